# Optimizing a Trainium2 kernel written in Bass

```python
import jax, jax.numpy as jnp
from jax import lax
import numpy as np

D_MODEL = 2048
BATCH = 2
SEQ = 8192
DEPTH = 1
DEC_BATCH = 32
DEC_SEQ = 8
PAST_LEN = 16384
PAGE_SIZE = 128

CONV_CH = D_MODEL // 2
N_HEADS = 8
HEAD_DIM = (D_MODEL - CONV_CH) // N_HEADS
N_KV_HEADS = 2
MIX_WIDTH = CONV_CH + N_HEADS * HEAD_DIM
N_IDX_HEADS = 16
IDX_DIM = 64
TOPK_MAX = 256
CONV_WIDTH = 31
D_FF = -(-8 * D_MODEL // (3 * 256)) * 256
QBLOCK = 128
EPS = 1e-6
ATTN_SCALE = HEAD_DIM ** -0.5
INDEX_SCALE = (N_IDX_HEADS * IDX_DIM) ** -0.5
IN_SIZES = (CONV_CH, CONV_CH, N_HEADS * HEAD_DIM, N_KV_HEADS * HEAD_DIM,
            N_KV_HEADS * HEAD_DIM, N_IDX_HEADS * IDX_DIM, IDX_DIM, N_IDX_HEADS)
N_IN = sum(IN_SIZES)

kernel_name = "hymba_conformer_conv_dsa_sparse_attn_step"


def rms_norm(x, g):
    xf = x.astype(jnp.float32)
    y = xf * lax.rsqrt(jnp.mean(xf * xf, axis=-1, keepdims=True) + EPS)
    return (y * g.astype(jnp.float32)).astype(x.dtype)


def layer_norm(x, g, b):
    xf = x.astype(jnp.float32)
    mu = jnp.mean(xf, axis=-1, keepdims=True)
    xc = xf - mu
    y = xc * lax.rsqrt(jnp.mean(xc * xc, axis=-1, keepdims=True) + EPS)
    return (y * g.astype(jnp.float32) + b.astype(jnp.float32)).astype(x.dtype)


def project_in(x, g_pre, w_in):
    B, T, _ = x.shape
    proj = rms_norm(x, g_pre) @ w_in
    offs = np.cumsum(IN_SIZES)[:-1].tolist()
    u_a, u_g, q, k, v, qi, ki, wi = jnp.split(proj, offs, axis=-1)
    return (u_a, u_g,
            q.reshape(B, T, N_HEADS, HEAD_DIM),
            k.reshape(B, T, N_KV_HEADS, HEAD_DIM),
            v.reshape(B, T, N_KV_HEADS, HEAD_DIM),
            qi.reshape(B, T, N_IDX_HEADS, IDX_DIM),
            ki, wi)


def conformer_conv(u_a, u_g, hist, conv_w, conv_b, ln_g, ln_b):
    u = u_a * jax.nn.sigmoid(u_g)
    full = jnp.concatenate([hist.astype(u.dtype), u], axis=1)
    y = lax.conv_general_dilated(
        full, conv_w[:, None, :].astype(full.dtype), window_strides=(1,),
        padding='VALID', dimension_numbers=('NWC', 'WIO', 'NWC'),
        feature_group_count=CONV_CH)
    y = y + conv_b.astype(y.dtype)
    y = jax.nn.silu(layer_norm(y, ln_g, ln_b))
    return y, full[:, -(CONV_WIDTH - 1):]


def indexer_topk(qi, wi, k_idx, q_pos, k_top):
    L = k_idx.shape[1]
    logits = jnp.einsum('bthd,bsd->bths', qi.astype(jnp.float32), k_idx.astype(jnp.float32))
    score = jnp.einsum('bth,bths->bts', wi.astype(jnp.float32) * INDEX_SCALE,
                       jax.nn.relu(logits))
    causal = jnp.arange(L, dtype=jnp.int32)[None, :] <= q_pos[:, None]
    score = jnp.where(causal[None], score, -jnp.inf)
    _, idx = lax.top_k(score, k_top)
    valid = idx <= q_pos[None, :, None]
    return idx, valid


def gathered_attention(q, k_sel, v_sel, valid):
    B, T, H, Dh = q.shape
    qg = q.reshape(B, T, N_KV_HEADS, H // N_KV_HEADS, Dh)
    s = jnp.einsum('btcgd,btscd->btcgs', qg, k_sel).astype(jnp.float32) * ATTN_SCALE
    s = jnp.where(valid[:, :, None, None, :], s, -jnp.inf)
    p = jax.nn.softmax(s, axis=-1).astype(v_sel.dtype)
    o = jnp.einsum('btcgs,btscd->btcgd', p, v_sel)
    return o.reshape(B, T, H * Dh)


def prompt_sparse_attention(q, k, v, qi, wi, k_idx):
    B, S = q.shape[:2]
    k_top = min(TOPK_MAX, S // 4)
    nb = S // QBLOCK

    def to_blocks(a):
        return jnp.moveaxis(a.reshape((B, nb, QBLOCK) + a.shape[2:]), 1, 0)

    pos = jnp.arange(S, dtype=jnp.int32).reshape(nb, QBLOCK)

    def one_block(args):
        q_b, qi_b, wi_b, pos_b = args
        idx, valid = indexer_topk(qi_b, wi_b, k_idx, pos_b, k_top)
        k_sel = jax.vmap(lambda kb, ib: kb[ib])(k, idx)
        v_sel = jax.vmap(lambda vb, ib: vb[ib])(v, idx)
        return gathered_attention(q_b, k_sel, v_sel, valid)

    out = lax.map(one_block, (to_blocks(q), to_blocks(qi), to_blocks(wi), pos))
    return jnp.moveaxis(out, 0, 1).reshape(B, S, N_HEADS * HEAD_DIM)


def sample_sparse_attention(q, k_new, v_new, qi, wi, ki_new,
                            cache_k, cache_v, cache_idx_k, page_table, layer):
    Bd, T = q.shape[:2]
    n_pages = page_table.shape[1]
    past = n_pages * PAGE_SIZE
    k_top = min(TOPK_MAX, (past + T) // 4)
    k_idx_past = cache_idx_k[layer, page_table].reshape(Bd, past, IDX_DIM)
    k_idx = jnp.concatenate([k_idx_past.astype(ki_new.dtype), ki_new], axis=1)
    pos = past + jnp.arange(T, dtype=jnp.int32)
    idx, valid = indexer_topk(qi, wi, k_idx, pos, k_top)
    is_past = idx < past
    phys = jax.vmap(lambda pt, pi: pt[pi])(page_table, jnp.minimum(idx // PAGE_SIZE, n_pages - 1))
    row = idx % PAGE_SIZE
    new_i = jnp.clip(idx - past, 0, T - 1)

    def gather(pool, new):
        from_pool = pool[layer, phys, row].astype(new.dtype)
        from_new = jax.vmap(lambda nb, ib: nb[ib])(new, new_i)
        return jnp.where(is_past[..., None, None], from_pool, from_new)

    return gathered_attention(q, gather(cache_k, k_new), gather(cache_v, v_new), valid)


def residual_update(x, conv_o, attn_o, w_out, g_post_mix, g_pre_ffn,
                    w_gate, w_up, w_down, g_post_ffn):
    mixed = jnp.concatenate([conv_o, attn_o], axis=-1) @ w_out
    x = x + rms_norm(mixed, g_post_mix)
    h = rms_norm(x, g_pre_ffn)
    f = (jax.nn.silu(h @ w_gate) * (h @ w_up)) @ w_down
    return x + rms_norm(f, g_post_ffn)


def setup_inputs(seed: int = 0) -> dict:
    key = jax.random.key(seed)
    ks = jax.random.split(key, 24)
    n_pages = PAST_LEN // PAGE_SIZE
    n_used = DEC_BATCH * n_pages
    n_pool = n_used + (n_used + 3) // 4
    nrm = jax.random.normal
    f32 = jnp.float32

    def gain(k, n):
        return 1.0 + 0.05 * nrm(k, (DEPTH, n), f32)

    page_table = jax.random.permutation(ks[0], n_pool)[:n_used].reshape(DEC_BATCH, n_pages).astype(jnp.int32)
    return {
        "x_prompt": nrm(ks[1], (BATCH, SEQ, D_MODEL), f32),
        "x_sample": nrm(ks[2], (DEC_BATCH, DEC_SEQ, D_MODEL), f32),
        "cache_k": nrm(ks[3], (DEPTH, n_pool, PAGE_SIZE, N_KV_HEADS, HEAD_DIM), f32),
        "cache_v": nrm(ks[4], (DEPTH, n_pool, PAGE_SIZE, N_KV_HEADS, HEAD_DIM), f32),
        "cache_idx_k": nrm(ks[5], (DEPTH, n_pool, PAGE_SIZE, IDX_DIM), f32),
        "state_conv": 0.5 * nrm(ks[6], (DEPTH, DEC_BATCH, CONV_WIDTH - 1, CONV_CH), f32),
        "page_table": page_table,
        "g_pre_mix": gain(ks[7], D_MODEL),
        "w_in": nrm(ks[8], (DEPTH, D_MODEL, N_IN), f32) * D_MODEL ** -0.5,
        "conv_w": nrm(ks[9], (DEPTH, CONV_WIDTH, CONV_CH), f32) * CONV_WIDTH ** -0.5,
        "conv_b": 0.02 * nrm(ks[10], (DEPTH, CONV_CH), f32),
        "conv_ln_g": gain(ks[11], CONV_CH),
        "conv_ln_b": 0.02 * nrm(ks[12], (DEPTH, CONV_CH), f32),
        "w_out": nrm(ks[13], (DEPTH, MIX_WIDTH, D_MODEL), f32) * MIX_WIDTH ** -0.5,
        "g_post_mix": gain(ks[14], D_MODEL),
        "g_pre_ffn": gain(ks[15], D_MODEL),
        "w_gate": nrm(ks[16], (DEPTH, D_MODEL, D_FF), f32) * D_MODEL ** -0.5,
        "w_up": nrm(ks[17], (DEPTH, D_MODEL, D_FF), f32) * D_MODEL ** -0.5,
        "w_down": nrm(ks[18], (DEPTH, D_FF, D_MODEL), f32) * D_FF ** -0.5,
        "g_post_ffn": gain(ks[19], D_MODEL),
    }


def reference(x_prompt, x_sample, cache_k, cache_v, cache_idx_k, state_conv, page_table,
              g_pre_mix, w_in, conv_w, conv_b, conv_ln_g, conv_ln_b, w_out,
              g_post_mix, g_pre_ffn, w_gate, w_up, w_down, g_post_ffn):
    xp, xs = x_prompt, x_sample
    kp_l, vp_l, ip_l, cp_l, ks_l, vs_l, is_l, cs_l = [], [], [], [], [], [], [], []
    for l in range(DEPTH):
        u_a, u_g, q, k, v, qi, ki, wi = project_in(xp, g_pre_mix[l], w_in[l])
        hist0 = jnp.zeros((xp.shape[0], CONV_WIDTH - 1, CONV_CH), xp.dtype)
        conv_o, conv_st = conformer_conv(u_a, u_g, hist0, conv_w[l], conv_b[l],
                                         conv_ln_g[l], conv_ln_b[l])
        attn_o = prompt_sparse_attention(q, k, v, qi, wi, ki)
        xp = residual_update(xp, conv_o, attn_o, w_out[l], g_post_mix[l], g_pre_ffn[l],
                             w_gate[l], w_up[l], w_down[l], g_post_ffn[l])
        kp_l.append(k); vp_l.append(v); ip_l.append(ki); cp_l.append(conv_st)

        u_a, u_g, q, k, v, qi, ki, wi = project_in(xs, g_pre_mix[l], w_in[l])
        conv_o, conv_st = conformer_conv(u_a, u_g, state_conv[l], conv_w[l], conv_b[l],
                                         conv_ln_g[l], conv_ln_b[l])
        attn_o = sample_sparse_attention(q, k, v, qi, wi, ki, cache_k, cache_v,
                                         cache_idx_k, page_table, l)
        xs = residual_update(xs, conv_o, attn_o, w_out[l], g_post_mix[l], g_pre_ffn[l],
                             w_gate[l], w_up[l], w_down[l], g_post_ffn[l])
        ks_l.append(k); vs_l.append(v); is_l.append(ki); cs_l.append(conv_st)

    return (xp, xs,
            jnp.stack(kp_l), jnp.stack(vp_l), jnp.stack(ip_l), jnp.stack(cp_l),
            jnp.stack(ks_l), jnp.stack(vs_l), jnp.stack(is_l), jnp.stack(cs_l))
```

```python
import numpy as np
import ml_dtypes
import concourse.bass as bass
import concourse.mybir as mybir
from concourse.bass_utils import run_bass_kernel_spmd

F32 = mybir.dt.float32
BF16 = mybir.dt.bfloat16
I32 = mybir.dt.int32
U32 = mybir.dt.uint32
U8 = mybir.dt.uint8
AF = mybir.ActivationFunctionType
ALU = mybir.AluOpType
AX = mybir.AxisListType

D = 2048
SEQ = 8192
NB = 2
CONV_CH = 1024
NH = 8
HD = 128
NKV = 2
NIH = 16
IDIM = 64
TOPK = 256
CW = 31
DFF = 5632
N_IN = 4688
EPS = 1e-6
ATTN_SCALE = HD ** -0.5
INDEX_SCALE = (NIH * IDIM) ** -0.5
DEC_B = 32
DEC_T = 8
PAST = 16384
PAGE = 128
NPAGES = PAST // PAGE
NEG = -30000.0
NBIS = 20


class Res:
    __slots__ = ("name", "w", "rd")

    def __init__(self, name=""):
        self.name = name
        self.w = None
        self.rd = {}


class S:
    R = 8

    def __init__(self, nc, stack):
        self.nc = nc
        self.eng = {"pe": nc.tensor, "act": nc.scalar, "dve": nc.vector, "pool": nc.gpsimd, "sp": nc.sync}
        self.sem = {k: stack.enter_context(nc.semaphore("s_" + k)) for k in self.eng}
        self.cnt = {k: 0 for k in self.eng}
        self.dsem = {k: [stack.enter_context(nc.semaphore("d_%s%d" % (k, i))) for i in range(self.R)]
                     for k in ("sp", "pool", "act")}
        self.dn = {k: 0 for k in self.dsem}
        self.waited = {k: {} for k in self.eng}
        self.pending_dma = []
        self.nres = 0

    def res(self, name=""):
        return Res(name)

    def _wait(self, e, tok):
        if tok is None:
            return
        kind, key, val = tok
        if kind == "c":
            if key == e and e == "pe":
                return
            sem = self.sem[key]
            wk = ("c", key)
        else:
            sem = self.dsem[key[0]][key[1]]
            wk = ("d", key)
        if self.waited[e].get(wk, 0) >= val:
            return
        self.waited[e][wk] = val
        self.eng[e].wait_ge(sem, val)

    def _deps(self, e, reads, writes):
        toks = []
        for r in reads:
            if r.w is not None:
                toks.append(r.w)
        for w in writes:
            if w.w is not None:
                toks.append(w.w)
            for k, t in w.rd.items():
                if isinstance(t, list):
                    toks.extend(t)
                else:
                    toks.append(t)
        for t in toks:
            self._wait(e, t)

    def _mark(self, e, tok, reads, writes, is_dma):
        for r in reads:
            if is_dma:
                r.rd.setdefault("dma", []).append(tok)
            else:
                r.rd[e] = tok
        for w in writes:
            w.w = tok
            w.rd = {}

    def op(self, e, fn, reads=(), writes=()):
        self._deps(e, reads, writes)
        inst = fn(self.eng[e])
        self.cnt[e] += 1
        inst.then_inc(self.sem[e], 1)
        tok = ("c", e, self.cnt[e])
        self._mark(e, tok, reads, writes, False)
        return tok

    def dma(self, e, fn, reads=(), writes=()):
        n = self.dn[e]
        slot = n % self.R
        val = 16 * (n // self.R + 1)
        if val > 16:
            self._wait(e, ("d", (e, slot), val - 16))
        self._deps(e, reads, writes)
        inst = fn(self.eng[e])
        inst.then_inc(self.dsem[e][slot], 16)
        self.dn[e] = n + 1
        tok = ("d", (e, slot), val)
        self._mark(e, tok, reads, writes, True)
        self.pending_dma.append(tok)
        return tok

    def barrier(self):
        toks = [("c", k, self.cnt[k]) for k in self.eng if self.cnt[k] > 0]
        toks += self.pending_dma
        self.pending_dma = []
        for e in self.eng:
            for t in toks:
                if t[0] == "c" and t[1] == e:
                    continue
                self._wait(e, t)

    def finish(self):
        self.barrier()


_UN = [0]


def _un(name):
    _UN[0] += 1
    return "t%d_%s" % (_UN[0], name)


def _bf(a):
    return np.ascontiguousarray(a).astype(ml_dtypes.bfloat16)


def build(cfg):
    from contextlib import ExitStack
    NT = cfg.get("NT", SEQ // 128)
    NOWN = NT // 4
    SB = min(4, NOWN)
    NPG = cfg.get("NPG", NPAGES)
    NPOOL = cfg.get("NPOOL", 5120)
    NFT = DFF // 128
    NBLK = NOWN + 1
    SBLK = NOWN
    PH = cfg.get("PH", "UABSC")
    nc = bass.Bass("TRN2", target_bir_lowering=False)

    def din(name, shape, dtype=F32):
        return nc.dram_tensor(name, shape, dtype, kind="ExternalInput").ap()

    def dout(name, shape, dtype=F32):
        return nc.dram_tensor(name, shape, dtype, kind="ExternalOutput").ap()

    def dscr(name, shape, dtype):
        return nc.dram_tensor(name, shape, dtype, kind="Internal").ap()

    xb = din("xb", [NT * 128, D])
    xo = din("xo", [NOWN, 160, D])
    xs = din("xs", [160, D])
    w_in = din("w_in", [D, N_IN])
    w_out = din("w_out", [D, D])
    w_gate = din("w_gate", [D, DFF])
    w_up = din("w_up", [D, DFF])
    w_down = din("w_down", [DFF, D])
    g_pre_mix = din("g_pre_mix", [1, D])
    g_post_mix = din("g_post_mix", [1, D])
    g_pre_ffn = din("g_pre_ffn", [1, D])
    g_post_ffn = din("g_post_ffn", [1, D])
    conv_w = din("conv_w", [CW, CONV_CH])
    conv_b = din("conv_b", [1, CONV_CH])
    ln_g = din("conv_ln_g", [1, CONV_CH])
    ln_b = din("conv_ln_b", [1, CONV_CH])
    cache_k = din("cache_k", [NPOOL * PAGE, NKV * HD])
    cache_v = din("cache_v", [NPOOL * PAGE, NKV * HD])
    cache_ik = din("cache_ik", [NPOOL, PAGE * IDIM])
    state_conv = din("state_conv", [4, CW - 1, CONV_CH])
    ptab = din("ptab", [4, NPG], I32)
    identb_d = din("identb", [128, 128], BF16)
    ident32_d = din("ident32", [128, 128])
    cmask_d = din("cmask", [128, 512])
    maskg_d = din("maskg", [128, 16, 128], BF16)
    identrep_d = din("identrep", [128, 4, 128], BF16)
    cmask_s_d = din("cmask_s", [128, 128])
    selrep_d = din("selrep", [32, 4, 32], BF16)
    iota_d = din("iota_p", [128, 1])

    o_k = dout("o_k", [NT * 128, NKV * HD])
    o_v = dout("o_v", [NT * 128, NKV * HD])
    o_ik = dout("o_ik", [NT * 128, IDIM])
    o_y = dout("o_y", [NBLK, 128, D])
    o_convp = dout("o_convp", [CW - 1, CONV_CH])
    o_ks = dout("o_ks", [128, NKV * HD])
    o_vs = dout("o_vs", [128, NKV * HD])
    o_iks = dout("o_iks", [128, IDIM])
    o_convs = dout("o_convs", [4, CW - 1, CONV_CH])

    QT_scr = dscr("QT_scr", [NBLK, 128, 8, 128], BF16)
    qiT_scr = dscr("qiT_scr", [NBLK, 64, 16, 2, 64], BF16)
    Wsel_scr = dscr("Wsel_scr", [NBLK, 128, 16, 128], BF16)
    mixT_scr = dscr("mixT_scr", [NBLK, 128, 16, 128], BF16)
    x1_scr = dscr("x1_scr", [NBLK, 128, D], F32)
    hT_scr = dscr("hT_scr", [NBLK, 128, 16, 128], BF16)

    w_in_r = w_in.rearrange("(k p) n -> p k n", p=128)
    w_out_r = w_out.rearrange("(k p) n -> p k n", p=128)
    w_gate_r = w_gate.rearrange("(k p) n -> p k n", p=128)
    w_up_r = w_up.rearrange("(k p) n -> p k n", p=128)
    w_down_r = w_down.rearrange("(k p) n -> p k n", p=128)

    with ExitStack() as st:
        s = S(nc, st)
        sb = lambda name, shape, dtype: st.enter_context(nc.sbuf_tensor(_un(name), shape, dtype))
        PS = st.enter_context(nc.psum_tensor("PS", [128, 8 * 512], F32))
        rb = [s.res("bank%d" % i) for i in range(8)]

        def bk(i, n=1):
            return PS[:, i * 512:(i + n) * 512]

        def bkb(i, n=1):
            return PS[:, i * 512:(i + n) * 512].bitcast(BF16)

        ident = sb("ident", [128, 128], BF16)
        ident32 = sb("ident32", [128, 128], F32)
        ones_bf = sb("ones_bf", [128, 128], BF16)
        ones32 = sb("ones32", [128, 128], F32)
        r_c = s.res("consts")
        s.dma("sp", lambda e: e.dma_start(out=ident[:], in_=identb_d[:, :]), writes=[r_c])
        s.dma("sp", lambda e: e.dma_start(out=ident32[:], in_=ident32_d[:, :]), writes=[r_c])
        s.op("dve", lambda e: e.memset(ones_bf[:], 1.0), writes=[r_c])
        s.op("dve", lambda e: e.memset(ones32[:], 1.0), writes=[r_c])

        def load_gain(tile, g_ap, r):
            s.dma("sp", lambda e: e.dma_start(out=tile[:], in_=g_ap.to_broadcast([128, D])), writes=[r])

        def rms_to_bf(P, x_ap, r_x, g_tile, r_g, out_ap, r_out, tmp):
            junk, r_junk, ssq, r_ss = tmp
            s.op("act", lambda e: e.activation(out=junk[0:P, :], in_=x_ap, func=AF.Square, accum_out=ssq[0:P, :]),
                 reads=[r_x], writes=[r_junk, r_ss])
            s.op("dve", lambda e: e.tensor_scalar(out=ssq[0:P, :], in0=ssq[0:P, :], scalar1=1.0 / D, scalar2=EPS,
                                                  op0=ALU.mult, op1=ALU.add), writes=[r_ss])
            s.op("act", lambda e: e.activation(out=ssq[0:P, :], in_=ssq[0:P, :], func=AF.Sqrt), writes=[r_ss])
            s.op("dve", lambda e: e.reciprocal(out=ssq[0:P, :], in_=ssq[0:P, :]), writes=[r_ss])
            s.op("dve", lambda e: e.scalar_tensor_tensor(out=out_ap, in0=x_ap, scalar=ssq[0:P, 0:1], in1=g_tile[0:P, :],
                                                         op0=ALU.mult, op1=ALU.mult),
                 reads=[r_x, r_ss, r_g], writes=[r_out])

        if "U" in PH:
          with ExitStack() as pu:
            sbu = lambda name, shape, dtype: pu.enter_context(nc.sbuf_tensor(_un(name), shape, dtype))
            gmix = sbu("gmixU", [128, D], F32); r_gmix = s.res()
            load_gain(gmix, g_pre_mix, r_gmix)
            cvec = sbu("cvec", [128, 3, 8], F32); r_cvec = s.res()
            for vi, v_ap in enumerate((conv_b, ln_g, ln_b)):
                s.dma("sp", lambda e, vi=vi, v_ap=v_ap: e.dma_start(out=cvec[:, vi, :], in_=v_ap.rearrange("o (c p) -> p (o c)", p=128),
                                                                       allow_slow_non_contiguous=True), writes=[r_cvec])
            cw_sb = sbu("cw_sb", [CW, CONV_CH], F32); r_cw = s.res()
            s.dma("sp", lambda e: e.dma_start(out=cw_sb[:], in_=conv_w[:, :]), writes=[r_cw])
            cwT = sbu("cwT", [128, 8, CW], F32); r_cwT = s.res()
            for c in range(8):
                s.op("pe", lambda e, c=c: e.transpose(bk(0)[:, c * 32:c * 32 + CW], cw_sb[:, c * 128:(c + 1) * 128], ident32[0:CW, 0:CW]),
                     reads=[r_cw, r_c], writes=[rb[0]])
            s.op("dve", lambda e: e.tensor_copy(out=cwT[:], in_=bk(0)[:, 0:256].rearrange("p (c j) -> p c j", j=32)[:, :, 0:CW]),
                 writes=[rb[0], r_cwT])
            diag = sbu("diag", [128, 8, CW, 128], BF16); r_diag = s.res()
            for c in range(8):
                for j in range(CW):
                    s.op("pool", lambda e, c=c, j=j: e.tensor_scalar(out=diag[:, c, j, :], in0=ident[:], scalar1=cwT[:, c, j:j + 1],
                                                                     scalar2=None, op0=ALU.mult),
                         reads=[r_cwT, r_c], writes=[r_diag])
            wwi = sbu("wwi", [128, 16, 16], BF16); r_wwi = s.res()
            s.dma("pool", lambda e: e.dma_start(out=wwi[:], in_=w_in_r[:, :, 4672:4688]), writes=[r_wwi])
            wwirep = sbu("wwirep", [128, 16, 128], BF16); r_wrep = s.res()
            for h2 in range(2):
                for hp in range(8):
                    h = 2 * hp + h2
                    col = h2 * 64 + hp * 8
                    s.op("pool", lambda e, h=h, col=col: e.tensor_copy(
                        out=wwirep[:, :, col:col + 8], in_=wwi[:, :, h:h + 1].to_broadcast([128, 16, 8])),
                        reads=[r_wwi], writes=[r_wrep])
            maskg = sbu("maskg", [128, 16, 128], BF16); r_maskg = s.res()
            s.dma("sp", lambda e: e.dma_start(out=maskg[:], in_=maskg_d[:, :, :]), writes=[r_maskg])

            xt = sbu("xtU", [128, D], F32); r_xt = s.res()
            xh = sbu("xhU", [32, D], F32); r_xh = s.res()
            junk = sbu("junkU", [128, D], BF16); r_junk = s.res()
            ssq = sbu("ssqU", [128, 1], F32); r_ssq = s.res()
            ssq2 = sbu("ssq2U", [128, 1], F32); r_ssq2 = s.res()
            xn = sbu("xnU", [128, D], BF16); r_xn = s.res()
            xnh = sbu("xnhU", [32, D], BF16); r_xnh = s.res()
            xnT = sbu("xnTU", [128, 16, SB, 160], BF16); r_xnT = s.res()
            Wt = [sbu("WtU%d" % i, [128, 16, 128], BF16) for i in range(2)]
            r_Wt = [s.res() for _ in range(2)]
            uT = sbu("uT", [128, 8, SB, 160], BF16); r_uT = s.res()
            sig = sbu("sig", [128, SB, 160], F32); r_sig = s.res()
            u32 = sbu("u32", [128, 8, 32], F32); r_u32 = s.res()
            Qst = sbu("Qst", [128, SB, 8, 128], BF16); r_Qst = s.res()
            Qi_sb = sbu("Qi_sb", [128, SB, 16, 8, 8], BF16); r_Qi = s.res()
            WIrep = sbu("WIrep", [128, SB, 128], F32); r_WIrep = s.res()
            Wsel = sbu("WselU", [128, 16, 128], BF16); r_Wsel = s.res()
            ycv = sbu("ycv", [128, 8, 128], F32); r_ycv = s.res()
            ysq = sbu("ysq", [128, 8, 128], F32); r_ysq = s.res()
            stat = sbu("stat", [128, 3, 128], F32); r_stat = s.res()
            co = sbu("co", [128, 8, 128], BF16); r_co = s.res()
            stt = sbu("stt", [CW - 1, CONV_CH], F32); r_stt = s.res()
            fullT = sbu("fullT", [128, 8, 4, 38], BF16); r_fullT = s.res()
            cvo = sbu("cvo", [32, CONV_CH], F32); r_cvo = s.res()

            wcnt = [0]

            def conv_tail(blk, TW):
                Y = bk(0, 2).rearrange("p (c t) -> p c t", t=128)
                s.op("dve", lambda e: e.tensor_tensor(out=ycv[:, :, 0:TW], in0=Y[:, :, 0:TW],
                                                      in1=cvec[:, 0, :].unsqueeze(2).to_broadcast([128, 8, TW]), op=ALU.add),
                     reads=[r_cvec], writes=[rb[0], rb[1], r_ycv])
                s.op("act", lambda e: e.activation(out=ysq[:, :, 0:TW], in_=ycv[:, :, 0:TW], func=AF.Square),
                     reads=[r_ycv], writes=[r_ysq])
                for c in range(8):
                    s.op("pe", lambda e, c=c: e.matmul(bk(2)[:, 0:TW], ones32[:], ycv[:, c, 0:TW], start=(c == 0), stop=(c == 7)),
                         reads=[r_ycv, r_c], writes=[rb[2]])
                for c in range(8):
                    s.op("pe", lambda e, c=c: e.matmul(bk(3)[:, 0:TW], ones32[:], ysq[:, c, 0:TW], start=(c == 0), stop=(c == 7)),
                         reads=[r_ysq, r_c], writes=[rb[3]])
                mean, msq, rstd = stat[:, 0, 0:TW], stat[:, 1, 0:TW], stat[:, 2, 0:TW]
                s.op("dve", lambda e: e.tensor_scalar(out=mean, in0=bk(2)[:, 0:TW], scalar1=1.0 / CONV_CH, scalar2=None, op0=ALU.mult),
                     writes=[rb[2], r_stat])
                s.op("dve", lambda e: e.tensor_tensor(out=msq, in0=mean, in1=mean, op=ALU.mult), writes=[r_stat])
                s.op("dve", lambda e: e.scalar_tensor_tensor(out=rstd, in0=bk(3)[:, 0:TW], scalar=1.0 / CONV_CH, in1=msq,
                                                             op0=ALU.mult, op1=ALU.subtract), writes=[rb[3], r_stat])
                s.op("dve", lambda e: e.tensor_scalar(out=rstd, in0=rstd, scalar1=EPS, scalar2=None, op0=ALU.add), writes=[r_stat])
                s.op("act", lambda e: e.activation(out=rstd, in_=rstd, func=AF.Sqrt), writes=[r_stat])
                s.op("dve", lambda e: e.reciprocal(out=rstd, in_=rstd), writes=[r_stat])
                s.op("dve", lambda e: e.tensor_tensor(out=ycv[:, :, 0:TW], in0=ycv[:, :, 0:TW],
                                                      in1=stat[:, 0:1, 0:TW].to_broadcast([128, 8, TW]), op=ALU.subtract),
                     reads=[r_stat], writes=[r_ycv])
                s.op("dve", lambda e: e.tensor_tensor(out=ycv[:, :, 0:TW], in0=ycv[:, :, 0:TW],
                                                      in1=stat[:, 2:3, 0:TW].to_broadcast([128, 8, TW]), op=ALU.mult),
                     reads=[r_stat], writes=[r_ycv])
                if TW < 128:
                    s.op("pool", lambda e: e.memset(co[:], 0.0), writes=[r_co])
                for c in range(8):
                    s.op("act", lambda e, c=c: e.activation(out=co[:, c, 0:TW], in_=ycv[:, c, 0:TW], func=AF.Silu,
                                                            scale=cvec[:, 1, c:c + 1], bias=cvec[:, 2, c:c + 1]),
                         reads=[r_ycv, r_cvec], writes=[r_co])
                s.dma("sp", lambda e: e.dma_start(out=mixT_scr[blk, :, 0:8, :], in_=co[:]), reads=[r_co])

            def u32_out(ncols, dst_fn):
                for c in range(8):
                    s.op("pe", lambda e, c=c: e.transpose(bk(2, 2)[0:ncols, c * 128:(c + 1) * 128], u32[:, c, 0:ncols], ident32[:]),
                         reads=[r_u32, r_c], writes=[rb[2], rb[3]])
                s.op("dve", lambda e: e.tensor_copy(out=cvo[0:ncols, :], in_=bk(2, 2)[0:ncols, :]), writes=[rb[2], rb[3], r_cvo])
                dst_fn()

            supers = [list(range(i, min(i + SB, NOWN))) for i in range(0, NOWN, SB)] + [[SBLK]]
            for blks in supers:
                nb = len(blks)
                is_s = blks[0] == SBLK
                for bi, blk in enumerate(blks):
                    src = xs if is_s else xo[blk]
                    s.dma("sp", lambda e: e.dma_start(out=xh[:], in_=src[0:32, :]), writes=[r_xh])
                    s.dma("sp", lambda e: e.dma_start(out=xt[:], in_=src[32:160, :]), writes=[r_xt])
                    rms_to_bf(32, xh[:], r_xh, gmix, r_gmix, xnh[:], r_xnh, (junk, r_junk, ssq2, r_ssq2))
                    rms_to_bf(128, xt[:], r_xt, gmix, r_gmix, xn[:], r_xn, (junk, r_junk, ssq, r_ssq))
                    tpm = bkb(0, 2).rearrange("p (k t) -> p k t", t=128)
                    tph = bkb(2).rearrange("p (k t) -> p k t", t=64)
                    for k in range(16):
                        s.op("pe", lambda e, k=k: e.transpose(tpm[:, k, :], xn[:, k * 128:(k + 1) * 128], ident[:]),
                             reads=[r_xn, r_c], writes=[rb[0], rb[1]])
                    for k in range(16):
                        s.op("pe", lambda e, k=k: e.transpose(tph[:, k, 0:32], xnh[:, k * 128:(k + 1) * 128], ident[0:32, 0:32]),
                             reads=[r_xnh, r_c], writes=[rb[2]])
                    s.op("act", lambda e: e.copy(out=xnT[:, :, bi, 32:160], in_=tpm[:, :, :]), writes=[rb[0], rb[1], r_xnT])
                    s.op("dve", lambda e: e.tensor_copy(out=xnT[:, :, bi, 0:32], in_=tph[:, :, 0:32]), writes=[rb[2], r_xnT])
                halves = [(0, min(2, nb))] + ([(2, nb)] if nb > 2 else [])

                def proj(ct_cols, lhs_fn, pair):
                    for hi, (b0, b1) in enumerate(halves):
                        n = (b1 - b0) * 160
                        for k in range(16):
                            s.op("pe", lambda e, k=k: e.matmul(bk(pair + hi)[:, 0:n], lhs_fn(k), xnT[:, k, b0:b1, :],
                                                               start=(k == 0), stop=(k == 15)),
                                 reads=[r_xnT] + ct_cols, writes=[rb[pair + hi]])

                def load_w(col0):
                    i = wcnt[0] % 2
                    wcnt[0] += 1
                    s.dma("pool", lambda e: e.dma_start(out=Wt[i][:], in_=w_in_r[:, :, col0:col0 + 128]), writes=[r_Wt[i]])
                    return i

                def pview(pair, hi, b0, b1):
                    return bk(pair + hi)[:, 0:(b1 - b0) * 160].rearrange("p (b t) -> p b t", t=160)

                for c in range(8):
                    ia = load_w(c * 128)
                    proj([r_Wt[ia]], lambda k, ia=ia: Wt[ia][:, k, :], 4)
                    ig = load_w(1024 + c * 128)
                    proj([r_Wt[ig]], lambda k, ig=ig: Wt[ig][:, k, :], 6)
                    for hi, (b0, b1) in enumerate(halves):
                        s.op("act", lambda e: e.activation(out=sig[:, b0:b1, :], in_=pview(6, hi, b0, b1), func=AF.Sigmoid),
                             writes=[rb[6 + hi], r_sig])
                        s.op("dve", lambda e: e.tensor_tensor(out=uT[:, c, b0:b1, :], in0=pview(4, hi, b0, b1), in1=sig[:, b0:b1, :], op=ALU.mult),
                             reads=[r_sig], writes=[rb[4 + hi], r_uT])
                        if is_s:
                            s.op("dve", lambda e: e.tensor_tensor(out=u32[:, c, :], in0=pview(4, hi, b0, b1)[:, 0, 32:64], in1=sig[:, 0, 32:64], op=ALU.mult),
                                 reads=[r_sig], writes=[rb[4 + hi], r_u32])
                        elif blks[-1] == NOWN - 1 and b1 == nb:
                            s.op("dve", lambda e: e.tensor_tensor(out=u32[:, c, 0:30], in0=pview(4, hi, b0, b1)[:, b1 - b0 - 1, 130:160],
                                                                  in1=sig[:, nb - 1, 130:160], op=ALU.mult),
                                 reads=[r_sig], writes=[rb[4 + hi], r_u32])
                for h in range(8):
                    iw = load_w(2048 + h * 128)
                    pair = 4 + 2 * (h % 2)
                    proj([r_Wt[iw]], lambda k, iw=iw: Wt[iw][:, k, :], pair)
                    for hi, (b0, b1) in enumerate(halves):
                        s.op("act", lambda e: e.activation(out=Qst[:, b0:b1, h, :], in_=pview(pair, hi, b0, b1)[:, :, 32:160],
                                                           func=AF.Copy, scale=ATTN_SCALE),
                             writes=[rb[pair + hi], r_Qst])
                for hp in range(8):
                    iw = load_w(3584 + hp * 128)
                    pair = 4 + 2 * (hp % 2)
                    proj([r_Wt[iw]], lambda k, iw=iw: Wt[iw][:, k, :], pair)
                    for hi, (b0, b1) in enumerate(halves):
                        for b in range(b0, b1):
                            s.op("dve", lambda e, b=b: e.tensor_copy(
                                out=Qi_sb[:, b, :, hp, :],
                                in_=pview(pair, hi, b0, b1)[:, b - b0, 32:160].rearrange("p (g t) -> p g t", t=8)),
                                writes=[rb[pair + hi], r_Qi])
                proj([r_wrep], lambda k: wwirep[:, k, :], 4)
                for hi, (b0, b1) in enumerate(halves):
                    s.op("act", lambda e: e.activation(out=WIrep[:, b0:b1, :], in_=pview(4, hi, b0, b1)[:, :, 32:160],
                                                       func=AF.Copy, scale=INDEX_SCALE),
                         writes=[rb[4 + hi], r_WIrep])
                for bi, blk in enumerate(blks):
                    s.dma("sp", lambda e: e.dma_start(out=QT_scr[blk], in_=Qst[:, bi, :, :]), reads=[r_Qst])
                    for h2 in range(2):
                        s.dma("sp", lambda e, h2=h2: e.dma_start(
                            out=qiT_scr[blk, :, :, h2, :],
                            in_=Qi_sb[h2 * 64:(h2 + 1) * 64, bi, :, :, :].rearrange("p g a b -> p g (a b)")),
                            reads=[r_Qi])
                    s.op("dve", lambda e: e.tensor_tensor(out=Wsel[:], in0=maskg[:],
                                                          in1=WIrep[:, bi:bi + 1, :].to_broadcast([128, 16, 128]), op=ALU.mult),
                         reads=[r_maskg, r_WIrep], writes=[r_Wsel])
                    s.dma("sp", lambda e: e.dma_start(out=Wsel_scr[blk], in_=Wsel[:]), reads=[r_Wsel])
                    Y = bk(0, 2).rearrange("p (c t) -> p c t", t=128)
                    if not is_s:
                        for c in range(8):
                            for j in range(CW):
                                s.op("pe", lambda e, c=c, j=j: e.matmul(Y[:, c, :], diag[:, c, j, :], uT[:, c, bi, 2 + j:2 + j + 128],
                                                                        start=(j == 0), stop=(j == CW - 1)),
                                     reads=[r_diag, r_uT], writes=[rb[0], rb[1]])
                        conv_tail(blk, 128)
                    else:
                        for q in range(4):
                            s.dma("sp", lambda e, q=q: e.dma_start(out=stt[:], in_=state_conv[q]), writes=[r_stt])
                            tq = bk(2)[:, 0:256].rearrange("p (c j) -> p c j", j=32)
                            for c in range(8):
                                s.op("pe", lambda e, c=c: e.transpose(tq[:, c, 0:CW - 1], stt[:, c * 128:(c + 1) * 128], ident32[0:CW - 1, 0:CW - 1]),
                                     reads=[r_stt, r_c], writes=[rb[2]])
                            s.op("dve", lambda e, q=q: e.tensor_copy(out=fullT[:, :, q, 0:CW - 1], in_=tq[:, :, 0:CW - 1]),
                                 writes=[rb[2], r_fullT])
                            s.op("dve", lambda e, q=q: e.tensor_copy(out=fullT[:, :, q, CW - 1:CW + 7], in_=uT[:, :, 0, 32 + 8 * q:40 + 8 * q]),
                                 reads=[r_uT], writes=[r_fullT])
                            s.dma("sp", lambda e, q=q: e.dma_start(out=o_convs[q, 0:CW - 9, :], in_=state_conv[q, 8:CW - 1, :]))
                        for c in range(8):
                            for q in range(4):
                                for j in range(CW):
                                    s.op("pe", lambda e, c=c, q=q, j=j: e.matmul(Y[:, c, 8 * q:8 * q + 8], diag[:, c, j, :], fullT[:, c, q, j:j + 8],
                                                                                 start=(j == 0), stop=(j == CW - 1)),
                                         reads=[r_diag, r_fullT], writes=[rb[0], rb[1]])
                        conv_tail(blk, 32)
                        u32_out(32, lambda: [s.dma("sp", lambda e, q=q: e.dma_start(out=o_convs[q, CW - 9:CW - 1, :], in_=cvo[8 * q:8 * q + 8, :]),
                                                   reads=[r_cvo]) for q in range(4)])
                if (not is_s) and blks[-1] == NOWN - 1:
                    u32_out(30, lambda: s.dma("sp", lambda e: e.dma_start(out=o_convp[:, :], in_=cvo[0:30, :]), reads=[r_cvo]))
            s.barrier()

        def kv_env(stack, pfx):
            sba = lambda name, shape, dtype: stack.enter_context(nc.sbuf_tensor(_un(name), shape, dtype))
            gmix = sba(pfx + "gmixA", [128, D], F32); r_gmix = s.res()
            load_gain(gmix, g_pre_mix, r_gmix)
            Wkv = sba(pfx + "Wkv", [128, 16, 576], BF16)
            r_Wkv = s.res()
            for k4 in range(4):
                s.dma("pool", lambda e, k4=k4: e.dma_start(out=Wkv[:, 4 * k4:4 * k4 + 4, 0:512],
                                                            in_=w_in_r[:, 4 * k4:4 * k4 + 4, 3072:3584]), writes=[r_Wkv])
                s.dma("pool", lambda e, k4=k4: e.dma_start(out=Wkv[:, 4 * k4:4 * k4 + 4, 512:576],
                                                            in_=w_in_r[:, 4 * k4:4 * k4 + 4, 4608:4672]), writes=[r_Wkv])
            xt = [sba(pfx + "xt%d" % i, [128, D], F32) for i in range(2)]
            r_xt = [s.res() for _ in range(2)]
            junk = sba(pfx + "junkA", [128, D], BF16); r_junk = s.res()
            xn = [sba(pfx + "xn%d" % i, [128, D], BF16) for i in range(2)]
            r_xn = [s.res() for _ in range(2)]
            xnT = [sba(pfx + "xnT%d" % i, [128, 16, 128], BF16) for i in range(2)]
            r_xnT = [s.res() for _ in range(2)]
            ssq = [sba(pfx + "ssA%d" % i, [128, 1], F32) for i in range(2)]
            r_ss = [s.res() for _ in range(2)]
            kvf = [sba(pfx + "kvf%d" % i, [128, 576], F32) for i in range(2)]
            r_kvf = [s.res() for _ in range(2)]
            kb = [sba(pfx + "kb%d" % i, [128, 320], BF16) for i in range(2)]
            r_kb = [s.res() for _ in range(2)]
            tp = bkb(0, 2).rearrange("p (k t) -> p k t", t=128)
            tp2 = bkb(6).rearrange("p (k t) -> p k t", t=128)

            kvcnt = [0]

            def kv_tile(src, KT_dst, V_dst, kiT_dst, r_KT, r_V, r_kiT, dk, dv, dik):
                b = kvcnt[0] % 2; kvcnt[0] += 1
                pkv, pki = bk(2 + b), bk(4 + b)
                r_pkv = [rb[2 + b], rb[4 + b]]
                s.dma("sp", lambda e: e.dma_start(out=xt[b][:], in_=src), writes=[r_xt[b]])
                rms_to_bf(128, xt[b][:], r_xt[b], gmix, r_gmix, xn[b][:], r_xn[b], (junk, r_junk, ssq[b], r_ss[b]))
                for k in range(16):
                    s.op("pe", lambda e, k=k: e.transpose(tp[:, k, :], xn[b][:, k * 128:(k + 1) * 128], ident[:]),
                         reads=[r_xn[b], r_c], writes=[rb[0], rb[1]])
                s.op("act", lambda e: e.copy(out=xnT[b][:], in_=tp[:, :, :]), writes=[rb[0], rb[1], r_xnT[b]])
                for k in range(16):
                    s.op("pe", lambda e, k=k: e.matmul(pkv[:, :], xnT[b][:, k, :], Wkv[:, k, 0:512], start=(k == 0), stop=(k == 15)),
                         reads=[r_xnT[b], r_Wkv], writes=r_pkv)
                for k in range(16):
                    s.op("pe", lambda e, k=k: e.matmul(pki[:, 0:64], xnT[b][:, k, :], Wkv[:, k, 512:576], start=(k == 0), stop=(k == 15)),
                         reads=[r_xnT[b], r_Wkv], writes=r_pkv)
                s.op("dve", lambda e: e.tensor_copy(out=kvf[b][:, 0:512], in_=pkv[:, :]), writes=r_pkv + [r_kvf[b]])
                s.op("dve", lambda e: e.tensor_copy(out=kvf[b][:, 512:576], in_=pki[:, 0:64]), writes=r_pkv + [r_kvf[b]])
                s.op("act", lambda e: e.copy(out=kb[b][:, 0:256], in_=pkv[:, 0:256]), writes=r_pkv + [r_kb[b]])
                s.op("act", lambda e: e.copy(out=V_dst, in_=pkv[:, 256:512]), writes=r_pkv + [r_V])
                s.op("act", lambda e: e.copy(out=kb[b][:, 256:320], in_=pki[:, 0:64]), writes=r_pkv + [r_kb[b]])
                s.op("pe", lambda e: e.transpose(tp2[:, 0, :], kb[b][:, 0:128], ident[:]), reads=[r_kb[b], r_c], writes=[rb[6]])
                s.op("pe", lambda e: e.transpose(tp2[:, 1, :], kb[b][:, 128:256], ident[:]), reads=[r_kb[b], r_c], writes=[rb[6]])
                s.op("pe", lambda e: e.transpose(tp2[0:64, 2, :], kb[b][:, 256:320], ident[:]), reads=[r_kb[b], r_c], writes=[rb[6]])
                s.op("dve", lambda e: e.tensor_copy(out=KT_dst, in_=tp2[:, 0:2, :]), writes=[rb[6], r_KT])
                s.op("dve", lambda e: e.tensor_copy(out=kiT_dst, in_=tp2[0:64, 2, :]), writes=[rb[6], r_kiT])
                s.dma("sp", lambda e: e.dma_start(out=dk, in_=kvf[b][:, 0:256]), reads=[r_kvf[b]])
                s.dma("sp", lambda e: e.dma_start(out=dv, in_=kvf[b][:, 256:512]), reads=[r_kvf[b]])
                s.dma("sp", lambda e: e.dma_start(out=dik, in_=kvf[b][:, 512:576]), reads=[r_kvf[b]])

            return kv_tile

        def attn_env(stack, pfx, NSC):
            sbb = lambda name, shape, dtype: stack.enter_context(nc.sbuf_tensor(_un(name), shape, dtype))
            score = sbb(pfx + "score", [128, NSC], F32); r_score = s.res()
            cjunk = sbb(pfx + "cjunk", [128, NSC], U8); r_cjunk = s.res()
            cmask = sbb(pfx + "cmask", [128, 512], F32)
            cmask_s = sbb(pfx + "cmask_s", [128, 128], F32)
            identrep = sbb(pfx + "identrep", [128, 4, 128], BF16)
            selrep = sbb(pfx + "selrep", [32, 4, 32], BF16)
            iota_p = sbb(pfx + "iota_p", [128, 1], F32)
            r_bc = s.res()
            s.dma("sp", lambda e: e.dma_start(out=cmask[:], in_=cmask_d[:, :]), writes=[r_bc])
            s.dma("sp", lambda e: e.dma_start(out=cmask_s[:], in_=cmask_s_d[:, :]), writes=[r_bc])
            s.dma("sp", lambda e: e.dma_start(out=identrep[:], in_=identrep_d[:, :, :]), writes=[r_bc])
            s.dma("sp", lambda e: e.dma_start(out=selrep[:], in_=selrep_d[:, :, :]), writes=[r_bc])
            s.dma("sp", lambda e: e.dma_start(out=iota_p[:], in_=iota_d[:, :]), writes=[r_bc])
            QTb = sbb(pfx + "QTb", [128, 8, 128], BF16); r_QTb = s.res()
            qiTb = sbb(pfx + "qiTb", [64, 16, 128], BF16); r_qiTb = s.res()
            Wselb = sbb(pfx + "Wselb", [128, 16, 128], BF16); r_Wselb = s.res()
            Rt = [sbb(pfx + "Rt%d" % i, [128, 512], BF16) for i in range(4)]
            r_Rt = [s.res() for _ in range(4)]
            Mb = [sbb(pfx + "Mb%d" % i, [128, 512], BF16) for i in range(2)]
            r_Mb = [s.res() for _ in range(2)]
            Pt = [sbb(pfx + "Pt%d" % i, [128, 512], BF16) for i in range(3)]
            r_Pt = [s.res() for _ in range(3)]
            sm = sbb(pfx + "smallB", [128, 8], F32); r_sm = s.res()
            rsum = sbb(pfx + "rsum", [128, 2, 512], F32); r_rsum = s.res()
            aoT = sbb(pfx + "aoT", [128, 8, 128], BF16); r_aoT = s.res()
            cnt = {"r": 0, "m": 0, "p": 0, "l": 0}

            def load_block(blk):
                s.dma("sp", lambda e: e.dma_start(out=QTb[:], in_=QT_scr[blk]), writes=[r_QTb])
                s.dma("sp", lambda e: e.dma_start(out=qiTb[:], in_=qiT_scr[blk].rearrange("d g a b -> d g (a b)")), writes=[r_qiTb])
                s.dma("sp", lambda e: e.dma_start(out=Wselb[:], in_=Wsel_scr[blk]), writes=[r_Wselb])

            def indexer_tile(groups, ki_ap, r_ki, ncol, dst_col, mask_ap, accumulate):
                for gi, g in enumerate(groups):
                    lb = cnt["l"] % 3; cnt["l"] += 1
                    s.op("pe", lambda e: e.matmul(bk(lb)[:, 0:ncol], qiTb[:, g, :], ki_ap, start=True, stop=True),
                         reads=[r_qiTb, r_ki], writes=[rb[lb]])
                    ri = cnt["r"] % 4; cnt["r"] += 1
                    if ri % 2 == 0:
                        s.op("act", lambda e: e.activation(out=Rt[ri][:, 0:ncol], in_=bk(lb)[:, 0:ncol], func=AF.Relu),
                             writes=[rb[lb], r_Rt[ri]])
                    else:
                        s.op("dve", lambda e: e.tensor_scalar(out=Rt[ri][:, 0:ncol], in0=bk(lb)[:, 0:ncol], scalar1=0.0, scalar2=None, op0=ALU.max),
                             writes=[rb[lb], r_Rt[ri]])
                    s.op("pe", lambda e: e.matmul(bk(3)[:, 0:ncol], Wselb[:, g, :], Rt[ri][:, 0:ncol],
                                                  start=(gi == 0), stop=(gi == len(groups) - 1)),
                         reads=[r_Wselb, r_Rt[ri]], writes=[rb[3]])
                dst = score[:, dst_col:dst_col + ncol]
                if accumulate:
                    s.op("dve", lambda e: e.tensor_tensor(out=dst, in0=bk(3)[:, 0:ncol], in1=dst, op=ALU.add), writes=[rb[3], r_score])
                    if mask_ap is not None:
                        s.op("dve", lambda e: e.tensor_tensor(out=dst, in0=dst, in1=mask_ap, op=ALU.add), reads=[r_bc], writes=[r_score])
                elif mask_ap is not None:
                    s.op("dve", lambda e: e.tensor_tensor(out=dst, in0=bk(3)[:, 0:ncol], in1=mask_ap, op=ALU.add),
                         reads=[r_bc], writes=[rb[3], r_score])
                else:
                    s.op("dve", lambda e: e.tensor_copy(out=dst, in_=bk(3)[:, 0:ncol]), writes=[rb[3], r_score])

            def threshold(ncols, premask_cols=None):
                sc = score[:, 0:ncols]
                lo, hi, mid, cn, sel, d1 = (sm[:, i:i + 1] for i in range(6))
                s.op("dve", lambda e: e.tensor_reduce(out=hi, in_=sc, axis=AX.X, op=ALU.max), reads=[r_score], writes=[r_sm])
                s.op("dve", lambda e: e.tensor_reduce(out=lo, in_=sc, axis=AX.X, op=ALU.min), reads=[r_score], writes=[r_sm])
                if premask_cols is not None:
                    c0, c1, m_ap = premask_cols
                    s.op("dve", lambda e: e.tensor_tensor(out=score[:, c0:c1], in0=score[:, c0:c1], in1=m_ap, op=ALU.add),
                         reads=[r_bc], writes=[r_score])
                s.op("dve", lambda e: e.tensor_tensor(out=d1, in0=hi, in1=lo, op=ALU.subtract), writes=[r_sm])
                s.op("dve", lambda e: e.scalar_tensor_tensor(out=hi, in0=d1, scalar=1e-3, in1=hi, op0=ALU.mult, op1=ALU.add), writes=[r_sm])
                s.op("dve", lambda e: e.tensor_scalar(out=hi, in0=hi, scalar1=1e-6, scalar2=None, op0=ALU.add), writes=[r_sm])
                for it in range(NBIS):
                    s.op("dve", lambda e: e.tensor_tensor(out=mid, in0=lo, in1=hi, op=ALU.add), writes=[r_sm])
                    s.op("dve", lambda e: e.tensor_scalar(out=mid, in0=mid, scalar1=0.5, scalar2=None, op0=ALU.mult), writes=[r_sm])
                    s.op("dve", lambda e: e.tensor_scalar(out=cjunk[:, 0:ncols], in0=sc, scalar1=mid, scalar2=None,
                                                          op0=ALU.is_ge, op1=ALU.add, accum_out=cn),
                         reads=[r_score], writes=[r_cjunk, r_sm])
                    s.op("dve", lambda e: e.tensor_scalar(out=sel, in0=cn, scalar1=float(TOPK) - 0.5, scalar2=None, op0=ALU.is_ge), writes=[r_sm])
                    s.op("dve", lambda e: e.tensor_tensor(out=d1, in0=mid, in1=lo, op=ALU.subtract), writes=[r_sm])
                    s.op("dve", lambda e: e.scalar_tensor_tensor(out=lo, in0=d1, scalar=sel, in1=lo, op0=ALU.mult, op1=ALU.add), writes=[r_sm])
                    s.op("dve", lambda e: e.tensor_tensor(out=d1, in0=hi, in1=mid, op=ALU.subtract), writes=[r_sm])
                    s.op("dve", lambda e: e.scalar_tensor_tensor(out=hi, in0=d1, scalar=sel, in1=mid, op0=ALU.mult, op1=ALU.add), writes=[r_sm])

            def make_mb(c0, ncol):
                mi = cnt["m"] % 2; cnt["m"] += 1
                s.op("dve", lambda e: e.tensor_scalar(out=Mb[mi][:, 0:ncol], in0=score[:, c0:c0 + ncol], scalar1=sm[:, 0:1], scalar2=NEG,
                                                      op0=ALU.is_lt, op1=ALU.mult),
                     reads=[r_score, r_sm], writes=[r_Mb[mi]])
                return mi

            def attend_chunk(first, last, kp, kt_fn, v_fn, r_kv, q_fn, r_q, nq, mb_l, mb_r, r_mb):
                if nq == 512:
                    for kvh in range(2):
                        pi = cnt["p"] % 3; cnt["p"] += 1
                        sb_ = pi % 2
                        s.op("pe", lambda e: e.matmul(bk(sb_)[0:kp, :], kt_fn(kvh), q_fn(kvh), start=True, stop=False),
                             reads=r_kv + [r_q], writes=[rb[sb_]])
                        s.op("pe", lambda e: e.matmul(bk(sb_)[0:kp, :], mb_l, mb_r, start=False, stop=True),
                             reads=[r_mb, r_bc], writes=[rb[sb_]])
                        s.op("act", lambda e: e.activation(out=Pt[pi][0:kp, :], in_=bk(sb_)[0:kp, :], func=AF.Exp), writes=[rb[sb_], r_Pt[pi]])
                        s.op("pe", lambda e: e.matmul(bk(4 + kvh)[:, :], v_fn(kvh), Pt[pi][0:kp, :], start=first, stop=last),
                             reads=r_kv + [r_Pt[pi]], writes=[rb[4 + kvh]])
                        s.op("pe", lambda e: e.matmul(bk(6 + kvh)[:, :], ones_bf[0:kp, :], Pt[pi][0:kp, :], start=first, stop=last),
                             reads=[r_Pt[pi], r_c], writes=[rb[6 + kvh]])
                else:
                    pi = cnt["p"] % 3; cnt["p"] += 1
                    sb_ = pi % 2
                    for kvh in range(2):
                        s.op("pe", lambda e: e.matmul(bk(sb_)[0:kp, kvh * nq:(kvh + 1) * nq], kt_fn(kvh), q_fn(kvh), start=True, stop=False),
                             reads=r_kv + [r_q], writes=[rb[sb_]])
                        s.op("pe", lambda e: e.matmul(bk(sb_)[0:kp, kvh * nq:(kvh + 1) * nq], mb_l, mb_r, start=False, stop=True),
                             reads=[r_mb, r_bc], writes=[rb[sb_]])
                    s.op("act", lambda e: e.activation(out=Pt[pi][0:kp, 0:2 * nq], in_=bk(sb_)[0:kp, 0:2 * nq], func=AF.Exp),
                         writes=[rb[sb_], r_Pt[pi]])
                    for kvh in range(2):
                        s.op("pe", lambda e: e.matmul(bk(4 + kvh)[:, 0:nq], v_fn(kvh), Pt[pi][0:kp, kvh * nq:(kvh + 1) * nq],
                                                      start=first, stop=last),
                             reads=r_kv + [r_Pt[pi]], writes=[rb[4 + kvh]])
                    s.op("pe", lambda e: e.matmul(bk(6)[:, 0:2 * nq], ones_bf[0:kp, :], Pt[pi][0:kp, 0:2 * nq], start=first, stop=last),
                         reads=[r_Pt[pi], r_c], writes=[rb[6]])


            return dict(score=score, r_score=r_score, sm=sm, r_sm=r_sm, QTb=QTb, r_QTb=r_QTb, aoT=aoT, r_aoT=r_aoT, rsum=rsum, r_rsum=r_rsum,
                        Mb=Mb, r_Mb=r_Mb, cmask=cmask, cmask_s=cmask_s, identrep=identrep, selrep=selrep, iota_p=iota_p, r_bc=r_bc, cnt=cnt,
                        load_block=load_block, indexer_tile=indexer_tile, threshold=threshold, make_mb=make_mb, attend_chunk=attend_chunk)


        if "S" in PH:
            with ExitStack() as pss:
                sbs = lambda name, shape, dtype: pss.enter_context(nc.sbuf_tensor(_un(name), shape, dtype))
                NCS = NPG * 128 + 128
                env = attn_env(pss, "s_", NCS)
                score, sm, QTb, aoT, rsum, selrep, cmask_s = env["score"], env["sm"], env["QTb"], env["aoT"], env["rsum"], env["selrep"], env["cmask_s"]
                r_score, r_sm, r_QTb, r_aoT, r_rsum, r_bc = env["r_score"], env["r_sm"], env["r_QTb"], env["r_aoT"], env["r_rsum"], env["r_bc"]
                KTn = sbs("KTn", [128, 2, 128], BF16)
                Vn = sbs("Vn", [128, 256], BF16)
                kiTn = sbs("kiTn", [64, 128], BF16)
                r_kvn = s.res()
                with ExitStack() as pkn:
                    kvt = kv_env(pkn, "s_")
                    kvt(xs[32:160, :], KTn[:], Vn[:], kiTn[:], r_kvn, r_kvn, r_kvn, o_ks[:, :], o_vs[:, :], o_iks[:, :])
                    s.barrier()
                env["load_block"](SBLK)
                ptab_sb = sbs("ptab_sb", [NPG, 4], I32); r_pt = s.res()
                s.dma("sp", lambda e: e.dma_start(out=ptab_sb[:], in_=ptab.rearrange("q p -> p q"), allow_slow_non_contiguous=True), writes=[r_pt])
                RG = 16
                NJ = 128 // RG
                ptf = sbs("ptf", [NPG, 4], F32)
                idxf = sbs("idxf", [NPG, 4, NJ], F32)
                idxK = sbs("idxK", [NPG, 4, NJ], I32); r_idxK = s.res()
                s.op("dve", lambda e: e.tensor_copy(out=ptf[:], in_=ptab_sb[:]), reads=[r_pt], writes=[r_idxK])
                for jj in range(NJ):
                    s.op("dve", lambda e, jj=jj: e.tensor_scalar(out=idxf[:, :, jj], in0=ptf[:], scalar1=float(NJ), scalar2=float(jj),
                                                                op0=ALU.mult, op1=ALU.add), writes=[r_idxK])
                s.op("dve", lambda e: e.tensor_copy(out=idxK[:], in_=idxf[:]), writes=[r_idxK])
                with ExitStack() as psi:
                    sbi = lambda name, shape, dtype: psi.enter_context(nc.sbuf_tensor(_un(name), shape, dtype))
                    idxpg = sbi("idxpg", [NPG, PAGE * IDIM], F32); r_idxpg = s.res()
                    kiTs = sbi("kiTs", [64, NPG * 128], BF16); r_kiTs = s.res()
                    kvw = kiTs[:].rearrange("d (pg r) -> d pg r", r=128)
                    s.op("pool", lambda e: e.memset(score[:, 0:NCS], 0.0), writes=[r_score])
                    for q in range(4):
                        s.dma("pool", lambda e, q=q: e.indirect_dma_start(
                            out=idxpg[:], out_offset=None, in_=cache_ik[:, :],
                            in_offset=bass.IndirectOffsetOnAxis(ap=ptab_sb[:, q:q + 1], axis=0)), reads=[r_pt], writes=[r_idxpg])
                        for r0 in range(0, 128, 4):
                            bank = 4 + (r0 // 4) % 2
                            tq = bk(bank)[0:64, 0:4 * NPG].rearrange("d (r pg) -> d r pg", pg=NPG)
                            for rr in range(4):
                                s.op("pe", lambda e, rr=rr: e.transpose(tq[:, rr, :], idxpg[:, (r0 + rr) * 64:(r0 + rr + 1) * 64], ident32[0:NPG, 0:NPG]),
                                     reads=[r_idxpg, r_c], writes=[rb[bank]])
                            eng = "act" if (r0 // 4) % 2 == 0 else "dve"
                            if eng == "act":
                                s.op("act", lambda e: e.copy(out=kvw[:, :, r0:r0 + 4].rearrange("d pg r -> d r pg"), in_=tq), writes=[rb[bank], r_kiTs])
                            else:
                                s.op("dve", lambda e: e.tensor_copy(out=kvw[:, :, r0:r0 + 4].rearrange("d pg r -> d r pg"), in_=tq), writes=[rb[bank], r_kiTs])
                        for kc in range(NPG * 128 // 512):
                            env["indexer_tile"]([q], kiTs[:, kc * 512:(kc + 1) * 512], r_kiTs, 512, kc * 512, None, True)
                        env["indexer_tile"]([q], kiTn[:, :], r_kvn, 128, NPG * 128, None, True)
                    s.barrier()
                env["threshold"](NCS, (NPG * 128, NCS, cmask_s[:]))
                with ExitStack() as psa:
                    sbt = lambda name, shape, dtype: psa.enter_context(nc.sbuf_tensor(_un(name), shape, dtype))
                    Kg = [sbt("Kg%d" % i, [NPG, RG * 256], F32) for i in range(2)]
                    Vg = [sbt("Vg%d" % i, [NPG, RG * 256], F32) for i in range(2)]
                    r_Kg = [s.res() for _ in range(2)]
                    r_Vg = [s.res() for _ in range(2)]
                    KTc = [sbt("KTc%d" % i, [128, 2, NPG], BF16) for i in range(2)]
                    Vc = [sbt("Vc%d" % i, [NPG, 256], BF16) for i in range(2)]
                    Mbc = [sbt("Mbc%d" % i, [32, 128], BF16) for i in range(2)]
                    r_KTc = [s.res() for _ in range(2)]
                    r_Vc = [s.res() for _ in range(2)]
                    r_Mbc = [s.res() for _ in range(2)]
                    QTq = sbt("QTq", [128, 8, 8], BF16); r_QTq = s.res()
                    ck = cache_k.rearrange("(n r) d -> n (r d)", r=RG)
                    cv = cache_v.rearrange("(n r) d -> n (r d)", r=RG)
                    s.op("pool", lambda e: e.memset(aoT[:], 0.0), writes=[r_aoT])
                    cc = 0
                    for q in range(4):
                        s.op("dve", lambda e: e.tensor_copy(out=QTq[:], in_=QTb[:, :, 8 * q:8 * q + 8]), reads=[r_QTb], writes=[r_QTq])
                        qf = lambda kvh: QTq[:, 4 * kvh:4 * kvh + 4, :]
                        for jj in range(NJ):
                            gi = (q * NJ + jj) % 2
                            s.dma("pool", lambda e: e.indirect_dma_start(
                                out=Kg[gi][:], out_offset=None, in_=ck[:, :],
                                in_offset=bass.IndirectOffsetOnAxis(ap=idxK[:, q, jj:jj + 1], axis=0)), reads=[r_idxK], writes=[r_Kg[gi]])
                            s.dma("pool", lambda e: e.indirect_dma_start(
                                out=Vg[gi][:], out_offset=None, in_=cv[:, :],
                                in_offset=bass.IndirectOffsetOnAxis(ap=idxK[:, q, jj:jj + 1], axis=0)), reads=[r_idxK], writes=[r_Vg[gi]])
                            for rl in range(RG):
                                r = jj * RG + rl
                                ci = cc % 2; cc += 1
                                tk = bk(2)[:, 0:2 * NPG].rearrange("p (a b) -> p a b", b=NPG)
                                for kvh in range(2):
                                    s.op("pe", lambda e, kvh=kvh: e.transpose(tk[:, kvh, :], Kg[gi][:, rl * 256 + kvh * 128:rl * 256 + (kvh + 1) * 128],
                                                                              ident32[0:NPG, 0:NPG]),
                                         reads=[r_Kg[gi], r_c], writes=[rb[2]])
                                s.op("act", lambda e: e.copy(out=KTc[ci][:], in_=tk), writes=[rb[2], r_KTc[ci]])
                                s.op("pool", lambda e: e.tensor_copy(out=Vc[ci][:], in_=Vg[gi][:, rl * 256:(rl + 1) * 256]), reads=[r_Vg[gi]], writes=[r_Vc[ci]])
                                s.op("dve", lambda e: e.tensor_scalar(out=Mbc[ci][:, 0:NPG], in0=score[0:32, r:NPG * 128:128], scalar1=sm[0:32, 0:1], scalar2=NEG,
                                                                      op0=ALU.is_lt, op1=ALU.mult), reads=[r_score, r_sm], writes=[r_Mbc[ci]])
                                env["attend_chunk"](r == 0, False, NPG, lambda kvh: KTc[ci][:, kvh, :], lambda kvh: Vc[ci][:, kvh * 128:(kvh + 1) * 128],
                                                    [r_KTc[ci], r_Vc[ci]], qf, r_QTq, 32, Mbc[ci][:, 0:NPG], selrep[:, q, :], r_Mbc[ci])
                        ci = cc % 2; cc += 1
                        s.op("dve", lambda e: e.tensor_scalar(out=Mbc[ci][:, :], in0=score[0:32, NPG * 128:NPG * 128 + 128], scalar1=sm[0:32, 0:1], scalar2=NEG,
                                                              op0=ALU.is_lt, op1=ALU.mult), reads=[r_score, r_sm], writes=[r_Mbc[ci]])
                        env["attend_chunk"](False, True, 128, lambda kvh: KTn[:, kvh, :], lambda kvh: Vn[:, kvh * 128:(kvh + 1) * 128],
                                            [r_kvn], qf, r_QTq, 32, Mbc[ci][:, :], selrep[:, q, :], r_Mbc[ci])
                        s.op("dve", lambda e: e.reciprocal(out=rsum[:, 0, 0:64], in_=bk(6)[:, 0:64]), writes=[rb[6], r_rsum])
                        for kvh in range(2):
                            s.op("dve", lambda e, kvh=kvh: e.tensor_tensor(out=aoT[:, 4 * kvh:4 * kvh + 4, 8 * q:8 * q + 8],
                                                                           in0=bk(4 + kvh)[:, 0:32].rearrange("p (h t) -> p h t", t=8),
                                                                           in1=rsum[:, 0, 32 * kvh:32 * kvh + 32].rearrange("p (h t) -> p h t", t=8), op=ALU.mult),
                                 reads=[r_rsum], writes=[rb[4 + kvh], r_aoT])
                    s.dma("sp", lambda e: e.dma_start(out=mixT_scr[SBLK, :, 8:16, :], in_=aoT[:]), reads=[r_aoT])
                    s.barrier()


        with ExitStack() as pkv_stack:
            sbk = lambda name, shape, dtype: pkv_stack.enter_context(nc.sbuf_tensor(_un(name), shape, dtype))
            if "A" in PH or "B" in PH:
                KT = sbk("KT", [128, NKV, NT * 128], BF16)
                Vr = sbk("Vr", [128, NT, NKV * HD], BF16)
                kiT = sbk("kiT", [64, NT * 128], BF16)
                r_KT, r_V, r_kiT = s.res(), s.res(), s.res()
            if "A" in PH:
                with ExitStack() as pa:
                    kvt = kv_env(pa, "a_")
                    for n in range(NT):
                        kvt(xb[n * 128:(n + 1) * 128, :], KT[:, :, n * 128:(n + 1) * 128], Vr[:, n, :], kiT[:, n * 128:(n + 1) * 128],
                            r_KT, r_V, r_kiT, o_k[n * 128:(n + 1) * 128, :], o_v[n * 128:(n + 1) * 128, :], o_ik[n * 128:(n + 1) * 128, :])
                    s.barrier()
            if "B" in PH:
                with ExitStack() as pb:
                    env = attn_env(pb, "b_", NT * 128)
                    score, sm, QTb, aoT, rsum, cmask, identrep, Mb = env["score"], env["sm"], env["QTb"], env["aoT"], env["rsum"], env["cmask"], env["identrep"], env["Mb"]
                    r_score, r_sm, r_QTb, r_aoT, r_rsum, r_bc, r_Mb = env["r_score"], env["r_sm"], env["r_QTb"], env["r_aoT"], env["r_rsum"], env["r_bc"], env["r_Mb"]
                    load_block, indexer_tile, threshold, make_mb, attend_chunk = env["load_block"], env["indexer_tile"], env["threshold"], env["make_mb"], env["attend_chunk"]
                    for i in range(NOWN):
                      load_block(i)
                      nkc = i + 1
                      for kc in range(nkc):
                          indexer_tile(list(range(16)), kiT[:, kc * 512:(kc + 1) * 512], r_kiT, 512, kc * 512, None, False)
                      ncols = nkc * 512
                      threshold(ncols, (ncols - 512, ncols, cmask[:]))
                      nch = 4 * nkc
                      for c in range(nch):
                          if c % 4 == 0:
                              mi = make_mb(c * 128, 512)
                          attend_chunk(c == 0, c == nch - 1, 128,
                                       lambda kvh: KT[:, kvh, c * 128:(c + 1) * 128],
                                       lambda kvh: Vr[:, c, kvh * 128:(kvh + 1) * 128], [r_KT, r_V],
                                       lambda kvh: QTb[:, 4 * kvh:4 * kvh + 4, :], r_QTb, 512,
                                       Mb[mi][:, (c % 4) * 128:(c % 4 + 1) * 128], identrep[:], r_Mb[mi])
                      s.op("dve", lambda e: e.reciprocal(out=rsum[:], in_=bk(6, 2).rearrange("p (a b) -> p a b", b=512)),
                           writes=[rb[6], rb[7], r_rsum])
                      s.op("dve", lambda e: e.tensor_tensor(out=aoT[:], in0=bk(4, 2).rearrange("p (h t) -> p h t", t=128),
                                                            in1=rsum[:].rearrange("p a (h t) -> p (a h) t", t=128), op=ALU.mult),
                           reads=[r_rsum], writes=[rb[4], rb[5], r_aoT])
                      s.dma("sp", lambda e: e.dma_start(out=mixT_scr[i, :, 8:16, :], in_=aoT[:]), reads=[r_aoT])

                    s.barrier()


        if "C" in PH:
            supers = [list(range(i, min(i + SB, NOWN))) for i in range(0, NOWN, SB)] + [[SBLK]]
            with ExitStack() as pc1:
                sbc = lambda name, shape, dtype: pc1.enter_context(nc.sbuf_tensor(_un(name), shape, dtype))
                gpm = sbc("gpm", [128, D], F32); r_gpm = s.res(); load_gain(gpm, g_post_mix, r_gpm)
                gpf = sbc("gpf", [128, D], F32); r_gpf = s.res(); load_gain(gpf, g_pre_ffn, r_gpf)
                mixT = sbc("mixT", [128, 16, SB, 128], BF16); r_mixT = s.res()
                Wo = [sbc("Wo%d" % i, [128, 16, 512], BF16) for i in range(2)]
                r_Wo = [s.res() for _ in range(2)]
                mixed = sbc("mixed", [128, SB, D], F32); r_mixed = s.res()
                xt = sbc("xtC", [128, D], F32); r_xt = s.res()
                tmp = sbc("tmpC", [128, D], F32); r_tmp = s.res()
                hbf = sbc("hbf", [128, D], BF16); r_hbf = s.res()
                hT = sbc("hTC", [128, 16, 128], BF16); r_hT = s.res()
                junk = sbc("junkC", [128, D], BF16); r_junk = s.res()
                ssq = sbc("ssqC", [128, 1], F32); r_ssq = s.res()
                wc = 0
                mc = 0
                for blks in supers:
                    nb = len(blks)
                    is_s = blks[0] == SBLK
                    for bi, blk in enumerate(blks):
                        s.dma("sp", lambda e: e.dma_start(out=mixT[:, :, bi, :], in_=mixT_scr[blk]), writes=[r_mixT])
                    for nt in range(4):
                        wi_ = wc % 2; wc += 1
                        for k4 in range(4):
                            s.dma("pool", lambda e, k4=k4: e.dma_start(out=Wo[wi_][:, 4 * k4:4 * k4 + 4, :], in_=w_out_r[:, 4 * k4:4 * k4 + 4, nt * 512:(nt + 1) * 512]),
                                  writes=[r_Wo[wi_]])
                        for bi in range(nb):
                            pbk = mc % 4; mc += 1
                            for k in range(16):
                                s.op("pe", lambda e, k=k: e.matmul(bk(pbk)[:, :], mixT[:, k, bi, :], Wo[wi_][:, k, :], start=(k == 0), stop=(k == 15)),
                                     reads=[r_mixT, r_Wo[wi_]], writes=[rb[pbk]])
                            s.op("act", lambda e: e.copy(out=mixed[:, bi, nt * 512:(nt + 1) * 512], in_=bk(pbk)[:, :]), writes=[rb[pbk], r_mixed])
                    for bi, blk in enumerate(blks):
                        src = xs[32:160, :] if is_s else xo[blk, 32:160, :]
                        s.dma("sp", lambda e: e.dma_start(out=xt[:], in_=src), writes=[r_xt])
                        rms_to_bf(128, mixed[:, bi, :], r_mixed, gpm, r_gpm, tmp[:], r_tmp, (junk, r_junk, ssq, r_ssq))
                        s.op("pool", lambda e: e.tensor_tensor(out=xt[:], in0=xt[:], in1=tmp[:], op=ALU.add), reads=[r_tmp], writes=[r_xt])
                        s.dma("sp", lambda e: e.dma_start(out=x1_scr[blk], in_=xt[:]), reads=[r_xt])
                        rms_to_bf(128, xt[:], r_xt, gpf, r_gpf, hbf[:], r_hbf, (junk, r_junk, ssq, r_ssq))
                        tp = bkb(4, 2).rearrange("p (k t) -> p k t", t=128)
                        for k in range(16):
                            s.op("pe", lambda e, k=k: e.transpose(tp[:, k, :], hbf[:, k * 128:(k + 1) * 128], ident[:]),
                                 reads=[r_hbf, r_c], writes=[rb[4], rb[5]])
                        s.op("act", lambda e: e.copy(out=hT[:], in_=tp[:, :, :]), writes=[rb[4], rb[5], r_hT])
                        s.dma("sp", lambda e: e.dma_start(out=hT_scr[blk], in_=hT[:]), reads=[r_hT])
                s.barrier()
            with ExitStack() as pc2:
                sbc = lambda name, shape, dtype: pc2.enter_context(nc.sbuf_tensor(_un(name), shape, dtype))
                gff = sbc("gff", [128, D], F32); r_gff = s.res(); load_gain(gff, g_post_ffn, r_gff)
                hTs = sbc("hTs", [128, 16, SB, 128], BF16); r_hTs = s.res()
                aT = sbc("aT", [128, NFT, SB * 128], BF16); r_aT = s.res()
                Wg = [sbc("Wg%d" % i, [128, 16, 128], BF16) for i in range(2)]
                Wu = [sbc("Wu%d" % i, [128, 16, 128], BF16) for i in range(2)]
                r_Wg = [s.res() for _ in range(2)]
                r_Wu = [s.res() for _ in range(2)]
                Wd = sbc("Wd", [128, NFT, 256], BF16); r_Wd = s.res()
                sg = sbc("sg", [128, SB * 128], F32); r_sg = s.res()
                fo = sbc("fo", [128, SB, D], F32); r_fo = s.res()
                x1t = sbc("x1t", [128, D], F32); r_x1t = s.res()
                tmp = sbc("tmpC2", [128, D], F32); r_tmp = s.res()
                junk = sbc("junkC2", [128, D], BF16); r_junk = s.res()
                ssq = sbc("ssqC2", [128, 1], F32); r_ssq = s.res()
                dc = 0
                for blks in supers:
                    nb = len(blks)
                    N = nb * 128
                    for bi, blk in enumerate(blks):
                        s.dma("sp", lambda e: e.dma_start(out=hTs[:, :, bi, :], in_=hT_scr[blk]), writes=[r_hTs])
                    for ft in range(NFT):
                        wi_ = ft % 2
                        s.dma("pool", lambda e: e.dma_start(out=Wg[wi_][:], in_=w_gate_r[:, :, ft * 128:(ft + 1) * 128]), writes=[r_Wg[wi_]])
                        s.dma("pool", lambda e: e.dma_start(out=Wu[wi_][:], in_=w_up_r[:, :, ft * 128:(ft + 1) * 128]), writes=[r_Wu[wi_]])
                        gb, ub = 2 * wi_, 2 * wi_ + 1
                        for k in range(16):
                            s.op("pe", lambda e, k=k: e.matmul(bk(gb)[:, 0:N], Wg[wi_][:, k, :], hTs[:, k, 0:nb, :], start=(k == 0), stop=(k == 15)),
                                 reads=[r_hTs, r_Wg[wi_]], writes=[rb[gb]])
                        for k in range(16):
                            s.op("pe", lambda e, k=k: e.matmul(bk(ub)[:, 0:N], Wu[wi_][:, k, :], hTs[:, k, 0:nb, :], start=(k == 0), stop=(k == 15)),
                                 reads=[r_hTs, r_Wu[wi_]], writes=[rb[ub]])
                        s.op("act", lambda e: e.activation(out=sg[:, 0:N], in_=bk(gb)[:, 0:N], func=AF.Silu), writes=[rb[gb], r_sg])
                        s.op("dve", lambda e: e.tensor_tensor(out=aT[:, ft, 0:N], in0=bk(ub)[:, 0:N], in1=sg[:, 0:N], op=ALU.mult),
                             reads=[r_sg], writes=[rb[ub], r_aT])
                    for nt in range(8):
                        for f4 in range(4):
                            s.dma("pool", lambda e, f4=f4: e.dma_start(out=Wd[:, 11 * f4:11 * f4 + 11, :], in_=w_down_r[:, 11 * f4:11 * f4 + 11, nt * 256:(nt + 1) * 256]),
                                  writes=[r_Wd])
                        for bi in range(nb):
                            pbk = 4 + dc % 4; dc += 1
                            for ft in range(NFT):
                                s.op("pe", lambda e, ft=ft: e.matmul(bk(pbk)[:, 0:256], aT[:, ft, bi * 128:(bi + 1) * 128], Wd[:, ft, :], start=(ft == 0), stop=(ft == NFT - 1)),
                                     reads=[r_aT, r_Wd], writes=[rb[pbk]])
                            s.op("act", lambda e: e.copy(out=fo[:, bi, nt * 256:(nt + 1) * 256], in_=bk(pbk)[:, 0:256]), writes=[rb[pbk], r_fo])
                    for bi, blk in enumerate(blks):
                        s.dma("sp", lambda e: e.dma_start(out=x1t[:], in_=x1_scr[blk]), writes=[r_x1t])
                        rms_to_bf(128, fo[:, bi, :], r_fo, gff, r_gff, tmp[:], r_tmp, (junk, r_junk, ssq, r_ssq))
                        s.op("pool", lambda e: e.tensor_tensor(out=x1t[:], in0=x1t[:], in1=tmp[:], op=ALU.add), reads=[r_tmp], writes=[r_x1t])
                        s.dma("sp", lambda e: e.dma_start(out=o_y[blk], in_=x1t[:]), reads=[r_x1t])
                s.barrier()
        s.finish()

    return nc


def _consts(j):
    c = {}
    c["identb"] = _bf(np.eye(128, dtype=np.float32))
    c["ident32"] = np.eye(128, dtype=np.float32)
    r = np.arange(128)[:, None]
    sp = np.arange(512)[None, :]
    c["cmask"] = np.where(sp <= 128 * j + r, 0.0, -1e30).astype(np.float32)
    mg = np.zeros((128, 16, 128), np.float32)
    for g in range(16):
        for row in range(128):
            mg[row, g, 8 * g + row % 8] = 1.0
    c["maskg"] = _bf(mg)
    c["identrep"] = _bf(np.repeat(np.eye(128, dtype=np.float32)[:, None, :], 4, axis=1))
    cs = np.full((128, 128), -1e30, np.float32)
    for row in range(32):
        q, t = row // 8, row % 8
        for col in range(32):
            q2, t2 = col // 8, col % 8
            if q2 == q and t2 <= t:
                cs[row, col] = 0.0
    c["cmask_s"] = cs
    sr = np.zeros((32, 4, 32), np.float32)
    for q in range(4):
        for h in range(4):
            for t in range(8):
                sr[8 * q + t, q, h * 8 + t] = 1.0
    c["selrep"] = _bf(sr)
    c["iota_p"] = np.arange(128, dtype=np.float32)[:, None]
    return c


def make_in_map(c, inp, NT, NPG):
    b, j = c // 4, c % 4
    NOWN = NT // 4
    f = lambda a: np.ascontiguousarray(a, dtype=np.float32)
    xp = inp["x_prompt"][b]
    m = {"xb": f(xp[:NT * 128])}
    xo = np.zeros((NOWN, 160, D), np.float32)
    for i in range(NOWN):
        g0 = (4 * i + j) * 128
        xo[i, 32:160] = xp[g0:g0 + 128]
        if g0 > 0:
            xo[i, 2:32] = xp[g0 - 30:g0]
    m["xo"] = xo
    xs_ = np.zeros((160, D), np.float32)
    xs_[32:64] = inp["x_sample"][4 * c:4 * c + 4].reshape(32, D)
    m["xs"] = xs_
    for k in ("w_in", "w_out", "w_gate", "w_up", "w_down", "g_pre_mix", "g_post_mix", "g_pre_ffn", "g_post_ffn",
              "conv_w", "conv_b", "conv_ln_g", "conv_ln_b"):
        m[k] = f(inp[k][0]) if inp[k][0].ndim == 2 else f(inp[k][0])[None, :]
    npool = inp["cache_k"].shape[1]
    m["cache_k"] = f(inp["cache_k"][0]).reshape(npool * PAGE, NKV * HD)
    m["cache_v"] = f(inp["cache_v"][0]).reshape(npool * PAGE, NKV * HD)
    m["cache_ik"] = f(inp["cache_idx_k"][0]).reshape(npool, PAGE * IDIM)
    m["state_conv"] = f(inp["state_conv"][0, 4 * c:4 * c + 4])
    m["ptab"] = np.ascontiguousarray(inp["page_table"][4 * c:4 * c + 4, :NPG], dtype=np.int32)
    m.update(_consts(j))
    return m


_NC_CACHE = {}


def kernel(**inp):
    NT, NPG = SEQ // 128, NPAGES
    npool = inp["cache_k"].shape[1]
    key = (NT, NPG, npool)
    if key not in _NC_CACHE:
        _NC_CACHE[key] = build({"NT": NT, "NPG": NPG, "NPOOL": npool})
    nc = _NC_CACHE[key]
    in_maps = [make_in_map(c, inp, NT, NPG) for c in range(8)]
    res = run_bass_kernel_spmd(nc, in_maps, core_ids=list(range(8))).results
    return assemble(res, NT)


def assemble(res, NT):
    NOWN = NT // 4
    S_ = NT * 128
    y_p = np.zeros((NB, S_, D), np.float32)
    y_s = np.zeros((DEC_B, DEC_T, D), np.float32)
    nk = np.zeros((1, NB, S_, NKV, HD), np.float32)
    nv = np.zeros((1, NB, S_, NKV, HD), np.float32)
    nik = np.zeros((1, NB, S_, IDIM), np.float32)
    ncp = np.zeros((1, NB, CW - 1, CONV_CH), np.float32)
    nks = np.zeros((1, DEC_B, DEC_T, NKV, HD), np.float32)
    nvs = np.zeros((1, DEC_B, DEC_T, NKV, HD), np.float32)
    niks = np.zeros((1, DEC_B, DEC_T, IDIM), np.float32)
    ncs = np.zeros((1, DEC_B, CW - 1, CONV_CH), np.float32)
    for c in range(len(res)):
        r = res[c]
        if r is None:
            continue
        b, j = c // 4, c % 4
        oy = np.asarray(r["o_y"])
        for i in range(NOWN):
            g0 = (4 * i + j) * 128
            y_p[b, g0:g0 + 128] = oy[i]
        y_s[4 * c:4 * c + 4] = oy[NOWN][0:32].reshape(4, DEC_T, D)
        if j == 0:
            nk[0, b] = np.asarray(r["o_k"]).reshape(S_, NKV, HD)
            nv[0, b] = np.asarray(r["o_v"]).reshape(S_, NKV, HD)
            nik[0, b] = np.asarray(r["o_ik"])
        if j == 3:
            ncp[0, b] = np.asarray(r["o_convp"])
        nks[0, 4 * c:4 * c + 4] = np.asarray(r["o_ks"])[0:32].reshape(4, DEC_T, NKV, HD)
        nvs[0, 4 * c:4 * c + 4] = np.asarray(r["o_vs"])[0:32].reshape(4, DEC_T, NKV, HD)
        niks[0, 4 * c:4 * c + 4] = np.asarray(r["o_iks"])[0:32].reshape(4, DEC_T, IDIM)
        ncs[0, 4 * c:4 * c + 4] = np.asarray(r["o_convs"])
    return (y_p, y_s, nk, nv, nik, ncp, nks, nvs, niks, ncs)
```

```python
import numpy as np
import ml_dtypes
import concourse.bass as bass
import concourse.mybir as mybir
from concourse.bass_utils import run_bass_kernel_spmd

F32 = mybir.dt.float32
BF16 = mybir.dt.bfloat16
I32 = mybir.dt.int32
U32 = mybir.dt.uint32
U8 = mybir.dt.uint8
AF = mybir.ActivationFunctionType
ALU = mybir.AluOpType
AX = mybir.AxisListType

D = 2048
SEQ = 8192
NB = 2
CONV_CH = 1024
NH = 8
HD = 128
NKV = 2
NIH = 16
IDIM = 64
TOPK = 256
CW = 31
DFF = 5632
N_IN = 4688
EPS = 1e-6
ATTN_SCALE = HD ** -0.5
INDEX_SCALE = (NIH * IDIM) ** -0.5
DEC_B = 32
DEC_T = 8
PAST = 16384
PAGE = 128
NPAGES = PAST // PAGE
NEG = -30000.0
NBIS = 20


class Res:
    __slots__ = ("name", "w", "rd")

    def __init__(self, name=""):
        self.name = name
        self.w = None
        self.rd = {}


class S:
    R = 8

    def __init__(self, nc, stack):
        self.nc = nc
        self.eng = {"pe": nc.tensor, "act": nc.scalar, "dve": nc.vector, "pool": nc.gpsimd, "sp": nc.sync}
        self.sem = {k: stack.enter_context(nc.semaphore("s_" + k)) for k in self.eng}
        self.cnt = {k: 0 for k in self.eng}
        self.dsem = {k: [stack.enter_context(nc.semaphore("d_%s%d" % (k, i))) for i in range(self.R)]
                     for k in ("sp", "pool", "act")}
        self.dn = {k: 0 for k in self.dsem}
        self.waited = {k: {} for k in self.eng}
        self.pending_dma = []
        self.nres = 0

    def res(self, name=""):
        return Res(name)

    def _wait(self, e, tok):
        if tok is None:
            return
        kind, key, val = tok
        if kind == "c":
            if key == e and e == "pe":
                return
            sem = self.sem[key]
            wk = ("c", key)
        else:
            sem = self.dsem[key[0]][key[1]]
            wk = ("d", key)
        if self.waited[e].get(wk, 0) >= val:
            return
        self.waited[e][wk] = val
        self.eng[e].wait_ge(sem, val)

    def _deps(self, e, reads, writes):
        toks = []
        for r in reads:
            if r.w is not None:
                toks.append(r.w)
        for w in writes:
            if w.w is not None:
                toks.append(w.w)
            for k, t in w.rd.items():
                if isinstance(t, list):
                    toks.extend(t)
                else:
                    toks.append(t)
        for t in toks:
            self._wait(e, t)

    def _mark(self, e, tok, reads, writes, is_dma):
        for r in reads:
            if is_dma:
                r.rd.setdefault("dma", []).append(tok)
            else:
                r.rd[e] = tok
        for w in writes:
            w.w = tok
            w.rd = {}

    def op(self, e, fn, reads=(), writes=()):
        self._deps(e, reads, writes)
        inst = fn(self.eng[e])
        self.cnt[e] += 1
        inst.then_inc(self.sem[e], 1)
        tok = ("c", e, self.cnt[e])
        self._mark(e, tok, reads, writes, False)
        return tok

    def dma(self, e, fn, reads=(), writes=()):
        n = self.dn[e]
        slot = n % self.R
        val = 16 * (n // self.R + 1)
        if val > 16:
            self._wait(e, ("d", (e, slot), val - 16))
        self._deps(e, reads, writes)
        inst = fn(self.eng[e])
        inst.then_inc(self.dsem[e][slot], 16)
        self.dn[e] = n + 1
        tok = ("d", (e, slot), val)
        self._mark(e, tok, reads, writes, True)
        self.pending_dma.append(tok)
        return tok

    def barrier(self):
        toks = [("c", k, self.cnt[k]) for k in self.eng if self.cnt[k] > 0]
        toks += self.pending_dma
        self.pending_dma = []
        for e in self.eng:
            for t in toks:
                if t[0] == "c" and t[1] == e:
                    continue
                self._wait(e, t)

    def finish(self):
        self.barrier()


_UN = [0]


def _un(name):
    _UN[0] += 1
    return "t%d_%s" % (_UN[0], name)


def _bf(a):
    return np.ascontiguousarray(a).astype(ml_dtypes.bfloat16)


def build(cfg):
    from contextlib import ExitStack
    NT = cfg.get("NT", SEQ // 128)
    NOWN = NT // 4
    SB = min(4, NOWN)
    NPG = cfg.get("NPG", NPAGES)
    NPOOL = cfg.get("NPOOL", 5120)
    NFT = DFF // 128
    NBLK = NOWN + 1
    SBLK = NOWN
    PH = cfg.get("PH", "UABSC")
    nc = bass.Bass("TRN2", target_bir_lowering=False)

    def din(name, shape, dtype=F32):
        return nc.dram_tensor(name, shape, dtype, kind="ExternalInput").ap()

    def dout(name, shape, dtype=F32):
        return nc.dram_tensor(name, shape, dtype, kind="ExternalOutput").ap()

    def dscr(name, shape, dtype):
        return nc.dram_tensor(name, shape, dtype, kind="Internal").ap()

    xb = din("xb", [NT * 128, D])
    xo = din("xo", [NOWN, 160, D])
    xs = din("xs", [160, D])
    w_in = din("w_in", [D, N_IN])
    w_out = din("w_out", [D, D])
    w_gate = din("w_gate", [D, DFF])
    w_up = din("w_up", [D, DFF])
    w_down = din("w_down", [DFF, D])
    g_pre_mix = din("g_pre_mix", [1, D])
    g_post_mix = din("g_post_mix", [1, D])
    g_pre_ffn = din("g_pre_ffn", [1, D])
    g_post_ffn = din("g_post_ffn", [1, D])
    conv_w = din("conv_w", [CW, CONV_CH])
    conv_b = din("conv_b", [1, CONV_CH])
    ln_g = din("conv_ln_g", [1, CONV_CH])
    ln_b = din("conv_ln_b", [1, CONV_CH])
    cache_k = din("cache_k", [NPOOL * PAGE, NKV * HD])
    cache_v = din("cache_v", [NPOOL * PAGE, NKV * HD])
    cache_ik = din("cache_ik", [NPOOL, PAGE * IDIM])
    state_conv = din("state_conv", [4, CW - 1, CONV_CH])
    ptab = din("ptab", [4, NPG], I32)
    identb_d = din("identb", [128, 128], BF16)
    ident32_d = din("ident32", [128, 128])
    cmask_d = din("cmask", [128, 512])
    maskg_d = din("maskg", [128, 16, 128], BF16)
    identrep_d = din("identrep", [128, 4, 128], BF16)
    cmask_s_d = din("cmask_s", [128, 128])
    selrep_d = din("selrep", [32, 4, 32], BF16)
    iota_d = din("iota_p", [128, 1])

    o_k = dout("o_k", [NT * 128, NKV * HD])
    o_v = dout("o_v", [NT * 128, NKV * HD])
    o_ik = dout("o_ik", [NT * 128, IDIM])
    o_y = dout("o_y", [NBLK, 128, D])
    o_convp = dout("o_convp", [CW - 1, CONV_CH])
    o_ks = dout("o_ks", [128, NKV * HD])
    o_vs = dout("o_vs", [128, NKV * HD])
    o_iks = dout("o_iks", [128, IDIM])
    o_convs = dout("o_convs", [4, CW - 1, CONV_CH])

    QT_scr = dscr("QT_scr", [NBLK, 128, 8, 128], BF16)
    qiT_scr = dscr("qiT_scr", [NBLK, 64, 16, 2, 64], BF16)
    Wsel_scr = dscr("Wsel_scr", [NBLK, 128, 16, 128], BF16)
    mixT_scr = dscr("mixT_scr", [NBLK, 128, 16, 128], BF16)
    x1_scr = dscr("x1_scr", [NBLK, 128, D], F32)
    hT_scr = dscr("hT_scr", [NBLK, 128, 16, 128], BF16)

    w_in_r = w_in.rearrange("(k p) n -> p k n", p=128)
    w_out_r = w_out.rearrange("(k p) n -> p k n", p=128)
    w_gate_r = w_gate.rearrange("(k p) n -> p k n", p=128)
    w_up_r = w_up.rearrange("(k p) n -> p k n", p=128)
    w_down_r = w_down.rearrange("(k p) n -> p k n", p=128)

    with ExitStack() as st:
        s = S(nc, st)
        sb = lambda name, shape, dtype: st.enter_context(nc.sbuf_tensor(_un(name), shape, dtype))
        PS = st.enter_context(nc.psum_tensor("PS", [128, 8 * 512], F32))
        rb = [s.res("bank%d" % i) for i in range(8)]

        def bk(i, n=1):
            return PS[:, i * 512:(i + n) * 512]

        def bkb(i, n=1):
            return PS[:, i * 512:(i + n) * 512].bitcast(BF16)

        ident = sb("ident", [128, 128], BF16)
        ident32 = sb("ident32", [128, 128], F32)
        ones_bf = sb("ones_bf", [128, 128], BF16)
        ones32 = sb("ones32", [128, 128], F32)
        r_c = s.res("consts")
        s.dma("sp", lambda e: e.dma_start(out=ident[:], in_=identb_d[:, :]), writes=[r_c])
        s.dma("sp", lambda e: e.dma_start(out=ident32[:], in_=ident32_d[:, :]), writes=[r_c])
        s.op("dve", lambda e: e.memset(ones_bf[:], 1.0), writes=[r_c])
        s.op("dve", lambda e: e.memset(ones32[:], 1.0), writes=[r_c])

        def load_gain(tile, g_ap, r):
            s.dma("sp", lambda e: e.dma_start(out=tile[:], in_=g_ap.to_broadcast([128, D])), writes=[r])

        def rms_to_bf(P, x_ap, r_x, g_tile, r_g, out_ap, r_out, tmp):
            junk, r_junk, ssq, r_ss = tmp
            s.op("act", lambda e: e.activation(out=junk[0:P, :], in_=x_ap, func=AF.Square, accum_out=ssq[0:P, :]),
                 reads=[r_x], writes=[r_junk, r_ss])
            s.op("dve", lambda e: e.tensor_scalar(out=ssq[0:P, :], in0=ssq[0:P, :], scalar1=1.0 / D, scalar2=EPS,
                                                  op0=ALU.mult, op1=ALU.add), writes=[r_ss])
            s.op("act", lambda e: e.activation(out=ssq[0:P, :], in_=ssq[0:P, :], func=AF.Sqrt), writes=[r_ss])
            s.op("dve", lambda e: e.reciprocal(out=ssq[0:P, :], in_=ssq[0:P, :]), writes=[r_ss])
            s.op("dve", lambda e: e.scalar_tensor_tensor(out=out_ap, in0=x_ap, scalar=ssq[0:P, 0:1], in1=g_tile[0:P, :],
                                                         op0=ALU.mult, op1=ALU.mult),
                 reads=[r_x, r_ss, r_g], writes=[r_out])

        if "U" in PH:
          with ExitStack() as pu:
            sbu = lambda name, shape, dtype: pu.enter_context(nc.sbuf_tensor(_un(name), shape, dtype))
            gmix = sbu("gmixU", [128, D], F32); r_gmix = s.res()
            load_gain(gmix, g_pre_mix, r_gmix)
            cvec = sbu("cvec", [128, 3, 8], F32); r_cvec = s.res()
            for vi, v_ap in enumerate((conv_b, ln_g, ln_b)):
                s.dma("sp", lambda e, vi=vi, v_ap=v_ap: e.dma_start(out=cvec[:, vi, :], in_=v_ap.rearrange("o (c p) -> p (o c)", p=128),
                                                                       allow_slow_non_contiguous=True), writes=[r_cvec])
            cw_sb = sbu("cw_sb", [CW, CONV_CH], F32); r_cw = s.res()
            s.dma("sp", lambda e: e.dma_start(out=cw_sb[:], in_=conv_w[:, :]), writes=[r_cw])
            cwT = sbu("cwT", [128, 8, CW], F32); r_cwT = s.res()
            for c in range(8):
                s.op("pe", lambda e, c=c: e.transpose(bk(0)[:, c * 32:c * 32 + CW], cw_sb[:, c * 128:(c + 1) * 128], ident32[0:CW, 0:CW]),
                     reads=[r_cw, r_c], writes=[rb[0]])
            s.op("dve", lambda e: e.tensor_copy(out=cwT[:], in_=bk(0)[:, 0:256].rearrange("p (c j) -> p c j", j=32)[:, :, 0:CW]),
                 writes=[rb[0], r_cwT])
            diag = sbu("diag", [128, 8, CW, 128], BF16); r_diag = s.res()
            for c in range(8):
                for j in range(CW):
                    s.op("pool", lambda e, c=c, j=j: e.tensor_scalar(out=diag[:, c, j, :], in0=ident[:], scalar1=cwT[:, c, j:j + 1],
                                                                     scalar2=None, op0=ALU.mult),
                         reads=[r_cwT, r_c], writes=[r_diag])
            wwi = sbu("wwi", [128, 16, 16], BF16); r_wwi = s.res()
            s.dma("pool", lambda e: e.dma_start(out=wwi[:], in_=w_in_r[:, :, 4672:4688]), writes=[r_wwi])
            wwirep = sbu("wwirep", [128, 16, 128], BF16); r_wrep = s.res()
            for h2 in range(2):
                for hp in range(8):
                    h = 2 * hp + h2
                    col = h2 * 64 + hp * 8
                    s.op("pool", lambda e, h=h, col=col: e.tensor_copy(
                        out=wwirep[:, :, col:col + 8], in_=wwi[:, :, h:h + 1].to_broadcast([128, 16, 8])),
                        reads=[r_wwi], writes=[r_wrep])
            maskg = sbu("maskg", [128, 16, 128], BF16); r_maskg = s.res()
            s.dma("sp", lambda e: e.dma_start(out=maskg[:], in_=maskg_d[:, :, :]), writes=[r_maskg])

            xt = sbu("xtU", [128, D], F32); r_xt = s.res()
            xh = sbu("xhU", [32, D], F32); r_xh = s.res()
            junk = sbu("junkU", [128, D], BF16); r_junk = s.res()
            ssq = sbu("ssqU", [128, 1], F32); r_ssq = s.res()
            ssq2 = sbu("ssq2U", [128, 1], F32); r_ssq2 = s.res()
            xn = sbu("xnU", [128, D], BF16); r_xn = s.res()
            xnh = sbu("xnhU", [32, D], BF16); r_xnh = s.res()
            xnT = sbu("xnTU", [128, 16, SB, 160], BF16); r_xnT = s.res()
            Wt = [sbu("WtU%d" % i, [128, 16, 128], BF16) for i in range(2)]
            r_Wt = [s.res() for _ in range(2)]
            uT = sbu("uT", [128, 8, SB, 160], BF16); r_uT = s.res()
            sig = sbu("sig", [128, SB, 160], F32); r_sig = s.res()
            u32 = sbu("u32", [128, 8, 32], F32); r_u32 = s.res()
            Qst = sbu("Qst", [128, SB, 8, 128], BF16); r_Qst = s.res()
            Qi_sb = sbu("Qi_sb", [128, SB, 16, 8, 8], BF16); r_Qi = s.res()
            WIrep = sbu("WIrep", [128, SB, 128], F32); r_WIrep = s.res()
            Wsel = sbu("WselU", [128, 16, 128], BF16); r_Wsel = s.res()
            ycv = sbu("ycv", [128, 8, 128], F32); r_ycv = s.res()
            ysq = sbu("ysq", [128, 8, 128], F32); r_ysq = s.res()
            stat = sbu("stat", [128, 3, 128], F32); r_stat = s.res()
            co = sbu("co", [128, 8, 128], BF16); r_co = s.res()
            stt = sbu("stt", [CW - 1, CONV_CH], F32); r_stt = s.res()
            fullT = sbu("fullT", [128, 8, 4, 38], BF16); r_fullT = s.res()
            cvo = sbu("cvo", [32, CONV_CH], F32); r_cvo = s.res()

            wcnt = [0]

            def conv_tail(blk, TW):
                Y = bk(0, 2).rearrange("p (c t) -> p c t", t=128)
                s.op("dve", lambda e: e.tensor_tensor(out=ycv[:, :, 0:TW], in0=Y[:, :, 0:TW],
                                                      in1=cvec[:, 0, :].unsqueeze(2).to_broadcast([128, 8, TW]), op=ALU.add),
                     reads=[r_cvec], writes=[rb[0], rb[1], r_ycv])
                s.op("act", lambda e: e.activation(out=ysq[:, :, 0:TW], in_=ycv[:, :, 0:TW], func=AF.Square),
                     reads=[r_ycv], writes=[r_ysq])
                for c in range(8):
                    s.op("pe", lambda e, c=c: e.matmul(bk(2)[:, 0:TW], ones32[:], ycv[:, c, 0:TW], start=(c == 0), stop=(c == 7)),
                         reads=[r_ycv, r_c], writes=[rb[2]])
                for c in range(8):
                    s.op("pe", lambda e, c=c: e.matmul(bk(3)[:, 0:TW], ones32[:], ysq[:, c, 0:TW], start=(c == 0), stop=(c == 7)),
                         reads=[r_ysq, r_c], writes=[rb[3]])
                mean, msq, rstd = stat[:, 0, 0:TW], stat[:, 1, 0:TW], stat[:, 2, 0:TW]
                s.op("dve", lambda e: e.tensor_scalar(out=mean, in0=bk(2)[:, 0:TW], scalar1=1.0 / CONV_CH, scalar2=None, op0=ALU.mult),
                     writes=[rb[2], r_stat])
                s.op("dve", lambda e: e.tensor_tensor(out=msq, in0=mean, in1=mean, op=ALU.mult), writes=[r_stat])
                s.op("dve", lambda e: e.scalar_tensor_tensor(out=rstd, in0=bk(3)[:, 0:TW], scalar=1.0 / CONV_CH, in1=msq,
                                                             op0=ALU.mult, op1=ALU.subtract), writes=[rb[3], r_stat])
                s.op("dve", lambda e: e.tensor_scalar(out=rstd, in0=rstd, scalar1=EPS, scalar2=None, op0=ALU.add), writes=[r_stat])
                s.op("act", lambda e: e.activation(out=rstd, in_=rstd, func=AF.Sqrt), writes=[r_stat])
                s.op("dve", lambda e: e.reciprocal(out=rstd, in_=rstd), writes=[r_stat])
                s.op("dve", lambda e: e.tensor_tensor(out=ycv[:, :, 0:TW], in0=ycv[:, :, 0:TW],
                                                      in1=stat[:, 0:1, 0:TW].to_broadcast([128, 8, TW]), op=ALU.subtract),
                     reads=[r_stat], writes=[r_ycv])
                s.op("dve", lambda e: e.tensor_tensor(out=ycv[:, :, 0:TW], in0=ycv[:, :, 0:TW],
                                                      in1=stat[:, 2:3, 0:TW].to_broadcast([128, 8, TW]), op=ALU.mult),
                     reads=[r_stat], writes=[r_ycv])
                if TW < 128:
                    s.op("pool", lambda e: e.memset(co[:], 0.0), writes=[r_co])
                for c in range(8):
                    s.op("act", lambda e, c=c: e.activation(out=co[:, c, 0:TW], in_=ycv[:, c, 0:TW], func=AF.Silu,
                                                            scale=cvec[:, 1, c:c + 1], bias=cvec[:, 2, c:c + 1]),
                         reads=[r_ycv, r_cvec], writes=[r_co])
                s.dma("sp", lambda e: e.dma_start(out=mixT_scr[blk, :, 0:8, :], in_=co[:]), reads=[r_co])

            def u32_out(ncols, dst_fn):
                for c in range(8):
                    s.op("pe", lambda e, c=c: e.transpose(bk(2, 2)[0:ncols, c * 128:(c + 1) * 128], u32[:, c, 0:ncols], ident32[:]),
                         reads=[r_u32, r_c], writes=[rb[2], rb[3]])
                s.op("dve", lambda e: e.tensor_copy(out=cvo[0:ncols, :], in_=bk(2, 2)[0:ncols, :]), writes=[rb[2], rb[3], r_cvo])
                dst_fn()

            supers = [list(range(i, min(i + SB, NOWN))) for i in range(0, NOWN, SB)] + [[SBLK]]
            for blks in supers:
                nb = len(blks)
                is_s = blks[0] == SBLK
                for bi, blk in enumerate(blks):
                    src = xs if is_s else xo[blk]
                    s.dma("sp", lambda e: e.dma_start(out=xh[:], in_=src[0:32, :]), writes=[r_xh])
                    s.dma("sp", lambda e: e.dma_start(out=xt[:], in_=src[32:160, :]), writes=[r_xt])
                    rms_to_bf(32, xh[:], r_xh, gmix, r_gmix, xnh[:], r_xnh, (junk, r_junk, ssq2, r_ssq2))
                    rms_to_bf(128, xt[:], r_xt, gmix, r_gmix, xn[:], r_xn, (junk, r_junk, ssq, r_ssq))
                    tpm = bkb(0, 2).rearrange("p (k t) -> p k t", t=128)
                    tph = bkb(2).rearrange("p (k t) -> p k t", t=64)
                    for k in range(16):
                        s.op("pe", lambda e, k=k: e.transpose(tpm[:, k, :], xn[:, k * 128:(k + 1) * 128], ident[:]),
                             reads=[r_xn, r_c], writes=[rb[0], rb[1]])
                    for k in range(16):
                        s.op("pe", lambda e, k=k: e.transpose(tph[:, k, 0:32], xnh[:, k * 128:(k + 1) * 128], ident[0:32, 0:32]),
                             reads=[r_xnh, r_c], writes=[rb[2]])
                    s.op("act", lambda e: e.copy(out=xnT[:, :, bi, 32:160], in_=tpm[:, :, :]), writes=[rb[0], rb[1], r_xnT])
                    s.op("dve", lambda e: e.tensor_copy(out=xnT[:, :, bi, 0:32], in_=tph[:, :, 0:32]), writes=[rb[2], r_xnT])
                halves = [(0, min(2, nb))] + ([(2, nb)] if nb > 2 else [])

                def proj(ct_cols, lhs_fn, pair):
                    for hi, (b0, b1) in enumerate(halves):
                        n = (b1 - b0) * 160
                        for k in range(16):
                            s.op("pe", lambda e, k=k: e.matmul(bk(pair + hi)[:, 0:n], lhs_fn(k), xnT[:, k, b0:b1, :],
                                                               start=(k == 0), stop=(k == 15)),
                                 reads=[r_xnT] + ct_cols, writes=[rb[pair + hi]])

                def load_w(col0):
                    i = wcnt[0] % 2
                    wcnt[0] += 1
                    s.dma("pool", lambda e: e.dma_start(out=Wt[i][:], in_=w_in_r[:, :, col0:col0 + 128]), writes=[r_Wt[i]])
                    return i

                def pview(pair, hi, b0, b1):
                    return bk(pair + hi)[:, 0:(b1 - b0) * 160].rearrange("p (b t) -> p b t", t=160)

                for c in range(8):
                    ia = load_w(c * 128)
                    proj([r_Wt[ia]], lambda k, ia=ia: Wt[ia][:, k, :], 4)
                    ig = load_w(1024 + c * 128)
                    proj([r_Wt[ig]], lambda k, ig=ig: Wt[ig][:, k, :], 6)
                    for hi, (b0, b1) in enumerate(halves):
                        s.op("act", lambda e: e.activation(out=sig[:, b0:b1, :], in_=pview(6, hi, b0, b1), func=AF.Sigmoid),
                             writes=[rb[6 + hi], r_sig])
                        s.op("dve", lambda e: e.tensor_tensor(out=uT[:, c, b0:b1, :], in0=pview(4, hi, b0, b1), in1=sig[:, b0:b1, :], op=ALU.mult),
                             reads=[r_sig], writes=[rb[4 + hi], r_uT])
                        if is_s:
                            s.op("dve", lambda e: e.tensor_tensor(out=u32[:, c, :], in0=pview(4, hi, b0, b1)[:, 0, 32:64], in1=sig[:, 0, 32:64], op=ALU.mult),
                                 reads=[r_sig], writes=[rb[4 + hi], r_u32])
                        elif blks[-1] == NOWN - 1 and b1 == nb:
                            s.op("dve", lambda e: e.tensor_tensor(out=u32[:, c, 0:30], in0=pview(4, hi, b0, b1)[:, b1 - b0 - 1, 130:160],
                                                                  in1=sig[:, nb - 1, 130:160], op=ALU.mult),
                                 reads=[r_sig], writes=[rb[4 + hi], r_u32])
                for h in range(8):
                    iw = load_w(2048 + h * 128)
                    pair = 4 + 2 * (h % 2)
                    proj([r_Wt[iw]], lambda k, iw=iw: Wt[iw][:, k, :], pair)
                    for hi, (b0, b1) in enumerate(halves):
                        s.op("act", lambda e: e.activation(out=Qst[:, b0:b1, h, :], in_=pview(pair, hi, b0, b1)[:, :, 32:160],
                                                           func=AF.Copy, scale=ATTN_SCALE),
                             writes=[rb[pair + hi], r_Qst])
                for hp in range(8):
                    iw = load_w(3584 + hp * 128)
                    pair = 4 + 2 * (hp % 2)
                    proj([r_Wt[iw]], lambda k, iw=iw: Wt[iw][:, k, :], pair)
                    for hi, (b0, b1) in enumerate(halves):
                        for b in range(b0, b1):
                            s.op("dve", lambda e, b=b: e.tensor_copy(
                                out=Qi_sb[:, b, :, hp, :],
                                in_=pview(pair, hi, b0, b1)[:, b - b0, 32:160].rearrange("p (g t) -> p g t", t=8)),
                                writes=[rb[pair + hi], r_Qi])
                proj([r_wrep], lambda k: wwirep[:, k, :], 4)
                for hi, (b0, b1) in enumerate(halves):
                    s.op("act", lambda e: e.activation(out=WIrep[:, b0:b1, :], in_=pview(4, hi, b0, b1)[:, :, 32:160],
                                                       func=AF.Copy, scale=INDEX_SCALE),
                         writes=[rb[4 + hi], r_WIrep])
                for bi, blk in enumerate(blks):
                    s.dma("sp", lambda e: e.dma_start(out=QT_scr[blk], in_=Qst[:, bi, :, :]), reads=[r_Qst])
                    for h2 in range(2):
                        s.dma("sp", lambda e, h2=h2: e.dma_start(
                            out=qiT_scr[blk, :, :, h2, :],
                            in_=Qi_sb[h2 * 64:(h2 + 1) * 64, bi, :, :, :].rearrange("p g a b -> p g (a b)")),
                            reads=[r_Qi])
                    s.op("dve", lambda e: e.tensor_tensor(out=Wsel[:], in0=maskg[:],
                                                          in1=WIrep[:, bi:bi + 1, :].to_broadcast([128, 16, 128]), op=ALU.mult),
                         reads=[r_maskg, r_WIrep], writes=[r_Wsel])
                    s.dma("sp", lambda e: e.dma_start(out=Wsel_scr[blk], in_=Wsel[:]), reads=[r_Wsel])
                    Y = bk(0, 2).rearrange("p (c t) -> p c t", t=128)
                    if not is_s:
                        for c in range(8):
                            for j in range(CW):
                                s.op("pe", lambda e, c=c, j=j: e.matmul(Y[:, c, :], diag[:, c, j, :], uT[:, c, bi, 2 + j:2 + j + 128],
                                                                        start=(j == 0), stop=(j == CW - 1)),
                                     reads=[r_diag, r_uT], writes=[rb[0], rb[1]])
                        conv_tail(blk, 128)
                    else:
                        for q in range(4):
                            s.dma("sp", lambda e, q=q: e.dma_start(out=stt[:], in_=state_conv[q]), writes=[r_stt])
                            tq = bk(2)[:, 0:256].rearrange("p (c j) -> p c j", j=32)
                            for c in range(8):
                                s.op("pe", lambda e, c=c: e.transpose(tq[:, c, 0:CW - 1], stt[:, c * 128:(c + 1) * 128], ident32[0:CW - 1, 0:CW - 1]),
                                     reads=[r_stt, r_c], writes=[rb[2]])
                            s.op("dve", lambda e, q=q: e.tensor_copy(out=fullT[:, :, q, 0:CW - 1], in_=tq[:, :, 0:CW - 1]),
                                 writes=[rb[2], r_fullT])
                            s.op("dve", lambda e, q=q: e.tensor_copy(out=fullT[:, :, q, CW - 1:CW + 7], in_=uT[:, :, 0, 32 + 8 * q:40 + 8 * q]),
                                 reads=[r_uT], writes=[r_fullT])
                            s.dma("sp", lambda e, q=q: e.dma_start(out=o_convs[q, 0:CW - 9, :], in_=state_conv[q, 8:CW - 1, :]))
                        for c in range(8):
                            for q in range(4):
                                for j in range(CW):
                                    s.op("pe", lambda e, c=c, q=q, j=j: e.matmul(Y[:, c, 8 * q:8 * q + 8], diag[:, c, j, :], fullT[:, c, q, j:j + 8],
                                                                                 start=(j == 0), stop=(j == CW - 1)),
                                         reads=[r_diag, r_fullT], writes=[rb[0], rb[1]])
                        conv_tail(blk, 32)
                        u32_out(32, lambda: [s.dma("sp", lambda e, q=q: e.dma_start(out=o_convs[q, CW - 9:CW - 1, :], in_=cvo[8 * q:8 * q + 8, :]),
                                                   reads=[r_cvo]) for q in range(4)])
                if (not is_s) and blks[-1] == NOWN - 1:
                    u32_out(30, lambda: s.dma("sp", lambda e: e.dma_start(out=o_convp[:, :], in_=cvo[0:30, :]), reads=[r_cvo]))
            s.barrier()

        def kv_env(stack, pfx):
            sba = lambda name, shape, dtype: stack.enter_context(nc.sbuf_tensor(_un(name), shape, dtype))
            gmix = sba(pfx + "gmixA", [128, D], F32); r_gmix = s.res()
            load_gain(gmix, g_pre_mix, r_gmix)
            Wkv = sba(pfx + "Wkv", [128, 16, 576], BF16)
            r_Wkv = s.res()
            for k4 in range(4):
                s.dma("pool", lambda e, k4=k4: e.dma_start(out=Wkv[:, 4 * k4:4 * k4 + 4, 0:512],
                                                            in_=w_in_r[:, 4 * k4:4 * k4 + 4, 3072:3584]), writes=[r_Wkv])
                s.dma("pool", lambda e, k4=k4: e.dma_start(out=Wkv[:, 4 * k4:4 * k4 + 4, 512:576],
                                                            in_=w_in_r[:, 4 * k4:4 * k4 + 4, 4608:4672]), writes=[r_Wkv])
            xt = [sba(pfx + "xt%d" % i, [128, D], F32) for i in range(2)]
            r_xt = [s.res() for _ in range(2)]
            junk = sba(pfx + "junkA", [128, D], BF16); r_junk = s.res()
            xn = [sba(pfx + "xn%d" % i, [128, D], BF16) for i in range(2)]
            r_xn = [s.res() for _ in range(2)]
            xnT = [sba(pfx + "xnT%d" % i, [128, 16, 128], BF16) for i in range(2)]
            r_xnT = [s.res() for _ in range(2)]
            ssq = [sba(pfx + "ssA%d" % i, [128, 1], F32) for i in range(2)]
            r_ss = [s.res() for _ in range(2)]
            kvf = [sba(pfx + "kvf%d" % i, [128, 576], F32) for i in range(2)]
            r_kvf = [s.res() for _ in range(2)]
            kb = [sba(pfx + "kb%d" % i, [128, 320], BF16) for i in range(2)]
            r_kb = [s.res() for _ in range(2)]
            tp = bkb(0, 2).rearrange("p (k t) -> p k t", t=128)
            tp2 = bkb(6).rearrange("p (k t) -> p k t", t=128)

            kvcnt = [0]

            def kv_tile(src, KT_dst, V_dst, kiT_dst, r_KT, r_V, r_kiT, dk, dv, dik):
                b = kvcnt[0] % 2; kvcnt[0] += 1
                pkv, pki = bk(2 + b), bk(4 + b)
                r_pkv = [rb[2 + b], rb[4 + b]]
                s.dma("sp", lambda e: e.dma_start(out=xt[b][:], in_=src), writes=[r_xt[b]])
                rms_to_bf(128, xt[b][:], r_xt[b], gmix, r_gmix, xn[b][:], r_xn[b], (junk, r_junk, ssq[b], r_ss[b]))
                for k in range(16):
                    s.op("pe", lambda e, k=k: e.transpose(tp[:, k, :], xn[b][:, k * 128:(k + 1) * 128], ident[:]),
                         reads=[r_xn[b], r_c], writes=[rb[0], rb[1]])
                s.op("act", lambda e: e.copy(out=xnT[b][:], in_=tp[:, :, :]), writes=[rb[0], rb[1], r_xnT[b]])
                for k in range(16):
                    s.op("pe", lambda e, k=k: e.matmul(pkv[:, :], xnT[b][:, k, :], Wkv[:, k, 0:512], start=(k == 0), stop=(k == 15)),
                         reads=[r_xnT[b], r_Wkv], writes=r_pkv)
                for k in range(16):
                    s.op("pe", lambda e, k=k: e.matmul(pki[:, 0:64], xnT[b][:, k, :], Wkv[:, k, 512:576], start=(k == 0), stop=(k == 15)),
                         reads=[r_xnT[b], r_Wkv], writes=r_pkv)
                s.op("dve", lambda e: e.tensor_copy(out=kvf[b][:, 0:512], in_=pkv[:, :]), writes=r_pkv + [r_kvf[b]])
                s.op("dve", lambda e: e.tensor_copy(out=kvf[b][:, 512:576], in_=pki[:, 0:64]), writes=r_pkv + [r_kvf[b]])
                s.op("act", lambda e: e.copy(out=kb[b][:, 0:256], in_=pkv[:, 0:256]), writes=r_pkv + [r_kb[b]])
                s.op("act", lambda e: e.copy(out=V_dst, in_=pkv[:, 256:512]), writes=r_pkv + [r_V])
                s.op("act", lambda e: e.copy(out=kb[b][:, 256:320], in_=pki[:, 0:64]), writes=r_pkv + [r_kb[b]])
                s.op("pe", lambda e: e.transpose(tp2[:, 0, :], kb[b][:, 0:128], ident[:]), reads=[r_kb[b], r_c], writes=[rb[6]])
                s.op("pe", lambda e: e.transpose(tp2[:, 1, :], kb[b][:, 128:256], ident[:]), reads=[r_kb[b], r_c], writes=[rb[6]])
                s.op("pe", lambda e: e.transpose(tp2[0:64, 2, :], kb[b][:, 256:320], ident[:]), reads=[r_kb[b], r_c], writes=[rb[6]])
                s.op("dve", lambda e: e.tensor_copy(out=KT_dst, in_=tp2[:, 0:2, :]), writes=[rb[6], r_KT])
                s.op("dve", lambda e: e.tensor_copy(out=kiT_dst, in_=tp2[0:64, 2, :]), writes=[rb[6], r_kiT])
                s.dma("sp", lambda e: e.dma_start(out=dk, in_=kvf[b][:, 0:256]), reads=[r_kvf[b]])
                s.dma("sp", lambda e: e.dma_start(out=dv, in_=kvf[b][:, 256:512]), reads=[r_kvf[b]])
                s.dma("sp", lambda e: e.dma_start(out=dik, in_=kvf[b][:, 512:576]), reads=[r_kvf[b]])

            return kv_tile

        def attn_env(stack, pfx, NSC):
            sbb = lambda name, shape, dtype: stack.enter_context(nc.sbuf_tensor(_un(name), shape, dtype))
            NS_ = 2 if pfx == "b_" else 1
            scores = [sbb(pfx + "score%d" % i, [128, NSC], F32) for i in range(NS_)]
            r_scores = [s.res() for _ in range(NS_)]
            cur = {"i": 0}
            wk = sbb(pfx + "wk", [128, NBIS], F32); r_wk = s.res()
            pw2 = sbb(pfx + "pw2", [128, NBIS], F32); r_pw2 = s.res()
            for it_ in range(NBIS):
                s.op("pool", lambda e, it_=it_: e.memset(pw2[:, it_:it_ + 1], 0.5 ** (it_ + 1)), writes=[r_pw2])
            cjunk = sbb(pfx + "cjunk", [128, NSC], U8); r_cjunk = s.res()
            cmask = sbb(pfx + "cmask", [128, 512], F32)
            cmask_s = sbb(pfx + "cmask_s", [128, 128], F32)
            identrep = sbb(pfx + "identrep", [128, 4, 128], BF16)
            selrep = sbb(pfx + "selrep", [32, 4, 32], BF16)
            iota_p = sbb(pfx + "iota_p", [128, 1], F32)
            r_bc = s.res()
            s.dma("sp", lambda e: e.dma_start(out=cmask[:], in_=cmask_d[:, :]), writes=[r_bc])
            s.dma("sp", lambda e: e.dma_start(out=cmask_s[:], in_=cmask_s_d[:, :]), writes=[r_bc])
            s.dma("sp", lambda e: e.dma_start(out=identrep[:], in_=identrep_d[:, :, :]), writes=[r_bc])
            s.dma("sp", lambda e: e.dma_start(out=selrep[:], in_=selrep_d[:, :, :]), writes=[r_bc])
            s.dma("sp", lambda e: e.dma_start(out=iota_p[:], in_=iota_d[:, :]), writes=[r_bc])
            QTb = sbb(pfx + "QTb", [128, 8, 128], BF16); r_QTb = s.res()
            qiTb = sbb(pfx + "qiTb", [64, 16, 128], BF16); r_qiTb = s.res()
            Wselb = sbb(pfx + "Wselb", [128, 16, 128], BF16); r_Wselb = s.res()
            Rt = [sbb(pfx + "Rt%d" % i, [128, 512], BF16) for i in range(4)]
            r_Rt = [s.res() for _ in range(4)]
            Mb = [sbb(pfx + "Mb%d" % i, [128, 512], BF16) for i in range(2)]
            r_Mb = [s.res() for _ in range(2)]
            Pt = [sbb(pfx + "Pt%d" % i, [128, 512], BF16) for i in range(3)]
            r_Pt = [s.res() for _ in range(3)]
            sms = [sbb(pfx + "smallB%d" % i, [128, 8], F32) for i in range(NS_)]
            r_sms = [s.res() for _ in range(NS_)]
            rsum = sbb(pfx + "rsum", [128, 2, 512], F32); r_rsum = s.res()
            aoT = sbb(pfx + "aoT", [128, 8, 128], BF16); r_aoT = s.res()
            cnt = {"r": 0, "m": 0, "p": 0, "l": 0}

            def load_q(blk):
                s.dma("sp", lambda e: e.dma_start(out=QTb[:], in_=QT_scr[blk]), writes=[r_QTb])

            def load_block(blk):
                s.dma("sp", lambda e: e.dma_start(out=qiTb[:], in_=qiT_scr[blk].rearrange("d g a b -> d g (a b)")), writes=[r_qiTb])
                s.dma("sp", lambda e: e.dma_start(out=Wselb[:], in_=Wsel_scr[blk]), writes=[r_Wselb])

            def indexer_tile(groups, ki_ap, r_ki, ncol, dst_col, mask_ap, accumulate):
                for gi, g in enumerate(groups):
                    lb = cnt["l"] % 3; cnt["l"] += 1
                    s.op("pe", lambda e: e.matmul(bk(lb)[:, 0:ncol], qiTb[:, g, :], ki_ap, start=True, stop=True),
                         reads=[r_qiTb, r_ki], writes=[rb[lb]])
                    ri = cnt["r"] % 4; cnt["r"] += 1
                    if ri % 2 == 0 or not accumulate:
                        s.op("act", lambda e: e.activation(out=Rt[ri][:, 0:ncol], in_=bk(lb)[:, 0:ncol], func=AF.Relu),
                             writes=[rb[lb], r_Rt[ri]])
                    else:
                        s.op("dve", lambda e: e.tensor_scalar(out=Rt[ri][:, 0:ncol], in0=bk(lb)[:, 0:ncol], scalar1=0.0, scalar2=None, op0=ALU.max),
                             writes=[rb[lb], r_Rt[ri]])
                    s.op("pe", lambda e: e.matmul(bk(3)[:, 0:ncol], Wselb[:, g, :], Rt[ri][:, 0:ncol],
                                                  start=(gi == 0), stop=(gi == len(groups) - 1)),
                         reads=[r_Wselb, r_Rt[ri]], writes=[rb[3]])
                score, r_score = scores[cur["i"]], r_scores[cur["i"]]
                dst = score[:, dst_col:dst_col + ncol]
                if accumulate:
                    s.op("dve", lambda e: e.tensor_tensor(out=dst, in0=bk(3)[:, 0:ncol], in1=dst, op=ALU.add), writes=[rb[3], r_score])
                    if mask_ap is not None:
                        s.op("dve", lambda e: e.tensor_tensor(out=dst, in0=dst, in1=mask_ap, op=ALU.add), reads=[r_bc], writes=[r_score])
                elif mask_ap is not None:
                    s.op("dve", lambda e: e.tensor_tensor(out=dst, in0=bk(3)[:, 0:ncol], in1=mask_ap, op=ALU.add),
                         reads=[r_bc], writes=[rb[3], r_score])
                else:
                    s.op("act", lambda e: e.copy(out=dst, in_=bk(3)[:, 0:ncol]), writes=[rb[3], r_score])

            def threshold(ncols, premask_cols=None):
                score, r_score = scores[cur["i"]], r_scores[cur["i"]]
                sm, r_sm = sms[cur["i"]], r_sms[cur["i"]]
                sc = score[:, 0:ncols]
                lo, hi, mid, cn, sel, d1 = (sm[:, i:i + 1] for i in range(6))
                s.op("dve", lambda e: e.tensor_reduce(out=hi, in_=sc, axis=AX.X, op=ALU.max), reads=[r_score], writes=[r_sm])
                s.op("dve", lambda e: e.tensor_reduce(out=lo, in_=sc, axis=AX.X, op=ALU.min), reads=[r_score], writes=[r_sm])
                if premask_cols is not None:
                    c0, c1, m_ap = premask_cols
                    s.op("dve", lambda e: e.tensor_tensor(out=score[:, c0:c1], in0=score[:, c0:c1], in1=m_ap, op=ALU.add),
                         reads=[r_bc], writes=[r_score])
                s.op("dve", lambda e: e.tensor_tensor(out=d1, in0=hi, in1=lo, op=ALU.subtract), writes=[r_sm])
                s.op("dve", lambda e: e.tensor_scalar(out=d1, in0=d1, scalar1=1.001, scalar2=1e-6, op0=ALU.mult, op1=ALU.add), writes=[r_sm])
                s.op("dve", lambda e: e.tensor_scalar(out=wk[:], in0=pw2[:], scalar1=d1, scalar2=None, op0=ALU.mult),
                     reads=[r_pw2], writes=[r_sm, r_wk])
                for it in range(NBIS):
                    s.op("dve", lambda e: e.tensor_tensor(out=mid, in0=lo, in1=wk[:, it:it + 1], op=ALU.add), reads=[r_wk], writes=[r_sm])
                    s.op("dve", lambda e: e.tensor_scalar(out=cjunk[:, 0:ncols], in0=sc, scalar1=mid, scalar2=None,
                                                          op0=ALU.is_ge, op1=ALU.add, accum_out=cn),
                         reads=[r_score], writes=[r_cjunk, r_sm])
                    s.op("dve", lambda e: e.scalar_tensor_tensor(out=sel, in0=cn, scalar=float(TOPK) - 0.5, in1=wk[:, it:it + 1],
                                                                 op0=ALU.is_ge, op1=ALU.mult), reads=[r_wk], writes=[r_sm])
                    s.op("dve", lambda e: e.tensor_tensor(out=lo, in0=lo, in1=sel, op=ALU.add), writes=[r_sm])

            def make_mb(c0, ncol):
                mi = cnt["m"] % 2; cnt["m"] += 1
                score, r_score = scores[cur["i"]], r_scores[cur["i"]]
                sm, r_sm = sms[cur["i"]], r_sms[cur["i"]]
                s.op("dve", lambda e: e.tensor_scalar(out=Mb[mi][:, 0:ncol], in0=score[:, c0:c0 + ncol], scalar1=sm[:, 0:1], scalar2=NEG,
                                                      op0=ALU.is_lt, op1=ALU.mult),
                     reads=[r_score, r_sm], writes=[r_Mb[mi]])
                return mi

            def attend_chunk(first, last, kp, kt_fn, v_fn, r_kv, q_fn, r_q, nq, mb_l, mb_r, r_mb):
                if nq == 512:
                    for kvh in range(2):
                        pi = cnt["p"] % 3; cnt["p"] += 1
                        sb_ = pi % 2
                        s.op("pe", lambda e: e.matmul(bk(sb_)[0:kp, :], kt_fn(kvh), q_fn(kvh), start=True, stop=False),
                             reads=r_kv + [r_q], writes=[rb[sb_]])
                        s.op("pe", lambda e: e.matmul(bk(sb_)[0:kp, :], mb_l, mb_r, start=False, stop=True),
                             reads=[r_mb, r_bc], writes=[rb[sb_]])
                        s.op("act", lambda e: e.activation(out=Pt[pi][0:kp, :], in_=bk(sb_)[0:kp, :], func=AF.Exp), writes=[rb[sb_], r_Pt[pi]])
                        s.op("pe", lambda e: e.matmul(bk(4 + kvh)[:, :], v_fn(kvh), Pt[pi][0:kp, :], start=first, stop=last),
                             reads=r_kv + [r_Pt[pi]], writes=[rb[4 + kvh]])
                        s.op("pe", lambda e: e.matmul(bk(6 + kvh)[:, :], ones_bf[0:kp, :], Pt[pi][0:kp, :], start=first, stop=last),
                             reads=[r_Pt[pi], r_c], writes=[rb[6 + kvh]])
                else:
                    pi = cnt["p"] % 3; cnt["p"] += 1
                    sb_ = pi % 2
                    for kvh in range(2):
                        s.op("pe", lambda e: e.matmul(bk(sb_)[0:kp, kvh * nq:(kvh + 1) * nq], kt_fn(kvh), q_fn(kvh), start=True, stop=False),
                             reads=r_kv + [r_q], writes=[rb[sb_]])
                        s.op("pe", lambda e: e.matmul(bk(sb_)[0:kp, kvh * nq:(kvh + 1) * nq], mb_l, mb_r, start=False, stop=True),
                             reads=[r_mb, r_bc], writes=[rb[sb_]])
                    s.op("act", lambda e: e.activation(out=Pt[pi][0:kp, 0:2 * nq], in_=bk(sb_)[0:kp, 0:2 * nq], func=AF.Exp),
                         writes=[rb[sb_], r_Pt[pi]])
                    for kvh in range(2):
                        s.op("pe", lambda e: e.matmul(bk(4 + kvh)[:, 0:nq], v_fn(kvh), Pt[pi][0:kp, kvh * nq:(kvh + 1) * nq],
                                                      start=first, stop=last),
                             reads=r_kv + [r_Pt[pi]], writes=[rb[4 + kvh]])
                    s.op("pe", lambda e: e.matmul(bk(6)[:, 0:2 * nq], ones_bf[0:kp, :], Pt[pi][0:kp, 0:2 * nq], start=first, stop=last),
                         reads=[r_Pt[pi], r_c], writes=[rb[6]])


            return dict(score=scores[0], r_score=r_scores[0], sm=sms[0], r_sm=r_sms[0], cur=cur, load_q=load_q, Pt=Pt, r_Pt=r_Pt, QTb=QTb, r_QTb=r_QTb, aoT=aoT, r_aoT=r_aoT, rsum=rsum, r_rsum=r_rsum,
                        Mb=Mb, r_Mb=r_Mb, cmask=cmask, cmask_s=cmask_s, identrep=identrep, selrep=selrep, iota_p=iota_p, r_bc=r_bc, cnt=cnt,
                        load_block=load_block, indexer_tile=indexer_tile, threshold=threshold, make_mb=make_mb, attend_chunk=attend_chunk)


        if "S" in PH:
            with ExitStack() as pss:
                sbs = lambda name, shape, dtype: pss.enter_context(nc.sbuf_tensor(_un(name), shape, dtype))
                NCS = NPG * 128 + 128
                env = attn_env(pss, "s_", NCS)
                score, sm, QTb, aoT, rsum, selrep, cmask_s = env["score"], env["sm"], env["QTb"], env["aoT"], env["rsum"], env["selrep"], env["cmask_s"]
                r_score, r_sm, r_QTb, r_aoT, r_rsum, r_bc = env["r_score"], env["r_sm"], env["r_QTb"], env["r_aoT"], env["r_rsum"], env["r_bc"]
                KTn = sbs("KTn", [128, 2, 128], BF16)
                Vn = sbs("Vn", [128, 256], BF16)
                kiTn = sbs("kiTn", [64, 128], BF16)
                r_kvn = s.res()
                with ExitStack() as pkn:
                    kvt = kv_env(pkn, "s_")
                    kvt(xs[32:160, :], KTn[:], Vn[:], kiTn[:], r_kvn, r_kvn, r_kvn, o_ks[:, :], o_vs[:, :], o_iks[:, :])
                    s.barrier()
                env["load_block"](SBLK)
                env["load_q"](SBLK)
                ptab_sb = sbs("ptab_sb", [NPG, 4], I32); r_pt = s.res()
                s.dma("sp", lambda e: e.dma_start(out=ptab_sb[:], in_=ptab.rearrange("q p -> p q"), allow_slow_non_contiguous=True), writes=[r_pt])
                RG = 16
                NJ = 128 // RG
                ptf = sbs("ptf", [NPG, 4], F32)
                idxf = sbs("idxf", [NPG, 4, NJ], F32)
                idxK = sbs("idxK", [NPG, 4, NJ], I32); r_idxK = s.res()
                s.op("dve", lambda e: e.tensor_copy(out=ptf[:], in_=ptab_sb[:]), reads=[r_pt], writes=[r_idxK])
                for jj in range(NJ):
                    s.op("dve", lambda e, jj=jj: e.tensor_scalar(out=idxf[:, :, jj], in0=ptf[:], scalar1=float(NJ), scalar2=float(jj),
                                                                op0=ALU.mult, op1=ALU.add), writes=[r_idxK])
                s.op("dve", lambda e: e.tensor_copy(out=idxK[:], in_=idxf[:]), writes=[r_idxK])
                with ExitStack() as psi:
                    sbi = lambda name, shape, dtype: psi.enter_context(nc.sbuf_tensor(_un(name), shape, dtype))
                    idxpg = sbi("idxpg", [NPG, PAGE * IDIM], F32); r_idxpg = s.res()
                    kiTs = sbi("kiTs", [64, NPG * 128], BF16); r_kiTs = s.res()
                    kvw = kiTs[:].rearrange("d (pg r) -> d pg r", r=128)
                    s.op("pool", lambda e: e.memset(score[:, 0:NCS], 0.0), writes=[r_score])
                    for q in range(4):
                        s.dma("pool", lambda e, q=q: e.indirect_dma_start(
                            out=idxpg[:], out_offset=None, in_=cache_ik[:, :],
                            in_offset=bass.IndirectOffsetOnAxis(ap=ptab_sb[:, q:q + 1], axis=0)), reads=[r_pt], writes=[r_idxpg])
                        for r0 in range(0, 128, 4):
                            bank = 4 + (r0 // 4) % 2
                            tq = bk(bank)[0:64, 0:4 * NPG].rearrange("d (r pg) -> d r pg", pg=NPG)
                            for rr in range(4):
                                s.op("pe", lambda e, rr=rr: e.transpose(tq[:, rr, :], idxpg[:, (r0 + rr) * 64:(r0 + rr + 1) * 64], ident32[0:NPG, 0:NPG]),
                                     reads=[r_idxpg, r_c], writes=[rb[bank]])
                            eng = "act" if (r0 // 4) % 2 == 0 else "dve"
                            if eng == "act":
                                s.op("act", lambda e: e.copy(out=kvw[:, :, r0:r0 + 4].rearrange("d pg r -> d r pg"), in_=tq), writes=[rb[bank], r_kiTs])
                            else:
                                s.op("dve", lambda e: e.tensor_copy(out=kvw[:, :, r0:r0 + 4].rearrange("d pg r -> d r pg"), in_=tq), writes=[rb[bank], r_kiTs])
                        for kc in range(NPG * 128 // 512):
                            env["indexer_tile"]([q], kiTs[:, kc * 512:(kc + 1) * 512], r_kiTs, 512, kc * 512, None, True)
                        env["indexer_tile"]([q], kiTn[:, :], r_kvn, 128, NPG * 128, None, True)
                    s.barrier()
                env["threshold"](NCS, (NPG * 128, NCS, cmask_s[:]))
                with ExitStack() as psa:
                    sbt = lambda name, shape, dtype: psa.enter_context(nc.sbuf_tensor(_un(name), shape, dtype))
                    Kg = [sbt("Kg%d" % i, [NPG, RG * 256], F32) for i in range(2)]
                    Vg = [sbt("Vg%d" % i, [NPG, RG * 256], F32) for i in range(2)]
                    r_Kg = [s.res() for _ in range(2)]
                    r_Vg = [s.res() for _ in range(2)]
                    KTc = [sbt("KTc%d" % i, [128, 4, 2, NPG], BF16) for i in range(2)]
                    Vc = [sbt("Vc%d" % i, [NPG, 4, 256], BF16) for i in range(2)]
                    Mbc = [sbt("Mbc%d" % i, [32, 4, 128], BF16) for i in range(2)]
                    r_KTc = [s.res() for _ in range(2)]
                    r_Vc = [s.res() for _ in range(2)]
                    r_Mbc = [s.res() for _ in range(2)]
                    QTq = sbt("QTq", [128, 8, 8], BF16); r_QTq = s.res()
                    ck = cache_k.rearrange("(n r) d -> n (r d)", r=RG)
                    cv = cache_v.rearrange("(n r) d -> n (r d)", r=RG)
                    s.op("pool", lambda e: e.memset(aoT[:], 0.0), writes=[r_aoT])
                    cc = 0
                    for q in range(4):
                        s.op("dve", lambda e: e.tensor_copy(out=QTq[:], in_=QTb[:, :, 8 * q:8 * q + 8]), reads=[r_QTb], writes=[r_QTq])
                        qf = lambda kvh: QTq[:, 4 * kvh:4 * kvh + 4, :]
                        for jj in range(NJ):
                            gi = (q * NJ + jj) % 2
                            s.dma("pool", lambda e: e.indirect_dma_start(
                                out=Kg[gi][:], out_offset=None, in_=ck[:, :],
                                in_offset=bass.IndirectOffsetOnAxis(ap=idxK[:, q, jj:jj + 1], axis=0)), reads=[r_idxK], writes=[r_Kg[gi]])
                            s.dma("pool", lambda e: e.indirect_dma_start(
                                out=Vg[gi][:], out_offset=None, in_=cv[:, :],
                                in_offset=bass.IndirectOffsetOnAxis(ap=idxK[:, q, jj:jj + 1], axis=0)), reads=[r_idxK], writes=[r_Vg[gi]])
                            for bb in range(RG // 4):
                                r0 = jj * RG + 4 * bb
                                ci = cc % 2; cc += 1
                                tk = bk(2, 2)[:, 0:8 * NPG].rearrange("p (r a b) -> p r a b", a=2, b=NPG)
                                for rl in range(4):
                                    for kvh in range(2):
                                        c0 = (4 * bb + rl) * 256 + kvh * 128
                                        s.op("pe", lambda e, rl=rl, kvh=kvh, c0=c0: e.transpose(tk[:, rl, kvh, :], Kg[gi][:, c0:c0 + 128], ident32[0:NPG, 0:NPG]),
                                             reads=[r_Kg[gi], r_c], writes=[rb[2], rb[3]])
                                s.op("act", lambda e: e.copy(out=KTc[ci][:], in_=tk), writes=[rb[2], rb[3], r_KTc[ci]])
                                s.op("pool", lambda e: e.tensor_copy(out=Vc[ci][:], in_=Vg[gi][:, 4 * bb * 256:(4 * bb + 4) * 256]),
                                     reads=[r_Vg[gi]], writes=[r_Vc[ci]])
                                s.op("dve", lambda e: e.tensor_scalar(
                                    out=Mbc[ci][:, :, 0:NPG], in0=score[0:32, 0:NPG * 128].rearrange("p (pg r) -> p r pg", r=128)[:, r0:r0 + 4, :],
                                    scalar1=sm[0:32, 0:1], scalar2=NEG, op0=ALU.is_lt, op1=ALU.mult), reads=[r_score, r_sm], writes=[r_Mbc[ci]])
                                pi = env["cnt"]["p"] % 3; env["cnt"]["p"] += 1
                                sb_ = pi % 2
                                Pt, r_Pt = env["Pt"], env["r_Pt"]
                                for rl in range(4):
                                    for kvh in range(2):
                                        oc = rl * 64 + kvh * 32
                                        s.op("pe", lambda e, rl=rl, kvh=kvh, oc=oc: e.matmul(bk(sb_)[0:NPG, oc:oc + 32], KTc[ci][:, rl, kvh, :], qf(kvh),
                                                                                              start=True, stop=False),
                                             reads=[r_KTc[ci], r_QTq], writes=[rb[sb_]])
                                        s.op("pe", lambda e, rl=rl, kvh=kvh, oc=oc: e.matmul(bk(sb_)[0:NPG, oc:oc + 32], Mbc[ci][:, rl, 0:NPG], selrep[:, q, :],
                                                                                              start=False, stop=True),
                                             reads=[r_Mbc[ci], r_bc], writes=[rb[sb_]])
                                s.op("act", lambda e: e.activation(out=Pt[pi][0:NPG, 0:256], in_=bk(sb_)[0:NPG, 0:256], func=AF.Exp),
                                     writes=[rb[sb_], r_Pt[pi]])
                                for rl in range(4):
                                    first = (r0 + rl == 0)
                                    for kvh in range(2):
                                        oc = rl * 64 + kvh * 32
                                        s.op("pe", lambda e, rl=rl, kvh=kvh, oc=oc: e.matmul(bk(4 + kvh)[:, 0:32], Vc[ci][:, rl, kvh * 128:(kvh + 1) * 128],
                                                                                              Pt[pi][0:NPG, oc:oc + 32], start=first, stop=False),
                                             reads=[r_Vc[ci], r_Pt[pi]], writes=[rb[4 + kvh]])
                                    s.op("pe", lambda e, rl=rl: e.matmul(bk(6)[:, 0:64], ones_bf[0:NPG, :], Pt[pi][0:NPG, rl * 64:rl * 64 + 64],
                                                                         start=first, stop=False),
                                         reads=[r_Pt[pi], r_c], writes=[rb[6]])
                        ci = cc % 2; cc += 1
                        s.op("dve", lambda e: e.tensor_scalar(out=Mbc[ci][:, 0, :], in0=score[0:32, NPG * 128:NPG * 128 + 128], scalar1=sm[0:32, 0:1], scalar2=NEG,
                                                              op0=ALU.is_lt, op1=ALU.mult), reads=[r_score, r_sm], writes=[r_Mbc[ci]])
                        env["attend_chunk"](False, True, 128, lambda kvh: KTn[:, kvh, :], lambda kvh: Vn[:, kvh * 128:(kvh + 1) * 128],
                                            [r_kvn], qf, r_QTq, 32, Mbc[ci][:, 0, :], selrep[:, q, :], r_Mbc[ci])
                        s.op("dve", lambda e: e.reciprocal(out=rsum[:, 0, 0:64], in_=bk(6)[:, 0:64]), writes=[rb[6], r_rsum])
                        for kvh in range(2):
                            s.op("dve", lambda e, kvh=kvh: e.tensor_tensor(out=aoT[:, 4 * kvh:4 * kvh + 4, 8 * q:8 * q + 8],
                                                                           in0=bk(4 + kvh)[:, 0:32].rearrange("p (h t) -> p h t", t=8),
                                                                           in1=rsum[:, 0, 32 * kvh:32 * kvh + 32].rearrange("p (h t) -> p h t", t=8), op=ALU.mult),
                                 reads=[r_rsum], writes=[rb[4 + kvh], r_aoT])
                    s.dma("sp", lambda e: e.dma_start(out=mixT_scr[SBLK, :, 8:16, :], in_=aoT[:]), reads=[r_aoT])
                    s.barrier()


        with ExitStack() as pkv_stack:
            sbk = lambda name, shape, dtype: pkv_stack.enter_context(nc.sbuf_tensor(_un(name), shape, dtype))
            if "A" in PH or "B" in PH:
                KT = sbk("KT", [128, NKV, NT * 128], BF16)
                Vr = sbk("Vr", [128, NT, NKV * HD], BF16)
                kiT = sbk("kiT", [64, NT * 128], BF16)
                r_KT, r_V, r_kiT = s.res(), s.res(), s.res()
            if "A" in PH:
                with ExitStack() as pa:
                    kvt = kv_env(pa, "a_")
                    for n in range(NT):
                        kvt(xb[n * 128:(n + 1) * 128, :], KT[:, :, n * 128:(n + 1) * 128], Vr[:, n, :], kiT[:, n * 128:(n + 1) * 128],
                            r_KT, r_V, r_kiT, o_k[n * 128:(n + 1) * 128, :], o_v[n * 128:(n + 1) * 128, :], o_ik[n * 128:(n + 1) * 128, :])
                    s.barrier()
            if "B" in PH:
                with ExitStack() as pb:
                    env = attn_env(pb, "b_", NT * 128)
                    score, sm, QTb, aoT, rsum, cmask, identrep, Mb = env["score"], env["sm"], env["QTb"], env["aoT"], env["rsum"], env["cmask"], env["identrep"], env["Mb"]
                    r_score, r_sm, r_QTb, r_aoT, r_rsum, r_bc, r_Mb = env["r_score"], env["r_sm"], env["r_QTb"], env["r_aoT"], env["r_rsum"], env["r_bc"], env["r_Mb"]
                    load_block, indexer_tile, threshold, make_mb, attend_chunk = env["load_block"], env["indexer_tile"], env["threshold"], env["make_mb"], env["attend_chunk"]
                    cur, load_q = env["cur"], env["load_q"]

                    def do_indexer(i):
                        cur["i"] = i % 2
                        load_block(i)
                        for kc in range(i + 1):
                            indexer_tile(list(range(16)), kiT[:, kc * 512:(kc + 1) * 512], r_kiT, 512, kc * 512, None, False)

                    def do_thr(i):
                        cur["i"] = i % 2
                        ncols = (i + 1) * 512
                        threshold(ncols, (ncols - 512, ncols, cmask[:]))

                    def do_attn(i):
                        cur["i"] = i % 2
                        load_q(i)
                        nch = 4 * (i + 1)
                        for c in range(nch):
                            if c % 4 == 0:
                                mi = make_mb(c * 128, 512)
                            attend_chunk(c == 0, c == nch - 1, 128,
                                         lambda kvh: KT[:, kvh, c * 128:(c + 1) * 128],
                                         lambda kvh: Vr[:, c, kvh * 128:(kvh + 1) * 128], [r_KT, r_V],
                                         lambda kvh: QTb[:, 4 * kvh:4 * kvh + 4, :], r_QTb, 512,
                                         Mb[mi][:, (c % 4) * 128:(c % 4 + 1) * 128], identrep[:], r_Mb[mi])
                        s.op("dve", lambda e: e.reciprocal(out=rsum[:], in_=bk(6, 2).rearrange("p (a b) -> p a b", b=512)),
                             writes=[rb[6], rb[7], r_rsum])
                        s.op("dve", lambda e: e.tensor_tensor(out=aoT[:], in0=bk(4, 2).rearrange("p (h t) -> p h t", t=128),
                                                              in1=rsum[:].rearrange("p a (h t) -> p (a h) t", t=128), op=ALU.mult),
                             reads=[r_rsum], writes=[rb[4], rb[5], r_aoT])
                        s.dma("sp", lambda e: e.dma_start(out=mixT_scr[i, :, 8:16, :], in_=aoT[:]), reads=[r_aoT])

                    do_indexer(0)
                    for i in range(NOWN):
                        if i + 1 < NOWN:
                            do_indexer(i + 1)
                        do_thr(i)
                        do_attn(i)

                    s.barrier()


        if "C" in PH:
            supers = [list(range(i, min(i + SB, NOWN))) for i in range(0, NOWN, SB)] + [[SBLK]]
            with ExitStack() as pc1:
                sbc = lambda name, shape, dtype: pc1.enter_context(nc.sbuf_tensor(_un(name), shape, dtype))
                gpm = sbc("gpm", [128, D], F32); r_gpm = s.res(); load_gain(gpm, g_post_mix, r_gpm)
                gpf = sbc("gpf", [128, D], F32); r_gpf = s.res(); load_gain(gpf, g_pre_ffn, r_gpf)
                mixT = sbc("mixT", [128, 16, SB, 128], BF16); r_mixT = s.res()
                Wo = [sbc("Wo%d" % i, [128, 16, 512], BF16) for i in range(2)]
                r_Wo = [s.res() for _ in range(2)]
                mixed = sbc("mixed", [128, SB, D], F32); r_mixed = s.res()
                xt = sbc("xtC", [128, D], F32); r_xt = s.res()
                tmp = sbc("tmpC", [128, D], F32); r_tmp = s.res()
                hbf = sbc("hbf", [128, D], BF16); r_hbf = s.res()
                hT = sbc("hTC", [128, 16, 128], BF16); r_hT = s.res()
                junk = sbc("junkC", [128, D], BF16); r_junk = s.res()
                ssq = sbc("ssqC", [128, 1], F32); r_ssq = s.res()
                wc = 0
                mc = 0
                for blks in supers:
                    nb = len(blks)
                    is_s = blks[0] == SBLK
                    for bi, blk in enumerate(blks):
                        s.dma("sp", lambda e: e.dma_start(out=mixT[:, :, bi, :], in_=mixT_scr[blk]), writes=[r_mixT])
                    for nt in range(4):
                        wi_ = wc % 2; wc += 1
                        for k4 in range(4):
                            s.dma("pool", lambda e, k4=k4: e.dma_start(out=Wo[wi_][:, 4 * k4:4 * k4 + 4, :], in_=w_out_r[:, 4 * k4:4 * k4 + 4, nt * 512:(nt + 1) * 512]),
                                  writes=[r_Wo[wi_]])
                        for bi in range(nb):
                            pbk = mc % 4; mc += 1
                            for k in range(16):
                                s.op("pe", lambda e, k=k: e.matmul(bk(pbk)[:, :], mixT[:, k, bi, :], Wo[wi_][:, k, :], start=(k == 0), stop=(k == 15)),
                                     reads=[r_mixT, r_Wo[wi_]], writes=[rb[pbk]])
                            s.op("act", lambda e: e.copy(out=mixed[:, bi, nt * 512:(nt + 1) * 512], in_=bk(pbk)[:, :]), writes=[rb[pbk], r_mixed])
                    for bi, blk in enumerate(blks):
                        src = xs[32:160, :] if is_s else xo[blk, 32:160, :]
                        s.dma("sp", lambda e: e.dma_start(out=xt[:], in_=src), writes=[r_xt])
                        rms_to_bf(128, mixed[:, bi, :], r_mixed, gpm, r_gpm, tmp[:], r_tmp, (junk, r_junk, ssq, r_ssq))
                        s.op("pool", lambda e: e.tensor_tensor(out=xt[:], in0=xt[:], in1=tmp[:], op=ALU.add), reads=[r_tmp], writes=[r_xt])
                        s.dma("sp", lambda e: e.dma_start(out=x1_scr[blk], in_=xt[:]), reads=[r_xt])
                        rms_to_bf(128, xt[:], r_xt, gpf, r_gpf, hbf[:], r_hbf, (junk, r_junk, ssq, r_ssq))
                        tp = bkb(4, 2).rearrange("p (k t) -> p k t", t=128)
                        for k in range(16):
                            s.op("pe", lambda e, k=k: e.transpose(tp[:, k, :], hbf[:, k * 128:(k + 1) * 128], ident[:]),
                                 reads=[r_hbf, r_c], writes=[rb[4], rb[5]])
                        s.op("act", lambda e: e.copy(out=hT[:], in_=tp[:, :, :]), writes=[rb[4], rb[5], r_hT])
                        s.dma("sp", lambda e: e.dma_start(out=hT_scr[blk], in_=hT[:]), reads=[r_hT])
                s.barrier()
            with ExitStack() as pc2:
                sbc = lambda name, shape, dtype: pc2.enter_context(nc.sbuf_tensor(_un(name), shape, dtype))
                gff = sbc("gff", [128, D], F32); r_gff = s.res(); load_gain(gff, g_post_ffn, r_gff)
                hTs = sbc("hTs", [128, 16, SB, 128], BF16); r_hTs = s.res()
                aT = sbc("aT", [128, NFT, SB * 128], BF16); r_aT = s.res()
                Wg = [sbc("Wg%d" % i, [128, 16, 128], BF16) for i in range(2)]
                Wu = [sbc("Wu%d" % i, [128, 16, 128], BF16) for i in range(2)]
                r_Wg = [s.res() for _ in range(2)]
                r_Wu = [s.res() for _ in range(2)]
                Wd = sbc("Wd", [128, NFT, 256], BF16); r_Wd = s.res()
                sg = sbc("sg", [128, SB * 128], F32); r_sg = s.res()
                fo = sbc("fo", [128, SB, D], F32); r_fo = s.res()
                x1t = sbc("x1t", [128, D], F32); r_x1t = s.res()
                tmp = sbc("tmpC2", [128, D], F32); r_tmp = s.res()
                junk = sbc("junkC2", [128, D], BF16); r_junk = s.res()
                ssq = sbc("ssqC2", [128, 1], F32); r_ssq = s.res()
                dc = 0
                for blks in supers:
                    nb = len(blks)
                    N = nb * 128
                    for bi, blk in enumerate(blks):
                        s.dma("sp", lambda e: e.dma_start(out=hTs[:, :, bi, :], in_=hT_scr[blk]), writes=[r_hTs])
                    for ft in range(NFT):
                        wi_ = ft % 2
                        s.dma("pool", lambda e: e.dma_start(out=Wg[wi_][:], in_=w_gate_r[:, :, ft * 128:(ft + 1) * 128]), writes=[r_Wg[wi_]])
                        s.dma("pool", lambda e: e.dma_start(out=Wu[wi_][:], in_=w_up_r[:, :, ft * 128:(ft + 1) * 128]), writes=[r_Wu[wi_]])
                        gb, ub = 2 * wi_, 2 * wi_ + 1
                        for k in range(16):
                            s.op("pe", lambda e, k=k: e.matmul(bk(gb)[:, 0:N], Wg[wi_][:, k, :], hTs[:, k, 0:nb, :], start=(k == 0), stop=(k == 15)),
                                 reads=[r_hTs, r_Wg[wi_]], writes=[rb[gb]])
                        for k in range(16):
                            s.op("pe", lambda e, k=k: e.matmul(bk(ub)[:, 0:N], Wu[wi_][:, k, :], hTs[:, k, 0:nb, :], start=(k == 0), stop=(k == 15)),
                                 reads=[r_hTs, r_Wu[wi_]], writes=[rb[ub]])
                        s.op("act", lambda e: e.activation(out=sg[:, 0:N], in_=bk(gb)[:, 0:N], func=AF.Silu), writes=[rb[gb], r_sg])
                        s.op("dve", lambda e: e.tensor_tensor(out=aT[:, ft, 0:N], in0=bk(ub)[:, 0:N], in1=sg[:, 0:N], op=ALU.mult),
                             reads=[r_sg], writes=[rb[ub], r_aT])
                    for nt in range(8):
                        for f4 in range(4):
                            s.dma("pool", lambda e, f4=f4: e.dma_start(out=Wd[:, 11 * f4:11 * f4 + 11, :], in_=w_down_r[:, 11 * f4:11 * f4 + 11, nt * 256:(nt + 1) * 256]),
                                  writes=[r_Wd])
                        for bi in range(nb):
                            pbk = 4 + dc % 4; dc += 1
                            for ft in range(NFT):
                                s.op("pe", lambda e, ft=ft: e.matmul(bk(pbk)[:, 0:256], aT[:, ft, bi * 128:(bi + 1) * 128], Wd[:, ft, :], start=(ft == 0), stop=(ft == NFT - 1)),
                                     reads=[r_aT, r_Wd], writes=[rb[pbk]])
                            s.op("act", lambda e: e.copy(out=fo[:, bi, nt * 256:(nt + 1) * 256], in_=bk(pbk)[:, 0:256]), writes=[rb[pbk], r_fo])
                    for bi, blk in enumerate(blks):
                        s.dma("sp", lambda e: e.dma_start(out=x1t[:], in_=x1_scr[blk]), writes=[r_x1t])
                        rms_to_bf(128, fo[:, bi, :], r_fo, gff, r_gff, tmp[:], r_tmp, (junk, r_junk, ssq, r_ssq))
                        s.op("pool", lambda e: e.tensor_tensor(out=x1t[:], in0=x1t[:], in1=tmp[:], op=ALU.add), reads=[r_tmp], writes=[r_x1t])
                        s.dma("sp", lambda e: e.dma_start(out=o_y[blk], in_=x1t[:]), reads=[r_x1t])
                s.barrier()
        s.finish()

    return nc


def _consts(j):
    c = {}
    c["identb"] = _bf(np.eye(128, dtype=np.float32))
    c["ident32"] = np.eye(128, dtype=np.float32)
    r = np.arange(128)[:, None]
    sp = np.arange(512)[None, :]
    c["cmask"] = np.where(sp <= 128 * j + r, 0.0, -1e30).astype(np.float32)
    mg = np.zeros((128, 16, 128), np.float32)
    for g in range(16):
        for row in range(128):
            mg[row, g, 8 * g + row % 8] = 1.0
    c["maskg"] = _bf(mg)
    c["identrep"] = _bf(np.repeat(np.eye(128, dtype=np.float32)[:, None, :], 4, axis=1))
    cs = np.full((128, 128), -1e30, np.float32)
    for row in range(32):
        q, t = row // 8, row % 8
        for col in range(32):
            q2, t2 = col // 8, col % 8
            if q2 == q and t2 <= t:
                cs[row, col] = 0.0
    c["cmask_s"] = cs
    sr = np.zeros((32, 4, 32), np.float32)
    for q in range(4):
        for h in range(4):
            for t in range(8):
                sr[8 * q + t, q, h * 8 + t] = 1.0
    c["selrep"] = _bf(sr)
    c["iota_p"] = np.arange(128, dtype=np.float32)[:, None]
    return c


def make_in_map(c, inp, NT, NPG):
    b, j = c // 4, c % 4
    NOWN = NT // 4
    f = lambda a: np.ascontiguousarray(a, dtype=np.float32)
    xp = inp["x_prompt"][b]
    m = {"xb": f(xp[:NT * 128])}
    xo = np.zeros((NOWN, 160, D), np.float32)
    for i in range(NOWN):
        g0 = (4 * i + j) * 128
        xo[i, 32:160] = xp[g0:g0 + 128]
        if g0 > 0:
            xo[i, 2:32] = xp[g0 - 30:g0]
    m["xo"] = xo
    xs_ = np.zeros((160, D), np.float32)
    xs_[32:64] = inp["x_sample"][4 * c:4 * c + 4].reshape(32, D)
    m["xs"] = xs_
    for k in ("w_in", "w_out", "w_gate", "w_up", "w_down", "g_pre_mix", "g_post_mix", "g_pre_ffn", "g_post_ffn",
              "conv_w", "conv_b", "conv_ln_g", "conv_ln_b"):
        m[k] = f(inp[k][0]) if inp[k][0].ndim == 2 else f(inp[k][0])[None, :]
    npool = inp["cache_k"].shape[1]
    m["cache_k"] = f(inp["cache_k"][0]).reshape(npool * PAGE, NKV * HD)
    m["cache_v"] = f(inp["cache_v"][0]).reshape(npool * PAGE, NKV * HD)
    m["cache_ik"] = f(inp["cache_idx_k"][0]).reshape(npool, PAGE * IDIM)
    m["state_conv"] = f(inp["state_conv"][0, 4 * c:4 * c + 4])
    m["ptab"] = np.ascontiguousarray(inp["page_table"][4 * c:4 * c + 4, :NPG], dtype=np.int32)
    m.update(_consts(j))
    return m


_NC_CACHE = {}


def kernel(**inp):
    NT, NPG = SEQ // 128, NPAGES
    npool = inp["cache_k"].shape[1]
    key = (NT, NPG, npool)
    if key not in _NC_CACHE:
        _NC_CACHE[key] = build({"NT": NT, "NPG": NPG, "NPOOL": npool})
    nc = _NC_CACHE[key]
    in_maps = [make_in_map(c, inp, NT, NPG) for c in range(8)]
    res = run_bass_kernel_spmd(nc, in_maps, core_ids=list(range(8))).results
    return assemble(res, NT)


def assemble(res, NT):
    NOWN = NT // 4
    S_ = NT * 128
    y_p = np.zeros((NB, S_, D), np.float32)
    y_s = np.zeros((DEC_B, DEC_T, D), np.float32)
    nk = np.zeros((1, NB, S_, NKV, HD), np.float32)
    nv = np.zeros((1, NB, S_, NKV, HD), np.float32)
    nik = np.zeros((1, NB, S_, IDIM), np.float32)
    ncp = np.zeros((1, NB, CW - 1, CONV_CH), np.float32)
    nks = np.zeros((1, DEC_B, DEC_T, NKV, HD), np.float32)
    nvs = np.zeros((1, DEC_B, DEC_T, NKV, HD), np.float32)
    niks = np.zeros((1, DEC_B, DEC_T, IDIM), np.float32)
    ncs = np.zeros((1, DEC_B, CW - 1, CONV_CH), np.float32)
    for c in range(len(res)):
        r = res[c]
        if r is None:
            continue
        b, j = c // 4, c % 4
        oy = np.asarray(r["o_y"])
        for i in range(NOWN):
            g0 = (4 * i + j) * 128
            y_p[b, g0:g0 + 128] = oy[i]
        y_s[4 * c:4 * c + 4] = oy[NOWN][0:32].reshape(4, DEC_T, D)
        if j == 0:
            nk[0, b] = np.asarray(r["o_k"]).reshape(S_, NKV, HD)
            nv[0, b] = np.asarray(r["o_v"]).reshape(S_, NKV, HD)
            nik[0, b] = np.asarray(r["o_ik"])
        if j == 3:
            ncp[0, b] = np.asarray(r["o_convp"])
        nks[0, 4 * c:4 * c + 4] = np.asarray(r["o_ks"])[0:32].reshape(4, DEC_T, NKV, HD)
        nvs[0, 4 * c:4 * c + 4] = np.asarray(r["o_vs"])[0:32].reshape(4, DEC_T, NKV, HD)
        niks[0, 4 * c:4 * c + 4] = np.asarray(r["o_iks"])[0:32].reshape(4, DEC_T, IDIM)
        ncs[0, 4 * c:4 * c + 4] = np.asarray(r["o_convs"])
    return (y_p, y_s, nk, nv, nik, ncp, nks, nvs, niks, ncs)
```

```python
import numpy as np
import ml_dtypes
import concourse.bass as bass
import concourse.mybir as mybir
from concourse.bass_utils import run_bass_kernel_spmd

F32 = mybir.dt.float32
BF16 = mybir.dt.bfloat16
I32 = mybir.dt.int32
U32 = mybir.dt.uint32
U8 = mybir.dt.uint8
AF = mybir.ActivationFunctionType
ALU = mybir.AluOpType
AX = mybir.AxisListType

D = 2048
SEQ = 8192
NB = 2
CONV_CH = 1024
NH = 8
HD = 128
NKV = 2
NIH = 16
IDIM = 64
TOPK = 256
CW = 31
DFF = 5632
N_IN = 4688
EPS = 1e-6
ATTN_SCALE = HD ** -0.5
INDEX_SCALE = (NIH * IDIM) ** -0.5
DEC_B = 32
DEC_T = 8
PAST = 16384
PAGE = 128
NPAGES = PAST // PAGE
NEG = -30000.0
NBIS = 20


class Res:
    __slots__ = ("name", "w", "rd")

    def __init__(self, name=""):
        self.name = name
        self.w = None
        self.rd = {}


class S:
    R = 8

    def __init__(self, nc, stack):
        self.nc = nc
        self.eng = {"pe": nc.tensor, "act": nc.scalar, "dve": nc.vector, "pool": nc.gpsimd, "sp": nc.sync}
        self.sem = {k: stack.enter_context(nc.semaphore("s_" + k)) for k in self.eng}
        self.cnt = {k: 0 for k in self.eng}
        self.dsem = {k: [stack.enter_context(nc.semaphore("d_%s%d" % (k, i))) for i in range(self.R)]
                     for k in ("sp", "pool", "act")}
        self.dn = {k: 0 for k in self.dsem}
        self.waited = {k: {} for k in self.eng}
        self.pending_dma = []
        self.nres = 0

    def res(self, name=""):
        return Res(name)

    def _wait(self, e, tok):
        if tok is None:
            return
        kind, key, val = tok
        if kind == "c":
            if key == e and e == "pe":
                return
            sem = self.sem[key]
            wk = ("c", key)
        else:
            sem = self.dsem[key[0]][key[1]]
            wk = ("d", key)
        if self.waited[e].get(wk, 0) >= val:
            return
        self.waited[e][wk] = val
        self.eng[e].wait_ge(sem, val)

    def _deps(self, e, reads, writes):
        toks = []
        for r in reads:
            if r.w is not None:
                toks.append(r.w)
        for w in writes:
            if w.w is not None:
                toks.append(w.w)
            for k, t in w.rd.items():
                if isinstance(t, list):
                    toks.extend(t)
                else:
                    toks.append(t)
        for t in toks:
            self._wait(e, t)

    def _mark(self, e, tok, reads, writes, is_dma):
        for r in reads:
            if is_dma:
                r.rd.setdefault("dma", []).append(tok)
            else:
                r.rd[e] = tok
        for w in writes:
            w.w = tok
            w.rd = {}

    def op(self, e, fn, reads=(), writes=()):
        self._deps(e, reads, writes)
        inst = fn(self.eng[e])
        self.cnt[e] += 1
        inst.then_inc(self.sem[e], 1)
        tok = ("c", e, self.cnt[e])
        self._mark(e, tok, reads, writes, False)
        return tok

    def dma(self, e, fn, reads=(), writes=()):
        n = self.dn[e]
        slot = n % self.R
        val = 16 * (n // self.R + 1)
        if val > 16:
            self._wait(e, ("d", (e, slot), val - 16))
        self._deps(e, reads, writes)
        inst = fn(self.eng[e])
        inst.then_inc(self.dsem[e][slot], 16)
        self.dn[e] = n + 1
        tok = ("d", (e, slot), val)
        self._mark(e, tok, reads, writes, True)
        self.pending_dma.append(tok)
        return tok

    def barrier(self):
        toks = [("c", k, self.cnt[k]) for k in self.eng if self.cnt[k] > 0]
        toks += self.pending_dma
        self.pending_dma = []
        for e in self.eng:
            for t in toks:
                if t[0] == "c" and t[1] == e:
                    continue
                self._wait(e, t)

    def finish(self):
        self.barrier()


_UN = [0]


def _un(name):
    _UN[0] += 1
    return "t%d_%s" % (_UN[0], name)


def _bf(a):
    return np.ascontiguousarray(a).astype(ml_dtypes.bfloat16)


def build(cfg):
    from contextlib import ExitStack
    NT = cfg.get("NT", SEQ // 128)
    NOWN = NT // 4
    SB = min(4, NOWN)
    NPG = cfg.get("NPG", NPAGES)
    NPOOL = cfg.get("NPOOL", 5120)
    NFT = DFF // 128
    NBLK = NOWN + 1
    SBLK = NOWN
    PH = cfg.get("PH", "UABSC")
    nc = bass.Bass("TRN2", target_bir_lowering=False)

    def din(name, shape, dtype=F32):
        return nc.dram_tensor(name, shape, dtype, kind="ExternalInput").ap()

    def dout(name, shape, dtype=F32):
        return nc.dram_tensor(name, shape, dtype, kind="ExternalOutput").ap()

    def dscr(name, shape, dtype):
        return nc.dram_tensor(name, shape, dtype, kind="Internal").ap()

    xb = din("xb", [NT * 128, D])
    xo = din("xo", [NOWN, 160, D])
    xs = din("xs", [160, D])
    w_in = din("w_in", [D, N_IN])
    w_out = din("w_out", [D, D])
    w_gate = din("w_gate", [D, DFF])
    w_up = din("w_up", [D, DFF])
    w_down = din("w_down", [DFF, D])
    g_pre_mix = din("g_pre_mix", [1, D])
    g_post_mix = din("g_post_mix", [1, D])
    g_pre_ffn = din("g_pre_ffn", [1, D])
    g_post_ffn = din("g_post_ffn", [1, D])
    conv_w = din("conv_w", [CW, CONV_CH])
    conv_b = din("conv_b", [1, CONV_CH])
    ln_g = din("conv_ln_g", [1, CONV_CH])
    ln_b = din("conv_ln_b", [1, CONV_CH])
    cache_k = din("cache_k", [NPOOL * PAGE, NKV * HD])
    cache_v = din("cache_v", [NPOOL * PAGE, NKV * HD])
    cache_ik = din("cache_ik", [NPOOL, PAGE * IDIM])
    state_conv = din("state_conv", [4, CW - 1, CONV_CH])
    ptab = din("ptab", [4, NPG], I32)
    identb_d = din("identb", [128, 128], BF16)
    ident32_d = din("ident32", [128, 128])
    cmask_d = din("cmask", [128, 512])
    maskg_d = din("maskg", [128, 16, 128], BF16)
    identrep_d = din("identrep", [128, 4, 128], BF16)
    cmask_s_d = din("cmask_s", [128, 128])
    selrep_d = din("selrep", [32, 4, 32], BF16)
    iota_d = din("iota_p", [128, 1])

    o_k = dout("o_k", [NT * 128, NKV * HD])
    o_v = dout("o_v", [NT * 128, NKV * HD])
    o_ik = dout("o_ik", [NT * 128, IDIM])
    o_y = dout("o_y", [NBLK, 128, D])
    o_convp = dout("o_convp", [CW - 1, CONV_CH])
    o_ks = dout("o_ks", [128, NKV * HD])
    o_vs = dout("o_vs", [128, NKV * HD])
    o_iks = dout("o_iks", [128, IDIM])
    o_convs = dout("o_convs", [4, CW - 1, CONV_CH])

    QT_scr = dscr("QT_scr", [NBLK, 128, 8, 128], BF16)
    qiT_scr = dscr("qiT_scr", [NBLK, 64, 16, 2, 64], BF16)
    Wsel_scr = dscr("Wsel_scr", [NBLK, 128, 16, 128], BF16)
    mixT_scr = dscr("mixT_scr", [NBLK, 128, 16, 128], BF16)
    x1_scr = dscr("x1_scr", [NBLK, 128, D], F32)
    hT_scr = dscr("hT_scr", [NBLK, 128, 16, 128], BF16)

    w_in_r = w_in.rearrange("(k p) n -> p k n", p=128)
    w_out_r = w_out.rearrange("(k p) n -> p k n", p=128)
    w_gate_r = w_gate.rearrange("(k p) n -> p k n", p=128)
    w_up_r = w_up.rearrange("(k p) n -> p k n", p=128)
    w_down_r = w_down.rearrange("(k p) n -> p k n", p=128)

    with ExitStack() as st:
        s = S(nc, st)
        sb = lambda name, shape, dtype: st.enter_context(nc.sbuf_tensor(_un(name), shape, dtype))
        PS = st.enter_context(nc.psum_tensor("PS", [128, 8 * 512], F32))
        rb = [s.res("bank%d" % i) for i in range(8)]

        def bk(i, n=1):
            return PS[:, i * 512:(i + n) * 512]

        def bkb(i, n=1):
            return PS[:, i * 512:(i + n) * 512].bitcast(BF16)

        ident = sb("ident", [128, 128], BF16)
        ident32 = sb("ident32", [128, 128], F32)
        ones_bf = sb("ones_bf", [128, 128], BF16)
        ones32 = sb("ones32", [128, 128], F32)
        r_c = s.res("consts")
        s.dma("sp", lambda e: e.dma_start(out=ident[:], in_=identb_d[:, :]), writes=[r_c])
        s.dma("sp", lambda e: e.dma_start(out=ident32[:], in_=ident32_d[:, :]), writes=[r_c])
        s.op("dve", lambda e: e.memset(ones_bf[:], 1.0), writes=[r_c])
        s.op("dve", lambda e: e.memset(ones32[:], 1.0), writes=[r_c])

        def load_gain(tile, g_ap, r):
            s.dma("sp", lambda e: e.dma_start(out=tile[:], in_=g_ap.to_broadcast([128, D])), writes=[r])

        def rms_to_bf(P, x_ap, r_x, g_tile, r_g, out_ap, r_out, tmp):
            junk, r_junk, ssq, r_ss = tmp
            s.op("act", lambda e: e.activation(out=junk[0:P, :], in_=x_ap, func=AF.Square, accum_out=ssq[0:P, :]),
                 reads=[r_x], writes=[r_junk, r_ss])
            s.op("dve", lambda e: e.tensor_scalar(out=ssq[0:P, :], in0=ssq[0:P, :], scalar1=1.0 / D, scalar2=EPS,
                                                  op0=ALU.mult, op1=ALU.add), writes=[r_ss])
            s.op("act", lambda e: e.activation(out=ssq[0:P, :], in_=ssq[0:P, :], func=AF.Sqrt), writes=[r_ss])
            s.op("dve", lambda e: e.reciprocal(out=ssq[0:P, :], in_=ssq[0:P, :]), writes=[r_ss])
            s.op("dve", lambda e: e.scalar_tensor_tensor(out=out_ap, in0=x_ap, scalar=ssq[0:P, 0:1], in1=g_tile[0:P, :],
                                                         op0=ALU.mult, op1=ALU.mult),
                 reads=[r_x, r_ss, r_g], writes=[r_out])

        if "U" in PH:
          with ExitStack() as pu:
            sbu = lambda name, shape, dtype: pu.enter_context(nc.sbuf_tensor(_un(name), shape, dtype))
            gmix = sbu("gmixU", [128, D], F32); r_gmix = s.res()
            load_gain(gmix, g_pre_mix, r_gmix)
            cvec = sbu("cvec", [128, 3, 8], F32); r_cvec = s.res()
            for vi, v_ap in enumerate((conv_b, ln_g, ln_b)):
                s.dma("sp", lambda e, vi=vi, v_ap=v_ap: e.dma_start(out=cvec[:, vi, :], in_=v_ap.rearrange("o (c p) -> p (o c)", p=128),
                                                                       allow_slow_non_contiguous=True), writes=[r_cvec])
            cw_sb = sbu("cw_sb", [CW, CONV_CH], F32); r_cw = s.res()
            s.dma("sp", lambda e: e.dma_start(out=cw_sb[:], in_=conv_w[:, :]), writes=[r_cw])
            cwT = sbu("cwT", [128, 8, CW], F32); r_cwT = s.res()
            for c in range(8):
                s.op("pe", lambda e, c=c: e.transpose(bk(0)[:, c * 32:c * 32 + CW], cw_sb[:, c * 128:(c + 1) * 128], ident32[0:CW, 0:CW]),
                     reads=[r_cw, r_c], writes=[rb[0]])
            s.op("dve", lambda e: e.tensor_copy(out=cwT[:], in_=bk(0)[:, 0:256].rearrange("p (c j) -> p c j", j=32)[:, :, 0:CW]),
                 writes=[rb[0], r_cwT])
            diag = sbu("diag", [128, 8, CW, 128], BF16); r_diag = s.res()
            for c in range(8):
                for j in range(CW):
                    s.op("pool", lambda e, c=c, j=j: e.tensor_scalar(out=diag[:, c, j, :], in0=ident[:], scalar1=cwT[:, c, j:j + 1],
                                                                     scalar2=None, op0=ALU.mult),
                         reads=[r_cwT, r_c], writes=[r_diag])
            wwi = sbu("wwi", [128, 16, 16], BF16); r_wwi = s.res()
            s.dma("pool", lambda e: e.dma_start(out=wwi[:], in_=w_in_r[:, :, 4672:4688]), writes=[r_wwi])
            wwirep = sbu("wwirep", [128, 16, 128], BF16); r_wrep = s.res()
            for h2 in range(2):
                for hp in range(8):
                    h = 2 * hp + h2
                    col = h2 * 64 + hp * 8
                    s.op("pool", lambda e, h=h, col=col: e.tensor_copy(
                        out=wwirep[:, :, col:col + 8], in_=wwi[:, :, h:h + 1].to_broadcast([128, 16, 8])),
                        reads=[r_wwi], writes=[r_wrep])
            maskg = sbu("maskg", [128, 16, 128], BF16); r_maskg = s.res()
            s.dma("sp", lambda e: e.dma_start(out=maskg[:], in_=maskg_d[:, :, :]), writes=[r_maskg])

            xt = sbu("xtU", [128, D], F32); r_xt = s.res()
            xh = sbu("xhU", [32, D], F32); r_xh = s.res()
            junk = sbu("junkU", [128, D], BF16); r_junk = s.res()
            ssq = sbu("ssqU", [128, 1], F32); r_ssq = s.res()
            ssq2 = sbu("ssq2U", [128, 1], F32); r_ssq2 = s.res()
            xn = sbu("xnU", [128, D], BF16); r_xn = s.res()
            xnh = sbu("xnhU", [32, D], BF16); r_xnh = s.res()
            xnT = sbu("xnTU", [128, 16, SB, 160], BF16); r_xnT = s.res()
            Wt = [sbu("WtU%d" % i, [128, 16, 128], BF16) for i in range(2)]
            r_Wt = [s.res() for _ in range(2)]
            uT = sbu("uT", [128, 8, SB, 160], BF16); r_uT = s.res()
            sig = sbu("sig", [128, SB, 160], F32); r_sig = s.res()
            u32 = sbu("u32", [128, 8, 32], F32); r_u32 = s.res()
            Qst = sbu("Qst", [128, SB, 8, 128], BF16); r_Qst = s.res()
            Qi_sb = sbu("Qi_sb", [128, SB, 16, 8, 8], BF16); r_Qi = s.res()
            WIrep = sbu("WIrep", [128, SB, 128], F32); r_WIrep = s.res()
            Wsel = sbu("WselU", [128, 16, 128], BF16); r_Wsel = s.res()
            ycv = sbu("ycv", [128, 8, 128], F32); r_ycv = s.res()
            ysq = sbu("ysq", [128, 8, 128], F32); r_ysq = s.res()
            stat = sbu("stat", [128, 3, 128], F32); r_stat = s.res()
            co = sbu("co", [128, 8, 128], BF16); r_co = s.res()
            stt = sbu("stt", [CW - 1, CONV_CH], F32); r_stt = s.res()
            fullT = sbu("fullT", [128, 8, 4, 38], BF16); r_fullT = s.res()
            cvo = sbu("cvo", [32, CONV_CH], F32); r_cvo = s.res()

            wcnt = [0]

            def conv_tail(blk, TW):
                Y = bk(0, 2).rearrange("p (c t) -> p c t", t=128)
                s.op("dve", lambda e: e.tensor_tensor(out=ycv[:, :, 0:TW], in0=Y[:, :, 0:TW],
                                                      in1=cvec[:, 0, :].unsqueeze(2).to_broadcast([128, 8, TW]), op=ALU.add),
                     reads=[r_cvec], writes=[rb[0], rb[1], r_ycv])
                s.op("act", lambda e: e.activation(out=ysq[:, :, 0:TW], in_=ycv[:, :, 0:TW], func=AF.Square),
                     reads=[r_ycv], writes=[r_ysq])
                for c in range(8):
                    s.op("pe", lambda e, c=c: e.matmul(bk(2)[:, 0:TW], ones32[:], ycv[:, c, 0:TW], start=(c == 0), stop=(c == 7)),
                         reads=[r_ycv, r_c], writes=[rb[2]])
                for c in range(8):
                    s.op("pe", lambda e, c=c: e.matmul(bk(3)[:, 0:TW], ones32[:], ysq[:, c, 0:TW], start=(c == 0), stop=(c == 7)),
                         reads=[r_ysq, r_c], writes=[rb[3]])
                mean, msq, rstd = stat[:, 0, 0:TW], stat[:, 1, 0:TW], stat[:, 2, 0:TW]
                s.op("dve", lambda e: e.tensor_scalar(out=mean, in0=bk(2)[:, 0:TW], scalar1=1.0 / CONV_CH, scalar2=None, op0=ALU.mult),
                     writes=[rb[2], r_stat])
                s.op("dve", lambda e: e.tensor_tensor(out=msq, in0=mean, in1=mean, op=ALU.mult), writes=[r_stat])
                s.op("dve", lambda e: e.scalar_tensor_tensor(out=rstd, in0=bk(3)[:, 0:TW], scalar=1.0 / CONV_CH, in1=msq,
                                                             op0=ALU.mult, op1=ALU.subtract), writes=[rb[3], r_stat])
                s.op("dve", lambda e: e.tensor_scalar(out=rstd, in0=rstd, scalar1=EPS, scalar2=None, op0=ALU.add), writes=[r_stat])
                s.op("act", lambda e: e.activation(out=rstd, in_=rstd, func=AF.Sqrt), writes=[r_stat])
                s.op("dve", lambda e: e.reciprocal(out=rstd, in_=rstd), writes=[r_stat])
                s.op("dve", lambda e: e.tensor_tensor(out=ycv[:, :, 0:TW], in0=ycv[:, :, 0:TW],
                                                      in1=stat[:, 0:1, 0:TW].to_broadcast([128, 8, TW]), op=ALU.subtract),
                     reads=[r_stat], writes=[r_ycv])
                s.op("dve", lambda e: e.tensor_tensor(out=ycv[:, :, 0:TW], in0=ycv[:, :, 0:TW],
                                                      in1=stat[:, 2:3, 0:TW].to_broadcast([128, 8, TW]), op=ALU.mult),
                     reads=[r_stat], writes=[r_ycv])
                if TW < 128:
                    s.op("pool", lambda e: e.memset(co[:], 0.0), writes=[r_co])
                for c in range(8):
                    s.op("act", lambda e, c=c: e.activation(out=co[:, c, 0:TW], in_=ycv[:, c, 0:TW], func=AF.Silu,
                                                            scale=cvec[:, 1, c:c + 1], bias=cvec[:, 2, c:c + 1]),
                         reads=[r_ycv, r_cvec], writes=[r_co])
                s.dma("sp", lambda e: e.dma_start(out=mixT_scr[blk, :, 0:8, :], in_=co[:]), reads=[r_co])

            def u32_out(ncols, dst_fn):
                for c in range(8):
                    s.op("pe", lambda e, c=c: e.transpose(bk(2, 2)[0:ncols, c * 128:(c + 1) * 128], u32[:, c, 0:ncols], ident32[:]),
                         reads=[r_u32, r_c], writes=[rb[2], rb[3]])
                s.op("dve", lambda e: e.tensor_copy(out=cvo[0:ncols, :], in_=bk(2, 2)[0:ncols, :]), writes=[rb[2], rb[3], r_cvo])
                dst_fn()

            supers = [list(range(i, min(i + SB, NOWN))) for i in range(0, NOWN, SB)] + [[SBLK]]
            for blks in supers:
                nb = len(blks)
                is_s = blks[0] == SBLK
                for bi, blk in enumerate(blks):
                    src = xs if is_s else xo[blk]
                    s.dma("sp", lambda e: e.dma_start(out=xh[:], in_=src[0:32, :]), writes=[r_xh])
                    s.dma("sp", lambda e: e.dma_start(out=xt[:], in_=src[32:160, :]), writes=[r_xt])
                    rms_to_bf(32, xh[:], r_xh, gmix, r_gmix, xnh[:], r_xnh, (junk, r_junk, ssq2, r_ssq2))
                    rms_to_bf(128, xt[:], r_xt, gmix, r_gmix, xn[:], r_xn, (junk, r_junk, ssq, r_ssq))
                    tpm = bkb(0, 2).rearrange("p (k t) -> p k t", t=128)
                    tph = bkb(2).rearrange("p (k t) -> p k t", t=64)
                    for k in range(16):
                        s.op("pe", lambda e, k=k: e.transpose(tpm[:, k, :], xn[:, k * 128:(k + 1) * 128], ident[:]),
                             reads=[r_xn, r_c], writes=[rb[0], rb[1]])
                    for k in range(16):
                        s.op("pe", lambda e, k=k: e.transpose(tph[:, k, 0:32], xnh[:, k * 128:(k + 1) * 128], ident[0:32, 0:32]),
                             reads=[r_xnh, r_c], writes=[rb[2]])
                    s.op("act", lambda e: e.copy(out=xnT[:, :, bi, 32:160], in_=tpm[:, :, :]), writes=[rb[0], rb[1], r_xnT])
                    s.op("dve", lambda e: e.tensor_copy(out=xnT[:, :, bi, 0:32], in_=tph[:, :, 0:32]), writes=[rb[2], r_xnT])
                halves = [(0, min(2, nb))] + ([(2, nb)] if nb > 2 else [])

                def proj(ct_cols, lhs_fn, pair):
                    for hi, (b0, b1) in enumerate(halves):
                        n = (b1 - b0) * 160
                        for k in range(16):
                            s.op("pe", lambda e, k=k: e.matmul(bk(pair + hi)[:, 0:n], lhs_fn(k), xnT[:, k, b0:b1, :],
                                                               start=(k == 0), stop=(k == 15)),
                                 reads=[r_xnT] + ct_cols, writes=[rb[pair + hi]])

                def load_w(col0):
                    i = wcnt[0] % 2
                    wcnt[0] += 1
                    s.dma("pool", lambda e: e.dma_start(out=Wt[i][:], in_=w_in_r[:, :, col0:col0 + 128]), writes=[r_Wt[i]])
                    return i

                def pview(pair, hi, b0, b1):
                    return bk(pair + hi)[:, 0:(b1 - b0) * 160].rearrange("p (b t) -> p b t", t=160)

                for c in range(8):
                    ia = load_w(c * 128)
                    proj([r_Wt[ia]], lambda k, ia=ia: Wt[ia][:, k, :], 4)
                    ig = load_w(1024 + c * 128)
                    proj([r_Wt[ig]], lambda k, ig=ig: Wt[ig][:, k, :], 6)
                    for hi, (b0, b1) in enumerate(halves):
                        s.op("act", lambda e: e.activation(out=sig[:, b0:b1, :], in_=pview(6, hi, b0, b1), func=AF.Sigmoid),
                             writes=[rb[6 + hi], r_sig])
                        s.op("dve", lambda e: e.tensor_tensor(out=uT[:, c, b0:b1, :], in0=pview(4, hi, b0, b1), in1=sig[:, b0:b1, :], op=ALU.mult),
                             reads=[r_sig], writes=[rb[4 + hi], r_uT])
                        if is_s:
                            s.op("dve", lambda e: e.tensor_tensor(out=u32[:, c, :], in0=pview(4, hi, b0, b1)[:, 0, 32:64], in1=sig[:, 0, 32:64], op=ALU.mult),
                                 reads=[r_sig], writes=[rb[4 + hi], r_u32])
                        elif blks[-1] == NOWN - 1 and b1 == nb:
                            s.op("dve", lambda e: e.tensor_tensor(out=u32[:, c, 0:30], in0=pview(4, hi, b0, b1)[:, b1 - b0 - 1, 130:160],
                                                                  in1=sig[:, nb - 1, 130:160], op=ALU.mult),
                                 reads=[r_sig], writes=[rb[4 + hi], r_u32])
                for h in range(8):
                    iw = load_w(2048 + h * 128)
                    pair = 4 + 2 * (h % 2)
                    proj([r_Wt[iw]], lambda k, iw=iw: Wt[iw][:, k, :], pair)
                    for hi, (b0, b1) in enumerate(halves):
                        s.op("act", lambda e: e.activation(out=Qst[:, b0:b1, h, :], in_=pview(pair, hi, b0, b1)[:, :, 32:160],
                                                           func=AF.Copy, scale=ATTN_SCALE),
                             writes=[rb[pair + hi], r_Qst])
                for hp in range(8):
                    iw = load_w(3584 + hp * 128)
                    pair = 4 + 2 * (hp % 2)
                    proj([r_Wt[iw]], lambda k, iw=iw: Wt[iw][:, k, :], pair)
                    for hi, (b0, b1) in enumerate(halves):
                        for b in range(b0, b1):
                            s.op("dve", lambda e, b=b: e.tensor_copy(
                                out=Qi_sb[:, b, :, hp, :],
                                in_=pview(pair, hi, b0, b1)[:, b - b0, 32:160].rearrange("p (g t) -> p g t", t=8)),
                                writes=[rb[pair + hi], r_Qi])
                proj([r_wrep], lambda k: wwirep[:, k, :], 4)
                for hi, (b0, b1) in enumerate(halves):
                    s.op("act", lambda e: e.activation(out=WIrep[:, b0:b1, :], in_=pview(4, hi, b0, b1)[:, :, 32:160],
                                                       func=AF.Copy, scale=INDEX_SCALE),
                         writes=[rb[4 + hi], r_WIrep])
                for bi, blk in enumerate(blks):
                    s.dma("sp", lambda e: e.dma_start(out=QT_scr[blk], in_=Qst[:, bi, :, :]), reads=[r_Qst])
                    for h2 in range(2):
                        s.dma("sp", lambda e, h2=h2: e.dma_start(
                            out=qiT_scr[blk, :, :, h2, :],
                            in_=Qi_sb[h2 * 64:(h2 + 1) * 64, bi, :, :, :].rearrange("p g a b -> p g (a b)")),
                            reads=[r_Qi])
                    s.op("dve", lambda e: e.tensor_tensor(out=Wsel[:], in0=maskg[:],
                                                          in1=WIrep[:, bi:bi + 1, :].to_broadcast([128, 16, 128]), op=ALU.mult),
                         reads=[r_maskg, r_WIrep], writes=[r_Wsel])
                    s.dma("sp", lambda e: e.dma_start(out=Wsel_scr[blk], in_=Wsel[:]), reads=[r_Wsel])
                    Y = bk(0, 2).rearrange("p (c t) -> p c t", t=128)
                    if not is_s:
                        for c in range(8):
                            for j in range(CW):
                                s.op("pe", lambda e, c=c, j=j: e.matmul(Y[:, c, :], diag[:, c, j, :], uT[:, c, bi, 2 + j:2 + j + 128],
                                                                        start=(j == 0), stop=(j == CW - 1)),
                                     reads=[r_diag, r_uT], writes=[rb[0], rb[1]])
                        conv_tail(blk, 128)
                    else:
                        for q in range(4):
                            s.dma("sp", lambda e, q=q: e.dma_start(out=stt[:], in_=state_conv[q]), writes=[r_stt])
                            tq = bk(2)[:, 0:256].rearrange("p (c j) -> p c j", j=32)
                            for c in range(8):
                                s.op("pe", lambda e, c=c: e.transpose(tq[:, c, 0:CW - 1], stt[:, c * 128:(c + 1) * 128], ident32[0:CW - 1, 0:CW - 1]),
                                     reads=[r_stt, r_c], writes=[rb[2]])
                            s.op("dve", lambda e, q=q: e.tensor_copy(out=fullT[:, :, q, 0:CW - 1], in_=tq[:, :, 0:CW - 1]),
                                 writes=[rb[2], r_fullT])
                            s.op("dve", lambda e, q=q: e.tensor_copy(out=fullT[:, :, q, CW - 1:CW + 7], in_=uT[:, :, 0, 32 + 8 * q:40 + 8 * q]),
                                 reads=[r_uT], writes=[r_fullT])
                            s.dma("sp", lambda e, q=q: e.dma_start(out=o_convs[q, 0:CW - 9, :], in_=state_conv[q, 8:CW - 1, :]))
                        for c in range(8):
                            for q in range(4):
                                for j in range(CW):
                                    s.op("pe", lambda e, c=c, q=q, j=j: e.matmul(Y[:, c, 8 * q:8 * q + 8], diag[:, c, j, :], fullT[:, c, q, j:j + 8],
                                                                                 start=(j == 0), stop=(j == CW - 1)),
                                         reads=[r_diag, r_fullT], writes=[rb[0], rb[1]])
                        conv_tail(blk, 32)
                        u32_out(32, lambda: [s.dma("sp", lambda e, q=q: e.dma_start(out=o_convs[q, CW - 9:CW - 1, :], in_=cvo[8 * q:8 * q + 8, :]),
                                                   reads=[r_cvo]) for q in range(4)])
                if (not is_s) and blks[-1] == NOWN - 1:
                    u32_out(30, lambda: s.dma("sp", lambda e: e.dma_start(out=o_convp[:, :], in_=cvo[0:30, :]), reads=[r_cvo]))
            s.barrier()

        def kv_env(stack, pfx):
            sba = lambda name, shape, dtype: stack.enter_context(nc.sbuf_tensor(_un(name), shape, dtype))
            gmix = sba(pfx + "gmixA", [128, D], F32); r_gmix = s.res()
            load_gain(gmix, g_pre_mix, r_gmix)
            Wkv = sba(pfx + "Wkv", [128, 16, 576], BF16)
            r_Wkv = s.res()
            for k4 in range(4):
                s.dma("pool", lambda e, k4=k4: e.dma_start(out=Wkv[:, 4 * k4:4 * k4 + 4, 0:512],
                                                            in_=w_in_r[:, 4 * k4:4 * k4 + 4, 3072:3584]), writes=[r_Wkv])
                s.dma("pool", lambda e, k4=k4: e.dma_start(out=Wkv[:, 4 * k4:4 * k4 + 4, 512:576],
                                                            in_=w_in_r[:, 4 * k4:4 * k4 + 4, 4608:4672]), writes=[r_Wkv])
            xt = [sba(pfx + "xt%d" % i, [128, D], F32) for i in range(2)]
            r_xt = [s.res() for _ in range(2)]
            junk = sba(pfx + "junkA", [128, D], BF16); r_junk = s.res()
            xn = [sba(pfx + "xn%d" % i, [128, D], BF16) for i in range(2)]
            r_xn = [s.res() for _ in range(2)]
            xnT = [sba(pfx + "xnT%d" % i, [128, 16, 128], BF16) for i in range(2)]
            r_xnT = [s.res() for _ in range(2)]
            ssq = [sba(pfx + "ssA%d" % i, [128, 1], F32) for i in range(2)]
            r_ss = [s.res() for _ in range(2)]
            kvf = [sba(pfx + "kvf%d" % i, [128, 576], F32) for i in range(2)]
            r_kvf = [s.res() for _ in range(2)]
            kb = [sba(pfx + "kb%d" % i, [128, 320], BF16) for i in range(2)]
            r_kb = [s.res() for _ in range(2)]
            tp = bkb(0, 2).rearrange("p (k t) -> p k t", t=128)
            tp2 = bkb(6).rearrange("p (k t) -> p k t", t=128)

            kvcnt = [0]

            def kv_tile(src, KT_dst, V_dst, kiT_dst, r_KT, r_V, r_kiT, dk, dv, dik):
                b = kvcnt[0] % 2; kvcnt[0] += 1
                pkv, pki = bk(2 + b), bk(4 + b)
                r_pkv = [rb[2 + b], rb[4 + b]]
                s.dma("sp", lambda e: e.dma_start(out=xt[b][:], in_=src), writes=[r_xt[b]])
                rms_to_bf(128, xt[b][:], r_xt[b], gmix, r_gmix, xn[b][:], r_xn[b], (junk, r_junk, ssq[b], r_ss[b]))
                for k in range(16):
                    s.op("pe", lambda e, k=k: e.transpose(tp[:, k, :], xn[b][:, k * 128:(k + 1) * 128], ident[:]),
                         reads=[r_xn[b], r_c], writes=[rb[0], rb[1]])
                s.op("act", lambda e: e.copy(out=xnT[b][:], in_=tp[:, :, :]), writes=[rb[0], rb[1], r_xnT[b]])
                for k in range(16):
                    s.op("pe", lambda e, k=k: e.matmul(pkv[:, :], xnT[b][:, k, :], Wkv[:, k, 0:512], start=(k == 0), stop=(k == 15)),
                         reads=[r_xnT[b], r_Wkv], writes=r_pkv)
                for k in range(16):
                    s.op("pe", lambda e, k=k: e.matmul(pki[:, 0:64], xnT[b][:, k, :], Wkv[:, k, 512:576], start=(k == 0), stop=(k == 15)),
                         reads=[r_xnT[b], r_Wkv], writes=r_pkv)
                s.op("dve", lambda e: e.tensor_copy(out=kvf[b][:, 0:512], in_=pkv[:, :]), writes=r_pkv + [r_kvf[b]])
                s.op("dve", lambda e: e.tensor_copy(out=kvf[b][:, 512:576], in_=pki[:, 0:64]), writes=r_pkv + [r_kvf[b]])
                s.op("act", lambda e: e.copy(out=kb[b][:, 0:256], in_=pkv[:, 0:256]), writes=r_pkv + [r_kb[b]])
                s.op("act", lambda e: e.copy(out=V_dst, in_=pkv[:, 256:512]), writes=r_pkv + [r_V])
                s.op("act", lambda e: e.copy(out=kb[b][:, 256:320], in_=pki[:, 0:64]), writes=r_pkv + [r_kb[b]])
                s.op("pe", lambda e: e.transpose(tp2[:, 0, :], kb[b][:, 0:128], ident[:]), reads=[r_kb[b], r_c], writes=[rb[6]])
                s.op("pe", lambda e: e.transpose(tp2[:, 1, :], kb[b][:, 128:256], ident[:]), reads=[r_kb[b], r_c], writes=[rb[6]])
                s.op("pe", lambda e: e.transpose(tp2[0:64, 2, :], kb[b][:, 256:320], ident[:]), reads=[r_kb[b], r_c], writes=[rb[6]])
                s.op("dve", lambda e: e.tensor_copy(out=KT_dst, in_=tp2[:, 0:2, :]), writes=[rb[6], r_KT])
                s.op("dve", lambda e: e.tensor_copy(out=kiT_dst, in_=tp2[0:64, 2, :]), writes=[rb[6], r_kiT])
                s.dma("sp", lambda e: e.dma_start(out=dk, in_=kvf[b][:, 0:256]), reads=[r_kvf[b]])
                s.dma("sp", lambda e: e.dma_start(out=dv, in_=kvf[b][:, 256:512]), reads=[r_kvf[b]])
                s.dma("sp", lambda e: e.dma_start(out=dik, in_=kvf[b][:, 512:576]), reads=[r_kvf[b]])

            return kv_tile

        def attn_env(stack, pfx, NSC):
            sbb = lambda name, shape, dtype: stack.enter_context(nc.sbuf_tensor(_un(name), shape, dtype))
            NS_ = 2 if pfx == "b_" else 1
            scores = [sbb(pfx + "score%d" % i, [128, NSC], F32) for i in range(NS_)]
            r_scores = [s.res() for _ in range(NS_)]
            cur = {"i": 0}
            wk = sbb(pfx + "wk", [128, NBIS], F32); r_wk = s.res()
            pw2 = sbb(pfx + "pw2", [128, NBIS], F32); r_pw2 = s.res()
            for it_ in range(NBIS):
                s.op("pool", lambda e, it_=it_: e.memset(pw2[:, it_:it_ + 1], 0.5 ** (it_ + 1)), writes=[r_pw2])
            cjunk = sbb(pfx + "cjunk", [128, NSC], U8); r_cjunk = s.res()
            cmask = sbb(pfx + "cmask", [128, 512], F32)
            cmask_s = sbb(pfx + "cmask_s", [128, 128], F32)
            identrep = sbb(pfx + "identrep", [128, 4, 128], BF16)
            selrep = sbb(pfx + "selrep", [32, 4, 32], BF16)
            iota_p = sbb(pfx + "iota_p", [128, 1], F32)
            r_bc = s.res()
            s.dma("sp", lambda e: e.dma_start(out=cmask[:], in_=cmask_d[:, :]), writes=[r_bc])
            s.dma("sp", lambda e: e.dma_start(out=cmask_s[:], in_=cmask_s_d[:, :]), writes=[r_bc])
            s.dma("sp", lambda e: e.dma_start(out=identrep[:], in_=identrep_d[:, :, :]), writes=[r_bc])
            s.dma("sp", lambda e: e.dma_start(out=selrep[:], in_=selrep_d[:, :, :]), writes=[r_bc])
            s.dma("sp", lambda e: e.dma_start(out=iota_p[:], in_=iota_d[:, :]), writes=[r_bc])
            QTb = sbb(pfx + "QTb", [128, 8, 128], BF16); r_QTb = s.res()
            qiTb = sbb(pfx + "qiTb", [64, 16, 128], BF16); r_qiTb = s.res()
            Wselb = sbb(pfx + "Wselb", [128, 16, 128], BF16); r_Wselb = s.res()
            Rt = [sbb(pfx + "Rt%d" % i, [128, 512], BF16) for i in range(4)]
            r_Rt = [s.res() for _ in range(4)]
            Mb = [sbb(pfx + "Mb%d" % i, [128, 512], BF16) for i in range(2)]
            r_Mb = [s.res() for _ in range(2)]
            Pt = [sbb(pfx + "Pt%d" % i, [128, 512], BF16) for i in range(3)]
            r_Pt = [s.res() for _ in range(3)]
            sms = [sbb(pfx + "smallB%d" % i, [128, 8], F32) for i in range(NS_)]
            r_sms = [s.res() for _ in range(NS_)]
            rsum = sbb(pfx + "rsum", [128, 2, 512], F32); r_rsum = s.res()
            aoT = sbb(pfx + "aoT", [128, 8, 128], BF16); r_aoT = s.res()
            cnt = {"r": 0, "m": 0, "p": 0, "l": 0}

            def load_q(blk):
                s.dma("sp", lambda e: e.dma_start(out=QTb[:], in_=QT_scr[blk]), writes=[r_QTb])

            def load_block(blk):
                s.dma("sp", lambda e: e.dma_start(out=qiTb[:], in_=qiT_scr[blk].rearrange("d g a b -> d g (a b)")), writes=[r_qiTb])
                s.dma("sp", lambda e: e.dma_start(out=Wselb[:], in_=Wsel_scr[blk]), writes=[r_Wselb])

            def idx_L(it):
                lb = cnt["l"] % 3; cnt["l"] += 1
                it["lb"] = lb
                ncol = it["ncol"]
                s.op("pe", lambda e: e.matmul(bk(lb)[:, 0:ncol], qiTb[:, it["g"], :], it["ki"], start=True, stop=True),
                     reads=[r_qiTb, it["r_ki"]], writes=[rb[lb]])

            def idx_R(it):
                lb, ncol, accumulate = it["lb"], it["ncol"], it["acc"]
                ri = cnt["r"] % 4; cnt["r"] += 1
                if ri % 2 == 0 or not accumulate:
                    s.op("act", lambda e: e.activation(out=Rt[ri][:, 0:ncol], in_=bk(lb)[:, 0:ncol], func=AF.Relu),
                         writes=[rb[lb], r_Rt[ri]])
                else:
                    s.op("dve", lambda e: e.tensor_scalar(out=Rt[ri][:, 0:ncol], in0=bk(lb)[:, 0:ncol], scalar1=0.0, scalar2=None, op0=ALU.max),
                         writes=[rb[lb], r_Rt[ri]])
                s.op("pe", lambda e: e.matmul(bk(3)[:, 0:ncol], Wselb[:, it["g"], :], Rt[ri][:, 0:ncol],
                                              start=it["first"], stop=it["last"]),
                     reads=[r_Wselb, r_Rt[ri]], writes=[rb[3]])
                if it["last"]:
                    score, r_score = scores[it["sc"]], r_scores[it["sc"]]
                    dst = score[:, it["dst"]:it["dst"] + ncol]
                    if accumulate:
                        s.op("dve", lambda e: e.tensor_tensor(out=dst, in0=bk(3)[:, 0:ncol], in1=dst, op=ALU.add), writes=[rb[3], r_score])
                    else:
                        s.op("act", lambda e: e.copy(out=dst, in_=bk(3)[:, 0:ncol]), writes=[rb[3], r_score])

            def indexer_run(items, look=2):
                for it in items[:look]:
                    idx_L(it)
                for n, it in enumerate(items):
                    idx_R(it)
                    if n + look < len(items):
                        idx_L(items[n + look])

            def idx_items(groups, ki_ap, r_ki, ncol, dst_col, accumulate):
                return [dict(g=g, ki=ki_ap, r_ki=r_ki, ncol=ncol, dst=dst_col, acc=accumulate, sc=cur["i"],
                             first=(gi == 0), last=(gi == len(groups) - 1)) for gi, g in enumerate(groups)]

            def indexer_tile(groups, ki_ap, r_ki, ncol, dst_col, mask_ap, accumulate):
                indexer_run(idx_items(groups, ki_ap, r_ki, ncol, dst_col, accumulate))

            def threshold(ncols, premask_cols=None):
                score, r_score = scores[cur["i"]], r_scores[cur["i"]]
                sm, r_sm = sms[cur["i"]], r_sms[cur["i"]]
                sc = score[:, 0:ncols]
                lo, hi, mid, cn, sel, d1 = (sm[:, i:i + 1] for i in range(6))
                s.op("dve", lambda e: e.tensor_reduce(out=hi, in_=sc, axis=AX.X, op=ALU.max), reads=[r_score], writes=[r_sm])
                s.op("dve", lambda e: e.tensor_reduce(out=lo, in_=sc, axis=AX.X, op=ALU.min), reads=[r_score], writes=[r_sm])
                if premask_cols is not None:
                    c0, c1, m_ap = premask_cols
                    s.op("dve", lambda e: e.tensor_tensor(out=score[:, c0:c1], in0=score[:, c0:c1], in1=m_ap, op=ALU.add),
                         reads=[r_bc], writes=[r_score])
                s.op("dve", lambda e: e.tensor_tensor(out=d1, in0=hi, in1=lo, op=ALU.subtract), writes=[r_sm])
                s.op("dve", lambda e: e.tensor_scalar(out=d1, in0=d1, scalar1=1.001, scalar2=1e-6, op0=ALU.mult, op1=ALU.add), writes=[r_sm])
                s.op("dve", lambda e: e.tensor_scalar(out=wk[:], in0=pw2[:], scalar1=d1, scalar2=None, op0=ALU.mult),
                     reads=[r_pw2], writes=[r_sm, r_wk])
                for it in range(NBIS):
                    s.op("dve", lambda e: e.tensor_tensor(out=mid, in0=lo, in1=wk[:, it:it + 1], op=ALU.add), reads=[r_wk], writes=[r_sm])
                    s.op("dve", lambda e: e.tensor_scalar(out=cjunk[:, 0:ncols], in0=sc, scalar1=mid, scalar2=None,
                                                          op0=ALU.is_ge, op1=ALU.add, accum_out=cn),
                         reads=[r_score], writes=[r_cjunk, r_sm])
                    s.op("dve", lambda e: e.scalar_tensor_tensor(out=sel, in0=cn, scalar=float(TOPK) - 0.5, in1=wk[:, it:it + 1],
                                                                 op0=ALU.is_ge, op1=ALU.mult), reads=[r_wk], writes=[r_sm])
                    s.op("dve", lambda e: e.tensor_tensor(out=lo, in0=lo, in1=sel, op=ALU.add), writes=[r_sm])

            def make_mb(c0, ncol):
                mi = cnt["m"] % 2; cnt["m"] += 1
                score, r_score = scores[cur["i"]], r_scores[cur["i"]]
                sm, r_sm = sms[cur["i"]], r_sms[cur["i"]]
                s.op("dve", lambda e: e.tensor_scalar(out=Mb[mi][:, 0:ncol], in0=score[:, c0:c0 + ncol], scalar1=sm[:, 0:1], scalar2=NEG,
                                                      op0=ALU.is_lt, op1=ALU.mult),
                     reads=[r_score, r_sm], writes=[r_Mb[mi]])
                return mi

            def att_S(it):
                pi = cnt["p"] % 3; cnt["p"] += 1
                sb_ = pi % 2
                it["pi"], it["sb"] = pi, sb_
                s.op("pe", lambda e: e.matmul(bk(sb_)[:, :], it["kt"], it["q"], start=True, stop=False),
                     reads=it["r_kv"] + [it["r_q"]], writes=[rb[sb_]])
                s.op("pe", lambda e: e.matmul(bk(sb_)[:, :], it["mb_l"], it["mb_r"], start=False, stop=True),
                     reads=[it["r_mb"], r_bc], writes=[rb[sb_]])
                s.op("act", lambda e: e.activation(out=Pt[pi][:, :], in_=bk(sb_)[:, :], func=AF.Exp), writes=[rb[sb_], r_Pt[pi]])

            def att_PV(it):
                pi, kvh = it["pi"], it["kvh"]
                s.op("pe", lambda e: e.matmul(bk(4 + kvh)[:, :], it["v"], Pt[pi][:, :], start=it["first"], stop=it["last"]),
                     reads=it["r_kv"] + [r_Pt[pi]], writes=[rb[4 + kvh]])
                s.op("pe", lambda e: e.matmul(bk(6 + kvh)[:, :], ones_bf[:, :], Pt[pi][:, :], start=it["first"], stop=it["last"]),
                     reads=[r_Pt[pi], r_c], writes=[rb[6 + kvh]])

            def attend_chunk(first, last, kp, kt_fn, v_fn, r_kv, q_fn, r_q, nq, mb_l, mb_r, r_mb):
                if nq == 512:
                    for kvh in range(2):
                        pi = cnt["p"] % 3; cnt["p"] += 1
                        sb_ = pi % 2
                        s.op("pe", lambda e: e.matmul(bk(sb_)[0:kp, :], kt_fn(kvh), q_fn(kvh), start=True, stop=False),
                             reads=r_kv + [r_q], writes=[rb[sb_]])
                        s.op("pe", lambda e: e.matmul(bk(sb_)[0:kp, :], mb_l, mb_r, start=False, stop=True),
                             reads=[r_mb, r_bc], writes=[rb[sb_]])
                        s.op("act", lambda e: e.activation(out=Pt[pi][0:kp, :], in_=bk(sb_)[0:kp, :], func=AF.Exp), writes=[rb[sb_], r_Pt[pi]])
                        s.op("pe", lambda e: e.matmul(bk(4 + kvh)[:, :], v_fn(kvh), Pt[pi][0:kp, :], start=first, stop=last),
                             reads=r_kv + [r_Pt[pi]], writes=[rb[4 + kvh]])
                        s.op("pe", lambda e: e.matmul(bk(6 + kvh)[:, :], ones_bf[0:kp, :], Pt[pi][0:kp, :], start=first, stop=last),
                             reads=[r_Pt[pi], r_c], writes=[rb[6 + kvh]])
                else:
                    pi = cnt["p"] % 3; cnt["p"] += 1
                    sb_ = pi % 2
                    for kvh in range(2):
                        s.op("pe", lambda e: e.matmul(bk(sb_)[0:kp, kvh * nq:(kvh + 1) * nq], kt_fn(kvh), q_fn(kvh), start=True, stop=False),
                             reads=r_kv + [r_q], writes=[rb[sb_]])
                        s.op("pe", lambda e: e.matmul(bk(sb_)[0:kp, kvh * nq:(kvh + 1) * nq], mb_l, mb_r, start=False, stop=True),
                             reads=[r_mb, r_bc], writes=[rb[sb_]])
                    s.op("act", lambda e: e.activation(out=Pt[pi][0:kp, 0:2 * nq], in_=bk(sb_)[0:kp, 0:2 * nq], func=AF.Exp),
                         writes=[rb[sb_], r_Pt[pi]])
                    for kvh in range(2):
                        s.op("pe", lambda e: e.matmul(bk(4 + kvh)[:, 0:nq], v_fn(kvh), Pt[pi][0:kp, kvh * nq:(kvh + 1) * nq],
                                                      start=first, stop=last),
                             reads=r_kv + [r_Pt[pi]], writes=[rb[4 + kvh]])
                    s.op("pe", lambda e: e.matmul(bk(6)[:, 0:2 * nq], ones_bf[0:kp, :], Pt[pi][0:kp, 0:2 * nq], start=first, stop=last),
                         reads=[r_Pt[pi], r_c], writes=[rb[6]])


            return dict(score=scores[0], r_score=r_scores[0], sm=sms[0], r_sm=r_sms[0], cur=cur, load_q=load_q, Pt=Pt, r_Pt=r_Pt, QTb=QTb, r_QTb=r_QTb, aoT=aoT, r_aoT=r_aoT, rsum=rsum, r_rsum=r_rsum,
                        Mb=Mb, r_Mb=r_Mb, cmask=cmask, cmask_s=cmask_s, identrep=identrep, selrep=selrep, iota_p=iota_p, r_bc=r_bc, cnt=cnt,
                        load_block=load_block, indexer_tile=indexer_tile, indexer_run=indexer_run, idx_items=idx_items, att_S=att_S, att_PV=att_PV, threshold=threshold, make_mb=make_mb, attend_chunk=attend_chunk)


        if "S" in PH:
            with ExitStack() as pss:
                sbs = lambda name, shape, dtype: pss.enter_context(nc.sbuf_tensor(_un(name), shape, dtype))
                NCS = NPG * 128 + 128
                env = attn_env(pss, "s_", NCS)
                score, sm, QTb, aoT, rsum, selrep, cmask_s = env["score"], env["sm"], env["QTb"], env["aoT"], env["rsum"], env["selrep"], env["cmask_s"]
                r_score, r_sm, r_QTb, r_aoT, r_rsum, r_bc = env["r_score"], env["r_sm"], env["r_QTb"], env["r_aoT"], env["r_rsum"], env["r_bc"]
                KTn = sbs("KTn", [128, 2, 128], BF16)
                Vn = sbs("Vn", [128, 256], BF16)
                kiTn = sbs("kiTn", [64, 128], BF16)
                r_kvn = s.res()
                with ExitStack() as pkn:
                    kvt = kv_env(pkn, "s_")
                    kvt(xs[32:160, :], KTn[:], Vn[:], kiTn[:], r_kvn, r_kvn, r_kvn, o_ks[:, :], o_vs[:, :], o_iks[:, :])
                    s.barrier()
                env["load_block"](SBLK)
                env["load_q"](SBLK)
                ptab_sb = sbs("ptab_sb", [NPG, 4], I32); r_pt = s.res()
                s.dma("sp", lambda e: e.dma_start(out=ptab_sb[:], in_=ptab.rearrange("q p -> p q"), allow_slow_non_contiguous=True), writes=[r_pt])
                RG = 16
                NJ = 128 // RG
                ptf = sbs("ptf", [NPG, 4], F32)
                idxf = sbs("idxf", [NPG, 4, NJ], F32)
                idxK = sbs("idxK", [NPG, 4, NJ], I32); r_idxK = s.res()
                s.op("dve", lambda e: e.tensor_copy(out=ptf[:], in_=ptab_sb[:]), reads=[r_pt], writes=[r_idxK])
                for jj in range(NJ):
                    s.op("dve", lambda e, jj=jj: e.tensor_scalar(out=idxf[:, :, jj], in0=ptf[:], scalar1=float(NJ), scalar2=float(jj),
                                                                op0=ALU.mult, op1=ALU.add), writes=[r_idxK])
                s.op("dve", lambda e: e.tensor_copy(out=idxK[:], in_=idxf[:]), writes=[r_idxK])
                with ExitStack() as psi:
                    sbi = lambda name, shape, dtype: psi.enter_context(nc.sbuf_tensor(_un(name), shape, dtype))
                    idxpg = sbi("idxpg", [NPG, PAGE * IDIM], F32); r_idxpg = s.res()
                    kiTs = sbi("kiTs", [64, NPG * 128], BF16); r_kiTs = s.res()
                    kvw = kiTs[:].rearrange("d (pg r) -> d pg r", r=128)
                    s.op("pool", lambda e: e.memset(score[:, 0:NCS], 0.0), writes=[r_score])
                    for q in range(4):
                        s.dma("pool", lambda e, q=q: e.indirect_dma_start(
                            out=idxpg[:], out_offset=None, in_=cache_ik[:, :],
                            in_offset=bass.IndirectOffsetOnAxis(ap=ptab_sb[:, q:q + 1], axis=0)), reads=[r_pt], writes=[r_idxpg])
                        for r0 in range(0, 128, 4):
                            bank = 4 + (r0 // 4) % 2
                            tq = bk(bank)[0:64, 0:4 * NPG].rearrange("d (r pg) -> d r pg", pg=NPG)
                            for rr in range(4):
                                s.op("pe", lambda e, rr=rr: e.transpose(tq[:, rr, :], idxpg[:, (r0 + rr) * 64:(r0 + rr + 1) * 64], ident32[0:NPG, 0:NPG]),
                                     reads=[r_idxpg, r_c], writes=[rb[bank]])
                            eng = "act" if (r0 // 4) % 2 == 0 else "dve"
                            if eng == "act":
                                s.op("act", lambda e: e.copy(out=kvw[:, :, r0:r0 + 4].rearrange("d pg r -> d r pg"), in_=tq), writes=[rb[bank], r_kiTs])
                            else:
                                s.op("dve", lambda e: e.tensor_copy(out=kvw[:, :, r0:r0 + 4].rearrange("d pg r -> d r pg"), in_=tq), writes=[rb[bank], r_kiTs])
                        items = []
                        for kc in range(NPG * 128 // 512):
                            items += env["idx_items"]([q], kiTs[:, kc * 512:(kc + 1) * 512], r_kiTs, 512, kc * 512, True)
                        items += env["idx_items"]([q], kiTn[:, :], r_kvn, 128, NPG * 128, True)
                        env["indexer_run"](items)
                    s.barrier()
                env["threshold"](NCS, (NPG * 128, NCS, cmask_s[:]))
                with ExitStack() as psa:
                    sbt = lambda name, shape, dtype: psa.enter_context(nc.sbuf_tensor(_un(name), shape, dtype))
                    Kg = [sbt("Kg%d" % i, [NPG, RG * 256], F32) for i in range(2)]
                    Vg = [sbt("Vg%d" % i, [NPG, RG * 256], F32) for i in range(2)]
                    r_Kg = [s.res() for _ in range(2)]
                    r_Vg = [s.res() for _ in range(2)]
                    KTc = [sbt("KTc%d" % i, [128, 4, 2, NPG], BF16) for i in range(3)]
                    Vc = [sbt("Vc%d" % i, [NPG, 4, 256], BF16) for i in range(3)]
                    Mbc = [sbt("Mbc%d" % i, [32, 4, 128], BF16) for i in range(3)]
                    r_KTc = [s.res() for _ in range(3)]
                    r_Vc = [s.res() for _ in range(3)]
                    r_Mbc = [s.res() for _ in range(3)]
                    QTq = sbt("QTq", [128, 8, 8], BF16); r_QTq = s.res()
                    ck = cache_k.rearrange("(n r) d -> n (r d)", r=RG)
                    cv = cache_v.rearrange("(n r) d -> n (r d)", r=RG)
                    s.op("pool", lambda e: e.memset(aoT[:], 0.0), writes=[r_aoT])
                    cc = 0
                    for q in range(4):
                        s.op("dve", lambda e: e.tensor_copy(out=QTq[:], in_=QTb[:, :, 8 * q:8 * q + 8]), reads=[r_QTb], writes=[r_QTq])
                        qf = lambda kvh: QTq[:, 4 * kvh:4 * kvh + 4, :]
                        batches = []
                        gdone = set()

                        def gather(jj):
                            if jj in gdone or jj >= NJ:
                                return
                            gdone.add(jj)
                            gi = (q * NJ + jj) % 2
                            s.dma("pool", lambda e: e.indirect_dma_start(
                                out=Kg[gi][:], out_offset=None, in_=ck[:, :],
                                in_offset=bass.IndirectOffsetOnAxis(ap=idxK[:, q, jj:jj + 1], axis=0)), reads=[r_idxK], writes=[r_Kg[gi]])
                            s.dma("pool", lambda e: e.indirect_dma_start(
                                out=Vg[gi][:], out_offset=None, in_=cv[:, :],
                                in_offset=bass.IndirectOffsetOnAxis(ap=idxK[:, q, jj:jj + 1], axis=0)), reads=[r_idxK], writes=[r_Vg[gi]])

                        for jj in range(NJ):
                            gi = (q * NJ + jj) % 2
                            for bb in range(RG // 4):
                                batches.append(dict(gi=gi, bb=bb, jj=jj, r0=jj * RG + 4 * bb))

                        def stA(bt):
                            gi, bb, r0 = bt["gi"], bt["bb"], bt["r0"]
                            gather(bt["jj"])
                            if bb == RG // 4 - 1:
                                pass
                            ci = bt["ci"]
                            tk = bk(2, 2)[:, 0:8 * NPG].rearrange("p (r a b) -> p r a b", a=2, b=NPG)
                            for rl in range(4):
                                for kvh in range(2):
                                    c0 = (4 * bb + rl) * 256 + kvh * 128
                                    s.op("pe", lambda e, rl=rl, kvh=kvh, c0=c0: e.transpose(tk[:, rl, kvh, :], Kg[gi][:, c0:c0 + 128], ident32[0:NPG, 0:NPG]),
                                         reads=[r_Kg[gi], r_c], writes=[rb[2], rb[3]])
                            s.op("act", lambda e: e.copy(out=KTc[ci][:], in_=tk), writes=[rb[2], rb[3], r_KTc[ci]])
                            s.op("pool", lambda e: e.tensor_copy(out=Vc[ci][:], in_=Vg[gi][:, 4 * bb * 256:(4 * bb + 4) * 256]),
                                 reads=[r_Vg[gi]], writes=[r_Vc[ci]])
                            s.op("dve", lambda e: e.tensor_scalar(
                                out=Mbc[ci][:, :, 0:NPG], in0=score[0:32, 0:NPG * 128].rearrange("p (pg r) -> p r pg", r=128)[:, r0:r0 + 4, :],
                                scalar1=sm[0:32, 0:1], scalar2=NEG, op0=ALU.is_lt, op1=ALU.mult), reads=[r_score, r_sm], writes=[r_Mbc[ci]])

                        def stB(bt):
                            ci = bt["ci"]
                            pi = env["cnt"]["p"] % 3; env["cnt"]["p"] += 1
                            sb_ = pi % 2
                            bt["pi"] = pi
                            Pt, r_Pt = env["Pt"], env["r_Pt"]
                            for rl in range(4):
                                for kvh in range(2):
                                    oc = rl * 64 + kvh * 32
                                    s.op("pe", lambda e, rl=rl, kvh=kvh, oc=oc: e.matmul(bk(sb_)[0:NPG, oc:oc + 32], KTc[ci][:, rl, kvh, :], qf(kvh),
                                                                                          start=True, stop=False),
                                         reads=[r_KTc[ci], r_QTq], writes=[rb[sb_]])
                                    s.op("pe", lambda e, rl=rl, kvh=kvh, oc=oc: e.matmul(bk(sb_)[0:NPG, oc:oc + 32], Mbc[ci][:, rl, 0:NPG], selrep[:, q, :],
                                                                                          start=False, stop=True),
                                         reads=[r_Mbc[ci], r_bc], writes=[rb[sb_]])
                            s.op("act", lambda e: e.activation(out=Pt[pi][0:NPG, 0:256], in_=bk(sb_)[0:NPG, 0:256], func=AF.Exp),
                                 writes=[rb[sb_], r_Pt[pi]])

                        def stC(bt):
                            ci, pi, r0 = bt["ci"], bt["pi"], bt["r0"]
                            Pt, r_Pt = env["Pt"], env["r_Pt"]
                            for rl in range(4):
                                first = (r0 + rl == 0)
                                for kvh in range(2):
                                    oc = rl * 64 + kvh * 32
                                    s.op("pe", lambda e, rl=rl, kvh=kvh, oc=oc: e.matmul(bk(4 + kvh)[:, 0:32], Vc[ci][:, rl, kvh * 128:(kvh + 1) * 128],
                                                                                          Pt[pi][0:NPG, oc:oc + 32], start=first, stop=False),
                                         reads=[r_Vc[ci], r_Pt[pi]], writes=[rb[4 + kvh]])
                                s.op("pe", lambda e, rl=rl: e.matmul(bk(6)[:, 0:64], ones_bf[0:NPG, :], Pt[pi][0:NPG, rl * 64:rl * 64 + 64],
                                                                     start=first, stop=False),
                                     reads=[r_Pt[pi], r_c], writes=[rb[6]])

                        for bt in batches:
                            bt["ci"] = cc % 3; cc += 1
                        gather(0); gather(1)
                        stA(batches[0]); stB(batches[0])
                        for n, bt in enumerate(batches):
                            if n + 1 < len(batches):
                                stA(batches[n + 1]); stB(batches[n + 1])
                            stC(bt)
                            if bt["bb"] == RG // 4 - 1:
                                gather(bt["jj"] + 2)
                        ci = cc % 3; cc += 1
                        s.op("dve", lambda e: e.tensor_scalar(out=Mbc[ci][:, 0, :], in0=score[0:32, NPG * 128:NPG * 128 + 128], scalar1=sm[0:32, 0:1], scalar2=NEG,
                                                              op0=ALU.is_lt, op1=ALU.mult), reads=[r_score, r_sm], writes=[r_Mbc[ci]])
                        env["attend_chunk"](False, True, 128, lambda kvh: KTn[:, kvh, :], lambda kvh: Vn[:, kvh * 128:(kvh + 1) * 128],
                                            [r_kvn], qf, r_QTq, 32, Mbc[ci][:, 0, :], selrep[:, q, :], r_Mbc[ci])
                        s.op("dve", lambda e: e.reciprocal(out=rsum[:, 0, 0:64], in_=bk(6)[:, 0:64]), writes=[rb[6], r_rsum])
                        for kvh in range(2):
                            s.op("dve", lambda e, kvh=kvh: e.tensor_tensor(out=aoT[:, 4 * kvh:4 * kvh + 4, 8 * q:8 * q + 8],
                                                                           in0=bk(4 + kvh)[:, 0:32].rearrange("p (h t) -> p h t", t=8),
                                                                           in1=rsum[:, 0, 32 * kvh:32 * kvh + 32].rearrange("p (h t) -> p h t", t=8), op=ALU.mult),
                                 reads=[r_rsum], writes=[rb[4 + kvh], r_aoT])
                    s.dma("sp", lambda e: e.dma_start(out=mixT_scr[SBLK, :, 8:16, :], in_=aoT[:]), reads=[r_aoT])
                    s.barrier()


        with ExitStack() as pkv_stack:
            sbk = lambda name, shape, dtype: pkv_stack.enter_context(nc.sbuf_tensor(_un(name), shape, dtype))
            if "A" in PH or "B" in PH:
                KT = sbk("KT", [128, NKV, NT * 128], BF16)
                Vr = sbk("Vr", [128, NT, NKV * HD], BF16)
                kiT = sbk("kiT", [64, NT * 128], BF16)
                r_KT, r_V, r_kiT = s.res(), s.res(), s.res()
            if "A" in PH:
                with ExitStack() as pa:
                    kvt = kv_env(pa, "a_")
                    for n in range(NT):
                        kvt(xb[n * 128:(n + 1) * 128, :], KT[:, :, n * 128:(n + 1) * 128], Vr[:, n, :], kiT[:, n * 128:(n + 1) * 128],
                            r_KT, r_V, r_kiT, o_k[n * 128:(n + 1) * 128, :], o_v[n * 128:(n + 1) * 128, :], o_ik[n * 128:(n + 1) * 128, :])
                    s.barrier()
            if "B" in PH:
                with ExitStack() as pb:
                    env = attn_env(pb, "b_", NT * 128)
                    score, sm, QTb, aoT, rsum, cmask, identrep, Mb = env["score"], env["sm"], env["QTb"], env["aoT"], env["rsum"], env["cmask"], env["identrep"], env["Mb"]
                    r_score, r_sm, r_QTb, r_aoT, r_rsum, r_bc, r_Mb = env["r_score"], env["r_sm"], env["r_QTb"], env["r_aoT"], env["r_rsum"], env["r_bc"], env["r_Mb"]
                    load_block, indexer_tile, threshold, make_mb, attend_chunk = env["load_block"], env["indexer_tile"], env["threshold"], env["make_mb"], env["attend_chunk"]
                    cur, load_q = env["cur"], env["load_q"]

                    def do_indexer(i):
                        cur["i"] = i % 2
                        load_block(i)
                        items = []
                        for kc in range(i + 1):
                            items += env["idx_items"](list(range(16)), kiT[:, kc * 512:(kc + 1) * 512], r_kiT, 512, kc * 512, False)
                        env["indexer_run"](items)

                    def do_thr(i):
                        cur["i"] = i % 2
                        ncols = (i + 1) * 512
                        threshold(ncols, (ncols - 512, ncols, cmask[:]))

                    def do_attn(i):
                        cur["i"] = i % 2
                        load_q(i)
                        nch = 4 * (i + 1)
                        items = []
                        for c in range(nch):
                            for kvh in range(2):
                                items.append(dict(c=c, kvh=kvh, first=(c == 0), last=(c == nch - 1),
                                                  kt=KT[:, kvh, c * 128:(c + 1) * 128], v=Vr[:, c, kvh * 128:(kvh + 1) * 128],
                                                  r_kv=[r_KT, r_V], q=QTb[:, 4 * kvh:4 * kvh + 4, :], r_q=r_QTb, mb_r=identrep[:]))
                        mstate = {}

                        def prep(it):
                            if it["c"] % 4 == 0 and it["kvh"] == 0:
                                mstate["mi"] = make_mb(it["c"] * 128, 512)
                            mi = mstate["mi"]
                            it["mb_l"] = Mb[mi][:, (it["c"] % 4) * 128:(it["c"] % 4 + 1) * 128]
                            it["r_mb"] = r_Mb[mi]
                            env["att_S"](it)

                        prep(items[0])
                        for n, it in enumerate(items):
                            if n + 1 < len(items):
                                prep(items[n + 1])
                            env["att_PV"](it)
                        s.op("dve", lambda e: e.reciprocal(out=rsum[:], in_=bk(6, 2).rearrange("p (a b) -> p a b", b=512)),
                             writes=[rb[6], rb[7], r_rsum])
                        s.op("dve", lambda e: e.tensor_tensor(out=aoT[:], in0=bk(4, 2).rearrange("p (h t) -> p h t", t=128),
                                                              in1=rsum[:].rearrange("p a (h t) -> p (a h) t", t=128), op=ALU.mult),
                             reads=[r_rsum], writes=[rb[4], rb[5], r_aoT])
                        s.dma("sp", lambda e: e.dma_start(out=mixT_scr[i, :, 8:16, :], in_=aoT[:]), reads=[r_aoT])

                    do_indexer(0)
                    for i in range(NOWN):
                        if i + 1 < NOWN:
                            do_indexer(i + 1)
                        do_thr(i)
                        do_attn(i)

                    s.barrier()


        if "C" in PH:
            supers = [list(range(i, min(i + SB, NOWN))) for i in range(0, NOWN, SB)] + [[SBLK]]
            with ExitStack() as pc1:
                sbc = lambda name, shape, dtype: pc1.enter_context(nc.sbuf_tensor(_un(name), shape, dtype))
                gpm = sbc("gpm", [128, D], F32); r_gpm = s.res(); load_gain(gpm, g_post_mix, r_gpm)
                gpf = sbc("gpf", [128, D], F32); r_gpf = s.res(); load_gain(gpf, g_pre_ffn, r_gpf)
                mixT = sbc("mixT", [128, 16, SB, 128], BF16); r_mixT = s.res()
                Wo = [sbc("Wo%d" % i, [128, 16, 512], BF16) for i in range(2)]
                r_Wo = [s.res() for _ in range(2)]
                mixed = sbc("mixed", [128, SB, D], F32); r_mixed = s.res()
                xt = sbc("xtC", [128, D], F32); r_xt = s.res()
                tmp = sbc("tmpC", [128, D], F32); r_tmp = s.res()
                hbf = sbc("hbf", [128, D], BF16); r_hbf = s.res()
                hT = sbc("hTC", [128, 16, 128], BF16); r_hT = s.res()
                junk = sbc("junkC", [128, D], BF16); r_junk = s.res()
                ssq = sbc("ssqC", [128, 1], F32); r_ssq = s.res()
                wc = 0
                mc = 0
                for blks in supers:
                    nb = len(blks)
                    is_s = blks[0] == SBLK
                    for bi, blk in enumerate(blks):
                        s.dma("sp", lambda e: e.dma_start(out=mixT[:, :, bi, :], in_=mixT_scr[blk]), writes=[r_mixT])
                    for nt in range(4):
                        wi_ = wc % 2; wc += 1
                        for k4 in range(4):
                            s.dma("pool", lambda e, k4=k4: e.dma_start(out=Wo[wi_][:, 4 * k4:4 * k4 + 4, :], in_=w_out_r[:, 4 * k4:4 * k4 + 4, nt * 512:(nt + 1) * 512]),
                                  writes=[r_Wo[wi_]])
                        for bi in range(nb):
                            pbk = mc % 4; mc += 1
                            for k in range(16):
                                s.op("pe", lambda e, k=k: e.matmul(bk(pbk)[:, :], mixT[:, k, bi, :], Wo[wi_][:, k, :], start=(k == 0), stop=(k == 15)),
                                     reads=[r_mixT, r_Wo[wi_]], writes=[rb[pbk]])
                            s.op("act", lambda e: e.copy(out=mixed[:, bi, nt * 512:(nt + 1) * 512], in_=bk(pbk)[:, :]), writes=[rb[pbk], r_mixed])
                    for bi, blk in enumerate(blks):
                        src = xs[32:160, :] if is_s else xo[blk, 32:160, :]
                        s.dma("sp", lambda e: e.dma_start(out=xt[:], in_=src), writes=[r_xt])
                        rms_to_bf(128, mixed[:, bi, :], r_mixed, gpm, r_gpm, tmp[:], r_tmp, (junk, r_junk, ssq, r_ssq))
                        s.op("pool", lambda e: e.tensor_tensor(out=xt[:], in0=xt[:], in1=tmp[:], op=ALU.add), reads=[r_tmp], writes=[r_xt])
                        s.dma("sp", lambda e: e.dma_start(out=x1_scr[blk], in_=xt[:]), reads=[r_xt])
                        rms_to_bf(128, xt[:], r_xt, gpf, r_gpf, hbf[:], r_hbf, (junk, r_junk, ssq, r_ssq))
                        tp = bkb(4, 2).rearrange("p (k t) -> p k t", t=128)
                        for k in range(16):
                            s.op("pe", lambda e, k=k: e.transpose(tp[:, k, :], hbf[:, k * 128:(k + 1) * 128], ident[:]),
                                 reads=[r_hbf, r_c], writes=[rb[4], rb[5]])
                        s.op("act", lambda e: e.copy(out=hT[:], in_=tp[:, :, :]), writes=[rb[4], rb[5], r_hT])
                        s.dma("sp", lambda e: e.dma_start(out=hT_scr[blk], in_=hT[:]), reads=[r_hT])
                s.barrier()
            with ExitStack() as pc2:
                sbc = lambda name, shape, dtype: pc2.enter_context(nc.sbuf_tensor(_un(name), shape, dtype))
                gff = sbc("gff", [128, D], F32); r_gff = s.res(); load_gain(gff, g_post_ffn, r_gff)
                hTs = sbc("hTs", [128, 16, SB, 128], BF16); r_hTs = s.res()
                aT = sbc("aT", [128, NFT, SB * 128], BF16); r_aT = s.res()
                Wg = [sbc("Wg%d" % i, [128, 16, 256], BF16) for i in range(2)]
                Wu = [sbc("Wu%d" % i, [128, 16, 256], BF16) for i in range(2)]
                r_Wg = [s.res() for _ in range(2)]
                r_Wu = [s.res() for _ in range(2)]
                Wd = [sbc("Wd%d" % i, [128, NFT, 256], BF16) for i in range(2)]
                r_Wd = [s.res() for _ in range(2)]
                wdc = 0
                sg = [sbc("sg%d" % i, [128, SB * 128], F32) for i in range(2)]
                r_sg = [s.res() for _ in range(2)]
                fo = sbc("fo", [128, SB, D], F32); r_fo = s.res()
                x1t = sbc("x1t", [128, D], F32); r_x1t = s.res()
                tmp = sbc("tmpC2", [128, D], F32); r_tmp = s.res()
                junk = sbc("junkC2", [128, D], BF16); r_junk = s.res()
                ssq = sbc("ssqC2", [128, 1], F32); r_ssq = s.res()
                dc = 0
                for blks in supers:
                    nb = len(blks)
                    N = nb * 128
                    for bi, blk in enumerate(blks):
                        s.dma("sp", lambda e: e.dma_start(out=hTs[:, :, bi, :], in_=hT_scr[blk]), writes=[r_hTs])
                    for ft in range(NFT):
                        wi_ = (ft // 2) % 2
                        fo_ = ft % 2
                        if fo_ == 0:
                            s.dma("pool", lambda e: e.dma_start(out=Wg[wi_][:], in_=w_gate_r[:, :, ft * 128:(ft + 2) * 128]), writes=[r_Wg[wi_]])
                            s.dma("pool", lambda e: e.dma_start(out=Wu[wi_][:], in_=w_up_r[:, :, ft * 128:(ft + 2) * 128]), writes=[r_Wu[wi_]])
                        gb, ub = 2 * (ft % 2), 2 * (ft % 2) + 1
                        for k in range(16):
                            s.op("pe", lambda e, k=k: e.matmul(bk(gb)[:, 0:N], Wg[wi_][:, k, fo_ * 128:(fo_ + 1) * 128], hTs[:, k, 0:nb, :], start=(k == 0), stop=(k == 15)),
                                 reads=[r_hTs, r_Wg[wi_]], writes=[rb[gb]])
                        for k in range(16):
                            s.op("pe", lambda e, k=k: e.matmul(bk(ub)[:, 0:N], Wu[wi_][:, k, fo_ * 128:(fo_ + 1) * 128], hTs[:, k, 0:nb, :], start=(k == 0), stop=(k == 15)),
                                 reads=[r_hTs, r_Wu[wi_]], writes=[rb[ub]])
                        s.op("act", lambda e: e.activation(out=sg[fo_][:, 0:N], in_=bk(gb)[:, 0:N], func=AF.Silu), writes=[rb[gb], r_sg[fo_]])
                        s.op("dve", lambda e: e.tensor_tensor(out=aT[:, ft, 0:N], in0=bk(ub)[:, 0:N], in1=sg[fo_][:, 0:N], op=ALU.mult),
                             reads=[r_sg[fo_]], writes=[rb[ub], r_aT])
                    for nt in range(8):
                        wd_ = wdc % 2; wdc += 1
                        for f4 in range(4):
                            s.dma("pool", lambda e, f4=f4: e.dma_start(out=Wd[wd_][:, 11 * f4:11 * f4 + 11, :], in_=w_down_r[:, 11 * f4:11 * f4 + 11, nt * 256:(nt + 1) * 256]),
                                  writes=[r_Wd[wd_]])
                        for bi in range(nb):
                            pbk = 4 + dc % 4; dc += 1
                            for ft in range(NFT):
                                s.op("pe", lambda e, ft=ft: e.matmul(bk(pbk)[:, 0:256], aT[:, ft, bi * 128:(bi + 1) * 128], Wd[wd_][:, ft, :], start=(ft == 0), stop=(ft == NFT - 1)),
                                     reads=[r_aT, r_Wd[wd_]], writes=[rb[pbk]])
                            s.op("act", lambda e: e.copy(out=fo[:, bi, nt * 256:(nt + 1) * 256], in_=bk(pbk)[:, 0:256]), writes=[rb[pbk], r_fo])
                    for bi, blk in enumerate(blks):
                        s.dma("sp", lambda e: e.dma_start(out=x1t[:], in_=x1_scr[blk]), writes=[r_x1t])
                        rms_to_bf(128, fo[:, bi, :], r_fo, gff, r_gff, tmp[:], r_tmp, (junk, r_junk, ssq, r_ssq))
                        s.op("pool", lambda e: e.tensor_tensor(out=x1t[:], in0=x1t[:], in1=tmp[:], op=ALU.add), reads=[r_tmp], writes=[r_x1t])
                        s.dma("sp", lambda e: e.dma_start(out=o_y[blk], in_=x1t[:]), reads=[r_x1t])
                s.barrier()
        s.finish()

    return nc


def _consts(j):
    c = {}
    c["identb"] = _bf(np.eye(128, dtype=np.float32))
    c["ident32"] = np.eye(128, dtype=np.float32)
    r = np.arange(128)[:, None]
    sp = np.arange(512)[None, :]
    c["cmask"] = np.where(sp <= 128 * j + r, 0.0, -1e30).astype(np.float32)
    mg = np.zeros((128, 16, 128), np.float32)
    for g in range(16):
        for row in range(128):
            mg[row, g, 8 * g + row % 8] = 1.0
    c["maskg"] = _bf(mg)
    c["identrep"] = _bf(np.repeat(np.eye(128, dtype=np.float32)[:, None, :], 4, axis=1))
    cs = np.full((128, 128), -1e30, np.float32)
    for row in range(32):
        q, t = row // 8, row % 8
        for col in range(32):
            q2, t2 = col // 8, col % 8
            if q2 == q and t2 <= t:
                cs[row, col] = 0.0
    c["cmask_s"] = cs
    sr = np.zeros((32, 4, 32), np.float32)
    for q in range(4):
        for h in range(4):
            for t in range(8):
                sr[8 * q + t, q, h * 8 + t] = 1.0
    c["selrep"] = _bf(sr)
    c["iota_p"] = np.arange(128, dtype=np.float32)[:, None]
    return c


def make_in_map(c, inp, NT, NPG):
    b, j = c // 4, c % 4
    NOWN = NT // 4
    f = lambda a: np.ascontiguousarray(a, dtype=np.float32)
    xp = inp["x_prompt"][b]
    m = {"xb": f(xp[:NT * 128])}
    xo = np.zeros((NOWN, 160, D), np.float32)
    for i in range(NOWN):
        g0 = (4 * i + j) * 128
        xo[i, 32:160] = xp[g0:g0 + 128]
        if g0 > 0:
            xo[i, 2:32] = xp[g0 - 30:g0]
    m["xo"] = xo
    xs_ = np.zeros((160, D), np.float32)
    xs_[32:64] = inp["x_sample"][4 * c:4 * c + 4].reshape(32, D)
    m["xs"] = xs_
    for k in ("w_in", "w_out", "w_gate", "w_up", "w_down", "g_pre_mix", "g_post_mix", "g_pre_ffn", "g_post_ffn",
              "conv_w", "conv_b", "conv_ln_g", "conv_ln_b"):
        m[k] = f(inp[k][0]) if inp[k][0].ndim == 2 else f(inp[k][0])[None, :]
    npool = inp["cache_k"].shape[1]
    m["cache_k"] = f(inp["cache_k"][0]).reshape(npool * PAGE, NKV * HD)
    m["cache_v"] = f(inp["cache_v"][0]).reshape(npool * PAGE, NKV * HD)
    m["cache_ik"] = f(inp["cache_idx_k"][0]).reshape(npool, PAGE * IDIM)
    m["state_conv"] = f(inp["state_conv"][0, 4 * c:4 * c + 4])
    m["ptab"] = np.ascontiguousarray(inp["page_table"][4 * c:4 * c + 4, :NPG], dtype=np.int32)
    m.update(_consts(j))
    return m


_NC_CACHE = {}


def kernel(**inp):
    NT, NPG = SEQ // 128, NPAGES
    npool = inp["cache_k"].shape[1]
    key = (NT, NPG, npool)
    if key not in _NC_CACHE:
        _NC_CACHE[key] = build({"NT": NT, "NPG": NPG, "NPOOL": npool})
    nc = _NC_CACHE[key]
    in_maps = [make_in_map(c, inp, NT, NPG) for c in range(8)]
    res = run_bass_kernel_spmd(nc, in_maps, core_ids=list(range(8))).results
    return assemble(res, NT)


def assemble(res, NT):
    NOWN = NT // 4
    S_ = NT * 128
    y_p = np.zeros((NB, S_, D), np.float32)
    y_s = np.zeros((DEC_B, DEC_T, D), np.float32)
    nk = np.zeros((1, NB, S_, NKV, HD), np.float32)
    nv = np.zeros((1, NB, S_, NKV, HD), np.float32)
    nik = np.zeros((1, NB, S_, IDIM), np.float32)
    ncp = np.zeros((1, NB, CW - 1, CONV_CH), np.float32)
    nks = np.zeros((1, DEC_B, DEC_T, NKV, HD), np.float32)
    nvs = np.zeros((1, DEC_B, DEC_T, NKV, HD), np.float32)
    niks = np.zeros((1, DEC_B, DEC_T, IDIM), np.float32)
    ncs = np.zeros((1, DEC_B, CW - 1, CONV_CH), np.float32)
    for c in range(len(res)):
        r = res[c]
        if r is None:
            continue
        b, j = c // 4, c % 4
        oy = np.asarray(r["o_y"])
        for i in range(NOWN):
            g0 = (4 * i + j) * 128
            y_p[b, g0:g0 + 128] = oy[i]
        y_s[4 * c:4 * c + 4] = oy[NOWN][0:32].reshape(4, DEC_T, D)
        if j == 0:
            nk[0, b] = np.asarray(r["o_k"]).reshape(S_, NKV, HD)
            nv[0, b] = np.asarray(r["o_v"]).reshape(S_, NKV, HD)
            nik[0, b] = np.asarray(r["o_ik"])
        if j == 3:
            ncp[0, b] = np.asarray(r["o_convp"])
        nks[0, 4 * c:4 * c + 4] = np.asarray(r["o_ks"])[0:32].reshape(4, DEC_T, NKV, HD)
        nvs[0, 4 * c:4 * c + 4] = np.asarray(r["o_vs"])[0:32].reshape(4, DEC_T, NKV, HD)
        niks[0, 4 * c:4 * c + 4] = np.asarray(r["o_iks"])[0:32].reshape(4, DEC_T, IDIM)
        ncs[0, 4 * c:4 * c + 4] = np.asarray(r["o_convs"])
    return (y_p, y_s, nk, nv, nik, ncp, nks, nvs, niks, ncs)
```

```python
import numpy as np
import ml_dtypes
import concourse.bass as bass
import concourse.mybir as mybir
from concourse.bass_utils import run_bass_kernel_spmd

F32 = mybir.dt.float32
BF16 = mybir.dt.bfloat16
I32 = mybir.dt.int32
U32 = mybir.dt.uint32
U8 = mybir.dt.uint8
AF = mybir.ActivationFunctionType
ALU = mybir.AluOpType
AX = mybir.AxisListType

D = 2048
SEQ = 8192
NB = 2
CONV_CH = 1024
NH = 8
HD = 128
NKV = 2
NIH = 16
IDIM = 64
TOPK = 256
CW = 31
DFF = 5632
N_IN = 4688
EPS = 1e-6
ATTN_SCALE = HD ** -0.5
INDEX_SCALE = (NIH * IDIM) ** -0.5
DEC_B = 32
DEC_T = 8
PAST = 16384
PAGE = 128
NPAGES = PAST // PAGE
NEG = -30000.0
NBIS = 20


class Res:
    __slots__ = ("name", "w", "rd")

    def __init__(self, name=""):
        self.name = name
        self.w = None
        self.rd = {}


class S:
    R = 8

    def __init__(self, nc, stack):
        self.nc = nc
        self.eng = {"pe": nc.tensor, "act": nc.scalar, "dve": nc.vector, "pool": nc.gpsimd, "sp": nc.sync}
        self.sem = {k: stack.enter_context(nc.semaphore("s_" + k)) for k in self.eng}
        self.cnt = {k: 0 for k in self.eng}
        self.dsem = {k: [stack.enter_context(nc.semaphore("d_%s%d" % (k, i))) for i in range(self.R)]
                     for k in ("sp", "pool", "act")}
        self.dn = {k: 0 for k in self.dsem}
        self.waited = {k: {} for k in self.eng}
        self.pending_dma = []
        self.nres = 0

    def res(self, name=""):
        return Res(name)

    def _wait(self, e, tok):
        if tok is None:
            return
        kind, key, val = tok
        if kind == "c":
            if key == e and e == "pe":
                return
            sem = self.sem[key]
            wk = ("c", key)
        else:
            sem = self.dsem[key[0]][key[1]]
            wk = ("d", key)
        if self.waited[e].get(wk, 0) >= val:
            return
        self.waited[e][wk] = val
        self.eng[e].wait_ge(sem, val)

    def _deps(self, e, reads, writes):
        toks = []
        for r in reads:
            if r.w is not None:
                toks.append(r.w)
        for w in writes:
            if w.w is not None:
                toks.append(w.w)
            for k, t in w.rd.items():
                if isinstance(t, list):
                    toks.extend(t)
                else:
                    toks.append(t)
        for t in toks:
            self._wait(e, t)

    def _mark(self, e, tok, reads, writes, is_dma):
        for r in reads:
            if is_dma:
                r.rd.setdefault("dma", []).append(tok)
            else:
                r.rd[e] = tok
        for w in writes:
            w.w = tok
            w.rd = {}

    def op(self, e, fn, reads=(), writes=()):
        self._deps(e, reads, writes)
        inst = fn(self.eng[e])
        self.cnt[e] += 1
        inst.then_inc(self.sem[e], 1)
        tok = ("c", e, self.cnt[e])
        self._mark(e, tok, reads, writes, False)
        return tok

    def dma(self, e, fn, reads=(), writes=()):
        n = self.dn[e]
        slot = n % self.R
        val = 16 * (n // self.R + 1)
        if val > 16:
            self._wait(e, ("d", (e, slot), val - 16))
        self._deps(e, reads, writes)
        inst = fn(self.eng[e])
        inst.then_inc(self.dsem[e][slot], 16)
        self.dn[e] = n + 1
        tok = ("d", (e, slot), val)
        self._mark(e, tok, reads, writes, True)
        self.pending_dma.append(tok)
        return tok

    def barrier(self):
        toks = [("c", k, self.cnt[k]) for k in self.eng if self.cnt[k] > 0]
        toks += self.pending_dma
        self.pending_dma = []
        for e in self.eng:
            for t in toks:
                if t[0] == "c" and t[1] == e:
                    continue
                self._wait(e, t)

    def finish(self):
        self.barrier()


_UN = [0]


def _un(name):
    _UN[0] += 1
    return "t%d_%s" % (_UN[0], name)


def _bf(a):
    return np.ascontiguousarray(a).astype(ml_dtypes.bfloat16)


def build(cfg):
    from contextlib import ExitStack
    NT = cfg.get("NT", SEQ // 128)
    NOWN = NT // 4
    SB = min(4, NOWN)
    NPG = cfg.get("NPG", NPAGES)
    NPOOL = cfg.get("NPOOL", 5120)
    NFT = DFF // 128
    NBLK = NOWN + 1
    SBLK = NOWN
    PH = cfg.get("PH", "UABSC")
    nc = bass.Bass("TRN2", target_bir_lowering=False)

    def din(name, shape, dtype=F32):
        return nc.dram_tensor(name, shape, dtype, kind="ExternalInput").ap()

    def dout(name, shape, dtype=F32):
        return nc.dram_tensor(name, shape, dtype, kind="ExternalOutput").ap()

    def dscr(name, shape, dtype):
        return nc.dram_tensor(name, shape, dtype, kind="Internal").ap()

    xb = din("xb", [NT * 128, D])
    xo = din("xo", [NOWN, 160, D])
    xs = din("xs", [160, D])
    w_in = din("w_in", [D, N_IN])
    w_out = din("w_out", [D, D])
    w_gate = din("w_gate", [D, DFF])
    w_up = din("w_up", [D, DFF])
    w_down = din("w_down", [DFF, D])
    g_pre_mix = din("g_pre_mix", [1, D])
    g_post_mix = din("g_post_mix", [1, D])
    g_pre_ffn = din("g_pre_ffn", [1, D])
    g_post_ffn = din("g_post_ffn", [1, D])
    conv_w = din("conv_w", [CW, CONV_CH])
    conv_b = din("conv_b", [1, CONV_CH])
    ln_g = din("conv_ln_g", [1, CONV_CH])
    ln_b = din("conv_ln_b", [1, CONV_CH])
    cache_k = din("cache_k", [NPOOL * PAGE, NKV * HD])
    cache_v = din("cache_v", [NPOOL * PAGE, NKV * HD])
    cache_ik = din("cache_ik", [NPOOL, PAGE * IDIM])
    state_conv = din("state_conv", [4, CW - 1, CONV_CH])
    ptab = din("ptab", [4, NPG], I32)
    identb_d = din("identb", [128, 128], BF16)
    ident32_d = din("ident32", [128, 128])
    cmask_d = din("cmask", [128, 512])
    maskg_d = din("maskg", [128, 16, 128], BF16)
    identrep_d = din("identrep", [128, 4, 128], BF16)
    cmask_s_d = din("cmask_s", [128, 128])
    selrep_d = din("selrep", [32, 4, 32], BF16)
    iota_d = din("iota_p", [128, 1])

    o_k = dout("o_k", [NT * 128, NKV * HD])
    o_v = dout("o_v", [NT * 128, NKV * HD])
    o_ik = dout("o_ik", [NT * 128, IDIM])
    o_y = dout("o_y", [NBLK, 128, D])
    o_convp = dout("o_convp", [CW - 1, CONV_CH])
    o_ks = dout("o_ks", [128, NKV * HD])
    o_vs = dout("o_vs", [128, NKV * HD])
    o_iks = dout("o_iks", [128, IDIM])
    o_convs = dout("o_convs", [4, CW - 1, CONV_CH])

    QT_scr = dscr("QT_scr", [NBLK, 128, 8, 128], BF16)
    qiT_scr = dscr("qiT_scr", [NBLK, 64, 16, 2, 64], BF16)
    Wsel_scr = dscr("Wsel_scr", [NBLK, 128, 16, 128], BF16)
    mixT_scr = dscr("mixT_scr", [NBLK, 128, 16, 128], BF16)
    x1_scr = dscr("x1_scr", [NBLK, 128, D], F32)
    hT_scr = dscr("hT_scr", [NBLK, 128, 16, 128], BF16)

    w_in_r = w_in.rearrange("(k p) n -> p k n", p=128)
    w_out_r = w_out.rearrange("(k p) n -> p k n", p=128)
    w_gate_r = w_gate.rearrange("(k p) n -> p k n", p=128)
    w_up_r = w_up.rearrange("(k p) n -> p k n", p=128)
    w_down_r = w_down.rearrange("(k p) n -> p k n", p=128)

    with ExitStack() as st:
        s = S(nc, st)
        sb = lambda name, shape, dtype: st.enter_context(nc.sbuf_tensor(_un(name), shape, dtype))
        PS = st.enter_context(nc.psum_tensor("PS", [128, 8 * 512], F32))
        rb = [s.res("bank%d" % i) for i in range(8)]

        def bk(i, n=1):
            return PS[:, i * 512:(i + n) * 512]

        def bkb(i, n=1):
            return PS[:, i * 512:(i + n) * 512].bitcast(BF16)

        ident = sb("ident", [128, 128], BF16)
        ident32 = sb("ident32", [128, 128], F32)
        ones_bf = sb("ones_bf", [128, 128], BF16)
        ones32 = sb("ones32", [128, 128], F32)
        r_c = s.res("consts")
        s.dma("sp", lambda e: e.dma_start(out=ident[:], in_=identb_d[:, :]), writes=[r_c])
        s.dma("sp", lambda e: e.dma_start(out=ident32[:], in_=ident32_d[:, :]), writes=[r_c])
        s.op("dve", lambda e: e.memset(ones_bf[:], 1.0), writes=[r_c])
        s.op("dve", lambda e: e.memset(ones32[:], 1.0), writes=[r_c])

        def load_gain(tile, g_ap, r):
            s.dma("sp", lambda e: e.dma_start(out=tile[:], in_=g_ap.to_broadcast([128, D])), writes=[r])

        def rms_to_bf(P, x_ap, r_x, g_tile, r_g, out_ap, r_out, tmp):
            junk, r_junk, ssq, r_ss = tmp
            s.op("act", lambda e: e.activation(out=junk[0:P, :], in_=x_ap, func=AF.Square, accum_out=ssq[0:P, :]),
                 reads=[r_x], writes=[r_junk, r_ss])
            s.op("dve", lambda e: e.tensor_scalar(out=ssq[0:P, :], in0=ssq[0:P, :], scalar1=1.0 / D, scalar2=EPS,
                                                  op0=ALU.mult, op1=ALU.add), writes=[r_ss])
            s.op("act", lambda e: e.activation(out=ssq[0:P, :], in_=ssq[0:P, :], func=AF.Sqrt), writes=[r_ss])
            s.op("dve", lambda e: e.reciprocal(out=ssq[0:P, :], in_=ssq[0:P, :]), writes=[r_ss])
            s.op("dve", lambda e: e.scalar_tensor_tensor(out=out_ap, in0=x_ap, scalar=ssq[0:P, 0:1], in1=g_tile[0:P, :],
                                                         op0=ALU.mult, op1=ALU.mult),
                 reads=[r_x, r_ss, r_g], writes=[r_out])

        if "U" in PH:
          with ExitStack() as pu:
            sbu = lambda name, shape, dtype: pu.enter_context(nc.sbuf_tensor(_un(name), shape, dtype))
            gmix = sbu("gmixU", [128, D], F32); r_gmix = s.res()
            load_gain(gmix, g_pre_mix, r_gmix)
            cvec = sbu("cvec", [128, 3, 8], F32); r_cvec = s.res()
            for vi, v_ap in enumerate((conv_b, ln_g, ln_b)):
                s.dma("sp", lambda e, vi=vi, v_ap=v_ap: e.dma_start(out=cvec[:, vi, :], in_=v_ap.rearrange("o (c p) -> p (o c)", p=128),
                                                                       allow_slow_non_contiguous=True), writes=[r_cvec])
            cvo = sbu("cvo", [32, CONV_CH], F32); r_cvo = s.res()
            cw_sb = cvo[0:CW, :]; r_cw = r_cvo
            s.dma("sp", lambda e: e.dma_start(out=cw_sb, in_=conv_w[:, :]), writes=[r_cw])
            cwT = sbu("cwT", [128, 8, CW], F32); r_cwT = s.res()
            for c in range(8):
                s.op("pe", lambda e, c=c: e.transpose(bk(0)[:, c * 32:c * 32 + CW], cw_sb[:, c * 128:(c + 1) * 128], ident32[0:CW, 0:CW]),
                     reads=[r_cw, r_c], writes=[rb[0]])
            s.op("dve", lambda e: e.tensor_copy(out=cwT[:], in_=bk(0)[:, 0:256].rearrange("p (c j) -> p c j", j=32)[:, :, 0:CW]),
                 writes=[rb[0], r_cwT])
            diag = sbu("diag", [128, 8, CW, 128], BF16); r_diag = s.res()
            for c in range(8):
                for j in range(CW):
                    s.op("pool", lambda e, c=c, j=j: e.tensor_scalar(out=diag[:, c, j, :], in0=ident[:], scalar1=cwT[:, c, j:j + 1],
                                                                     scalar2=None, op0=ALU.mult),
                         reads=[r_cwT, r_c], writes=[r_diag])
            wwi = sbu("wwi", [128, 16, 16], BF16); r_wwi = s.res()
            s.dma("pool", lambda e: e.dma_start(out=wwi[:], in_=w_in_r[:, :, 4672:4688]), writes=[r_wwi])
            wwirep = sbu("wwirep", [128, 16, 128], BF16); r_wrep = s.res()
            for h2 in range(2):
                for hp in range(8):
                    h = 2 * hp + h2
                    col = h2 * 64 + hp * 8
                    s.op("pool", lambda e, h=h, col=col: e.tensor_copy(
                        out=wwirep[:, :, col:col + 8], in_=wwi[:, :, h:h + 1].to_broadcast([128, 16, 8])),
                        reads=[r_wwi], writes=[r_wrep])
            maskg = sbu("maskg", [128, 16, 128], BF16); r_maskg = s.res()
            s.dma("sp", lambda e: e.dma_start(out=maskg[:], in_=maskg_d[:, :, :]), writes=[r_maskg])

            xt_l = [sbu("xtU%d" % i, [128, D], F32) for i in range(1)] * 2; r_xt_l = [s.res()] * 2
            xh_l = [sbu("xhU%d" % i, [32, D], F32) for i in range(1)] * 2; r_xh_l = [s.res()] * 2
            ssq = sbu("ssqU", [128, 1], F32); r_ssq = s.res()
            ssq2 = sbu("ssq2U", [128, 1], F32); r_ssq2 = s.res()
            xn_l = [sbu("xnU%d" % i, [128, D], BF16) for i in range(1)] * 2; r_xn_l = [s.res()] * 2
            xnh_l = [sbu("xnhU%d" % i, [32, D], BF16) for i in range(1)] * 2; r_xnh_l = [s.res()] * 2
            xnT = sbu("xnTU", [128, 16, SB, 160], BF16); r_xnT = s.res()
            Wt = [sbu("WtU%d" % i, [128, 16, 256], BF16) for i in range(3)]
            r_Wt = [s.res() for _ in range(3)]
            worder = []
            for c2 in range(4):
                worder += [c2 * 256, 1024 + c2 * 256]
            worder += [2048 + c2 * 256 for c2 in range(4)] + [3584 + c2 * 256 for c2 in range(4)]
            wstate = {"next": 0, "map": {}}

            def w_reset():
                wstate["next"] = 0
                wstate["map"] = {}

            def w_fill(upto):
                while wstate["next"] < min(upto, len(worder)):
                    base = worder[wstate["next"]]
                    i = wcnt[0] % 3
                    wcnt[0] += 1
                    s.dma("pool", lambda e: e.dma_start(out=Wt[i][:], in_=w_in_r[:, :, base:base + 256]), writes=[r_Wt[i]])
                    wstate["map"][base] = i
                    wstate["next"] += 1
            uT = sbu("uT", [128, 8, SB, 160], BF16); r_uT = s.res()
            sig = sbu("sig", [128, SB, 160], F32); r_sig = s.res()
            u32 = sbu("u32", [128, 8, 32], F32); r_u32 = s.res()
            Qst = sbu("Qst", [128, SB, 8, 128], BF16); r_Qst = s.res()
            Qi_sb = sbu("Qi_sb", [128, SB, 16, 8, 8], BF16); r_Qi = s.res()
            WIrep = sbu("WIrep", [128, SB, 128], F32); r_WIrep = s.res()
            ycv = sbu("ycv", [128, 8, 128], F32); r_ycv = s.res()
            ysq = sbu("ysq", [128, 8, 128], F32); r_ysq = s.res()
            Wsel = ysq[:].rearrange("p c t -> p (c t)").bitcast(BF16).rearrange("p (g t) -> p g t", t=128); r_Wsel = r_ysq
            stat = sbu("stat", [128, 3, 128], F32); r_stat = s.res()
            co = sbu("co", [128, 8, 128], BF16); r_co = s.res()
            stt = sbu("stt", [CW - 1, CONV_CH], F32); r_stt = s.res()
            fullT = sbu("fullT", [128, 8, 4, 38], BF16); r_fullT = s.res()

            wcnt = [0]

            def conv_tail(blk, TW):
                Y = bk(0, 2).rearrange("p (c t) -> p c t", t=128)
                s.op("dve", lambda e: e.tensor_tensor(out=ycv[:, :, 0:TW], in0=Y[:, :, 0:TW],
                                                      in1=cvec[:, 0, :].unsqueeze(2).to_broadcast([128, 8, TW]), op=ALU.add),
                     reads=[r_cvec], writes=[rb[0], rb[1], r_ycv])
                s.op("act", lambda e: e.activation(out=ysq[:, :, 0:TW], in_=ycv[:, :, 0:TW], func=AF.Square),
                     reads=[r_ycv], writes=[r_ysq])
                for c in range(8):
                    s.op("pe", lambda e, c=c: e.matmul(bk(2)[:, 0:TW], ones32[:], ycv[:, c, 0:TW], start=(c == 0), stop=(c == 7)),
                         reads=[r_ycv, r_c], writes=[rb[2]])
                for c in range(8):
                    s.op("pe", lambda e, c=c: e.matmul(bk(3)[:, 0:TW], ones32[:], ysq[:, c, 0:TW], start=(c == 0), stop=(c == 7)),
                         reads=[r_ysq, r_c], writes=[rb[3]])
                mean, msq, rstd = stat[:, 0, 0:TW], stat[:, 1, 0:TW], stat[:, 2, 0:TW]
                s.op("dve", lambda e: e.tensor_scalar(out=mean, in0=bk(2)[:, 0:TW], scalar1=1.0 / CONV_CH, scalar2=None, op0=ALU.mult),
                     writes=[rb[2], r_stat])
                s.op("dve", lambda e: e.tensor_tensor(out=msq, in0=mean, in1=mean, op=ALU.mult), writes=[r_stat])
                s.op("dve", lambda e: e.scalar_tensor_tensor(out=rstd, in0=bk(3)[:, 0:TW], scalar=1.0 / CONV_CH, in1=msq,
                                                             op0=ALU.mult, op1=ALU.subtract), writes=[rb[3], r_stat])
                s.op("dve", lambda e: e.tensor_scalar(out=rstd, in0=rstd, scalar1=EPS, scalar2=None, op0=ALU.add), writes=[r_stat])
                s.op("act", lambda e: e.activation(out=rstd, in_=rstd, func=AF.Sqrt), writes=[r_stat])
                s.op("dve", lambda e: e.reciprocal(out=rstd, in_=rstd), writes=[r_stat])
                s.op("dve", lambda e: e.tensor_tensor(out=ycv[:, :, 0:TW], in0=ycv[:, :, 0:TW],
                                                      in1=stat[:, 0:1, 0:TW].to_broadcast([128, 8, TW]), op=ALU.subtract),
                     reads=[r_stat], writes=[r_ycv])
                s.op("dve", lambda e: e.tensor_tensor(out=ycv[:, :, 0:TW], in0=ycv[:, :, 0:TW],
                                                      in1=stat[:, 2:3, 0:TW].to_broadcast([128, 8, TW]), op=ALU.mult),
                     reads=[r_stat], writes=[r_ycv])
                if TW < 128:
                    s.op("pool", lambda e: e.memset(co[:], 0.0), writes=[r_co])
                for c in range(8):
                    s.op("act", lambda e, c=c: e.activation(out=co[:, c, 0:TW], in_=ycv[:, c, 0:TW], func=AF.Silu,
                                                            scale=cvec[:, 1, c:c + 1], bias=cvec[:, 2, c:c + 1]),
                         reads=[r_ycv, r_cvec], writes=[r_co])
                s.dma("sp", lambda e: e.dma_start(out=mixT_scr[blk, :, 0:8, :], in_=co[:]), reads=[r_co])

            def u32_out(ncols, dst_fn):
                for c in range(8):
                    s.op("pe", lambda e, c=c: e.transpose(bk(2, 2)[0:ncols, c * 128:(c + 1) * 128], u32[:, c, 0:ncols], ident32[:]),
                         reads=[r_u32, r_c], writes=[rb[2], rb[3]])
                s.op("dve", lambda e: e.tensor_copy(out=cvo[0:ncols, :], in_=bk(2, 2)[0:ncols, :]), writes=[rb[2], rb[3], r_cvo])
                dst_fn()

            supers = [list(range(i, min(i + SB, NOWN))) for i in range(0, NOWN, SB)] + [[SBLK]]
            for blks in supers:
                nb = len(blks)
                is_s = blks[0] == SBLK
                w_reset()
                w_fill(2)
                for bi, blk in enumerate(blks):
                    src = xs if is_s else xo[blk]
                    xt, r_xt, xh, r_xh = xt_l[bi % 2], r_xt_l[bi % 2], xh_l[bi % 2], r_xh_l[bi % 2]
                    xn, r_xn, xnh, r_xnh = xn_l[bi % 2], r_xn_l[bi % 2], xnh_l[bi % 2], r_xnh_l[bi % 2]
                    s.dma("sp", lambda e: e.dma_start(out=xh[:], in_=src[0:32, :]), writes=[r_xh])
                    s.dma("sp", lambda e: e.dma_start(out=xt[:], in_=src[32:160, :]), writes=[r_xt])
                    rms_to_bf(32, xh[:], r_xh, gmix, r_gmix, xnh[:], r_xnh, (xnh, r_xnh, ssq2, r_ssq2))
                    rms_to_bf(128, xt[:], r_xt, gmix, r_gmix, xn[:], r_xn, (xn, r_xn, ssq, r_ssq))
                    tpm = bkb(0, 2).rearrange("p (k t) -> p k t", t=128)
                    tph = bkb(2).rearrange("p (k t) -> p k t", t=64)
                    for k in range(16):
                        s.op("pe", lambda e, k=k: e.transpose(tpm[:, k, :], xn[:, k * 128:(k + 1) * 128], ident[:]),
                             reads=[r_xn, r_c], writes=[rb[0], rb[1]])
                    for k in range(16):
                        s.op("pe", lambda e, k=k: e.transpose(tph[:, k, 0:32], xnh[:, k * 128:(k + 1) * 128], ident[0:32, 0:32]),
                             reads=[r_xnh, r_c], writes=[rb[2]])
                    s.op("act", lambda e: e.copy(out=xnT[:, :, bi, 32:160], in_=tpm[:, :, :]), writes=[rb[0], rb[1], r_xnT])
                    s.op("dve", lambda e: e.tensor_copy(out=xnT[:, :, bi, 0:32], in_=tph[:, :, 0:32]), writes=[rb[2], r_xnT])
                halves = [(0, min(2, nb))] + ([(2, nb)] if nb > 2 else [])

                def proj(ct_cols, lhs_fn, pair):
                    for hi, (b0, b1) in enumerate(halves):
                        n = (b1 - b0) * 160
                        for k in range(16):
                            s.op("pe", lambda e, k=k: e.matmul(bk(pair + hi)[:, 0:n], lhs_fn(k), xnT[:, k, b0:b1, :],
                                                               start=(k == 0), stop=(k == 15)),
                                 reads=[r_xnT] + ct_cols, writes=[rb[pair + hi]])

                def load_w(col0):
                    base = col0 - (col0 % 256)
                    idx = worder.index(base)
                    w_fill(idx + 2)
                    return wstate["map"][base], col0 - base

                def pview(pair, hi, b0, b1):
                    return bk(pair + hi)[:, 0:(b1 - b0) * 160].rearrange("p (b t) -> p b t", t=160)

                for c in range(8):
                    ia, oa = load_w(c * 128)
                    proj([r_Wt[ia]], lambda k, ia=ia, oa=oa: Wt[ia][:, k, oa:oa + 128], 4)
                    ig, og = load_w(1024 + c * 128)
                    proj([r_Wt[ig]], lambda k, ig=ig, og=og: Wt[ig][:, k, og:og + 128], 6)
                    for hi, (b0, b1) in enumerate(halves):
                        s.op("act", lambda e: e.activation(out=sig[:, b0:b1, :], in_=pview(6, hi, b0, b1), func=AF.Sigmoid),
                             writes=[rb[6 + hi], r_sig])
                        s.op("dve", lambda e: e.tensor_tensor(out=uT[:, c, b0:b1, :], in0=pview(4, hi, b0, b1), in1=sig[:, b0:b1, :], op=ALU.mult),
                             reads=[r_sig], writes=[rb[4 + hi], r_uT])
                        if is_s:
                            s.op("dve", lambda e: e.tensor_tensor(out=u32[:, c, :], in0=pview(4, hi, b0, b1)[:, 0, 32:64], in1=sig[:, 0, 32:64], op=ALU.mult),
                                 reads=[r_sig], writes=[rb[4 + hi], r_u32])
                        elif blks[-1] == NOWN - 1 and b1 == nb:
                            s.op("dve", lambda e: e.tensor_tensor(out=u32[:, c, 0:30], in0=pview(4, hi, b0, b1)[:, b1 - b0 - 1, 130:160],
                                                                  in1=sig[:, nb - 1, 130:160], op=ALU.mult),
                                 reads=[r_sig], writes=[rb[4 + hi], r_u32])
                for h in range(8):
                    iw, ow = load_w(2048 + h * 128)
                    pair = 4 + 2 * (h % 2)
                    proj([r_Wt[iw]], lambda k, iw=iw, ow=ow: Wt[iw][:, k, ow:ow + 128], pair)
                    for hi, (b0, b1) in enumerate(halves):
                        s.op("act", lambda e: e.activation(out=Qst[:, b0:b1, h, :], in_=pview(pair, hi, b0, b1)[:, :, 32:160],
                                                           func=AF.Copy, scale=ATTN_SCALE),
                             writes=[rb[pair + hi], r_Qst])
                for hp in range(8):
                    iw, ow = load_w(3584 + hp * 128)
                    pair = 4 + 2 * (hp % 2)
                    proj([r_Wt[iw]], lambda k, iw=iw, ow=ow: Wt[iw][:, k, ow:ow + 128], pair)
                    for hi, (b0, b1) in enumerate(halves):
                        for b in range(b0, b1):
                            s.op("dve", lambda e, b=b: e.tensor_copy(
                                out=Qi_sb[:, b, :, hp, :],
                                in_=pview(pair, hi, b0, b1)[:, b - b0, 32:160].rearrange("p (g t) -> p g t", t=8)),
                                writes=[rb[pair + hi], r_Qi])
                proj([r_wrep], lambda k: wwirep[:, k, :], 4)
                for hi, (b0, b1) in enumerate(halves):
                    s.op("act", lambda e: e.activation(out=WIrep[:, b0:b1, :], in_=pview(4, hi, b0, b1)[:, :, 32:160],
                                                       func=AF.Copy, scale=INDEX_SCALE),
                         writes=[rb[4 + hi], r_WIrep])
                for bi, blk in enumerate(blks):
                    s.dma("sp", lambda e: e.dma_start(out=QT_scr[blk], in_=Qst[:, bi, :, :]), reads=[r_Qst])
                    for h2 in range(2):
                        s.dma("sp", lambda e, h2=h2: e.dma_start(
                            out=qiT_scr[blk, :, :, h2, :],
                            in_=Qi_sb[h2 * 64:(h2 + 1) * 64, bi, :, :, :].rearrange("p g a b -> p g (a b)")),
                            reads=[r_Qi])
                    s.op("dve", lambda e: e.tensor_tensor(out=Wsel, in0=maskg[:],
                                                          in1=WIrep[:, bi:bi + 1, :].to_broadcast([128, 16, 128]), op=ALU.mult),
                         reads=[r_maskg, r_WIrep], writes=[r_Wsel])
                    s.dma("sp", lambda e: e.dma_start(out=Wsel_scr[blk], in_=Wsel), reads=[r_Wsel])
                    Y = bk(0, 2).rearrange("p (c t) -> p c t", t=128)
                    if not is_s:
                        for c in range(8):
                            for j in range(CW):
                                s.op("pe", lambda e, c=c, j=j: e.matmul(Y[:, c, :], diag[:, c, j, :], uT[:, c, bi, 2 + j:2 + j + 128],
                                                                        start=(j == 0), stop=(j == CW - 1)),
                                     reads=[r_diag, r_uT], writes=[rb[0], rb[1]])
                        conv_tail(blk, 128)
                    else:
                        for q in range(4):
                            s.dma("sp", lambda e, q=q: e.dma_start(out=stt[:], in_=state_conv[q]), writes=[r_stt])
                            tq = bk(2)[:, 0:256].rearrange("p (c j) -> p c j", j=32)
                            for c in range(8):
                                s.op("pe", lambda e, c=c: e.transpose(tq[:, c, 0:CW - 1], stt[:, c * 128:(c + 1) * 128], ident32[0:CW - 1, 0:CW - 1]),
                                     reads=[r_stt, r_c], writes=[rb[2]])
                            s.op("dve", lambda e, q=q: e.tensor_copy(out=fullT[:, :, q, 0:CW - 1], in_=tq[:, :, 0:CW - 1]),
                                 writes=[rb[2], r_fullT])
                            s.op("dve", lambda e, q=q: e.tensor_copy(out=fullT[:, :, q, CW - 1:CW + 7], in_=uT[:, :, 0, 32 + 8 * q:40 + 8 * q]),
                                 reads=[r_uT], writes=[r_fullT])
                            s.dma("sp", lambda e, q=q: e.dma_start(out=o_convs[q, 0:CW - 9, :], in_=state_conv[q, 8:CW - 1, :]))
                        for c in range(8):
                            for q in range(4):
                                for j in range(CW):
                                    s.op("pe", lambda e, c=c, q=q, j=j: e.matmul(Y[:, c, 8 * q:8 * q + 8], diag[:, c, j, :], fullT[:, c, q, j:j + 8],
                                                                                 start=(j == 0), stop=(j == CW - 1)),
                                         reads=[r_diag, r_fullT], writes=[rb[0], rb[1]])
                        conv_tail(blk, 32)
                        u32_out(32, lambda: [s.dma("sp", lambda e, q=q: e.dma_start(out=o_convs[q, CW - 9:CW - 1, :], in_=cvo[8 * q:8 * q + 8, :]),
                                                   reads=[r_cvo]) for q in range(4)])
                if (not is_s) and blks[-1] == NOWN - 1:
                    u32_out(30, lambda: s.dma("sp", lambda e: e.dma_start(out=o_convp[:, :], in_=cvo[0:30, :]), reads=[r_cvo]))
            s.barrier()

        def kv_env(stack, pfx):
            sba = lambda name, shape, dtype: stack.enter_context(nc.sbuf_tensor(_un(name), shape, dtype))
            gmix = sba(pfx + "gmixA", [128, D], F32); r_gmix = s.res()
            load_gain(gmix, g_pre_mix, r_gmix)
            Wkv = sba(pfx + "Wkv", [128, 16, 576], BF16)
            r_Wkv = s.res()
            for k4 in range(4):
                s.dma("pool", lambda e, k4=k4: e.dma_start(out=Wkv[:, 4 * k4:4 * k4 + 4, 0:512],
                                                            in_=w_in_r[:, 4 * k4:4 * k4 + 4, 3072:3584]), writes=[r_Wkv])
                s.dma("pool", lambda e, k4=k4: e.dma_start(out=Wkv[:, 4 * k4:4 * k4 + 4, 512:576],
                                                            in_=w_in_r[:, 4 * k4:4 * k4 + 4, 4608:4672]), writes=[r_Wkv])
            xt = [sba(pfx + "xt%d" % i, [128, D], F32) for i in range(2)]
            r_xt = [s.res() for _ in range(2)]
            junk = sba(pfx + "junkA", [128, D], BF16); r_junk = s.res()
            xn = [sba(pfx + "xn%d" % i, [128, D], BF16) for i in range(2)]
            r_xn = [s.res() for _ in range(2)]
            xnT = [sba(pfx + "xnT%d" % i, [128, 16, 128], BF16) for i in range(2)]
            r_xnT = [s.res() for _ in range(2)]
            ssq = [sba(pfx + "ssA%d" % i, [128, 1], F32) for i in range(2)]
            r_ss = [s.res() for _ in range(2)]
            kvf = [sba(pfx + "kvf%d" % i, [128, 576], F32) for i in range(2)]
            r_kvf = [s.res() for _ in range(2)]
            kb = [sba(pfx + "kb%d" % i, [128, 320], BF16) for i in range(2)]
            r_kb = [s.res() for _ in range(2)]
            tp = bkb(0, 2).rearrange("p (k t) -> p k t", t=128)
            tp2 = bkb(6).rearrange("p (k t) -> p k t", t=128)

            kvcnt = [0]

            def kv_tile(src, KT_dst, V_dst, kiT_dst, r_KT, r_V, r_kiT, dk, dv, dik):
                b = kvcnt[0] % 2; kvcnt[0] += 1
                pkv, pki = bk(2 + b), bk(4 + b)
                r_pkv = [rb[2 + b], rb[4 + b]]
                s.dma("sp", lambda e: e.dma_start(out=xt[b][:], in_=src), writes=[r_xt[b]])
                rms_to_bf(128, xt[b][:], r_xt[b], gmix, r_gmix, xn[b][:], r_xn[b], (junk, r_junk, ssq[b], r_ss[b]))
                for k in range(16):
                    s.op("pe", lambda e, k=k: e.transpose(tp[:, k, :], xn[b][:, k * 128:(k + 1) * 128], ident[:]),
                         reads=[r_xn[b], r_c], writes=[rb[0], rb[1]])
                s.op("act", lambda e: e.copy(out=xnT[b][:], in_=tp[:, :, :]), writes=[rb[0], rb[1], r_xnT[b]])
                for k in range(16):
                    s.op("pe", lambda e, k=k: e.matmul(pkv[:, :], xnT[b][:, k, :], Wkv[:, k, 0:512], start=(k == 0), stop=(k == 15)),
                         reads=[r_xnT[b], r_Wkv], writes=r_pkv)
                for k in range(16):
                    s.op("pe", lambda e, k=k: e.matmul(pki[:, 0:64], xnT[b][:, k, :], Wkv[:, k, 512:576], start=(k == 0), stop=(k == 15)),
                         reads=[r_xnT[b], r_Wkv], writes=r_pkv)
                s.op("dve", lambda e: e.tensor_copy(out=kvf[b][:, 0:512], in_=pkv[:, :]), writes=r_pkv + [r_kvf[b]])
                s.op("dve", lambda e: e.tensor_copy(out=kvf[b][:, 512:576], in_=pki[:, 0:64]), writes=r_pkv + [r_kvf[b]])
                s.op("act", lambda e: e.copy(out=kb[b][:, 0:256], in_=pkv[:, 0:256]), writes=r_pkv + [r_kb[b]])
                s.op("act", lambda e: e.copy(out=V_dst, in_=pkv[:, 256:512]), writes=r_pkv + [r_V])
                s.op("act", lambda e: e.copy(out=kb[b][:, 256:320], in_=pki[:, 0:64]), writes=r_pkv + [r_kb[b]])
                s.op("pe", lambda e: e.transpose(tp2[:, 0, :], kb[b][:, 0:128], ident[:]), reads=[r_kb[b], r_c], writes=[rb[6]])
                s.op("pe", lambda e: e.transpose(tp2[:, 1, :], kb[b][:, 128:256], ident[:]), reads=[r_kb[b], r_c], writes=[rb[6]])
                s.op("pe", lambda e: e.transpose(tp2[0:64, 2, :], kb[b][:, 256:320], ident[:]), reads=[r_kb[b], r_c], writes=[rb[6]])
                s.op("dve", lambda e: e.tensor_copy(out=KT_dst, in_=tp2[:, 0:2, :]), writes=[rb[6], r_KT])
                s.op("dve", lambda e: e.tensor_copy(out=kiT_dst, in_=tp2[0:64, 2, :]), writes=[rb[6], r_kiT])
                s.dma("sp", lambda e: e.dma_start(out=dk, in_=kvf[b][:, 0:256]), reads=[r_kvf[b]])
                s.dma("sp", lambda e: e.dma_start(out=dv, in_=kvf[b][:, 256:512]), reads=[r_kvf[b]])
                s.dma("sp", lambda e: e.dma_start(out=dik, in_=kvf[b][:, 512:576]), reads=[r_kvf[b]])

            return kv_tile

        def attn_env(stack, pfx, NSC):
            sbb = lambda name, shape, dtype: stack.enter_context(nc.sbuf_tensor(_un(name), shape, dtype))
            NS_ = 2 if pfx == "b_" else 1
            scores = [sbb(pfx + "score%d" % i, [128, NSC], F32) for i in range(NS_)]
            r_scores = [s.res() for _ in range(NS_)]
            cur = {"i": 0}
            wk = sbb(pfx + "wk", [128, NBIS], F32); r_wk = s.res()
            pw2 = sbb(pfx + "pw2", [128, NBIS], F32); r_pw2 = s.res()
            for it_ in range(NBIS):
                s.op("pool", lambda e, it_=it_: e.memset(pw2[:, it_:it_ + 1], 0.5 ** (it_ + 1)), writes=[r_pw2])
            cjunk = sbb(pfx + "cjunk", [128, NSC], U8); r_cjunk = s.res()
            cmask = sbb(pfx + "cmask", [128, 512], F32)
            cmask_s = sbb(pfx + "cmask_s", [128, 128], F32)
            identrep = sbb(pfx + "identrep", [128, 4, 128], BF16)
            selrep = sbb(pfx + "selrep", [32, 4, 32], BF16)
            iota_p = sbb(pfx + "iota_p", [128, 1], F32)
            r_bc = s.res()
            s.dma("sp", lambda e: e.dma_start(out=cmask[:], in_=cmask_d[:, :]), writes=[r_bc])
            s.dma("sp", lambda e: e.dma_start(out=cmask_s[:], in_=cmask_s_d[:, :]), writes=[r_bc])
            s.dma("sp", lambda e: e.dma_start(out=identrep[:], in_=identrep_d[:, :, :]), writes=[r_bc])
            s.dma("sp", lambda e: e.dma_start(out=selrep[:], in_=selrep_d[:, :, :]), writes=[r_bc])
            s.dma("sp", lambda e: e.dma_start(out=iota_p[:], in_=iota_d[:, :]), writes=[r_bc])
            QTb = sbb(pfx + "QTb", [128, 8, 128], BF16); r_QTb = s.res()
            qiTb = sbb(pfx + "qiTb", [64, 16, 128], BF16); r_qiTb = s.res()
            Wselb = sbb(pfx + "Wselb", [128, 16, 128], BF16); r_Wselb = s.res()
            Rt = [sbb(pfx + "Rt%d" % i, [128, 512], BF16) for i in range(4)]
            r_Rt = [s.res() for _ in range(4)]
            Mb = [sbb(pfx + "Mb%d" % i, [128, 512], BF16) for i in range(2)]
            r_Mb = [s.res() for _ in range(2)]
            Pt = [sbb(pfx + "Pt%d" % i, [128, 512], BF16) for i in range(3)]
            r_Pt = [s.res() for _ in range(3)]
            sms = [sbb(pfx + "smallB%d" % i, [128, 8], F32) for i in range(NS_)]
            r_sms = [s.res() for _ in range(NS_)]
            rsum = sbb(pfx + "rsum", [128, 2, 512], F32); r_rsum = s.res()
            aoT = sbb(pfx + "aoT", [128, 8, 128], BF16); r_aoT = s.res()
            cnt = {"r": 0, "m": 0, "p": 0, "l": 0}

            def load_q(blk):
                s.dma("sp", lambda e: e.dma_start(out=QTb[:], in_=QT_scr[blk]), writes=[r_QTb])

            def load_block(blk):
                s.dma("sp", lambda e: e.dma_start(out=qiTb[:], in_=qiT_scr[blk].rearrange("d g a b -> d g (a b)")), writes=[r_qiTb])
                s.dma("sp", lambda e: e.dma_start(out=Wselb[:], in_=Wsel_scr[blk]), writes=[r_Wselb])

            def idx_L(it):
                lb = cnt["l"] % 3; cnt["l"] += 1
                it["lb"] = lb
                ncol = it["ncol"]
                s.op("pe", lambda e: e.matmul(bk(lb)[:, 0:ncol], qiTb[:, it["g"], :], it["ki"], start=True, stop=True),
                     reads=[r_qiTb, it["r_ki"]], writes=[rb[lb]])

            def idx_R(it):
                lb, ncol, accumulate = it["lb"], it["ncol"], it["acc"]
                ri = cnt["r"] % 4; cnt["r"] += 1
                if ri % 2 == 0 or not accumulate:
                    s.op("act", lambda e: e.activation(out=Rt[ri][:, 0:ncol], in_=bk(lb)[:, 0:ncol], func=AF.Relu),
                         writes=[rb[lb], r_Rt[ri]])
                else:
                    s.op("dve", lambda e: e.tensor_scalar(out=Rt[ri][:, 0:ncol], in0=bk(lb)[:, 0:ncol], scalar1=0.0, scalar2=None, op0=ALU.max),
                         writes=[rb[lb], r_Rt[ri]])
                s.op("pe", lambda e: e.matmul(bk(3)[:, 0:ncol], Wselb[:, it["g"], :], Rt[ri][:, 0:ncol],
                                              start=it["first"], stop=it["last"]),
                     reads=[r_Wselb, r_Rt[ri]], writes=[rb[3]])
                if it["last"]:
                    score, r_score = scores[it["sc"]], r_scores[it["sc"]]
                    dst = score[:, it["dst"]:it["dst"] + ncol]
                    if accumulate:
                        s.op("dve", lambda e: e.tensor_tensor(out=dst, in0=bk(3)[:, 0:ncol], in1=dst, op=ALU.add), writes=[rb[3], r_score])
                    else:
                        s.op("act", lambda e: e.copy(out=dst, in_=bk(3)[:, 0:ncol]), writes=[rb[3], r_score])

            def indexer_run(items, look=2):
                for it in items[:look]:
                    idx_L(it)
                for n, it in enumerate(items):
                    idx_R(it)
                    if n + look < len(items):
                        idx_L(items[n + look])

            def idx_items(groups, ki_ap, r_ki, ncol, dst_col, accumulate):
                return [dict(g=g, ki=ki_ap, r_ki=r_ki, ncol=ncol, dst=dst_col, acc=accumulate, sc=cur["i"],
                             first=(gi == 0), last=(gi == len(groups) - 1)) for gi, g in enumerate(groups)]

            def indexer_tile(groups, ki_ap, r_ki, ncol, dst_col, mask_ap, accumulate):
                indexer_run(idx_items(groups, ki_ap, r_ki, ncol, dst_col, accumulate))

            def threshold(ncols, premask_cols=None):
                score, r_score = scores[cur["i"]], r_scores[cur["i"]]
                sm, r_sm = sms[cur["i"]], r_sms[cur["i"]]
                sc = score[:, 0:ncols]
                lo, hi, mid, cn, sel, d1 = (sm[:, i:i + 1] for i in range(6))
                s.op("dve", lambda e: e.tensor_reduce(out=hi, in_=sc, axis=AX.X, op=ALU.max), reads=[r_score], writes=[r_sm])
                s.op("dve", lambda e: e.tensor_reduce(out=lo, in_=sc, axis=AX.X, op=ALU.min), reads=[r_score], writes=[r_sm])
                if premask_cols is not None:
                    c0, c1, m_ap = premask_cols
                    s.op("dve", lambda e: e.tensor_tensor(out=score[:, c0:c1], in0=score[:, c0:c1], in1=m_ap, op=ALU.add),
                         reads=[r_bc], writes=[r_score])
                s.op("dve", lambda e: e.tensor_tensor(out=d1, in0=hi, in1=lo, op=ALU.subtract), writes=[r_sm])
                s.op("dve", lambda e: e.tensor_scalar(out=d1, in0=d1, scalar1=1.001, scalar2=1e-6, op0=ALU.mult, op1=ALU.add), writes=[r_sm])
                s.op("dve", lambda e: e.tensor_scalar(out=wk[:], in0=pw2[:], scalar1=d1, scalar2=None, op0=ALU.mult),
                     reads=[r_pw2], writes=[r_sm, r_wk])
                for it in range(NBIS):
                    s.op("dve", lambda e: e.tensor_tensor(out=mid, in0=lo, in1=wk[:, it:it + 1], op=ALU.add), reads=[r_wk], writes=[r_sm])
                    s.op("dve", lambda e: e.tensor_scalar(out=cjunk[:, 0:ncols], in0=sc, scalar1=mid, scalar2=None,
                                                          op0=ALU.is_ge, op1=ALU.add, accum_out=cn),
                         reads=[r_score], writes=[r_cjunk, r_sm])
                    s.op("dve", lambda e: e.scalar_tensor_tensor(out=sel, in0=cn, scalar=float(TOPK) - 0.5, in1=wk[:, it:it + 1],
                                                                 op0=ALU.is_ge, op1=ALU.mult), reads=[r_wk], writes=[r_sm])
                    s.op("dve", lambda e: e.tensor_tensor(out=lo, in0=lo, in1=sel, op=ALU.add), writes=[r_sm])

            def make_mb(c0, ncol):
                mi = cnt["m"] % 2; cnt["m"] += 1
                score, r_score = scores[cur["i"]], r_scores[cur["i"]]
                sm, r_sm = sms[cur["i"]], r_sms[cur["i"]]
                s.op("dve", lambda e: e.tensor_scalar(out=Mb[mi][:, 0:ncol], in0=score[:, c0:c0 + ncol], scalar1=sm[:, 0:1], scalar2=NEG,
                                                      op0=ALU.is_lt, op1=ALU.mult),
                     reads=[r_score, r_sm], writes=[r_Mb[mi]])
                return mi

            def att_S(it):
                pi = cnt["p"] % 3; cnt["p"] += 1
                sb_ = pi % 2
                it["pi"], it["sb"] = pi, sb_
                s.op("pe", lambda e: e.matmul(bk(sb_)[:, :], it["kt"], it["q"], start=True, stop=False),
                     reads=it["r_kv"] + [it["r_q"]], writes=[rb[sb_]])
                s.op("pe", lambda e: e.matmul(bk(sb_)[:, :], it["mb_l"], it["mb_r"], start=False, stop=True),
                     reads=[it["r_mb"], r_bc], writes=[rb[sb_]])
                s.op("act", lambda e: e.activation(out=Pt[pi][:, :], in_=bk(sb_)[:, :], func=AF.Exp), writes=[rb[sb_], r_Pt[pi]])

            def att_PV(it):
                pi, kvh = it["pi"], it["kvh"]
                s.op("pe", lambda e: e.matmul(bk(4 + kvh)[:, :], it["v"], Pt[pi][:, :], start=it["first"], stop=it["last"]),
                     reads=it["r_kv"] + [r_Pt[pi]], writes=[rb[4 + kvh]])
                s.op("pe", lambda e: e.matmul(bk(6 + kvh)[:, :], ones_bf[:, :], Pt[pi][:, :], start=it["first"], stop=it["last"]),
                     reads=[r_Pt[pi], r_c], writes=[rb[6 + kvh]])

            def attend_chunk(first, last, kp, kt_fn, v_fn, r_kv, q_fn, r_q, nq, mb_l, mb_r, r_mb):
                if nq == 512:
                    for kvh in range(2):
                        pi = cnt["p"] % 3; cnt["p"] += 1
                        sb_ = pi % 2
                        s.op("pe", lambda e: e.matmul(bk(sb_)[0:kp, :], kt_fn(kvh), q_fn(kvh), start=True, stop=False),
                             reads=r_kv + [r_q], writes=[rb[sb_]])
                        s.op("pe", lambda e: e.matmul(bk(sb_)[0:kp, :], mb_l, mb_r, start=False, stop=True),
                             reads=[r_mb, r_bc], writes=[rb[sb_]])
                        s.op("act", lambda e: e.activation(out=Pt[pi][0:kp, :], in_=bk(sb_)[0:kp, :], func=AF.Exp), writes=[rb[sb_], r_Pt[pi]])
                        s.op("pe", lambda e: e.matmul(bk(4 + kvh)[:, :], v_fn(kvh), Pt[pi][0:kp, :], start=first, stop=last),
                             reads=r_kv + [r_Pt[pi]], writes=[rb[4 + kvh]])
                        s.op("pe", lambda e: e.matmul(bk(6 + kvh)[:, :], ones_bf[0:kp, :], Pt[pi][0:kp, :], start=first, stop=last),
                             reads=[r_Pt[pi], r_c], writes=[rb[6 + kvh]])
                else:
                    pi = cnt["p"] % 3; cnt["p"] += 1
                    sb_ = pi % 2
                    for kvh in range(2):
                        s.op("pe", lambda e: e.matmul(bk(sb_)[0:kp, kvh * nq:(kvh + 1) * nq], kt_fn(kvh), q_fn(kvh), start=True, stop=False),
                             reads=r_kv + [r_q], writes=[rb[sb_]])
                        s.op("pe", lambda e: e.matmul(bk(sb_)[0:kp, kvh * nq:(kvh + 1) * nq], mb_l, mb_r, start=False, stop=True),
                             reads=[r_mb, r_bc], writes=[rb[sb_]])
                    s.op("act", lambda e: e.activation(out=Pt[pi][0:kp, 0:2 * nq], in_=bk(sb_)[0:kp, 0:2 * nq], func=AF.Exp),
                         writes=[rb[sb_], r_Pt[pi]])
                    for kvh in range(2):
                        s.op("pe", lambda e: e.matmul(bk(4 + kvh)[:, 0:nq], v_fn(kvh), Pt[pi][0:kp, kvh * nq:(kvh + 1) * nq],
                                                      start=first, stop=last),
                             reads=r_kv + [r_Pt[pi]], writes=[rb[4 + kvh]])
                    s.op("pe", lambda e: e.matmul(bk(6)[:, 0:2 * nq], ones_bf[0:kp, :], Pt[pi][0:kp, 0:2 * nq], start=first, stop=last),
                         reads=[r_Pt[pi], r_c], writes=[rb[6]])


            return dict(score=scores[0], r_score=r_scores[0], sm=sms[0], r_sm=r_sms[0], cur=cur, load_q=load_q, Pt=Pt, r_Pt=r_Pt, QTb=QTb, r_QTb=r_QTb, aoT=aoT, r_aoT=r_aoT, rsum=rsum, r_rsum=r_rsum,
                        Mb=Mb, r_Mb=r_Mb, cmask=cmask, cmask_s=cmask_s, identrep=identrep, selrep=selrep, iota_p=iota_p, r_bc=r_bc, cnt=cnt,
                        load_block=load_block, indexer_tile=indexer_tile, indexer_run=indexer_run, idx_items=idx_items, att_S=att_S, att_PV=att_PV, threshold=threshold, make_mb=make_mb, attend_chunk=attend_chunk)


        if "S" in PH:
            with ExitStack() as pss:
                sbs = lambda name, shape, dtype: pss.enter_context(nc.sbuf_tensor(_un(name), shape, dtype))
                NCS = NPG * 128 + 128
                env = attn_env(pss, "s_", NCS)
                score, sm, QTb, aoT, rsum, selrep, cmask_s = env["score"], env["sm"], env["QTb"], env["aoT"], env["rsum"], env["selrep"], env["cmask_s"]
                r_score, r_sm, r_QTb, r_aoT, r_rsum, r_bc = env["r_score"], env["r_sm"], env["r_QTb"], env["r_aoT"], env["r_rsum"], env["r_bc"]
                KTn = sbs("KTn", [128, 2, 128], BF16)
                Vn = sbs("Vn", [128, 256], BF16)
                kiTn = sbs("kiTn", [64, 128], BF16)
                r_kvn = s.res()
                with ExitStack() as pkn:
                    kvt = kv_env(pkn, "s_")
                    kvt(xs[32:160, :], KTn[:], Vn[:], kiTn[:], r_kvn, r_kvn, r_kvn, o_ks[:, :], o_vs[:, :], o_iks[:, :])
                    s.barrier()
                env["load_block"](SBLK)
                env["load_q"](SBLK)
                ptab_sb = sbs("ptab_sb", [NPG, 4], I32); r_pt = s.res()
                s.dma("sp", lambda e: e.dma_start(out=ptab_sb[:], in_=ptab.rearrange("q p -> p q"), allow_slow_non_contiguous=True), writes=[r_pt])
                RG = 16
                NJ = 128 // RG
                ptf = sbs("ptf", [NPG, 4], F32)
                idxf = sbs("idxf", [NPG, 4, NJ], F32)
                idxK = sbs("idxK", [NPG, 4, NJ], I32); r_idxK = s.res()
                s.op("dve", lambda e: e.tensor_copy(out=ptf[:], in_=ptab_sb[:]), reads=[r_pt], writes=[r_idxK])
                for jj in range(NJ):
                    s.op("dve", lambda e, jj=jj: e.tensor_scalar(out=idxf[:, :, jj], in0=ptf[:], scalar1=float(NJ), scalar2=float(jj),
                                                                op0=ALU.mult, op1=ALU.add), writes=[r_idxK])
                s.op("dve", lambda e: e.tensor_copy(out=idxK[:], in_=idxf[:]), writes=[r_idxK])
                with ExitStack() as psi:
                    sbi = lambda name, shape, dtype: psi.enter_context(nc.sbuf_tensor(_un(name), shape, dtype))
                    idxpg = sbi("idxpg", [NPG, PAGE * IDIM], F32); r_idxpg = s.res()
                    kiTs = sbi("kiTs", [64, NPG * 128], BF16); r_kiTs = s.res()
                    kvw = kiTs[:].rearrange("d (pg r) -> d pg r", r=128)
                    s.op("pool", lambda e: e.memset(score[:, 0:NCS], 0.0), writes=[r_score])
                    for q in range(4):
                        s.dma("pool", lambda e, q=q: e.indirect_dma_start(
                            out=idxpg[:], out_offset=None, in_=cache_ik[:, :],
                            in_offset=bass.IndirectOffsetOnAxis(ap=ptab_sb[:, q:q + 1], axis=0)), reads=[r_pt], writes=[r_idxpg])
                        for r0 in range(0, 128, 4):
                            bank = 4 + (r0 // 4) % 2
                            tq = bk(bank)[0:64, 0:4 * NPG].rearrange("d (r pg) -> d r pg", pg=NPG)
                            for rr in range(4):
                                s.op("pe", lambda e, rr=rr: e.transpose(tq[:, rr, :], idxpg[:, (r0 + rr) * 64:(r0 + rr + 1) * 64], ident32[0:NPG, 0:NPG]),
                                     reads=[r_idxpg, r_c], writes=[rb[bank]])
                            eng = "act" if (r0 // 4) % 2 == 0 else "dve"
                            if eng == "act":
                                s.op("act", lambda e: e.copy(out=kvw[:, :, r0:r0 + 4].rearrange("d pg r -> d r pg"), in_=tq), writes=[rb[bank], r_kiTs])
                            else:
                                s.op("dve", lambda e: e.tensor_copy(out=kvw[:, :, r0:r0 + 4].rearrange("d pg r -> d r pg"), in_=tq), writes=[rb[bank], r_kiTs])
                        items = []
                        for kc in range(NPG * 128 // 512):
                            items += env["idx_items"]([q], kiTs[:, kc * 512:(kc + 1) * 512], r_kiTs, 512, kc * 512, True)
                        items += env["idx_items"]([q], kiTn[:, :], r_kvn, 128, NPG * 128, True)
                        env["indexer_run"](items)
                    s.barrier()
                env["threshold"](NCS, (NPG * 128, NCS, cmask_s[:]))
                with ExitStack() as psa:
                    sbt = lambda name, shape, dtype: psa.enter_context(nc.sbuf_tensor(_un(name), shape, dtype))
                    Kg = [sbt("Kg%d" % i, [NPG, RG * 256], F32) for i in range(2)]
                    Vg = [sbt("Vg%d" % i, [NPG, RG * 256], F32) for i in range(2)]
                    r_Kg = [s.res() for _ in range(2)]
                    r_Vg = [s.res() for _ in range(2)]
                    KTc = [sbt("KTc%d" % i, [128, 4, 2, NPG], BF16) for i in range(3)]
                    Vc = [sbt("Vc%d" % i, [NPG, 4, 256], BF16) for i in range(3)]
                    Mbc = [sbt("Mbc%d" % i, [32, 4, 128], BF16) for i in range(3)]
                    r_KTc = [s.res() for _ in range(3)]
                    r_Vc = [s.res() for _ in range(3)]
                    r_Mbc = [s.res() for _ in range(3)]
                    QTq = sbt("QTq", [128, 8, 8], BF16); r_QTq = s.res()
                    ck = cache_k.rearrange("(n r) d -> n (r d)", r=RG)
                    cv = cache_v.rearrange("(n r) d -> n (r d)", r=RG)
                    s.op("pool", lambda e: e.memset(aoT[:], 0.0), writes=[r_aoT])
                    cc = 0
                    for q in range(4):
                        s.op("dve", lambda e: e.tensor_copy(out=QTq[:], in_=QTb[:, :, 8 * q:8 * q + 8]), reads=[r_QTb], writes=[r_QTq])
                        qf = lambda kvh: QTq[:, 4 * kvh:4 * kvh + 4, :]
                        batches = []
                        gdone = set()

                        def gather(jj):
                            if jj in gdone or jj >= NJ:
                                return
                            gdone.add(jj)
                            gi = (q * NJ + jj) % 2
                            s.dma("pool", lambda e: e.indirect_dma_start(
                                out=Kg[gi][:], out_offset=None, in_=ck[:, :],
                                in_offset=bass.IndirectOffsetOnAxis(ap=idxK[:, q, jj:jj + 1], axis=0)), reads=[r_idxK], writes=[r_Kg[gi]])
                            s.dma("pool", lambda e: e.indirect_dma_start(
                                out=Vg[gi][:], out_offset=None, in_=cv[:, :],
                                in_offset=bass.IndirectOffsetOnAxis(ap=idxK[:, q, jj:jj + 1], axis=0)), reads=[r_idxK], writes=[r_Vg[gi]])

                        for jj in range(NJ):
                            gi = (q * NJ + jj) % 2
                            for bb in range(RG // 4):
                                batches.append(dict(gi=gi, bb=bb, jj=jj, r0=jj * RG + 4 * bb))

                        def stA(bt):
                            gi, bb, r0 = bt["gi"], bt["bb"], bt["r0"]
                            gather(bt["jj"])
                            if bb == RG // 4 - 1:
                                pass
                            ci = bt["ci"]
                            tk = bk(2, 2)[:, 0:8 * NPG].rearrange("p (r a b) -> p r a b", a=2, b=NPG)
                            for rl in range(4):
                                for kvh in range(2):
                                    c0 = (4 * bb + rl) * 256 + kvh * 128
                                    s.op("pe", lambda e, rl=rl, kvh=kvh, c0=c0: e.transpose(tk[:, rl, kvh, :], Kg[gi][:, c0:c0 + 128], ident32[0:NPG, 0:NPG]),
                                         reads=[r_Kg[gi], r_c], writes=[rb[2], rb[3]])
                            s.op("act", lambda e: e.copy(out=KTc[ci][:], in_=tk), writes=[rb[2], rb[3], r_KTc[ci]])
                            s.op("pool", lambda e: e.tensor_copy(out=Vc[ci][:], in_=Vg[gi][:, 4 * bb * 256:(4 * bb + 4) * 256]),
                                 reads=[r_Vg[gi]], writes=[r_Vc[ci]])
                            s.op("dve", lambda e: e.tensor_scalar(
                                out=Mbc[ci][:, :, 0:NPG], in0=score[0:32, 0:NPG * 128].rearrange("p (pg r) -> p r pg", r=128)[:, r0:r0 + 4, :],
                                scalar1=sm[0:32, 0:1], scalar2=NEG, op0=ALU.is_lt, op1=ALU.mult), reads=[r_score, r_sm], writes=[r_Mbc[ci]])

                        def stB(bt):
                            ci = bt["ci"]
                            pi = env["cnt"]["p"] % 3; env["cnt"]["p"] += 1
                            sb_ = pi % 2
                            bt["pi"] = pi
                            Pt, r_Pt = env["Pt"], env["r_Pt"]
                            for rl in range(4):
                                for kvh in range(2):
                                    oc = rl * 64 + kvh * 32
                                    s.op("pe", lambda e, rl=rl, kvh=kvh, oc=oc: e.matmul(bk(sb_)[0:NPG, oc:oc + 32], KTc[ci][:, rl, kvh, :], qf(kvh),
                                                                                          start=True, stop=False),
                                         reads=[r_KTc[ci], r_QTq], writes=[rb[sb_]])
                                    s.op("pe", lambda e, rl=rl, kvh=kvh, oc=oc: e.matmul(bk(sb_)[0:NPG, oc:oc + 32], Mbc[ci][:, rl, 0:NPG], selrep[:, q, :],
                                                                                          start=False, stop=True),
                                         reads=[r_Mbc[ci], r_bc], writes=[rb[sb_]])
                            s.op("act", lambda e: e.activation(out=Pt[pi][0:NPG, 0:256], in_=bk(sb_)[0:NPG, 0:256], func=AF.Exp),
                                 writes=[rb[sb_], r_Pt[pi]])

                        def stC(bt):
                            ci, pi, r0 = bt["ci"], bt["pi"], bt["r0"]
                            Pt, r_Pt = env["Pt"], env["r_Pt"]
                            for rl in range(4):
                                first = (r0 + rl == 0)
                                for kvh in range(2):
                                    oc = rl * 64 + kvh * 32
                                    s.op("pe", lambda e, rl=rl, kvh=kvh, oc=oc: e.matmul(bk(4 + kvh)[:, 0:32], Vc[ci][:, rl, kvh * 128:(kvh + 1) * 128],
                                                                                          Pt[pi][0:NPG, oc:oc + 32], start=first, stop=False),
                                         reads=[r_Vc[ci], r_Pt[pi]], writes=[rb[4 + kvh]])
                                s.op("pe", lambda e, rl=rl: e.matmul(bk(6)[:, 0:64], ones_bf[0:NPG, :], Pt[pi][0:NPG, rl * 64:rl * 64 + 64],
                                                                     start=first, stop=False),
                                     reads=[r_Pt[pi], r_c], writes=[rb[6]])

                        for bt in batches:
                            bt["ci"] = cc % 3; cc += 1
                        gather(0); gather(1)
                        stA(batches[0]); stB(batches[0])
                        for n, bt in enumerate(batches):
                            if n + 1 < len(batches):
                                stA(batches[n + 1]); stB(batches[n + 1])
                            stC(bt)
                            if bt["bb"] == RG // 4 - 1:
                                gather(bt["jj"] + 2)
                        ci = cc % 3; cc += 1
                        s.op("dve", lambda e: e.tensor_scalar(out=Mbc[ci][:, 0, :], in0=score[0:32, NPG * 128:NPG * 128 + 128], scalar1=sm[0:32, 0:1], scalar2=NEG,
                                                              op0=ALU.is_lt, op1=ALU.mult), reads=[r_score, r_sm], writes=[r_Mbc[ci]])
                        env["attend_chunk"](False, True, 128, lambda kvh: KTn[:, kvh, :], lambda kvh: Vn[:, kvh * 128:(kvh + 1) * 128],
                                            [r_kvn], qf, r_QTq, 32, Mbc[ci][:, 0, :], selrep[:, q, :], r_Mbc[ci])
                        s.op("dve", lambda e: e.reciprocal(out=rsum[:, 0, 0:64], in_=bk(6)[:, 0:64]), writes=[rb[6], r_rsum])
                        for kvh in range(2):
                            s.op("dve", lambda e, kvh=kvh: e.tensor_tensor(out=aoT[:, 4 * kvh:4 * kvh + 4, 8 * q:8 * q + 8],
                                                                           in0=bk(4 + kvh)[:, 0:32].rearrange("p (h t) -> p h t", t=8),
                                                                           in1=rsum[:, 0, 32 * kvh:32 * kvh + 32].rearrange("p (h t) -> p h t", t=8), op=ALU.mult),
                                 reads=[r_rsum], writes=[rb[4 + kvh], r_aoT])
                    s.dma("sp", lambda e: e.dma_start(out=mixT_scr[SBLK, :, 8:16, :], in_=aoT[:]), reads=[r_aoT])
                    s.barrier()


        with ExitStack() as pkv_stack:
            sbk = lambda name, shape, dtype: pkv_stack.enter_context(nc.sbuf_tensor(_un(name), shape, dtype))
            if "A" in PH or "B" in PH:
                KT = sbk("KT", [128, NKV, NT * 128], BF16)
                Vr = sbk("Vr", [128, NT, NKV * HD], BF16)
                kiT = sbk("kiT", [64, NT * 128], BF16)
                r_KT, r_V, r_kiT = s.res(), s.res(), s.res()
            if "A" in PH:
                with ExitStack() as pa:
                    kvt = kv_env(pa, "a_")
                    for n in range(NT):
                        kvt(xb[n * 128:(n + 1) * 128, :], KT[:, :, n * 128:(n + 1) * 128], Vr[:, n, :], kiT[:, n * 128:(n + 1) * 128],
                            r_KT, r_V, r_kiT, o_k[n * 128:(n + 1) * 128, :], o_v[n * 128:(n + 1) * 128, :], o_ik[n * 128:(n + 1) * 128, :])
                    s.barrier()
            if "B" in PH:
                with ExitStack() as pb:
                    env = attn_env(pb, "b_", NT * 128)
                    score, sm, QTb, aoT, rsum, cmask, identrep, Mb = env["score"], env["sm"], env["QTb"], env["aoT"], env["rsum"], env["cmask"], env["identrep"], env["Mb"]
                    r_score, r_sm, r_QTb, r_aoT, r_rsum, r_bc, r_Mb = env["r_score"], env["r_sm"], env["r_QTb"], env["r_aoT"], env["r_rsum"], env["r_bc"], env["r_Mb"]
                    load_block, indexer_tile, threshold, make_mb, attend_chunk = env["load_block"], env["indexer_tile"], env["threshold"], env["make_mb"], env["attend_chunk"]
                    cur, load_q = env["cur"], env["load_q"]

                    def do_indexer(i):
                        cur["i"] = i % 2
                        load_block(i)
                        items = []
                        for kc in range(i + 1):
                            items += env["idx_items"](list(range(16)), kiT[:, kc * 512:(kc + 1) * 512], r_kiT, 512, kc * 512, False)
                        env["indexer_run"](items)

                    def do_thr(i):
                        cur["i"] = i % 2
                        ncols = (i + 1) * 512
                        threshold(ncols, (ncols - 512, ncols, cmask[:]))

                    def do_attn(i):
                        cur["i"] = i % 2
                        load_q(i)
                        nch = 4 * (i + 1)
                        items = []
                        for c in range(nch):
                            for kvh in range(2):
                                items.append(dict(c=c, kvh=kvh, first=(c == 0), last=(c == nch - 1),
                                                  kt=KT[:, kvh, c * 128:(c + 1) * 128], v=Vr[:, c, kvh * 128:(kvh + 1) * 128],
                                                  r_kv=[r_KT, r_V], q=QTb[:, 4 * kvh:4 * kvh + 4, :], r_q=r_QTb, mb_r=identrep[:]))
                        mstate = {}

                        def prep(it):
                            if it["c"] % 4 == 0 and it["kvh"] == 0:
                                mstate["mi"] = make_mb(it["c"] * 128, 512)
                            mi = mstate["mi"]
                            it["mb_l"] = Mb[mi][:, (it["c"] % 4) * 128:(it["c"] % 4 + 1) * 128]
                            it["r_mb"] = r_Mb[mi]
                            env["att_S"](it)

                        prep(items[0])
                        for n, it in enumerate(items):
                            if n + 1 < len(items):
                                prep(items[n + 1])
                            env["att_PV"](it)
                        s.op("dve", lambda e: e.reciprocal(out=rsum[:], in_=bk(6, 2).rearrange("p (a b) -> p a b", b=512)),
                             writes=[rb[6], rb[7], r_rsum])
                        s.op("dve", lambda e: e.tensor_tensor(out=aoT[:], in0=bk(4, 2).rearrange("p (h t) -> p h t", t=128),
                                                              in1=rsum[:].rearrange("p a (h t) -> p (a h) t", t=128), op=ALU.mult),
                             reads=[r_rsum], writes=[rb[4], rb[5], r_aoT])
                        s.dma("sp", lambda e: e.dma_start(out=mixT_scr[i, :, 8:16, :], in_=aoT[:]), reads=[r_aoT])

                    do_indexer(0)
                    for i in range(NOWN):
                        if i + 1 < NOWN:
                            do_indexer(i + 1)
                        do_thr(i)
                        do_attn(i)

                    s.barrier()


        if "C" in PH:
            supers = [list(range(i, min(i + SB, NOWN))) for i in range(0, NOWN, SB)] + [[SBLK]]
            with ExitStack() as pc1:
                sbc = lambda name, shape, dtype: pc1.enter_context(nc.sbuf_tensor(_un(name), shape, dtype))
                gpm = sbc("gpm", [128, D], F32); r_gpm = s.res(); load_gain(gpm, g_post_mix, r_gpm)
                gpf = sbc("gpf", [128, D], F32); r_gpf = s.res(); load_gain(gpf, g_pre_ffn, r_gpf)
                mixT = sbc("mixT", [128, 16, SB, 128], BF16); r_mixT = s.res()
                Wo = [sbc("Wo%d" % i, [128, 16, 512], BF16) for i in range(2)]
                r_Wo = [s.res() for _ in range(2)]
                mixed = sbc("mixed", [128, SB, D], F32); r_mixed = s.res()
                xt = sbc("xtC", [128, D], F32); r_xt = s.res()
                tmp = sbc("tmpC", [128, D], F32); r_tmp = s.res()
                hbf = sbc("hbf", [128, D], BF16); r_hbf = s.res()
                hT = sbc("hTC", [128, 16, 128], BF16); r_hT = s.res()
                junk = sbc("junkC", [128, D], BF16); r_junk = s.res()
                ssq = sbc("ssqC", [128, 1], F32); r_ssq = s.res()
                wc = 0
                mc = 0
                for blks in supers:
                    nb = len(blks)
                    is_s = blks[0] == SBLK
                    for bi, blk in enumerate(blks):
                        s.dma("sp", lambda e: e.dma_start(out=mixT[:, :, bi, :], in_=mixT_scr[blk]), writes=[r_mixT])
                    for nt in range(4):
                        wi_ = wc % 2; wc += 1
                        for k4 in range(4):
                            s.dma("pool", lambda e, k4=k4: e.dma_start(out=Wo[wi_][:, 4 * k4:4 * k4 + 4, :], in_=w_out_r[:, 4 * k4:4 * k4 + 4, nt * 512:(nt + 1) * 512]),
                                  writes=[r_Wo[wi_]])
                        for bi in range(nb):
                            pbk = mc % 4; mc += 1
                            for k in range(16):
                                s.op("pe", lambda e, k=k: e.matmul(bk(pbk)[:, :], mixT[:, k, bi, :], Wo[wi_][:, k, :], start=(k == 0), stop=(k == 15)),
                                     reads=[r_mixT, r_Wo[wi_]], writes=[rb[pbk]])
                            s.op("act", lambda e: e.copy(out=mixed[:, bi, nt * 512:(nt + 1) * 512], in_=bk(pbk)[:, :]), writes=[rb[pbk], r_mixed])
                    for bi, blk in enumerate(blks):
                        src = xs[32:160, :] if is_s else xo[blk, 32:160, :]
                        s.dma("sp", lambda e: e.dma_start(out=xt[:], in_=src), writes=[r_xt])
                        rms_to_bf(128, mixed[:, bi, :], r_mixed, gpm, r_gpm, tmp[:], r_tmp, (junk, r_junk, ssq, r_ssq))
                        s.op("pool", lambda e: e.tensor_tensor(out=xt[:], in0=xt[:], in1=tmp[:], op=ALU.add), reads=[r_tmp], writes=[r_xt])
                        s.dma("sp", lambda e: e.dma_start(out=x1_scr[blk], in_=xt[:]), reads=[r_xt])
                        rms_to_bf(128, xt[:], r_xt, gpf, r_gpf, hbf[:], r_hbf, (junk, r_junk, ssq, r_ssq))
                        tp = bkb(4, 2).rearrange("p (k t) -> p k t", t=128)
                        for k in range(16):
                            s.op("pe", lambda e, k=k: e.transpose(tp[:, k, :], hbf[:, k * 128:(k + 1) * 128], ident[:]),
                                 reads=[r_hbf, r_c], writes=[rb[4], rb[5]])
                        s.op("act", lambda e: e.copy(out=hT[:], in_=tp[:, :, :]), writes=[rb[4], rb[5], r_hT])
                        s.dma("sp", lambda e: e.dma_start(out=hT_scr[blk], in_=hT[:]), reads=[r_hT])
                s.barrier()
            with ExitStack() as pc2:
                sbc = lambda name, shape, dtype: pc2.enter_context(nc.sbuf_tensor(_un(name), shape, dtype))
                gff = sbc("gff", [128, D], F32); r_gff = s.res(); load_gain(gff, g_post_ffn, r_gff)
                hTs = sbc("hTs", [128, 16, SB, 128], BF16); r_hTs = s.res()
                aT = sbc("aT", [128, NFT, SB * 128], BF16); r_aT = s.res()
                Wg = [sbc("Wg%d" % i, [128, 16, 256], BF16) for i in range(2)]
                Wu = [sbc("Wu%d" % i, [128, 16, 256], BF16) for i in range(2)]
                r_Wg = [s.res() for _ in range(2)]
                r_Wu = [s.res() for _ in range(2)]
                Wd = [sbc("Wd%d" % i, [128, NFT, 256], BF16) for i in range(2)]
                r_Wd = [s.res() for _ in range(2)]
                wdc = 0
                sg = [sbc("sg%d" % i, [128, SB * 128], F32) for i in range(2)]
                r_sg = [s.res() for _ in range(2)]
                fo = sbc("fo", [128, SB, D], F32); r_fo = s.res()
                x1t = sbc("x1t", [128, D], F32); r_x1t = s.res()
                tmp = sbc("tmpC2", [128, D], F32); r_tmp = s.res()
                junk = sbc("junkC2", [128, D], BF16); r_junk = s.res()
                ssq = sbc("ssqC2", [128, 1], F32); r_ssq = s.res()
                dc = 0
                for blks in supers:
                    nb = len(blks)
                    N = nb * 128
                    for bi, blk in enumerate(blks):
                        s.dma("sp", lambda e: e.dma_start(out=hTs[:, :, bi, :], in_=hT_scr[blk]), writes=[r_hTs])
                    for ft in range(NFT):
                        wi_ = (ft // 2) % 2
                        fo_ = ft % 2
                        if fo_ == 0:
                            s.dma("pool", lambda e: e.dma_start(out=Wg[wi_][:], in_=w_gate_r[:, :, ft * 128:(ft + 2) * 128]), writes=[r_Wg[wi_]])
                            s.dma("pool", lambda e: e.dma_start(out=Wu[wi_][:], in_=w_up_r[:, :, ft * 128:(ft + 2) * 128]), writes=[r_Wu[wi_]])
                        gb, ub = 2 * (ft % 2), 2 * (ft % 2) + 1
                        for k in range(16):
                            s.op("pe", lambda e, k=k: e.matmul(bk(gb)[:, 0:N], Wg[wi_][:, k, fo_ * 128:(fo_ + 1) * 128], hTs[:, k, 0:nb, :], start=(k == 0), stop=(k == 15)),
                                 reads=[r_hTs, r_Wg[wi_]], writes=[rb[gb]])
                        for k in range(16):
                            s.op("pe", lambda e, k=k: e.matmul(bk(ub)[:, 0:N], Wu[wi_][:, k, fo_ * 128:(fo_ + 1) * 128], hTs[:, k, 0:nb, :], start=(k == 0), stop=(k == 15)),
                                 reads=[r_hTs, r_Wu[wi_]], writes=[rb[ub]])
                        s.op("act", lambda e: e.activation(out=sg[fo_][:, 0:N], in_=bk(gb)[:, 0:N], func=AF.Silu), writes=[rb[gb], r_sg[fo_]])
                        s.op("dve", lambda e: e.tensor_tensor(out=aT[:, ft, 0:N], in0=bk(ub)[:, 0:N], in1=sg[fo_][:, 0:N], op=ALU.mult),
                             reads=[r_sg[fo_]], writes=[rb[ub], r_aT])
                    for nt in range(8):
                        wd_ = wdc % 2; wdc += 1
                        for f4 in range(4):
                            s.dma("pool", lambda e, f4=f4: e.dma_start(out=Wd[wd_][:, 11 * f4:11 * f4 + 11, :], in_=w_down_r[:, 11 * f4:11 * f4 + 11, nt * 256:(nt + 1) * 256]),
                                  writes=[r_Wd[wd_]])
                        for bi in range(nb):
                            pbk = 4 + dc % 4; dc += 1
                            for ft in range(NFT):
                                s.op("pe", lambda e, ft=ft: e.matmul(bk(pbk)[:, 0:256], aT[:, ft, bi * 128:(bi + 1) * 128], Wd[wd_][:, ft, :], start=(ft == 0), stop=(ft == NFT - 1)),
                                     reads=[r_aT, r_Wd[wd_]], writes=[rb[pbk]])
                            s.op("act", lambda e: e.copy(out=fo[:, bi, nt * 256:(nt + 1) * 256], in_=bk(pbk)[:, 0:256]), writes=[rb[pbk], r_fo])
                    for bi, blk in enumerate(blks):
                        s.dma("sp", lambda e: e.dma_start(out=x1t[:], in_=x1_scr[blk]), writes=[r_x1t])
                        rms_to_bf(128, fo[:, bi, :], r_fo, gff, r_gff, tmp[:], r_tmp, (junk, r_junk, ssq, r_ssq))
                        s.op("pool", lambda e: e.tensor_tensor(out=x1t[:], in0=x1t[:], in1=tmp[:], op=ALU.add), reads=[r_tmp], writes=[r_x1t])
                        s.dma("sp", lambda e: e.dma_start(out=o_y[blk], in_=x1t[:]), reads=[r_x1t])
                s.barrier()
        s.finish()

    return nc


def _consts(j):
    c = {}
    c["identb"] = _bf(np.eye(128, dtype=np.float32))
    c["ident32"] = np.eye(128, dtype=np.float32)
    r = np.arange(128)[:, None]
    sp = np.arange(512)[None, :]
    c["cmask"] = np.where(sp <= 128 * j + r, 0.0, -1e30).astype(np.float32)
    mg = np.zeros((128, 16, 128), np.float32)
    for g in range(16):
        for row in range(128):
            mg[row, g, 8 * g + row % 8] = 1.0
    c["maskg"] = _bf(mg)
    c["identrep"] = _bf(np.repeat(np.eye(128, dtype=np.float32)[:, None, :], 4, axis=1))
    cs = np.full((128, 128), -1e30, np.float32)
    for row in range(32):
        q, t = row // 8, row % 8
        for col in range(32):
            q2, t2 = col // 8, col % 8
            if q2 == q and t2 <= t:
                cs[row, col] = 0.0
    c["cmask_s"] = cs
    sr = np.zeros((32, 4, 32), np.float32)
    for q in range(4):
        for h in range(4):
            for t in range(8):
                sr[8 * q + t, q, h * 8 + t] = 1.0
    c["selrep"] = _bf(sr)
    c["iota_p"] = np.arange(128, dtype=np.float32)[:, None]
    return c


def make_in_map(c, inp, NT, NPG):
    b, j = c // 4, c % 4
    NOWN = NT // 4
    f = lambda a: np.ascontiguousarray(a, dtype=np.float32)
    xp = inp["x_prompt"][b]
    m = {"xb": f(xp[:NT * 128])}
    xo = np.zeros((NOWN, 160, D), np.float32)
    for i in range(NOWN):
        g0 = (4 * i + j) * 128
        xo[i, 32:160] = xp[g0:g0 + 128]
        if g0 > 0:
            xo[i, 2:32] = xp[g0 - 30:g0]
    m["xo"] = xo
    xs_ = np.zeros((160, D), np.float32)
    xs_[32:64] = inp["x_sample"][4 * c:4 * c + 4].reshape(32, D)
    m["xs"] = xs_
    for k in ("w_in", "w_out", "w_gate", "w_up", "w_down", "g_pre_mix", "g_post_mix", "g_pre_ffn", "g_post_ffn",
              "conv_w", "conv_b", "conv_ln_g", "conv_ln_b"):
        m[k] = f(inp[k][0]) if inp[k][0].ndim == 2 else f(inp[k][0])[None, :]
    npool = inp["cache_k"].shape[1]
    m["cache_k"] = f(inp["cache_k"][0]).reshape(npool * PAGE, NKV * HD)
    m["cache_v"] = f(inp["cache_v"][0]).reshape(npool * PAGE, NKV * HD)
    m["cache_ik"] = f(inp["cache_idx_k"][0]).reshape(npool, PAGE * IDIM)
    m["state_conv"] = f(inp["state_conv"][0, 4 * c:4 * c + 4])
    m["ptab"] = np.ascontiguousarray(inp["page_table"][4 * c:4 * c + 4, :NPG], dtype=np.int32)
    m.update(_consts(j))
    return m


_NC_CACHE = {}


def kernel(**inp):
    NT, NPG = SEQ // 128, NPAGES
    npool = inp["cache_k"].shape[1]
    key = (NT, NPG, npool)
    if key not in _NC_CACHE:
        _NC_CACHE[key] = build({"NT": NT, "NPG": NPG, "NPOOL": npool})
    nc = _NC_CACHE[key]
    in_maps = [make_in_map(c, inp, NT, NPG) for c in range(8)]
    res = run_bass_kernel_spmd(nc, in_maps, core_ids=list(range(8))).results
    return assemble(res, NT)


def assemble(res, NT):
    NOWN = NT // 4
    S_ = NT * 128
    y_p = np.zeros((NB, S_, D), np.float32)
    y_s = np.zeros((DEC_B, DEC_T, D), np.float32)
    nk = np.zeros((1, NB, S_, NKV, HD), np.float32)
    nv = np.zeros((1, NB, S_, NKV, HD), np.float32)
    nik = np.zeros((1, NB, S_, IDIM), np.float32)
    ncp = np.zeros((1, NB, CW - 1, CONV_CH), np.float32)
    nks = np.zeros((1, DEC_B, DEC_T, NKV, HD), np.float32)
    nvs = np.zeros((1, DEC_B, DEC_T, NKV, HD), np.float32)
    niks = np.zeros((1, DEC_B, DEC_T, IDIM), np.float32)
    ncs = np.zeros((1, DEC_B, CW - 1, CONV_CH), np.float32)
    for c in range(len(res)):
        r = res[c]
        if r is None:
            continue
        b, j = c // 4, c % 4
        oy = np.asarray(r["o_y"])
        for i in range(NOWN):
            g0 = (4 * i + j) * 128
            y_p[b, g0:g0 + 128] = oy[i]
        y_s[4 * c:4 * c + 4] = oy[NOWN][0:32].reshape(4, DEC_T, D)
        if j == 0:
            nk[0, b] = np.asarray(r["o_k"]).reshape(S_, NKV, HD)
            nv[0, b] = np.asarray(r["o_v"]).reshape(S_, NKV, HD)
            nik[0, b] = np.asarray(r["o_ik"])
        if j == 3:
            ncp[0, b] = np.asarray(r["o_convp"])
        nks[0, 4 * c:4 * c + 4] = np.asarray(r["o_ks"])[0:32].reshape(4, DEC_T, NKV, HD)
        nvs[0, 4 * c:4 * c + 4] = np.asarray(r["o_vs"])[0:32].reshape(4, DEC_T, NKV, HD)
        niks[0, 4 * c:4 * c + 4] = np.asarray(r["o_iks"])[0:32].reshape(4, DEC_T, IDIM)
        ncs[0, 4 * c:4 * c + 4] = np.asarray(r["o_convs"])
    return (y_p, y_s, nk, nv, nik, ncp, nks, nvs, niks, ncs)
```

```python
import numpy as np
import ml_dtypes
import concourse.bass as bass
import concourse.mybir as mybir
from concourse.bass_utils import run_bass_kernel_spmd

F32 = mybir.dt.float32
BF16 = mybir.dt.bfloat16
I32 = mybir.dt.int32
U32 = mybir.dt.uint32
U8 = mybir.dt.uint8
AF = mybir.ActivationFunctionType
ALU = mybir.AluOpType
AX = mybir.AxisListType

D = 2048
SEQ = 8192
NB = 2
CONV_CH = 1024
NH = 8
HD = 128
NKV = 2
NIH = 16
IDIM = 64
TOPK = 256
CW = 31
DFF = 5632
N_IN = 4688
EPS = 1e-6
ATTN_SCALE = HD ** -0.5
INDEX_SCALE = (NIH * IDIM) ** -0.5
DEC_B = 32
DEC_T = 8
PAST = 16384
PAGE = 128
NPAGES = PAST // PAGE
NEG = -30000.0
NBIS = 17


class Res:
    __slots__ = ("name", "w", "rd")

    def __init__(self, name=""):
        self.name = name
        self.w = None
        self.rd = {}


class S:
    R = 8

    def __init__(self, nc, stack):
        self.nc = nc
        self.eng = {"pe": nc.tensor, "act": nc.scalar, "dve": nc.vector, "pool": nc.gpsimd, "sp": nc.sync}
        self.sem = {k: stack.enter_context(nc.semaphore("s_" + k)) for k in self.eng}
        self.cnt = {k: 0 for k in self.eng}
        self.dsem = {k: [stack.enter_context(nc.semaphore("d_%s%d" % (k, i))) for i in range(self.R)]
                     for k in ("sp", "pool", "act")}
        self.dn = {k: 0 for k in self.dsem}
        self.waited = {k: {} for k in self.eng}
        self.pending_dma = []
        self.nres = 0

    def res(self, name=""):
        return Res(name)

    def _wait(self, e, tok):
        if tok is None:
            return
        kind, key, val = tok
        if kind == "c":
            if key == e and e == "pe":
                return
            sem = self.sem[key]
            wk = ("c", key)
        else:
            sem = self.dsem[key[0]][key[1]]
            wk = ("d", key)
        if self.waited[e].get(wk, 0) >= val:
            return
        self.waited[e][wk] = val
        self.eng[e].wait_ge(sem, val)

    def _deps(self, e, reads, writes):
        toks = []
        for r in reads:
            if r.w is not None:
                toks.append(r.w)
        for w in writes:
            if w.w is not None:
                toks.append(w.w)
            for k, t in w.rd.items():
                if isinstance(t, list):
                    toks.extend(t)
                else:
                    toks.append(t)
        for t in toks:
            self._wait(e, t)

    def _mark(self, e, tok, reads, writes, is_dma):
        for r in reads:
            if is_dma:
                r.rd.setdefault("dma", []).append(tok)
            else:
                r.rd[e] = tok
        for w in writes:
            w.w = tok
            w.rd = {}

    def op(self, e, fn, reads=(), writes=()):
        self._deps(e, reads, writes)
        inst = fn(self.eng[e])
        self.cnt[e] += 1
        inst.then_inc(self.sem[e], 1)
        tok = ("c", e, self.cnt[e])
        self._mark(e, tok, reads, writes, False)
        return tok

    def dma(self, e, fn, reads=(), writes=()):
        n = self.dn[e]
        slot = n % self.R
        val = 16 * (n // self.R + 1)
        if val > 16:
            self._wait(e, ("d", (e, slot), val - 16))
        self._deps(e, reads, writes)
        inst = fn(self.eng[e])
        inst.then_inc(self.dsem[e][slot], 16)
        self.dn[e] = n + 1
        tok = ("d", (e, slot), val)
        self._mark(e, tok, reads, writes, True)
        self.pending_dma.append(tok)
        return tok

    def barrier(self):
        toks = [("c", k, self.cnt[k]) for k in self.eng if self.cnt[k] > 0]
        toks += self.pending_dma
        self.pending_dma = []
        for e in self.eng:
            for t in toks:
                if t[0] == "c" and t[1] == e:
                    continue
                self._wait(e, t)

    def finish(self):
        self.barrier()


_UN = [0]


def _un(name):
    _UN[0] += 1
    return "t%d_%s" % (_UN[0], name)


def _bf(a):
    return np.ascontiguousarray(a).astype(ml_dtypes.bfloat16)


def build(cfg):
    from contextlib import ExitStack
    NT = cfg.get("NT", SEQ // 128)
    NOWN = NT // 4
    SB = min(4, NOWN)
    NPG = cfg.get("NPG", NPAGES)
    NPOOL = cfg.get("NPOOL", 5120)
    NFT = DFF // 128
    NBLK = NOWN + 1
    SBLK = NOWN
    PH = cfg.get("PH", "UABSC")
    nc = bass.Bass("TRN2", target_bir_lowering=False)

    def din(name, shape, dtype=F32):
        return nc.dram_tensor(name, shape, dtype, kind="ExternalInput").ap()

    def dout(name, shape, dtype=F32):
        return nc.dram_tensor(name, shape, dtype, kind="ExternalOutput").ap()

    def dscr(name, shape, dtype):
        return nc.dram_tensor(name, shape, dtype, kind="Internal").ap()

    xb = din("xb", [NT * 128, D])
    xo = din("xo", [NOWN, 160, D])
    xs = din("xs", [160, D])
    w_in = din("w_in", [D, N_IN])
    w_out = din("w_out", [D, D])
    w_gate = din("w_gate", [D, DFF])
    w_up = din("w_up", [D, DFF])
    w_down = din("w_down", [DFF, D])
    g_pre_mix = din("g_pre_mix", [1, D])
    g_post_mix = din("g_post_mix", [1, D])
    g_pre_ffn = din("g_pre_ffn", [1, D])
    g_post_ffn = din("g_post_ffn", [1, D])
    conv_w = din("conv_w", [CW, CONV_CH])
    conv_b = din("conv_b", [1, CONV_CH])
    ln_g = din("conv_ln_g", [1, CONV_CH])
    ln_b = din("conv_ln_b", [1, CONV_CH])
    cache_k = din("cache_k", [NPOOL * PAGE, NKV * HD])
    cache_v = din("cache_v", [NPOOL * PAGE, NKV * HD])
    cache_ik = din("cache_ik", [NPOOL, PAGE * IDIM])
    state_conv = din("state_conv", [4, CW - 1, CONV_CH])
    ptab = din("ptab", [4, NPG], I32)
    identb_d = din("identb", [128, 128], BF16)
    ident32_d = din("ident32", [128, 128])
    cmask_d = din("cmask", [128, 512])
    maskg_d = din("maskg", [128, 16, 128], BF16)
    identrep_d = din("identrep", [128, 4, 128], BF16)
    cmask_s_d = din("cmask_s", [128, 128])
    selrep_d = din("selrep", [32, 4, 32], BF16)
    iota_d = din("iota_p", [128, 1])

    o_k = dout("o_k", [NT * 128, NKV * HD])
    o_v = dout("o_v", [NT * 128, NKV * HD])
    o_ik = dout("o_ik", [NT * 128, IDIM])
    o_y = dout("o_y", [NBLK, 128, D])
    o_convp = dout("o_convp", [CW - 1, CONV_CH])
    o_ks = dout("o_ks", [128, NKV * HD])
    o_vs = dout("o_vs", [128, NKV * HD])
    o_iks = dout("o_iks", [128, IDIM])
    o_convs = dout("o_convs", [4, CW - 1, CONV_CH])

    QT_scr = dscr("QT_scr", [NBLK, 128, 8, 128], BF16)
    qiT_scr = dscr("qiT_scr", [NBLK, 64, 16, 2, 64], BF16)
    Wsel_scr = dscr("Wsel_scr", [NBLK, 128, 16, 128], BF16)
    mixT_scr = dscr("mixT_scr", [NBLK, 128, 16, 128], BF16)
    x1_scr = dscr("x1_scr", [NBLK, 128, D], F32)
    hT_scr = dscr("hT_scr", [NBLK, 128, 16, 128], BF16)

    w_in_r = w_in.rearrange("(k p) n -> p k n", p=128)
    w_out_r = w_out.rearrange("(k p) n -> p k n", p=128)
    w_gate_r = w_gate.rearrange("(k p) n -> p k n", p=128)
    w_up_r = w_up.rearrange("(k p) n -> p k n", p=128)
    w_down_r = w_down.rearrange("(k p) n -> p k n", p=128)

    with ExitStack() as st:
        s = S(nc, st)
        sb = lambda name, shape, dtype: st.enter_context(nc.sbuf_tensor(_un(name), shape, dtype))
        PS = st.enter_context(nc.psum_tensor("PS", [128, 8 * 512], F32))
        rb = [s.res("bank%d" % i) for i in range(8)]

        def bk(i, n=1):
            return PS[:, i * 512:(i + n) * 512]

        def bkb(i, n=1):
            return PS[:, i * 512:(i + n) * 512].bitcast(BF16)

        ident = sb("ident", [128, 128], BF16)
        ident32 = sb("ident32", [128, 128], F32)
        ones_bf = sb("ones_bf", [128, 128], BF16)
        ones32 = sb("ones32", [128, 128], F32)
        r_c = s.res("consts")
        s.dma("sp", lambda e: e.dma_start(out=ident[:], in_=identb_d[:, :]), writes=[r_c])
        s.dma("sp", lambda e: e.dma_start(out=ident32[:], in_=ident32_d[:, :]), writes=[r_c])
        s.op("dve", lambda e: e.memset(ones_bf[:], 1.0), writes=[r_c])
        s.op("dve", lambda e: e.memset(ones32[:], 1.0), writes=[r_c])

        def load_gain(tile, g_ap, r):
            s.dma("sp", lambda e: e.dma_start(out=tile[:], in_=g_ap.to_broadcast([128, D])), writes=[r])

        def rms_to_bf(P, x_ap, r_x, g_tile, r_g, out_ap, r_out, tmp):
            junk, r_junk, ssq, r_ss = tmp
            s.op("act", lambda e: e.activation(out=junk[0:P, 0:D], in_=x_ap, func=AF.Square, accum_out=ssq[0:P, :]),
                 reads=[r_x], writes=[r_junk, r_ss])
            s.op("dve", lambda e: e.tensor_scalar(out=ssq[0:P, :], in0=ssq[0:P, :], scalar1=1.0 / D, scalar2=EPS,
                                                  op0=ALU.mult, op1=ALU.add), writes=[r_ss])
            s.op("act", lambda e: e.activation(out=ssq[0:P, :], in_=ssq[0:P, :], func=AF.Sqrt), writes=[r_ss])
            s.op("dve", lambda e: e.reciprocal(out=ssq[0:P, :], in_=ssq[0:P, :]), writes=[r_ss])
            s.op("dve", lambda e: e.scalar_tensor_tensor(out=out_ap, in0=x_ap, scalar=ssq[0:P, 0:1], in1=g_tile[0:P, :],
                                                         op0=ALU.mult, op1=ALU.mult),
                 reads=[r_x, r_ss, r_g], writes=[r_out])

        if "U" in PH:
          with ExitStack() as pu:
            sbu = lambda name, shape, dtype: pu.enter_context(nc.sbuf_tensor(_un(name), shape, dtype))
            gmix = sbu("gmixU", [128, D], F32); r_gmix = s.res()
            load_gain(gmix, g_pre_mix, r_gmix)
            cvec = sbu("cvec", [128, 3, 8], F32); r_cvec = s.res()
            for vi, v_ap in enumerate((conv_b, ln_g, ln_b)):
                s.dma("sp", lambda e, vi=vi, v_ap=v_ap: e.dma_start(out=cvec[:, vi, :], in_=v_ap.rearrange("o (c p) -> p (o c)", p=128),
                                                                       allow_slow_non_contiguous=True), writes=[r_cvec])
            cvo = sbu("cvo", [32, CONV_CH], F32); r_cvo = s.res()
            cw_sb = cvo[0:CW, :]; r_cw = r_cvo
            s.dma("sp", lambda e: e.dma_start(out=cw_sb, in_=conv_w[:, :]), writes=[r_cw])
            cwT = sbu("cwT", [128, 8, CW], F32); r_cwT = s.res()
            for c in range(8):
                s.op("pe", lambda e, c=c: e.transpose(bk(0)[:, c * 32:c * 32 + CW], cw_sb[:, c * 128:(c + 1) * 128], ident32[0:CW, 0:CW]),
                     reads=[r_cw, r_c], writes=[rb[0]])
            s.op("dve", lambda e: e.tensor_copy(out=cwT[:], in_=bk(0)[:, 0:256].rearrange("p (c j) -> p c j", j=32)[:, :, 0:CW]),
                 writes=[rb[0], r_cwT])
            diag = sbu("diag", [128, 8, CW, 128], BF16); r_diag = s.res()
            for c in range(8):
                for j in range(CW):
                    s.op("pool", lambda e, c=c, j=j: e.tensor_scalar(out=diag[:, c, j, :], in0=ident[:], scalar1=cwT[:, c, j:j + 1],
                                                                     scalar2=None, op0=ALU.mult),
                         reads=[r_cwT, r_c], writes=[r_diag])
            wwi = sbu("wwi", [128, 16, 16], BF16); r_wwi = s.res()
            s.dma("pool", lambda e: e.dma_start(out=wwi[:], in_=w_in_r[:, :, 4672:4688]), writes=[r_wwi])
            wwirep = sbu("wwirep", [128, 16, 128], BF16); r_wrep = s.res()
            for h2 in range(2):
                for hp in range(8):
                    h = 2 * hp + h2
                    col = h2 * 64 + hp * 8
                    s.op("pool", lambda e, h=h, col=col: e.tensor_copy(
                        out=wwirep[:, :, col:col + 8], in_=wwi[:, :, h:h + 1].to_broadcast([128, 16, 8])),
                        reads=[r_wwi], writes=[r_wrep])
            maskg = sbu("maskg", [128, 16, 128], BF16); r_maskg = s.res()
            s.dma("sp", lambda e: e.dma_start(out=maskg[:], in_=maskg_d[:, :, :]), writes=[r_maskg])

            xt_l = [sbu("xtU%d" % i, [128, D], F32) for i in range(1)] * 2; r_xt_l = [s.res()] * 2
            xh_l = [sbu("xhU%d" % i, [32, D], F32) for i in range(1)] * 2; r_xh_l = [s.res()] * 2
            ssq = sbu("ssqU", [128, 1], F32); r_ssq = s.res()
            ssq2 = sbu("ssq2U", [128, 1], F32); r_ssq2 = s.res()
            xn_l = [sbu("xnU%d" % i, [128, D], BF16) for i in range(1)] * 2; r_xn_l = [s.res()] * 2
            xnh_l = [sbu("xnhU%d" % i, [32, D], BF16) for i in range(1)] * 2; r_xnh_l = [s.res()] * 2
            xnT = sbu("xnTU", [128, 16, SB, 160], BF16); r_xnT = s.res()
            Wt = [sbu("WtU%d" % i, [128, 16, 256], BF16) for i in range(3)]
            r_Wt = [s.res() for _ in range(3)]
            worder = []
            for c2 in range(4):
                worder += [c2 * 256, 1024 + c2 * 256]
            worder += [2048 + c2 * 256 for c2 in range(4)] + [3584 + c2 * 256 for c2 in range(4)]
            wstate = {"next": 0, "map": {}}

            def w_reset():
                wstate["next"] = 0
                wstate["map"] = {}

            def w_fill(upto):
                while wstate["next"] < min(upto, len(worder)):
                    base = worder[wstate["next"]]
                    i = wcnt[0] % 3
                    wcnt[0] += 1
                    s.dma("pool", lambda e: e.dma_start(out=Wt[i][:], in_=w_in_r[:, :, base:base + 256]), writes=[r_Wt[i]])
                    wstate["map"][base] = i
                    wstate["next"] += 1
            uT = sbu("uT", [128, 8, SB, 160], BF16); r_uT = s.res()
            sig = sbu("sig", [128, SB, 160], F32); r_sig = s.res()
            u32 = sbu("u32", [128, 8, 32], F32); r_u32 = s.res()
            Qst = sbu("Qst", [128, SB, 8, 128], BF16); r_Qst = s.res()
            Qi_sb = sbu("Qi_sb", [128, SB, 16, 8, 8], BF16); r_Qi = s.res()
            WIrep = sbu("WIrep", [128, SB, 128], F32); r_WIrep = s.res()
            ycv = sbu("ycv", [128, 8, 128], F32); r_ycv = s.res()
            ysq = sbu("ysq", [128, 8, 128], F32); r_ysq = s.res()
            Wsel = ysq[:].rearrange("p c t -> p (c t)").bitcast(BF16).rearrange("p (g t) -> p g t", t=128); r_Wsel = r_ysq
            stat = sbu("stat", [128, 3, 128], F32); r_stat = s.res()
            co = sbu("co", [128, 8, 128], BF16); r_co = s.res()
            stt = sbu("stt", [CW - 1, CONV_CH], F32); r_stt = s.res()
            fullT = sbu("fullT", [128, 8, 4, 38], BF16); r_fullT = s.res()

            wcnt = [0]

            def conv_tail(blk, TW):
                Y = bk(0, 2).rearrange("p (c t) -> p c t", t=128)
                s.op("dve", lambda e: e.tensor_tensor(out=ycv[:, :, 0:TW], in0=Y[:, :, 0:TW],
                                                      in1=cvec[:, 0, :].unsqueeze(2).to_broadcast([128, 8, TW]), op=ALU.add),
                     reads=[r_cvec], writes=[rb[0], rb[1], r_ycv])
                s.op("act", lambda e: e.activation(out=ysq[:, :, 0:TW], in_=ycv[:, :, 0:TW], func=AF.Square),
                     reads=[r_ycv], writes=[r_ysq])
                for c in range(8):
                    s.op("pe", lambda e, c=c: e.matmul(bk(2)[:, 0:TW], ones32[:], ycv[:, c, 0:TW], start=(c == 0), stop=(c == 7)),
                         reads=[r_ycv, r_c], writes=[rb[2]])
                for c in range(8):
                    s.op("pe", lambda e, c=c: e.matmul(bk(3)[:, 0:TW], ones32[:], ysq[:, c, 0:TW], start=(c == 0), stop=(c == 7)),
                         reads=[r_ysq, r_c], writes=[rb[3]])
                mean, msq, rstd = stat[:, 0, 0:TW], stat[:, 1, 0:TW], stat[:, 2, 0:TW]
                s.op("dve", lambda e: e.tensor_scalar(out=mean, in0=bk(2)[:, 0:TW], scalar1=1.0 / CONV_CH, scalar2=None, op0=ALU.mult),
                     writes=[rb[2], r_stat])
                s.op("dve", lambda e: e.tensor_tensor(out=msq, in0=mean, in1=mean, op=ALU.mult), writes=[r_stat])
                s.op("dve", lambda e: e.scalar_tensor_tensor(out=rstd, in0=bk(3)[:, 0:TW], scalar=1.0 / CONV_CH, in1=msq,
                                                             op0=ALU.mult, op1=ALU.subtract), writes=[rb[3], r_stat])
                s.op("dve", lambda e: e.tensor_scalar(out=rstd, in0=rstd, scalar1=EPS, scalar2=None, op0=ALU.add), writes=[r_stat])
                s.op("act", lambda e: e.activation(out=rstd, in_=rstd, func=AF.Sqrt), writes=[r_stat])
                s.op("dve", lambda e: e.reciprocal(out=rstd, in_=rstd), writes=[r_stat])
                s.op("dve", lambda e: e.tensor_tensor(out=ycv[:, :, 0:TW], in0=ycv[:, :, 0:TW],
                                                      in1=stat[:, 0:1, 0:TW].to_broadcast([128, 8, TW]), op=ALU.subtract),
                     reads=[r_stat], writes=[r_ycv])
                s.op("dve", lambda e: e.tensor_tensor(out=ycv[:, :, 0:TW], in0=ycv[:, :, 0:TW],
                                                      in1=stat[:, 2:3, 0:TW].to_broadcast([128, 8, TW]), op=ALU.mult),
                     reads=[r_stat], writes=[r_ycv])
                if TW < 128:
                    s.op("pool", lambda e: e.memset(co[:], 0.0), writes=[r_co])
                for c in range(8):
                    s.op("act", lambda e, c=c: e.activation(out=co[:, c, 0:TW], in_=ycv[:, c, 0:TW], func=AF.Silu,
                                                            scale=cvec[:, 1, c:c + 1], bias=cvec[:, 2, c:c + 1]),
                         reads=[r_ycv, r_cvec], writes=[r_co])
                s.dma("sp", lambda e: e.dma_start(out=mixT_scr[blk, :, 0:8, :], in_=co[:]), reads=[r_co])

            def u32_out(ncols, dst_fn):
                for c in range(8):
                    s.op("pe", lambda e, c=c: e.transpose(bk(2, 2)[0:ncols, c * 128:(c + 1) * 128], u32[:, c, 0:ncols], ident32[:]),
                         reads=[r_u32, r_c], writes=[rb[2], rb[3]])
                s.op("dve", lambda e: e.tensor_copy(out=cvo[0:ncols, :], in_=bk(2, 2)[0:ncols, :]), writes=[rb[2], rb[3], r_cvo])
                dst_fn()

            supers = [list(range(i, min(i + SB, NOWN))) for i in range(0, NOWN, SB)] + [[SBLK]]
            for blks in supers:
                nb = len(blks)
                is_s = blks[0] == SBLK
                w_reset()
                w_fill(2)
                for bi, blk in enumerate(blks):
                    src = xs if is_s else xo[blk]
                    xt, r_xt, xh, r_xh = xt_l[bi % 2], r_xt_l[bi % 2], xh_l[bi % 2], r_xh_l[bi % 2]
                    xn, r_xn, xnh, r_xnh = xn_l[bi % 2], r_xn_l[bi % 2], xnh_l[bi % 2], r_xnh_l[bi % 2]
                    s.dma("sp", lambda e: e.dma_start(out=xh[:], in_=src[0:32, :]), writes=[r_xh])
                    s.dma("sp", lambda e: e.dma_start(out=xt[:], in_=src[32:160, :]), writes=[r_xt])
                    rms_to_bf(32, xh[:], r_xh, gmix, r_gmix, xnh[:], r_xnh, (xnh, r_xnh, ssq2, r_ssq2))
                    rms_to_bf(128, xt[:], r_xt, gmix, r_gmix, xn[:], r_xn, (xn, r_xn, ssq, r_ssq))
                    tpm = bkb(0, 2).rearrange("p (k t) -> p k t", t=128)
                    tph = bkb(2).rearrange("p (k t) -> p k t", t=64)
                    for k in range(16):
                        s.op("pe", lambda e, k=k: e.transpose(tpm[:, k, :], xn[:, k * 128:(k + 1) * 128], ident[:]),
                             reads=[r_xn, r_c], writes=[rb[0], rb[1]])
                    for k in range(16):
                        s.op("pe", lambda e, k=k: e.transpose(tph[:, k, 0:32], xnh[:, k * 128:(k + 1) * 128], ident[0:32, 0:32]),
                             reads=[r_xnh, r_c], writes=[rb[2]])
                    s.op("act", lambda e: e.copy(out=xnT[:, :, bi, 32:160], in_=tpm[:, :, :]), writes=[rb[0], rb[1], r_xnT])
                    s.op("dve", lambda e: e.tensor_copy(out=xnT[:, :, bi, 0:32], in_=tph[:, :, 0:32]), writes=[rb[2], r_xnT])
                halves = [(0, min(2, nb))] + ([(2, nb)] if nb > 2 else [])

                def proj(ct_cols, lhs_fn, pair):
                    for hi, (b0, b1) in enumerate(halves):
                        n = (b1 - b0) * 160
                        for k in range(16):
                            s.op("pe", lambda e, k=k: e.matmul(bk(pair + hi)[:, 0:n], lhs_fn(k), xnT[:, k, b0:b1, :],
                                                               start=(k == 0), stop=(k == 15)),
                                 reads=[r_xnT] + ct_cols, writes=[rb[pair + hi]])

                def load_w(col0):
                    base = col0 - (col0 % 256)
                    idx = worder.index(base)
                    w_fill(idx + 2)
                    return wstate["map"][base], col0 - base

                def pview(pair, hi, b0, b1):
                    return bk(pair + hi)[:, 0:(b1 - b0) * 160].rearrange("p (b t) -> p b t", t=160)

                for c in range(8):
                    ia, oa = load_w(c * 128)
                    proj([r_Wt[ia]], lambda k, ia=ia, oa=oa: Wt[ia][:, k, oa:oa + 128], 4)
                    ig, og = load_w(1024 + c * 128)
                    proj([r_Wt[ig]], lambda k, ig=ig, og=og: Wt[ig][:, k, og:og + 128], 6)
                    for hi, (b0, b1) in enumerate(halves):
                        s.op("act", lambda e: e.activation(out=sig[:, b0:b1, :], in_=pview(6, hi, b0, b1), func=AF.Sigmoid),
                             writes=[rb[6 + hi], r_sig])
                        s.op("dve", lambda e: e.tensor_tensor(out=uT[:, c, b0:b1, :], in0=pview(4, hi, b0, b1), in1=sig[:, b0:b1, :], op=ALU.mult),
                             reads=[r_sig], writes=[rb[4 + hi], r_uT])
                        if is_s:
                            s.op("dve", lambda e: e.tensor_tensor(out=u32[:, c, :], in0=pview(4, hi, b0, b1)[:, 0, 32:64], in1=sig[:, 0, 32:64], op=ALU.mult),
                                 reads=[r_sig], writes=[rb[4 + hi], r_u32])
                        elif blks[-1] == NOWN - 1 and b1 == nb:
                            s.op("dve", lambda e: e.tensor_tensor(out=u32[:, c, 0:30], in0=pview(4, hi, b0, b1)[:, b1 - b0 - 1, 130:160],
                                                                  in1=sig[:, nb - 1, 130:160], op=ALU.mult),
                                 reads=[r_sig], writes=[rb[4 + hi], r_u32])
                for h in range(8):
                    iw, ow = load_w(2048 + h * 128)
                    pair = 4 + 2 * (h % 2)
                    proj([r_Wt[iw]], lambda k, iw=iw, ow=ow: Wt[iw][:, k, ow:ow + 128], pair)
                    for hi, (b0, b1) in enumerate(halves):
                        s.op("act", lambda e: e.activation(out=Qst[:, b0:b1, h, :], in_=pview(pair, hi, b0, b1)[:, :, 32:160],
                                                           func=AF.Copy, scale=ATTN_SCALE),
                             writes=[rb[pair + hi], r_Qst])
                for hp in range(8):
                    iw, ow = load_w(3584 + hp * 128)
                    pair = 4 + 2 * (hp % 2)
                    proj([r_Wt[iw]], lambda k, iw=iw, ow=ow: Wt[iw][:, k, ow:ow + 128], pair)
                    for hi, (b0, b1) in enumerate(halves):
                        for b in range(b0, b1):
                            s.op("dve", lambda e, b=b: e.tensor_copy(
                                out=Qi_sb[:, b, :, hp, :],
                                in_=pview(pair, hi, b0, b1)[:, b - b0, 32:160].rearrange("p (g t) -> p g t", t=8)),
                                writes=[rb[pair + hi], r_Qi])
                proj([r_wrep], lambda k: wwirep[:, k, :], 4)
                for hi, (b0, b1) in enumerate(halves):
                    s.op("act", lambda e: e.activation(out=WIrep[:, b0:b1, :], in_=pview(4, hi, b0, b1)[:, :, 32:160],
                                                       func=AF.Copy, scale=INDEX_SCALE),
                         writes=[rb[4 + hi], r_WIrep])
                for bi, blk in enumerate(blks):
                    s.dma("sp", lambda e: e.dma_start(out=QT_scr[blk], in_=Qst[:, bi, :, :]), reads=[r_Qst])
                    for h2 in range(2):
                        s.dma("sp", lambda e, h2=h2: e.dma_start(
                            out=qiT_scr[blk, :, :, h2, :],
                            in_=Qi_sb[h2 * 64:(h2 + 1) * 64, bi, :, :, :].rearrange("p g a b -> p g (a b)")),
                            reads=[r_Qi])
                    s.op("dve", lambda e: e.tensor_tensor(out=Wsel, in0=maskg[:],
                                                          in1=WIrep[:, bi:bi + 1, :].to_broadcast([128, 16, 128]), op=ALU.mult),
                         reads=[r_maskg, r_WIrep], writes=[r_Wsel])
                    s.dma("sp", lambda e: e.dma_start(out=Wsel_scr[blk], in_=Wsel), reads=[r_Wsel])
                    Y = bk(0, 2).rearrange("p (c t) -> p c t", t=128)
                    if not is_s:
                        for c in range(8):
                            for j in range(CW):
                                s.op("pe", lambda e, c=c, j=j: e.matmul(Y[:, c, :], diag[:, c, j, :], uT[:, c, bi, 2 + j:2 + j + 128],
                                                                        start=(j == 0), stop=(j == CW - 1)),
                                     reads=[r_diag, r_uT], writes=[rb[0], rb[1]])
                        conv_tail(blk, 128)
                    else:
                        for q in range(4):
                            s.dma("sp", lambda e, q=q: e.dma_start(out=stt[:], in_=state_conv[q]), writes=[r_stt])
                            tq = bk(2)[:, 0:256].rearrange("p (c j) -> p c j", j=32)
                            for c in range(8):
                                s.op("pe", lambda e, c=c: e.transpose(tq[:, c, 0:CW - 1], stt[:, c * 128:(c + 1) * 128], ident32[0:CW - 1, 0:CW - 1]),
                                     reads=[r_stt, r_c], writes=[rb[2]])
                            s.op("dve", lambda e, q=q: e.tensor_copy(out=fullT[:, :, q, 0:CW - 1], in_=tq[:, :, 0:CW - 1]),
                                 writes=[rb[2], r_fullT])
                            s.op("dve", lambda e, q=q: e.tensor_copy(out=fullT[:, :, q, CW - 1:CW + 7], in_=uT[:, :, 0, 32 + 8 * q:40 + 8 * q]),
                                 reads=[r_uT], writes=[r_fullT])
                            s.dma("sp", lambda e, q=q: e.dma_start(out=o_convs[q, 0:CW - 9, :], in_=state_conv[q, 8:CW - 1, :]))
                        for c in range(8):
                            for q in range(4):
                                for j in range(CW):
                                    s.op("pe", lambda e, c=c, q=q, j=j: e.matmul(Y[:, c, 8 * q:8 * q + 8], diag[:, c, j, :], fullT[:, c, q, j:j + 8],
                                                                                 start=(j == 0), stop=(j == CW - 1)),
                                         reads=[r_diag, r_fullT], writes=[rb[0], rb[1]])
                        conv_tail(blk, 32)
                        u32_out(32, lambda: [s.dma("sp", lambda e, q=q: e.dma_start(out=o_convs[q, CW - 9:CW - 1, :], in_=cvo[8 * q:8 * q + 8, :]),
                                                   reads=[r_cvo]) for q in range(4)])
                if (not is_s) and blks[-1] == NOWN - 1:
                    u32_out(30, lambda: s.dma("sp", lambda e: e.dma_start(out=o_convp[:, :], in_=cvo[0:30, :]), reads=[r_cvo]))
            s.barrier()

        def kv_env(stack, pfx):
            sba = lambda name, shape, dtype: stack.enter_context(nc.sbuf_tensor(_un(name), shape, dtype))
            gmix = sba(pfx + "gmixA", [128, D], F32); r_gmix = s.res()
            load_gain(gmix, g_pre_mix, r_gmix)
            Wkv = sba(pfx + "Wkv", [128, 16, 576], BF16)
            r_Wkv = s.res()
            for k4 in range(4):
                s.dma("pool", lambda e, k4=k4: e.dma_start(out=Wkv[:, 4 * k4:4 * k4 + 4, 0:512],
                                                            in_=w_in_r[:, 4 * k4:4 * k4 + 4, 3072:3584]), writes=[r_Wkv])
                s.dma("pool", lambda e, k4=k4: e.dma_start(out=Wkv[:, 4 * k4:4 * k4 + 4, 512:576],
                                                            in_=w_in_r[:, 4 * k4:4 * k4 + 4, 4608:4672]), writes=[r_Wkv])
            xt = [sba(pfx + "xt%d" % i, [128, D], F32) for i in range(2)]
            r_xt = [s.res() for _ in range(2)]
            junk = sba(pfx + "junkA", [128, D], BF16); r_junk = s.res()
            xn = [sba(pfx + "xn%d" % i, [128, D], BF16) for i in range(2)]
            r_xn = [s.res() for _ in range(2)]
            xnT = [sba(pfx + "xnT%d" % i, [128, 16, 128], BF16) for i in range(2)]
            r_xnT = [s.res() for _ in range(2)]
            ssq = [sba(pfx + "ssA%d" % i, [128, 1], F32) for i in range(2)]
            r_ss = [s.res() for _ in range(2)]
            kvf = [sba(pfx + "kvf%d" % i, [128, 576], F32) for i in range(2)]
            r_kvf = [s.res() for _ in range(2)]
            kb = [sba(pfx + "kb%d" % i, [128, 320], BF16) for i in range(2)]
            r_kb = [s.res() for _ in range(2)]
            tp = bkb(0, 2).rearrange("p (k t) -> p k t", t=128)
            tp2 = bkb(6).rearrange("p (k t) -> p k t", t=128)

            kvcnt = [0]

            def kv_tile(src, KT_dst, V_dst, kiT_dst, r_KT, r_V, r_kiT, dk, dv, dik):
                b = kvcnt[0] % 2; kvcnt[0] += 1
                pkv, pki = bk(2 + b), bk(4 + b)
                r_pkv = [rb[2 + b], rb[4 + b]]
                s.dma("sp", lambda e: e.dma_start(out=xt[b][:], in_=src), writes=[r_xt[b]])
                rms_to_bf(128, xt[b][:], r_xt[b], gmix, r_gmix, xn[b][:], r_xn[b], (junk, r_junk, ssq[b], r_ss[b]))
                for k in range(16):
                    s.op("pe", lambda e, k=k: e.transpose(tp[:, k, :], xn[b][:, k * 128:(k + 1) * 128], ident[:]),
                         reads=[r_xn[b], r_c], writes=[rb[0], rb[1]])
                s.op("act", lambda e: e.copy(out=xnT[b][:], in_=tp[:, :, :]), writes=[rb[0], rb[1], r_xnT[b]])
                for k in range(16):
                    s.op("pe", lambda e, k=k: e.matmul(pkv[:, :], xnT[b][:, k, :], Wkv[:, k, 0:512], start=(k == 0), stop=(k == 15)),
                         reads=[r_xnT[b], r_Wkv], writes=r_pkv)
                for k in range(16):
                    s.op("pe", lambda e, k=k: e.matmul(pki[:, 0:64], xnT[b][:, k, :], Wkv[:, k, 512:576], start=(k == 0), stop=(k == 15)),
                         reads=[r_xnT[b], r_Wkv], writes=r_pkv)
                s.op("dve", lambda e: e.tensor_copy(out=kvf[b][:, 0:512], in_=pkv[:, :]), writes=r_pkv + [r_kvf[b]])
                s.op("dve", lambda e: e.tensor_copy(out=kvf[b][:, 512:576], in_=pki[:, 0:64]), writes=r_pkv + [r_kvf[b]])
                s.op("act", lambda e: e.copy(out=kb[b][:, 0:256], in_=pkv[:, 0:256]), writes=r_pkv + [r_kb[b]])
                s.op("act", lambda e: e.copy(out=V_dst, in_=pkv[:, 256:512]), writes=r_pkv + [r_V])
                s.op("act", lambda e: e.copy(out=kb[b][:, 256:320], in_=pki[:, 0:64]), writes=r_pkv + [r_kb[b]])
                s.op("pe", lambda e: e.transpose(tp2[:, 0, :], kb[b][:, 0:128], ident[:]), reads=[r_kb[b], r_c], writes=[rb[6]])
                s.op("pe", lambda e: e.transpose(tp2[:, 1, :], kb[b][:, 128:256], ident[:]), reads=[r_kb[b], r_c], writes=[rb[6]])
                s.op("pe", lambda e: e.transpose(tp2[0:64, 2, :], kb[b][:, 256:320], ident[:]), reads=[r_kb[b], r_c], writes=[rb[6]])
                s.op("dve", lambda e: e.tensor_copy(out=KT_dst, in_=tp2[:, 0:2, :]), writes=[rb[6], r_KT])
                s.op("dve", lambda e: e.tensor_copy(out=kiT_dst, in_=tp2[0:64, 2, :]), writes=[rb[6], r_kiT])
                s.dma("sp", lambda e: e.dma_start(out=dk, in_=kvf[b][:, 0:256]), reads=[r_kvf[b]])
                s.dma("sp", lambda e: e.dma_start(out=dv, in_=kvf[b][:, 256:512]), reads=[r_kvf[b]])
                s.dma("sp", lambda e: e.dma_start(out=dik, in_=kvf[b][:, 512:576]), reads=[r_kvf[b]])

            return kv_tile

        def attn_env(stack, pfx, NSC):
            sbb = lambda name, shape, dtype: stack.enter_context(nc.sbuf_tensor(_un(name), shape, dtype))
            NS_ = 2 if pfx == "b_" else 1
            scores = [sbb(pfx + "score%d" % i, [128, NSC], F32) for i in range(NS_)]
            r_scores = [s.res() for _ in range(NS_)]
            cur = {"i": 0}
            wk = sbb(pfx + "wk", [128, NBIS], F32); r_wk = s.res()
            pw2 = sbb(pfx + "pw2", [128, NBIS], F32); r_pw2 = s.res()
            for it_ in range(NBIS):
                s.op("pool", lambda e, it_=it_: e.memset(pw2[:, it_:it_ + 1], 0.5 ** (it_ + 1)), writes=[r_pw2])
            cjunk = sbb(pfx + "cjunk", [128, NSC], U8); r_cjunk = s.res()
            cmask = sbb(pfx + "cmask", [128, 512], F32)
            cmask_s = sbb(pfx + "cmask_s", [128, 128], F32)
            identrep = sbb(pfx + "identrep", [128, 4, 128], BF16)
            selrep = sbb(pfx + "selrep", [32, 4, 32], BF16)
            iota_p = sbb(pfx + "iota_p", [128, 1], F32)
            r_bc = s.res()
            s.dma("sp", lambda e: e.dma_start(out=cmask[:], in_=cmask_d[:, :]), writes=[r_bc])
            s.dma("sp", lambda e: e.dma_start(out=cmask_s[:], in_=cmask_s_d[:, :]), writes=[r_bc])
            s.dma("sp", lambda e: e.dma_start(out=identrep[:], in_=identrep_d[:, :, :]), writes=[r_bc])
            s.dma("sp", lambda e: e.dma_start(out=selrep[:], in_=selrep_d[:, :, :]), writes=[r_bc])
            s.dma("sp", lambda e: e.dma_start(out=iota_p[:], in_=iota_d[:, :]), writes=[r_bc])
            QTb = sbb(pfx + "QTb", [128, 8, 128], BF16); r_QTb = s.res()
            qiTb = sbb(pfx + "qiTb", [64, 16, 128], BF16); r_qiTb = s.res()
            Wselb = sbb(pfx + "Wselb", [128, 16, 128], BF16); r_Wselb = s.res()
            Rt = [sbb(pfx + "Rt%d" % i, [128, 512], BF16) for i in range(4)]
            r_Rt = [s.res() for _ in range(4)]
            Mb = [sbb(pfx + "Mb%d" % i, [128, 512], BF16) for i in range(2)]
            r_Mb = [s.res() for _ in range(2)]
            Pt = [sbb(pfx + "Pt%d" % i, [128, 512], BF16) for i in range(3)]
            r_Pt = [s.res() for _ in range(3)]
            sms = [sbb(pfx + "smallB%d" % i, [128, 8], F32) for i in range(NS_)]
            r_sms = [s.res() for _ in range(NS_)]
            rsum = sbb(pfx + "rsum", [128, 2, 512], F32); r_rsum = s.res()
            aoT = sbb(pfx + "aoT", [128, 8, 128], BF16); r_aoT = s.res()
            cnt = {"r": 0, "m": 0, "p": 0, "l": 0}

            def load_q(blk):
                s.dma("sp", lambda e: e.dma_start(out=QTb[:], in_=QT_scr[blk]), writes=[r_QTb])

            def load_block(blk):
                s.dma("sp", lambda e: e.dma_start(out=qiTb[:], in_=qiT_scr[blk].rearrange("d g a b -> d g (a b)")), writes=[r_qiTb])
                s.dma("sp", lambda e: e.dma_start(out=Wselb[:], in_=Wsel_scr[blk]), writes=[r_Wselb])

            def idx_L(it):
                lb = cnt["l"] % 3; cnt["l"] += 1
                it["lb"] = lb
                ncol = it["ncol"]
                s.op("pe", lambda e: e.matmul(bk(lb)[:, 0:ncol], qiTb[:, it["g"], :], it["ki"], start=True, stop=True),
                     reads=[r_qiTb, it["r_ki"]], writes=[rb[lb]])

            def idx_R(it):
                lb, ncol, accumulate = it["lb"], it["ncol"], it["acc"]
                ri = cnt["r"] % 4; cnt["r"] += 1
                if ri % 2 == 0 or not accumulate:
                    s.op("act", lambda e: e.activation(out=Rt[ri][:, 0:ncol], in_=bk(lb)[:, 0:ncol], func=AF.Relu),
                         writes=[rb[lb], r_Rt[ri]])
                else:
                    s.op("dve", lambda e: e.tensor_scalar(out=Rt[ri][:, 0:ncol], in0=bk(lb)[:, 0:ncol], scalar1=0.0, scalar2=None, op0=ALU.max),
                         writes=[rb[lb], r_Rt[ri]])
                s.op("pe", lambda e: e.matmul(bk(3)[:, 0:ncol], Wselb[:, it["g"], :], Rt[ri][:, 0:ncol],
                                              start=it["first"], stop=it["last"]),
                     reads=[r_Wselb, r_Rt[ri]], writes=[rb[3]])
                if it["last"]:
                    score, r_score = scores[it["sc"]], r_scores[it["sc"]]
                    dst = score[:, it["dst"]:it["dst"] + ncol]
                    if accumulate:
                        s.op("dve", lambda e: e.tensor_tensor(out=dst, in0=bk(3)[:, 0:ncol], in1=dst, op=ALU.add), writes=[rb[3], r_score])
                    else:
                        s.op("act", lambda e: e.copy(out=dst, in_=bk(3)[:, 0:ncol]), writes=[rb[3], r_score])

            def indexer_run(items, look=2):
                for it in items[:look]:
                    idx_L(it)
                for n, it in enumerate(items):
                    idx_R(it)
                    if n + look < len(items):
                        idx_L(items[n + look])

            def idx_items(groups, ki_ap, r_ki, ncol, dst_col, accumulate):
                return [dict(g=g, ki=ki_ap, r_ki=r_ki, ncol=ncol, dst=dst_col, acc=accumulate, sc=cur["i"],
                             first=(gi == 0), last=(gi == len(groups) - 1)) for gi, g in enumerate(groups)]

            def indexer_tile(groups, ki_ap, r_ki, ncol, dst_col, mask_ap, accumulate):
                indexer_run(idx_items(groups, ki_ap, r_ki, ncol, dst_col, accumulate))

            def threshold(ncols, premask_cols=None):
                score, r_score = scores[cur["i"]], r_scores[cur["i"]]
                sm, r_sm = sms[cur["i"]], r_sms[cur["i"]]
                sc = score[:, 0:ncols]
                lo, hi, mid, cn, sel, d1 = (sm[:, i:i + 1] for i in range(6))
                s.op("dve", lambda e: e.tensor_reduce(out=hi, in_=sc, axis=AX.X, op=ALU.max), reads=[r_score], writes=[r_sm])
                s.op("dve", lambda e: e.tensor_reduce(out=lo, in_=sc, axis=AX.X, op=ALU.min), reads=[r_score], writes=[r_sm])
                if premask_cols is not None:
                    c0, c1, m_ap = premask_cols
                    s.op("dve", lambda e: e.tensor_tensor(out=score[:, c0:c1], in0=score[:, c0:c1], in1=m_ap, op=ALU.add),
                         reads=[r_bc], writes=[r_score])
                s.op("dve", lambda e: e.tensor_tensor(out=d1, in0=hi, in1=lo, op=ALU.subtract), writes=[r_sm])
                s.op("dve", lambda e: e.tensor_scalar(out=d1, in0=d1, scalar1=1.001, scalar2=1e-6, op0=ALU.mult, op1=ALU.add), writes=[r_sm])
                s.op("dve", lambda e: e.tensor_scalar(out=wk[:], in0=pw2[:], scalar1=d1, scalar2=None, op0=ALU.mult),
                     reads=[r_pw2], writes=[r_sm, r_wk])
                for it in range(NBIS):
                    s.op("dve", lambda e: e.tensor_tensor(out=mid, in0=lo, in1=wk[:, it:it + 1], op=ALU.add), reads=[r_wk], writes=[r_sm])
                    s.op("dve", lambda e: e.tensor_scalar(out=cjunk[:, 0:ncols], in0=sc, scalar1=mid, scalar2=None,
                                                          op0=ALU.is_ge, op1=ALU.add, accum_out=cn),
                         reads=[r_score], writes=[r_cjunk, r_sm])
                    s.op("dve", lambda e: e.scalar_tensor_tensor(out=sel, in0=cn, scalar=float(TOPK) - 0.5, in1=wk[:, it:it + 1],
                                                                 op0=ALU.is_ge, op1=ALU.mult), reads=[r_wk], writes=[r_sm])
                    s.op("dve", lambda e: e.tensor_tensor(out=lo, in0=lo, in1=sel, op=ALU.add), writes=[r_sm])

            def make_mb(c0, ncol):
                mi = cnt["m"] % 2; cnt["m"] += 1
                score, r_score = scores[cur["i"]], r_scores[cur["i"]]
                sm, r_sm = sms[cur["i"]], r_sms[cur["i"]]
                s.op("dve", lambda e: e.tensor_scalar(out=Mb[mi][:, 0:ncol], in0=score[:, c0:c0 + ncol], scalar1=sm[:, 0:1], scalar2=NEG,
                                                      op0=ALU.is_lt, op1=ALU.mult),
                     reads=[r_score, r_sm], writes=[r_Mb[mi]])
                return mi

            def att_S(it):
                pi = cnt["p"] % 3; cnt["p"] += 1
                sb_ = pi % 2
                it["pi"], it["sb"] = pi, sb_
                s.op("pe", lambda e: e.matmul(bk(sb_)[:, :], it["kt"], it["q"], start=True, stop=False),
                     reads=it["r_kv"] + [it["r_q"]], writes=[rb[sb_]])
                s.op("pe", lambda e: e.matmul(bk(sb_)[:, :], it["mb_l"], it["mb_r"], start=False, stop=True),
                     reads=[it["r_mb"], r_bc], writes=[rb[sb_]])
                s.op("act", lambda e: e.activation(out=Pt[pi][:, :], in_=bk(sb_)[:, :], func=AF.Exp), writes=[rb[sb_], r_Pt[pi]])

            def att_PV(it):
                pi, kvh = it["pi"], it["kvh"]
                s.op("pe", lambda e: e.matmul(bk(4 + kvh)[:, :], it["v"], Pt[pi][:, :], start=it["first"], stop=it["last"]),
                     reads=it["r_kv"] + [r_Pt[pi]], writes=[rb[4 + kvh]])
                s.op("pe", lambda e: e.matmul(bk(6 + kvh)[:, :], ones_bf[:, :], Pt[pi][:, :], start=it["first"], stop=it["last"]),
                     reads=[r_Pt[pi], r_c], writes=[rb[6 + kvh]])

            def attend_chunk(first, last, kp, kt_fn, v_fn, r_kv, q_fn, r_q, nq, mb_l, mb_r, r_mb):
                if nq == 512:
                    for kvh in range(2):
                        pi = cnt["p"] % 3; cnt["p"] += 1
                        sb_ = pi % 2
                        s.op("pe", lambda e: e.matmul(bk(sb_)[0:kp, :], kt_fn(kvh), q_fn(kvh), start=True, stop=False),
                             reads=r_kv + [r_q], writes=[rb[sb_]])
                        s.op("pe", lambda e: e.matmul(bk(sb_)[0:kp, :], mb_l, mb_r, start=False, stop=True),
                             reads=[r_mb, r_bc], writes=[rb[sb_]])
                        s.op("act", lambda e: e.activation(out=Pt[pi][0:kp, :], in_=bk(sb_)[0:kp, :], func=AF.Exp), writes=[rb[sb_], r_Pt[pi]])
                        s.op("pe", lambda e: e.matmul(bk(4 + kvh)[:, :], v_fn(kvh), Pt[pi][0:kp, :], start=first, stop=last),
                             reads=r_kv + [r_Pt[pi]], writes=[rb[4 + kvh]])
                        s.op("pe", lambda e: e.matmul(bk(6 + kvh)[:, :], ones_bf[0:kp, :], Pt[pi][0:kp, :], start=first, stop=last),
                             reads=[r_Pt[pi], r_c], writes=[rb[6 + kvh]])
                else:
                    pi = cnt["p"] % 3; cnt["p"] += 1
                    sb_ = pi % 2
                    for kvh in range(2):
                        s.op("pe", lambda e: e.matmul(bk(sb_)[0:kp, kvh * nq:(kvh + 1) * nq], kt_fn(kvh), q_fn(kvh), start=True, stop=False),
                             reads=r_kv + [r_q], writes=[rb[sb_]])
                        s.op("pe", lambda e: e.matmul(bk(sb_)[0:kp, kvh * nq:(kvh + 1) * nq], mb_l, mb_r, start=False, stop=True),
                             reads=[r_mb, r_bc], writes=[rb[sb_]])
                    s.op("act", lambda e: e.activation(out=Pt[pi][0:kp, 0:2 * nq], in_=bk(sb_)[0:kp, 0:2 * nq], func=AF.Exp),
                         writes=[rb[sb_], r_Pt[pi]])
                    for kvh in range(2):
                        s.op("pe", lambda e: e.matmul(bk(4 + kvh)[:, 0:nq], v_fn(kvh), Pt[pi][0:kp, kvh * nq:(kvh + 1) * nq],
                                                      start=first, stop=last),
                             reads=r_kv + [r_Pt[pi]], writes=[rb[4 + kvh]])
                    s.op("pe", lambda e: e.matmul(bk(6)[:, 0:2 * nq], ones_bf[0:kp, :], Pt[pi][0:kp, 0:2 * nq], start=first, stop=last),
                         reads=[r_Pt[pi], r_c], writes=[rb[6]])


            return dict(score=scores[0], r_score=r_scores[0], sm=sms[0], r_sm=r_sms[0], cur=cur, load_q=load_q, Pt=Pt, r_Pt=r_Pt, QTb=QTb, r_QTb=r_QTb, aoT=aoT, r_aoT=r_aoT, rsum=rsum, r_rsum=r_rsum,
                        Mb=Mb, r_Mb=r_Mb, cmask=cmask, cmask_s=cmask_s, identrep=identrep, selrep=selrep, iota_p=iota_p, r_bc=r_bc, cnt=cnt,
                        load_block=load_block, indexer_tile=indexer_tile, indexer_run=indexer_run, idx_items=idx_items, att_S=att_S, att_PV=att_PV, threshold=threshold, make_mb=make_mb, attend_chunk=attend_chunk)


        if "S" in PH:
            with ExitStack() as pss:
                sbs = lambda name, shape, dtype: pss.enter_context(nc.sbuf_tensor(_un(name), shape, dtype))
                NCS = NPG * 128 + 128
                env = attn_env(pss, "s_", NCS)
                score, sm, QTb, aoT, rsum, selrep, cmask_s = env["score"], env["sm"], env["QTb"], env["aoT"], env["rsum"], env["selrep"], env["cmask_s"]
                r_score, r_sm, r_QTb, r_aoT, r_rsum, r_bc = env["r_score"], env["r_sm"], env["r_QTb"], env["r_aoT"], env["r_rsum"], env["r_bc"]
                KTn = sbs("KTn", [128, 2, 128], BF16)
                Vn = sbs("Vn", [128, 256], BF16)
                kiTn = sbs("kiTn", [64, 128], BF16)
                r_kvn = s.res()
                with ExitStack() as pkn:
                    kvt = kv_env(pkn, "s_")
                    kvt(xs[32:160, :], KTn[:], Vn[:], kiTn[:], r_kvn, r_kvn, r_kvn, o_ks[:, :], o_vs[:, :], o_iks[:, :])
                    s.barrier()
                env["load_block"](SBLK)
                env["load_q"](SBLK)
                ptab_sb = sbs("ptab_sb", [NPG, 4], I32); r_pt = s.res()
                s.dma("sp", lambda e: e.dma_start(out=ptab_sb[:], in_=ptab.rearrange("q p -> p q"), allow_slow_non_contiguous=True), writes=[r_pt])
                RG = 16
                NJ = 128 // RG
                ptf = sbs("ptf", [NPG, 4], F32)
                idxf = sbs("idxf", [NPG, 4, NJ], F32)
                idxK = sbs("idxK", [NPG, 4, NJ], I32); r_idxK = s.res()
                s.op("dve", lambda e: e.tensor_copy(out=ptf[:], in_=ptab_sb[:]), reads=[r_pt], writes=[r_idxK])
                for jj in range(NJ):
                    s.op("dve", lambda e, jj=jj: e.tensor_scalar(out=idxf[:, :, jj], in0=ptf[:], scalar1=float(NJ), scalar2=float(jj),
                                                                op0=ALU.mult, op1=ALU.add), writes=[r_idxK])
                s.op("dve", lambda e: e.tensor_copy(out=idxK[:], in_=idxf[:]), writes=[r_idxK])
                with ExitStack() as psi:
                    sbi = lambda name, shape, dtype: psi.enter_context(nc.sbuf_tensor(_un(name), shape, dtype))
                    idxpg = sbi("idxpg", [NPG, PAGE * IDIM], F32); r_idxpg = s.res()
                    kiTs = sbi("kiTs", [64, NPG * 128], BF16); r_kiTs = s.res()
                    kvw = kiTs[:].rearrange("d (pg r) -> d pg r", r=128)
                    s.op("pool", lambda e: e.memset(score[:, 0:NCS], 0.0), writes=[r_score])
                    for q in range(4):
                        s.dma("pool", lambda e, q=q: e.indirect_dma_start(
                            out=idxpg[:], out_offset=None, in_=cache_ik[:, :],
                            in_offset=bass.IndirectOffsetOnAxis(ap=ptab_sb[:, q:q + 1], axis=0)), reads=[r_pt], writes=[r_idxpg])
                        for r0 in range(0, 128, 4):
                            bank = 4 + (r0 // 4) % 2
                            tq = bk(bank)[0:64, 0:4 * NPG].rearrange("d (r pg) -> d r pg", pg=NPG)
                            for rr in range(4):
                                s.op("pe", lambda e, rr=rr: e.transpose(tq[:, rr, :], idxpg[:, (r0 + rr) * 64:(r0 + rr + 1) * 64], ident32[0:NPG, 0:NPG]),
                                     reads=[r_idxpg, r_c], writes=[rb[bank]])
                            eng = "act" if (r0 // 4) % 2 == 0 else "dve"
                            if eng == "act":
                                s.op("act", lambda e: e.copy(out=kvw[:, :, r0:r0 + 4].rearrange("d pg r -> d r pg"), in_=tq), writes=[rb[bank], r_kiTs])
                            else:
                                s.op("dve", lambda e: e.tensor_copy(out=kvw[:, :, r0:r0 + 4].rearrange("d pg r -> d r pg"), in_=tq), writes=[rb[bank], r_kiTs])
                        items = []
                        for kc in range(NPG * 128 // 512):
                            items += env["idx_items"]([q], kiTs[:, kc * 512:(kc + 1) * 512], r_kiTs, 512, kc * 512, True)
                        items += env["idx_items"]([q], kiTn[:, :], r_kvn, 128, NPG * 128, True)
                        env["indexer_run"](items)
                    s.barrier()
                env["threshold"](NCS, (NPG * 128, NCS, cmask_s[:]))
                with ExitStack() as psa:
                    sbt = lambda name, shape, dtype: psa.enter_context(nc.sbuf_tensor(_un(name), shape, dtype))
                    Kg = [sbt("Kg%d" % i, [NPG, RG * 256], F32) for i in range(2)]
                    Vg = [sbt("Vg%d" % i, [NPG, RG * 256], F32) for i in range(2)]
                    r_Kg = [s.res() for _ in range(2)]
                    r_Vg = [s.res() for _ in range(2)]
                    KTc = [sbt("KTc%d" % i, [128, 4, 2, NPG], BF16) for i in range(3)]
                    Vc = [sbt("Vc%d" % i, [NPG, 4, 256], BF16) for i in range(3)]
                    Mbc = [sbt("Mbc%d" % i, [32, 4, 128], BF16) for i in range(3)]
                    r_KTc = [s.res() for _ in range(3)]
                    r_Vc = [s.res() for _ in range(3)]
                    r_Mbc = [s.res() for _ in range(3)]
                    QTq = sbt("QTq", [128, 8, 8], BF16); r_QTq = s.res()
                    ck = cache_k.rearrange("(n r) d -> n (r d)", r=RG)
                    cv = cache_v.rearrange("(n r) d -> n (r d)", r=RG)
                    s.op("pool", lambda e: e.memset(aoT[:], 0.0), writes=[r_aoT])
                    cc = 0
                    for q in range(4):
                        s.op("dve", lambda e: e.tensor_copy(out=QTq[:], in_=QTb[:, :, 8 * q:8 * q + 8]), reads=[r_QTb], writes=[r_QTq])
                        qf = lambda kvh: QTq[:, 4 * kvh:4 * kvh + 4, :]
                        batches = []
                        gdone = set()

                        def gather(jj):
                            if jj in gdone or jj >= NJ:
                                return
                            gdone.add(jj)
                            gi = (q * NJ + jj) % 2
                            s.dma("pool", lambda e: e.indirect_dma_start(
                                out=Kg[gi][:], out_offset=None, in_=ck[:, :],
                                in_offset=bass.IndirectOffsetOnAxis(ap=idxK[:, q, jj:jj + 1], axis=0)), reads=[r_idxK], writes=[r_Kg[gi]])
                            s.dma("pool", lambda e: e.indirect_dma_start(
                                out=Vg[gi][:], out_offset=None, in_=cv[:, :],
                                in_offset=bass.IndirectOffsetOnAxis(ap=idxK[:, q, jj:jj + 1], axis=0)), reads=[r_idxK], writes=[r_Vg[gi]])

                        for jj in range(NJ):
                            gi = (q * NJ + jj) % 2
                            for bb in range(RG // 4):
                                batches.append(dict(gi=gi, bb=bb, jj=jj, r0=jj * RG + 4 * bb))

                        def stA(bt):
                            gi, bb, r0 = bt["gi"], bt["bb"], bt["r0"]
                            gather(bt["jj"])
                            if bb == RG // 4 - 1:
                                pass
                            ci = bt["ci"]
                            tk = bk(2, 2)[:, 0:8 * NPG].rearrange("p (r a b) -> p r a b", a=2, b=NPG)
                            for rl in range(4):
                                for kvh in range(2):
                                    c0 = (4 * bb + rl) * 256 + kvh * 128
                                    s.op("pe", lambda e, rl=rl, kvh=kvh, c0=c0: e.transpose(tk[:, rl, kvh, :], Kg[gi][:, c0:c0 + 128], ident32[0:NPG, 0:NPG]),
                                         reads=[r_Kg[gi], r_c], writes=[rb[2], rb[3]])
                            s.op("act", lambda e: e.copy(out=KTc[ci][:], in_=tk), writes=[rb[2], rb[3], r_KTc[ci]])
                            s.op("pool", lambda e: e.tensor_copy(out=Vc[ci][:], in_=Vg[gi][:, 4 * bb * 256:(4 * bb + 4) * 256]),
                                 reads=[r_Vg[gi]], writes=[r_Vc[ci]])
                            s.op("dve", lambda e: e.tensor_scalar(
                                out=Mbc[ci][:, :, 0:NPG], in0=score[0:32, 0:NPG * 128].rearrange("p (pg r) -> p r pg", r=128)[:, r0:r0 + 4, :],
                                scalar1=sm[0:32, 0:1], scalar2=NEG, op0=ALU.is_lt, op1=ALU.mult), reads=[r_score, r_sm], writes=[r_Mbc[ci]])

                        def stB(bt):
                            ci = bt["ci"]
                            pi = env["cnt"]["p"] % 3; env["cnt"]["p"] += 1
                            sb_ = pi % 2
                            bt["pi"] = pi
                            Pt, r_Pt = env["Pt"], env["r_Pt"]
                            for rl in range(4):
                                for kvh in range(2):
                                    oc = rl * 64 + kvh * 32
                                    s.op("pe", lambda e, rl=rl, kvh=kvh, oc=oc: e.matmul(bk(sb_)[0:NPG, oc:oc + 32], KTc[ci][:, rl, kvh, :], qf(kvh),
                                                                                          start=True, stop=False),
                                         reads=[r_KTc[ci], r_QTq], writes=[rb[sb_]])
                                    s.op("pe", lambda e, rl=rl, kvh=kvh, oc=oc: e.matmul(bk(sb_)[0:NPG, oc:oc + 32], Mbc[ci][:, rl, 0:NPG], selrep[:, q, :],
                                                                                          start=False, stop=True),
                                         reads=[r_Mbc[ci], r_bc], writes=[rb[sb_]])
                            s.op("act", lambda e: e.activation(out=Pt[pi][0:NPG, 0:256], in_=bk(sb_)[0:NPG, 0:256], func=AF.Exp),
                                 writes=[rb[sb_], r_Pt[pi]])

                        def stC(bt):
                            ci, pi, r0 = bt["ci"], bt["pi"], bt["r0"]
                            Pt, r_Pt = env["Pt"], env["r_Pt"]
                            for rl in range(4):
                                first = (r0 + rl == 0)
                                for kvh in range(2):
                                    oc = rl * 64 + kvh * 32
                                    s.op("pe", lambda e, rl=rl, kvh=kvh, oc=oc: e.matmul(bk(4 + kvh)[:, 0:32], Vc[ci][:, rl, kvh * 128:(kvh + 1) * 128],
                                                                                          Pt[pi][0:NPG, oc:oc + 32], start=first, stop=False),
                                         reads=[r_Vc[ci], r_Pt[pi]], writes=[rb[4 + kvh]])
                                s.op("pe", lambda e, rl=rl: e.matmul(bk(6)[:, 0:64], ones_bf[0:NPG, :], Pt[pi][0:NPG, rl * 64:rl * 64 + 64],
                                                                     start=first, stop=False),
                                     reads=[r_Pt[pi], r_c], writes=[rb[6]])

                        for bt in batches:
                            bt["ci"] = cc % 3; cc += 1
                        gather(0); gather(1)
                        stA(batches[0]); stB(batches[0])
                        for n, bt in enumerate(batches):
                            if n + 1 < len(batches):
                                stA(batches[n + 1]); stB(batches[n + 1])
                            stC(bt)
                            if bt["bb"] == RG // 4 - 1:
                                gather(bt["jj"] + 2)
                        ci = cc % 3; cc += 1
                        s.op("dve", lambda e: e.tensor_scalar(out=Mbc[ci][:, 0, :], in0=score[0:32, NPG * 128:NPG * 128 + 128], scalar1=sm[0:32, 0:1], scalar2=NEG,
                                                              op0=ALU.is_lt, op1=ALU.mult), reads=[r_score, r_sm], writes=[r_Mbc[ci]])
                        env["attend_chunk"](False, True, 128, lambda kvh: KTn[:, kvh, :], lambda kvh: Vn[:, kvh * 128:(kvh + 1) * 128],
                                            [r_kvn], qf, r_QTq, 32, Mbc[ci][:, 0, :], selrep[:, q, :], r_Mbc[ci])
                        s.op("dve", lambda e: e.reciprocal(out=rsum[:, 0, 0:64], in_=bk(6)[:, 0:64]), writes=[rb[6], r_rsum])
                        for kvh in range(2):
                            s.op("dve", lambda e, kvh=kvh: e.tensor_tensor(out=aoT[:, 4 * kvh:4 * kvh + 4, 8 * q:8 * q + 8],
                                                                           in0=bk(4 + kvh)[:, 0:32].rearrange("p (h t) -> p h t", t=8),
                                                                           in1=rsum[:, 0, 32 * kvh:32 * kvh + 32].rearrange("p (h t) -> p h t", t=8), op=ALU.mult),
                                 reads=[r_rsum], writes=[rb[4 + kvh], r_aoT])
                    s.dma("sp", lambda e: e.dma_start(out=mixT_scr[SBLK, :, 8:16, :], in_=aoT[:]), reads=[r_aoT])
                    s.barrier()


        with ExitStack() as pkv_stack:
            sbk = lambda name, shape, dtype: pkv_stack.enter_context(nc.sbuf_tensor(_un(name), shape, dtype))
            if "A" in PH or "B" in PH:
                KT = sbk("KT", [128, NKV, NT * 128], BF16)
                Vr = sbk("Vr", [128, NT, NKV * HD], BF16)
                kiT = sbk("kiT", [64, NT * 128], BF16)
                r_KT, r_V, r_kiT = s.res(), s.res(), s.res()
            if "A" in PH:
                with ExitStack() as pa:
                    kvt = kv_env(pa, "a_")
                    for n in range(NT):
                        kvt(xb[n * 128:(n + 1) * 128, :], KT[:, :, n * 128:(n + 1) * 128], Vr[:, n, :], kiT[:, n * 128:(n + 1) * 128],
                            r_KT, r_V, r_kiT, o_k[n * 128:(n + 1) * 128, :], o_v[n * 128:(n + 1) * 128, :], o_ik[n * 128:(n + 1) * 128, :])
                    s.barrier()
            if "B" in PH:
                with ExitStack() as pb:
                    env = attn_env(pb, "b_", NT * 128)
                    score, sm, QTb, aoT, rsum, cmask, identrep, Mb = env["score"], env["sm"], env["QTb"], env["aoT"], env["rsum"], env["cmask"], env["identrep"], env["Mb"]
                    r_score, r_sm, r_QTb, r_aoT, r_rsum, r_bc, r_Mb = env["r_score"], env["r_sm"], env["r_QTb"], env["r_aoT"], env["r_rsum"], env["r_bc"], env["r_Mb"]
                    load_block, indexer_tile, threshold, make_mb, attend_chunk = env["load_block"], env["indexer_tile"], env["threshold"], env["make_mb"], env["attend_chunk"]
                    cur, load_q = env["cur"], env["load_q"]

                    def do_indexer(i):
                        cur["i"] = i % 2
                        load_block(i)
                        items = []
                        for kc in range(i + 1):
                            items += env["idx_items"](list(range(16)), kiT[:, kc * 512:(kc + 1) * 512], r_kiT, 512, kc * 512, False)
                        env["indexer_run"](items)

                    def do_thr(i):
                        cur["i"] = i % 2
                        ncols = (i + 1) * 512
                        threshold(ncols, (ncols - 512, ncols, cmask[:]))

                    def do_attn(i):
                        cur["i"] = i % 2
                        load_q(i)
                        nch = 4 * (i + 1)
                        items = []
                        for c in range(nch):
                            for kvh in range(2):
                                items.append(dict(c=c, kvh=kvh, first=(c == 0), last=(c == nch - 1),
                                                  kt=KT[:, kvh, c * 128:(c + 1) * 128], v=Vr[:, c, kvh * 128:(kvh + 1) * 128],
                                                  r_kv=[r_KT, r_V], q=QTb[:, 4 * kvh:4 * kvh + 4, :], r_q=r_QTb, mb_r=identrep[:]))
                        mstate = {}

                        def prep(it):
                            if it["c"] % 4 == 0 and it["kvh"] == 0:
                                mstate["mi"] = make_mb(it["c"] * 128, 512)
                            mi = mstate["mi"]
                            it["mb_l"] = Mb[mi][:, (it["c"] % 4) * 128:(it["c"] % 4 + 1) * 128]
                            it["r_mb"] = r_Mb[mi]
                            env["att_S"](it)

                        prep(items[0])
                        for n, it in enumerate(items):
                            if n + 1 < len(items):
                                prep(items[n + 1])
                            env["att_PV"](it)
                        s.op("dve", lambda e: e.reciprocal(out=rsum[:], in_=bk(6, 2).rearrange("p (a b) -> p a b", b=512)),
                             writes=[rb[6], rb[7], r_rsum])
                        s.op("dve", lambda e: e.tensor_tensor(out=aoT[:], in0=bk(4, 2).rearrange("p (h t) -> p h t", t=128),
                                                              in1=rsum[:].rearrange("p a (h t) -> p (a h) t", t=128), op=ALU.mult),
                             reads=[r_rsum], writes=[rb[4], rb[5], r_aoT])
                        s.dma("sp", lambda e: e.dma_start(out=mixT_scr[i, :, 8:16, :], in_=aoT[:]), reads=[r_aoT])

                    do_indexer(0)
                    for i in range(NOWN):
                        if i + 1 < NOWN:
                            do_indexer(i + 1)
                        do_thr(i)
                        do_attn(i)

                    s.barrier()


        if "C" in PH:
            supers = [list(range(i, min(i + SB, NOWN))) for i in range(0, NOWN, SB)] + [[SBLK]]
            with ExitStack() as pc1:
                sbc = lambda name, shape, dtype: pc1.enter_context(nc.sbuf_tensor(_un(name), shape, dtype))
                gpm = sbc("gpm", [128, D], F32); r_gpm = s.res(); load_gain(gpm, g_post_mix, r_gpm)
                gpf = sbc("gpf", [128, D], F32); r_gpf = s.res(); load_gain(gpf, g_pre_ffn, r_gpf)
                mixT = sbc("mixT", [128, 16, SB, 128], BF16); r_mixT = s.res()
                Wo = [sbc("Wo%d" % i, [128, 16, 512], BF16) for i in range(2)]
                r_Wo = [s.res() for _ in range(2)]
                mixed = sbc("mixed", [128, SB, D], F32); r_mixed = s.res()
                xt_l = [sbc("xtC%d" % i, [128, D], F32) for i in range(2)]; r_xt_l = [s.res() for _ in range(2)]
                tmp_l = [sbc("tmpC%d" % i, [128, D], F32) for i in range(2)]; r_tmp_l = [s.res() for _ in range(2)]
                hbf_l = [sbc("hbf%d" % i, [128, D], BF16) for i in range(2)]; r_hbf_l = [s.res() for _ in range(2)]
                hT_l = [sbc("hTC%d" % i, [128, 16, 128], BF16) for i in range(2)]; r_hT_l = [s.res() for _ in range(2)]
                ssq_l = [sbc("ssqC%d" % i, [128, 1], F32) for i in range(4)]; r_ssq_l = [s.res() for _ in range(4)]
                wc = 0
                mc = 0
                for blks in supers:
                    nb = len(blks)
                    is_s = blks[0] == SBLK
                    for bi, blk in enumerate(blks):
                        s.dma("sp", lambda e: e.dma_start(out=mixT[:, :, bi, :], in_=mixT_scr[blk]), writes=[r_mixT])
                    for nt in range(4):
                        wi_ = wc % 2; wc += 1
                        for k4 in range(4):
                            s.dma("pool", lambda e, k4=k4: e.dma_start(out=Wo[wi_][:, 4 * k4:4 * k4 + 4, :], in_=w_out_r[:, 4 * k4:4 * k4 + 4, nt * 512:(nt + 1) * 512]),
                                  writes=[r_Wo[wi_]])
                        for bi in range(nb):
                            pbk = mc % 4; mc += 1
                            for k in range(16):
                                s.op("pe", lambda e, k=k: e.matmul(bk(pbk)[:, :], mixT[:, k, bi, :], Wo[wi_][:, k, :], start=(k == 0), stop=(k == 15)),
                                     reads=[r_mixT, r_Wo[wi_]], writes=[rb[pbk]])
                            s.op("act", lambda e: e.copy(out=mixed[:, bi, nt * 512:(nt + 1) * 512], in_=bk(pbk)[:, :]), writes=[rb[pbk], r_mixed])
                    for bi, blk in enumerate(blks):
                        src = xs[32:160, :] if is_s else xo[blk, 32:160, :]
                        b2 = bi % 2
                        xt, r_xt, tmp, r_tmp = xt_l[b2], r_xt_l[b2], tmp_l[b2], r_tmp_l[b2]
                        hbf, r_hbf, hT, r_hT = hbf_l[b2], r_hbf_l[b2], hT_l[b2], r_hT_l[b2]
                        s.dma("sp", lambda e: e.dma_start(out=xt[:], in_=src), writes=[r_xt])
                        rms_to_bf(128, mixed[:, bi, :], r_mixed, gpm, r_gpm, tmp[:], r_tmp, (hbf, r_hbf, ssq_l[b2], r_ssq_l[b2]))
                        s.op("pool", lambda e: e.tensor_tensor(out=xt[:], in0=xt[:], in1=tmp[:], op=ALU.add), reads=[r_tmp], writes=[r_xt])
                        s.dma("sp", lambda e: e.dma_start(out=x1_scr[blk], in_=xt[:]), reads=[r_xt])
                        rms_to_bf(128, xt[:], r_xt, gpf, r_gpf, hbf[:], r_hbf, (hbf, r_hbf, ssq_l[2 + b2], r_ssq_l[2 + b2]))
                        tp = bkb(4, 2).rearrange("p (k t) -> p k t", t=128)
                        for k in range(16):
                            s.op("pe", lambda e, k=k: e.transpose(tp[:, k, :], hbf[:, k * 128:(k + 1) * 128], ident[:]),
                                 reads=[r_hbf, r_c], writes=[rb[4], rb[5]])
                        s.op("act", lambda e: e.copy(out=hT[:], in_=tp[:, :, :]), writes=[rb[4], rb[5], r_hT])
                        s.dma("sp", lambda e: e.dma_start(out=hT_scr[blk], in_=hT[:]), reads=[r_hT])
                s.barrier()
            with ExitStack() as pc2:
                sbc = lambda name, shape, dtype: pc2.enter_context(nc.sbuf_tensor(_un(name), shape, dtype))
                gff = sbc("gff", [128, D], F32); r_gff = s.res(); load_gain(gff, g_post_ffn, r_gff)
                hTs = sbc("hTs", [128, 16, SB, 128], BF16); r_hTs = s.res()
                aT = sbc("aT", [128, NFT, SB * 128], BF16); r_aT = s.res()
                Wg = [sbc("Wg%d" % i, [128, 16, 256], BF16) for i in range(2)]
                Wu = [sbc("Wu%d" % i, [128, 16, 256], BF16) for i in range(2)]
                r_Wg = [s.res() for _ in range(2)]
                r_Wu = [s.res() for _ in range(2)]
                Wd = [sbc("Wd%d" % i, [128, NFT, 256], BF16) for i in range(2)]
                r_Wd = [s.res() for _ in range(2)]
                wdc = 0
                sg = [sbc("sg%d" % i, [128, SB * 128], F32) for i in range(2)]
                r_sg = [s.res() for _ in range(2)]
                fo = sbc("fo", [128, SB, D], F32); r_fo = s.res()
                x1t_l = [sbc("x1t%d" % i, [128, D], F32) for i in range(2)]; r_x1t_l = [s.res() for _ in range(2)]
                tmp_l = [sbc("tmpC2%d" % i, [128, D], F32) for i in range(1)] * 2; r_tmp_l = [s.res()] * 2
                ssq_l = [sbc("ssqC2%d" % i, [128, 1], F32) for i in range(2)]; r_ssq_l = [s.res() for _ in range(2)]
                dc = 0
                for blks in supers:
                    nb = len(blks)
                    N = nb * 128
                    for bi, blk in enumerate(blks):
                        s.dma("sp", lambda e: e.dma_start(out=hTs[:, :, bi, :], in_=hT_scr[blk]), writes=[r_hTs])
                    for ft in range(NFT):
                        wi_ = (ft // 2) % 2
                        fo_ = ft % 2
                        if fo_ == 0:
                            s.dma("pool", lambda e: e.dma_start(out=Wg[wi_][:], in_=w_gate_r[:, :, ft * 128:(ft + 2) * 128]), writes=[r_Wg[wi_]])
                            s.dma("pool", lambda e: e.dma_start(out=Wu[wi_][:], in_=w_up_r[:, :, ft * 128:(ft + 2) * 128]), writes=[r_Wu[wi_]])
                        gb, ub = 2 * (ft % 2), 2 * (ft % 2) + 1
                        for k in range(16):
                            s.op("pe", lambda e, k=k: e.matmul(bk(gb)[:, 0:N], Wg[wi_][:, k, fo_ * 128:(fo_ + 1) * 128], hTs[:, k, 0:nb, :], start=(k == 0), stop=(k == 15)),
                                 reads=[r_hTs, r_Wg[wi_]], writes=[rb[gb]])
                        for k in range(16):
                            s.op("pe", lambda e, k=k: e.matmul(bk(ub)[:, 0:N], Wu[wi_][:, k, fo_ * 128:(fo_ + 1) * 128], hTs[:, k, 0:nb, :], start=(k == 0), stop=(k == 15)),
                                 reads=[r_hTs, r_Wu[wi_]], writes=[rb[ub]])
                        s.op("act", lambda e: e.activation(out=sg[fo_][:, 0:N], in_=bk(gb)[:, 0:N], func=AF.Silu), writes=[rb[gb], r_sg[fo_]])
                        s.op("dve", lambda e: e.tensor_tensor(out=aT[:, ft, 0:N], in0=bk(ub)[:, 0:N], in1=sg[fo_][:, 0:N], op=ALU.mult),
                             reads=[r_sg[fo_]], writes=[rb[ub], r_aT])
                    for nt in range(8):
                        wd_ = wdc % 2; wdc += 1
                        for f4 in range(4):
                            s.dma("pool", lambda e, f4=f4: e.dma_start(out=Wd[wd_][:, 11 * f4:11 * f4 + 11, :], in_=w_down_r[:, 11 * f4:11 * f4 + 11, nt * 256:(nt + 1) * 256]),
                                  writes=[r_Wd[wd_]])
                        for bi in range(nb):
                            pbk = 4 + dc % 4; dc += 1
                            for ft in range(NFT):
                                s.op("pe", lambda e, ft=ft: e.matmul(bk(pbk)[:, 0:256], aT[:, ft, bi * 128:(bi + 1) * 128], Wd[wd_][:, ft, :], start=(ft == 0), stop=(ft == NFT - 1)),
                                     reads=[r_aT, r_Wd[wd_]], writes=[rb[pbk]])
                            s.op("act", lambda e: e.copy(out=fo[:, bi, nt * 256:(nt + 1) * 256], in_=bk(pbk)[:, 0:256]), writes=[rb[pbk], r_fo])
                    for bi, blk in enumerate(blks):
                        b2 = bi % 2
                        x1t, r_x1t, tmp, r_tmp, ssq, r_ssq = x1t_l[b2], r_x1t_l[b2], tmp_l[b2], r_tmp_l[b2], ssq_l[b2], r_ssq_l[b2]
                        s.dma("sp", lambda e: e.dma_start(out=x1t[:], in_=x1_scr[blk]), writes=[r_x1t])
                        rms_to_bf(128, fo[:, bi, :], r_fo, gff, r_gff, tmp[:], r_tmp, (tmp[:].bitcast(BF16), r_tmp, ssq, r_ssq))
                        s.op("pool", lambda e: e.tensor_tensor(out=x1t[:], in0=x1t[:], in1=tmp[:], op=ALU.add), reads=[r_tmp], writes=[r_x1t])
                        s.dma("sp", lambda e: e.dma_start(out=o_y[blk], in_=x1t[:]), reads=[r_x1t])
                s.barrier()
        s.finish()

    return nc


def _consts(j):
    c = {}
    c["identb"] = _bf(np.eye(128, dtype=np.float32))
    c["ident32"] = np.eye(128, dtype=np.float32)
    r = np.arange(128)[:, None]
    sp = np.arange(512)[None, :]
    c["cmask"] = np.where(sp <= 128 * j + r, 0.0, -1e30).astype(np.float32)
    mg = np.zeros((128, 16, 128), np.float32)
    for g in range(16):
        for row in range(128):
            mg[row, g, 8 * g + row % 8] = 1.0
    c["maskg"] = _bf(mg)
    c["identrep"] = _bf(np.repeat(np.eye(128, dtype=np.float32)[:, None, :], 4, axis=1))
    cs = np.full((128, 128), -1e30, np.float32)
    for row in range(32):
        q, t = row // 8, row % 8
        for col in range(32):
            q2, t2 = col // 8, col % 8
            if q2 == q and t2 <= t:
                cs[row, col] = 0.0
    c["cmask_s"] = cs
    sr = np.zeros((32, 4, 32), np.float32)
    for q in range(4):
        for h in range(4):
            for t in range(8):
                sr[8 * q + t, q, h * 8 + t] = 1.0
    c["selrep"] = _bf(sr)
    c["iota_p"] = np.arange(128, dtype=np.float32)[:, None]
    return c


def make_in_map(c, inp, NT, NPG):
    b, j = c // 4, c % 4
    NOWN = NT // 4
    f = lambda a: np.ascontiguousarray(a, dtype=np.float32)
    xp = inp["x_prompt"][b]
    m = {"xb": f(xp[:NT * 128])}
    xo = np.zeros((NOWN, 160, D), np.float32)
    for i in range(NOWN):
        g0 = (4 * i + j) * 128
        xo[i, 32:160] = xp[g0:g0 + 128]
        if g0 > 0:
            xo[i, 2:32] = xp[g0 - 30:g0]
    m["xo"] = xo
    xs_ = np.zeros((160, D), np.float32)
    xs_[32:64] = inp["x_sample"][4 * c:4 * c + 4].reshape(32, D)
    m["xs"] = xs_
    for k in ("w_in", "w_out", "w_gate", "w_up", "w_down", "g_pre_mix", "g_post_mix", "g_pre_ffn", "g_post_ffn",
              "conv_w", "conv_b", "conv_ln_g", "conv_ln_b"):
        m[k] = f(inp[k][0]) if inp[k][0].ndim == 2 else f(inp[k][0])[None, :]
    npool = inp["cache_k"].shape[1]
    m["cache_k"] = f(inp["cache_k"][0]).reshape(npool * PAGE, NKV * HD)
    m["cache_v"] = f(inp["cache_v"][0]).reshape(npool * PAGE, NKV * HD)
    m["cache_ik"] = f(inp["cache_idx_k"][0]).reshape(npool, PAGE * IDIM)
    m["state_conv"] = f(inp["state_conv"][0, 4 * c:4 * c + 4])
    m["ptab"] = np.ascontiguousarray(inp["page_table"][4 * c:4 * c + 4, :NPG], dtype=np.int32)
    m.update(_consts(j))
    return m


_NC_CACHE = {}


def kernel(**inp):
    NT, NPG = SEQ // 128, NPAGES
    npool = inp["cache_k"].shape[1]
    key = (NT, NPG, npool)
    if key not in _NC_CACHE:
        _NC_CACHE[key] = build({"NT": NT, "NPG": NPG, "NPOOL": npool})
    nc = _NC_CACHE[key]
    in_maps = [make_in_map(c, inp, NT, NPG) for c in range(8)]
    res = run_bass_kernel_spmd(nc, in_maps, core_ids=list(range(8))).results
    return assemble(res, NT)


def assemble(res, NT):
    NOWN = NT // 4
    S_ = NT * 128
    y_p = np.zeros((NB, S_, D), np.float32)
    y_s = np.zeros((DEC_B, DEC_T, D), np.float32)
    nk = np.zeros((1, NB, S_, NKV, HD), np.float32)
    nv = np.zeros((1, NB, S_, NKV, HD), np.float32)
    nik = np.zeros((1, NB, S_, IDIM), np.float32)
    ncp = np.zeros((1, NB, CW - 1, CONV_CH), np.float32)
    nks = np.zeros((1, DEC_B, DEC_T, NKV, HD), np.float32)
    nvs = np.zeros((1, DEC_B, DEC_T, NKV, HD), np.float32)
    niks = np.zeros((1, DEC_B, DEC_T, IDIM), np.float32)
    ncs = np.zeros((1, DEC_B, CW - 1, CONV_CH), np.float32)
    for c in range(len(res)):
        r = res[c]
        if r is None:
            continue
        b, j = c // 4, c % 4
        oy = np.asarray(r["o_y"])
        for i in range(NOWN):
            g0 = (4 * i + j) * 128
            y_p[b, g0:g0 + 128] = oy[i]
        y_s[4 * c:4 * c + 4] = oy[NOWN][0:32].reshape(4, DEC_T, D)
        if j == 0:
            nk[0, b] = np.asarray(r["o_k"]).reshape(S_, NKV, HD)
            nv[0, b] = np.asarray(r["o_v"]).reshape(S_, NKV, HD)
            nik[0, b] = np.asarray(r["o_ik"])
        if j == 3:
            ncp[0, b] = np.asarray(r["o_convp"])
        nks[0, 4 * c:4 * c + 4] = np.asarray(r["o_ks"])[0:32].reshape(4, DEC_T, NKV, HD)
        nvs[0, 4 * c:4 * c + 4] = np.asarray(r["o_vs"])[0:32].reshape(4, DEC_T, NKV, HD)
        niks[0, 4 * c:4 * c + 4] = np.asarray(r["o_iks"])[0:32].reshape(4, DEC_T, IDIM)
        ncs[0, 4 * c:4 * c + 4] = np.asarray(r["o_convs"])
    return (y_p, y_s, nk, nv, nik, ncp, nks, nvs, niks, ncs)
```
